# Optimizing a Trainium2 kernel written in Bass

```python
import math
import jax, jax.numpy as jnp
from jax import lax
import numpy as np


D_MODEL = 1024
BATCH = 2
SEQ = 8192
DEPTH = 1

MIX_WIDTH = D_MODEL
HEAD_DIM = 64
ATTN_HEADS = (MIX_WIDTH // 2) // HEAD_DIM
ATTN_WIDTH = ATTN_HEADS * HEAD_DIM
SSM_GROUP_CH = 16
SSM_GROUPS = (MIX_WIDTH - ATTN_WIDTH) // SSM_GROUP_CH
SSM_WIDTH = SSM_GROUPS * SSM_GROUP_CH
SSM_STATE = 64
DT_MIN = 1e-3
DT_MAX = 1e-1
MOBA_BLOCK = 256
MOBA_TOPK = 3
QUERY_CHUNK = 128
ROPE_THETA = 10000.0
D_FF = 4 * D_MODEL
EPS = 1e-6
IN_PROJ_WIDTH = 3 * ATTN_WIDTH + SSM_WIDTH

kernel_name = 'hymba_moba_s5_hybrid_layer'


def rmsnorm(x, g):
    xf = x.astype(jnp.float32)
    y = xf * lax.rsqrt(jnp.mean(jnp.square(xf), axis=-1, keepdims=True) + EPS)
    return (y * g.astype(jnp.float32)).astype(x.dtype)


def rope(x):
    L = x.shape[2]
    half = HEAD_DIM // 2
    inv_freq = ROPE_THETA ** (-jnp.arange(half, dtype=jnp.float32) / half)
    ang = jnp.arange(L, dtype=jnp.float32)[:, None] * inv_freq[None, :]
    cos, sin = jnp.cos(ang), jnp.sin(ang)
    xf = x.astype(jnp.float32)
    x1, x2 = xf[..., :half], xf[..., half:]
    out = jnp.concatenate([x1 * cos - x2 * sin, x1 * sin + x2 * cos], axis=-1)
    return out.astype(x.dtype)


def moba_attention(q, k, v):
    B, H, L, D = q.shape
    nb = -(-L // MOBA_BLOCK)
    pad = nb * MOBA_BLOCK - L
    kp = jnp.pad(k, ((0, 0), (0, 0), (0, pad), (0, 0)))
    vp = jnp.pad(v, ((0, 0), (0, 0), (0, pad), (0, 0)))
    k_blk = kp.reshape(B, H, nb, MOBA_BLOCK, D)
    v_blk = vp.reshape(B, H, nb, MOBA_BLOCK, D)
    counts = jnp.clip(L - jnp.arange(nb) * MOBA_BLOCK, 1, MOBA_BLOCK).astype(jnp.float32)
    k_mean = k_blk.astype(jnp.float32).sum(axis=3) / counts[:, None]
    gate = jnp.einsum('bhld,bhnd->bhln', q.astype(jnp.float32), k_mean)
    q_blk_id = jnp.arange(L) // MOBA_BLOCK
    fully_past = jnp.arange(nb)[None, :] < q_blk_id[:, None]
    gate = jnp.where(fully_past, gate, -jnp.inf)
    topk = min(MOBA_TOPK, nb)
    _, sel = lax.top_k(gate, topk)
    sel_valid = sel < q_blk_id[None, None, :, None]

    scale = HEAD_DIM ** -0.5
    n_chunks = L // QUERY_CHUNK
    b_idx = jnp.arange(B)[:, None, None, None]
    h_idx = jnp.arange(H)[None, :, None, None]

    def one_chunk(ci):
        start = ci * QUERY_CHUNK
        qc = lax.dynamic_slice_in_dim(q, start, QUERY_CHUNK, axis=2)
        sc = lax.dynamic_slice_in_dim(sel, start, QUERY_CHUNK, axis=2)
        vc = lax.dynamic_slice_in_dim(sel_valid, start, QUERY_CHUNK, axis=2)
        own = start // MOBA_BLOCK
        k_sel = k_blk[b_idx, h_idx, sc]
        v_sel = v_blk[b_idx, h_idx, sc]
        k_own = lax.dynamic_index_in_dim(k_blk, own, axis=2, keepdims=False)
        v_own = lax.dynamic_index_in_dim(v_blk, own, axis=2, keepdims=False)
        s_sel = jnp.einsum('bhqd,bhqtkd->bhqtk', qc, k_sel,
                           preferred_element_type=jnp.float32) * scale
        s_sel = jnp.where(vc[..., None], s_sel, -jnp.inf).reshape(B, H, QUERY_CHUNK, topk * MOBA_BLOCK)
        s_own = jnp.einsum('bhqd,bhkd->bhqk', qc, k_own,
                           preferred_element_type=jnp.float32) * scale
        q_pos = start + jnp.arange(QUERY_CHUNK)
        k_pos = own * MOBA_BLOCK + jnp.arange(MOBA_BLOCK)
        s_own = jnp.where(k_pos[None, :] <= q_pos[:, None], s_own, -jnp.inf)
        p = jax.nn.softmax(jnp.concatenate([s_sel, s_own], axis=-1), axis=-1)
        p_sel = p[..., :topk * MOBA_BLOCK].reshape(B, H, QUERY_CHUNK, topk, MOBA_BLOCK).astype(v.dtype)
        p_own = p[..., topk * MOBA_BLOCK:].astype(v.dtype)
        return (jnp.einsum('bhqtk,bhqtkd->bhqd', p_sel, v_sel)
                + jnp.einsum('bhqk,bhkd->bhqd', p_own, v_own))

    outs = lax.map(one_chunk, jnp.arange(n_chunks))
    return outs.transpose(1, 2, 0, 3, 4).reshape(B, H, L, D)


def s5_mixer(u, lam_re, lam_im, log_dt, b_re, b_im, c_re, c_im, d_skip, w_glu, b_glu):
    Bsz, L, _ = u.shape
    uf = u.astype(jnp.float32).reshape(Bsz, L, SSM_GROUPS, SSM_GROUP_CH)
    lam = lax.complex(lam_re.astype(jnp.float32), lam_im.astype(jnp.float32))
    dt = jnp.exp(log_dt.astype(jnp.float32))[:, None]
    lam_bar = jnp.exp(lam * dt)
    b_mat = lax.complex(b_re.astype(jnp.float32), b_im.astype(jnp.float32))
    b_bar = ((lam_bar - 1.0) / lam)[..., None] * b_mat
    bu = jnp.einsum('gpn,blgn->blgp', b_bar, uf.astype(jnp.complex64))
    a = jnp.broadcast_to(lam_bar, bu.shape)

    def combine(e1, e2):
        a1, x1 = e1
        a2, x2 = e2
        return a1 * a2, a2 * x1 + x2

    _, states = lax.associative_scan(combine, (a, bu), axis=1)
    c_mat = lax.complex(c_re.astype(jnp.float32), c_im.astype(jnp.float32))
    y = jnp.einsum('gnp,blgp->blgn', c_mat, states).real + d_skip.astype(jnp.float32) * uf
    y = jax.nn.gelu(y.reshape(Bsz, L, SSM_WIDTH))
    y = y * jax.nn.sigmoid(y @ w_glu.astype(jnp.float32) + b_glu.astype(jnp.float32))
    return y.astype(u.dtype)


def setup_inputs(seed: int = 0) -> dict:
    key = jax.random.key(seed)
    ks = jax.random.split(key, 24)
    f32 = jnp.float32

    def nrm(k, shape, s):
        return s * jax.random.normal(k, shape, f32)

    n_idx = jnp.arange(SSM_STATE, dtype=f32)
    x = jax.random.normal(ks[0], (BATCH, SEQ, D_MODEL), f32)
    c = jax.random.normal(ks[1], (BATCH, D_MODEL), f32)
    w_ada = nrm(ks[2], (DEPTH, D_MODEL, 6 * D_MODEL), 0.5 * D_MODEL ** -0.5)
    b_ada = nrm(ks[3], (DEPTH, 6 * D_MODEL), 0.01)
    g_mix = 1.0 + nrm(ks[4], (DEPTH, D_MODEL), 0.02)
    w_in = nrm(ks[5], (DEPTH, D_MODEL, IN_PROJ_WIDTH), D_MODEL ** -0.5)
    g_attn_out = 1.0 + nrm(ks[6], (DEPTH, ATTN_WIDTH), 0.02)
    lam_re = -0.5 + nrm(ks[7], (DEPTH, SSM_GROUPS, SSM_STATE), 0.01)
    lam_im = math.pi * n_idx + nrm(ks[8], (DEPTH, SSM_GROUPS, SSM_STATE), 0.01)
    log_dt = jax.random.uniform(ks[9], (DEPTH, SSM_GROUPS), f32, math.log(DT_MIN), math.log(DT_MAX))
    b_re = nrm(ks[10], (DEPTH, SSM_GROUPS, SSM_STATE, SSM_GROUP_CH), (2 * SSM_GROUP_CH) ** -0.5)
    b_im = nrm(ks[11], (DEPTH, SSM_GROUPS, SSM_STATE, SSM_GROUP_CH), (2 * SSM_GROUP_CH) ** -0.5)
    c_re = nrm(ks[12], (DEPTH, SSM_GROUPS, SSM_GROUP_CH, SSM_STATE), SSM_STATE ** -0.5)
    c_im = nrm(ks[13], (DEPTH, SSM_GROUPS, SSM_GROUP_CH, SSM_STATE), SSM_STATE ** -0.5)
    d_skip = nrm(ks[14], (DEPTH, SSM_GROUPS, SSM_GROUP_CH), 1.0)
    w_glu = nrm(ks[15], (DEPTH, SSM_WIDTH, SSM_WIDTH), SSM_WIDTH ** -0.5)
    b_glu = nrm(ks[16], (DEPTH, SSM_WIDTH), 0.01)
    g_ssm_out = 1.0 + nrm(ks[17], (DEPTH, SSM_WIDTH), 0.02)
    w_out = nrm(ks[18], (DEPTH, MIX_WIDTH, D_MODEL), MIX_WIDTH ** -0.5)
    g_mlp = 1.0 + nrm(ks[19], (DEPTH, D_MODEL), 0.02)
    w_fc1 = nrm(ks[20], (DEPTH, D_MODEL, D_FF), D_MODEL ** -0.5)
    w_fc2 = nrm(ks[21], (DEPTH, D_FF, D_MODEL), D_FF ** -0.5)
    g_final = 1.0 + nrm(ks[22], (D_MODEL,), 0.02)
    return {'x': x, 'c': c, 'w_ada': w_ada, 'b_ada': b_ada, 'g_mix': g_mix, 'w_in': w_in,
            'g_attn_out': g_attn_out, 'lam_re': lam_re, 'lam_im': lam_im, 'log_dt': log_dt,
            'b_re': b_re, 'b_im': b_im, 'c_re': c_re, 'c_im': c_im, 'd_skip': d_skip,
            'w_glu': w_glu, 'b_glu': b_glu, 'g_ssm_out': g_ssm_out, 'w_out': w_out,
            'g_mlp': g_mlp, 'w_fc1': w_fc1, 'w_fc2': w_fc2, 'g_final': g_final}


def reference(x, c, w_ada, b_ada, g_mix, w_in, g_attn_out, lam_re, lam_im, log_dt,
              b_re, b_im, c_re, c_im, d_skip, w_glu, b_glu, g_ssm_out, w_out,
              g_mlp, w_fc1, w_fc2, g_final):
    Bsz, L, _ = x.shape

    def to_heads(t):
        return t.reshape(Bsz, L, ATTN_HEADS, HEAD_DIM).transpose(0, 2, 1, 3)

    for l in range(DEPTH):
        mod = jax.nn.silu(c) @ w_ada[l] + b_ada[l]
        sh1, sc1, gt1, sh2, sc2, gt2 = [m[:, None, :] for m in jnp.split(mod, 6, axis=-1)]

        h = rmsnorm(x, g_mix[l]) * (1.0 + sc1) + sh1
        proj = h @ w_in[l]
        q, k, v, u = jnp.split(proj, [ATTN_WIDTH, 2 * ATTN_WIDTH, 3 * ATTN_WIDTH], axis=-1)
        q = rope(to_heads(q))
        k = rope(to_heads(k))
        attn = moba_attention(q, k, to_heads(v)).transpose(0, 2, 1, 3).reshape(Bsz, L, ATTN_WIDTH)
        ssm = s5_mixer(u, lam_re[l], lam_im[l], log_dt[l], b_re[l], b_im[l], c_re[l], c_im[l],
                       d_skip[l], w_glu[l], b_glu[l])
        mix = jnp.concatenate([rmsnorm(attn, g_attn_out[l]), rmsnorm(ssm, g_ssm_out[l])], axis=-1)
        x = x + gt1 * (mix @ w_out[l])

        h = rmsnorm(x, g_mlp[l]) * (1.0 + sc2) + sh2
        x = x + gt2 * (jnp.square(jax.nn.relu(h @ w_fc1[l])) @ w_fc2[l])

    return rmsnorm(x, g_final)
```

```python
import math
import numpy as np
from contextlib import ExitStack

import concourse.bass as bass
import concourse.mybir as mybir
from concourse.bass_utils import run_bass_kernel_spmd

F32 = mybir.dt.float32
BF16 = mybir.dt.bfloat16
I32 = mybir.dt.int32
ALU = mybir.AluOpType
AF = mybir.ActivationFunctionType

PHYS = ["pe", "act", "dve", "pool", "sp"]
NDMA = 8
SAME_ENG_WAIT = True
EPS = 1e-6
NEGBIG = -30000.0


class Buf:
    __slots__ = ("name", "last_w", "readers", "excl")

    def __init__(self, name, excl=False):
        self.name = name
        self.last_w = None
        self.readers = []
        self.excl = excl


class Prog:
    def __init__(self, nc):
        self.nc = nc
        self.stack = ExitStack()
        self.semnames = ["pe", "act", "dve", "pool"] + [f"d{i}" for i in range(NDMA)]
        self.cnt = {s: 0 for s in self.semnames}
        self.seen = {e: {s: 0 for s in self.semnames} for e in PHYS}
        self.sems = {s: self.stack.enter_context(nc.semaphore("sem_" + s)) for s in self.semnames}
        self.engobjs = {"pe": nc.tensor, "act": nc.scalar, "dve": nc.vector, "pool": nc.gpsimd, "sp": nc.sync}
        self.pending = {e: {} for e in PHYS}
        self.nbuf = 0
        self.dma_rr = 0
        self.scopes = [self.stack]

    def buf(self, name=None, excl=False):
        self.nbuf += 1
        return Buf(name or f"b{self.nbuf}", excl)

    def pbuf(self):
        return self.buf(excl=True)

    def sb(self, name, shape, dtype):
        return self.scopes[-1].enter_context(self.nc.sbuf_tensor(name, list(shape), dtype))

    def ps(self, name, shape, dtype=F32):
        return self.scopes[-1].enter_context(self.nc.psum_tensor(name, list(shape), dtype))

    def push_scope(self):
        st = ExitStack()
        self.scopes.append(st)
        return st

    def pop_scope(self):
        st = self.scopes.pop()
        st.close()
        self.fence()

    def fence(self):
        for e in PHYS:
            for s in self.semnames:
                if self.cnt[s] > self.seen[e][s]:
                    self.pending[e][s] = self.cnt[s]

    def _op(self, phys, sem, inc, fn, reads, writes, extra_waits=()):
        waits = dict(self.pending[phys])
        self.pending[phys] = {}
        xr = [b for b in reads if b.excl]
        if xr:
            reads = [b for b in reads if not b.excl]
            writes = list(writes) + [b for b in xr if b not in writes]
        for (f, c) in extra_waits:
            waits[f] = max(waits.get(f, 0), c)

        def need(f):
            return f != phys or (SAME_ENG_WAIT and phys != "pe")
        for b in reads:
            if b.last_w is not None:
                f, c = b.last_w
                if need(f):
                    waits[f] = max(waits.get(f, 0), c)
        for b in writes:
            if b.last_w is not None:
                f, c = b.last_w
                if need(f):
                    waits[f] = max(waits.get(f, 0), c)
            for (f, c) in b.readers:
                if need(f):
                    waits[f] = max(waits.get(f, 0), c)
        engobj = self.engobjs[phys]
        for f, c in waits.items():
            if c > self.seen[phys][f]:
                self.seen[phys][f] = c
                engobj.wait_ge(self.sems[f], c)
        self.cnt[sem] += inc
        me = (sem, self.cnt[sem])
        ins = fn(engobj)
        ins.then_inc(self.sems[sem], inc)
        for b in reads:
            b.readers.append(me)
        for b in writes:
            b.last_w = me
            b.readers = []
        return me

    def op(self, eng, fn, reads=(), writes=()):
        return self._op(eng, eng, 1, fn, reads, writes)

    def dma(self, fn, reads=(), writes=(), q="sp"):
        k = self.dma_rr % NDMA
        self.dma_rr += 1
        sem = f"d{k}"
        prev = self.cnt[sem]
        extra = [(sem, prev)] if prev > 0 else []
        return self._op(q, sem, 16, fn, reads, writes, extra_waits=extra)

    def finish(self):
        for i in range(NDMA):
            s = f"d{i}"
            if self.cnt[s] > 0:
                self.nc.sync.wait_ge(self.sems[s], self.cnt[s])
        for s in ["pe", "act", "dve", "pool"]:
            if self.cnt[s] > 0:
                self.nc.sync.wait_ge(self.sems[s], self.cnt[s])
        while len(self.scopes) > 1:
            self.scopes.pop().close()
        self.stack.close()


def _rot(lst, i):
    return lst[i % len(lst)]


NT_C = 2048


def build_phase_c(nc, srcs=None):
    def din(name, shape, dt=F32):
        return nc.dram_tensor(name, list(shape), dt, kind="ExternalInput").ap()
    x = din("xc", [NT_C, 1024])
    if srcs is None:
        attn = din("attn", [NT_C, 520])
        yin = din("yin", [NT_C, 512])
    else:
        attn, yin = srcs["attn"], srcs["y"]
    cT2 = din("cT2", [128, 16])
    wada = din("wada_c", [1024, 4096])
    bada = din("bada_c", [1, 4096])
    rows = din("rows_c", [1, 3072])
    bglu = din("bglu", [1, 512])
    wglu = din("wglu", [512, 512])
    wout = din("wout", [1024, 1024])
    wfc1 = din("wfc1", [1024, 4096])
    wfc2 = din("wfc2", [4096, 1024])
    ident = din("ident", [128, 128])
    out = nc.dram_tensor("out", [NT_C, 1024], F32, kind="ExternalOutput").ap()
    x1s = nc.dram_tensor("x1s", [NT_C, 1024], F32).ap()

    P = Prog(nc)
    op, dma = P.op, P.dma

    ident_bf = P.sb("ident_bf", [128, 128], BF16); b_ident = P.buf()
    ones_f = P.sb("ones_f", [1, 128], F32); b_ones_f = P.buf()
    ones_bf = P.sb("ones_bf", [1, 512], BF16); b_ones_bf = P.buf()
    bglu_bf = P.sb("bglu_bf", [1, 512], BF16); b_bglu = P.buf()
    sh2T = P.sb("sh2T", [128, 16], BF16); b_sh2T = P.buf()
    Gcat = P.sb("Gcat", [128, 1024], F32); b_Gcat = P.buf()
    G1 = P.sb("G1", [128, 1024], F32); b_G1 = P.buf()
    S2b = P.sb("S2b", [128, 1024], F32); b_S2b = P.buf()
    G2 = P.sb("G2", [128, 1024], F32); b_G2 = P.buf()
    Gf = P.sb("Gf", [128, 1024], F32); b_Gf = P.buf()

    dma(lambda e: e.dma_start(out=ident_bf[:], in_=ident[:, :]), writes=[b_ident], q="pool")
    dma(lambda e: e.dma_start(out=bglu_bf[:], in_=bglu[:, :]), writes=[b_bglu], q="pool")
    op("dve", lambda e: e.memset(ones_f[:], 1.0), writes=[b_ones_f])
    op("dve", lambda e: e.memset(ones_bf[:], 1.0), writes=[b_ones_bf])

    P.push_scope()
    ct = P.sb("ct", [128, 16], F32); b_ct = P.buf()
    sct = P.sb("sct", [128, 16], F32); b_sct = P.buf()
    sgc = P.sb("sgc", [128, 16], F32)
    modrow = P.sb("modrow", [1, 4096], F32); b_modrow = P.buf()
    badas = P.sb("badas", [1, 4096], F32); b_badas = P.buf()
    rowss = P.sb("rowss", [1, 3072], F32); b_rowss = P.buf()
    s2row = P.sb("s2row", [1, 1024], F32); b_s2row = P.buf()
    pieces = [P.sb(f"wpiece{i}", [128, 8, 512], F32) for i in range(2)]
    b_pieces = [P.buf() for _ in range(2)]
    ps_row = [P.ps(f"ps_row{i}", [128, 512], F32) for i in range(2)]
    b_ps_row = [P.pbuf() for _ in range(2)]

    dma(lambda e: e.dma_start(out=ct[:], in_=cT2[:, :]), writes=[b_ct])
    dma(lambda e: e.dma_start(out=badas[:], in_=bada[:, :]), writes=[b_badas])
    dma(lambda e: e.dma_start(out=rowss[:], in_=rows[:, :]), writes=[b_rowss])
    op("act", lambda e: e.activation(out=sgc[:], in_=ct[:], func=AF.Sigmoid), reads=[b_ct], writes=[b_sct])
    op("dve", lambda e: e.tensor_tensor(out=sct[:], in0=sgc[:], in1=ct[:], op=ALU.mult), reads=[b_ct, b_sct], writes=[b_sct])
    for j in range(8):
        pc, bpc = _rot(pieces, j), _rot(b_pieces, j)
        pr, bpr = _rot(ps_row, j), _rot(b_ps_row, j)
        dma(lambda e: e.dma_start(out=pc[:], in_=wada[:, j * 512:(j + 1) * 512].rearrange("(k p) n -> p k n", p=128)),
            writes=[bpc])
        for k in range(8):
            op("pe", lambda e: e.matmul(pr[0:1, :], lhsT=sct[:, 2 * k:2 * k + 1], rhs=pc[:, k, :],
                                        start=(k == 0), stop=(k == 7)),
               reads=[b_sct, bpc], writes=[bpr])
        op("dve", lambda e: e.tensor_tensor(out=modrow[0:1, j * 512:(j + 1) * 512], in0=pr[0:1, :],
                                            in1=badas[0:1, j * 512:(j + 1) * 512], op=ALU.add),
           reads=[bpr, b_badas], writes=[b_modrow])
    op("dve", lambda e: e.scalar_tensor_tensor(out=s2row[:], in0=modrow[0:1, 2048:3072], scalar=1.0,
                                                in1=rowss[0:1, 1024:2048], op0=ALU.add, op1=ALU.mult),
       reads=[b_modrow, b_rowss], writes=[b_s2row])
    bc_list = [(Gcat, b_Gcat, rowss, b_rowss, 0), (G1, b_G1, modrow, b_modrow, 0), (S2b, b_S2b, s2row, b_s2row, 0),
               (G2, b_G2, modrow, b_modrow, 3072), (Gf, b_Gf, rowss, b_rowss, 2048)]
    i = 0
    for (dst, bdst, src, bsrc, off) in bc_list:
        for hf in range(2):
            pr, bpr = _rot(ps_row, i), _rot(b_ps_row, i)
            i += 1
            op("pe", lambda e: e.matmul(pr[:, :], lhsT=ones_f[0:1, :], rhs=src[0:1, off + hf * 512: off + (hf + 1) * 512],
                                        start=True, stop=True),
               reads=[b_ones_f, bsrc], writes=[bpr])
            op("act", lambda e: e.activation(out=dst[:, hf * 512:(hf + 1) * 512], in_=pr[:, :], func=AF.Copy),
               reads=[bpr], writes=[bdst])
    pr, bpr = ps_row[0], b_ps_row[0]
    for k in range(8):
        op("pe", lambda e: e.matmul(pr[:, 2 * k:2 * k + 2], lhsT=modrow[0:1, 1024 + k * 128:1024 + (k + 1) * 128],
                                    rhs=ones_f[0:1, 0:2], start=True, stop=True),
           reads=[b_ones_f, b_modrow], writes=[bpr])
    op("dve", lambda e: e.tensor_copy(out=sh2T[:], in_=pr[:, 0:16]), reads=[bpr], writes=[b_sh2T])
    P.pop_scope()

    P.push_scope()
    Wout = P.sb("Wout", [128, 8, 1024], BF16); b_Wout = P.buf()
    Wglu = P.sb("Wglu", [128, 4, 512], BF16); b_Wglu = P.buf()
    for k in range(4):
        dma(lambda e: e.dma_start(out=Wglu[:, k, :], in_=wglu[k * 128:(k + 1) * 128, :]), writes=[b_Wglu], q="pool")
    for k in range(8):
        dma(lambda e: e.dma_start(out=Wout[:, k, :], in_=wout[k * 128:(k + 1) * 128, :]), writes=[b_Wout], q="pool")
    NB = 2
    at = [P.sb(f"at{i}", [128, 8, 65], F32) for i in range(NB)]; b_at = [P.buf() for _ in range(NB)]
    yt = [P.sb(f"yt{i}", [128, 512], F32) for i in range(NB)]; b_yt = [P.buf() for _ in range(NB)]
    xt = [P.sb(f"xt{i}", [128, 1024], F32) for i in range(NB)]; b_xt = [P.buf() for _ in range(NB)]
    rl = P.sb("rl", [128, 8], F32); b_rl = P.buf()
    A = P.sb("A", [128, 8, 64], F32); b_A = P.buf()
    junk = P.sb("junk", [128, 512], F32); b_junk = P.buf()
    ss = P.sb("ss", [128, 2], F32); b_ss = P.buf()
    sq = P.sb("sq", [128, 2], F32); b_sq = P.buf()
    rstd = P.sb("rstd", [128, 2], F32); b_rstd = P.buf()
    g1 = P.sb("g1", [128, 512], F32); b_g1 = P.buf()
    g2 = P.sb("g2", [128, 512], F32); b_g2 = P.buf()
    sg = P.sb("sg", [128, 512], F32); b_sg = P.buf()
    yg = P.sb("yg", [128, 512], F32); b_yg = P.buf()
    ygb = P.sb("ygb", [128, 512], BF16); b_ygb = P.buf()
    ygT = P.sb("ygT", [128, 4, 128], BF16); b_ygT = P.buf()
    sig = P.sb("sig", [128, 512], F32); b_sig = P.buf()
    S = P.sb("S", [128, 512], F32); b_S = P.buf()
    mixh = P.sb("mixh", [128, 1024], BF16); b_mixh = P.buf()
    mixT = P.sb("mixT", [128, 8, 128], BF16); b_mixT = P.buf()
    tt = P.sb("tt", [128, 1024], F32); b_tt = P.buf()
    x1t = [P.sb(f"x1t{i}", [128, 1024], F32) for i in range(NB)]; b_x1t = [P.buf() for _ in range(NB)]
    ptr = P.ps("ptr", [128, 8, 128], BF16); b_ptr = P.pbuf()
    psg = P.ps("psg", [128, 512], F32); b_psg = P.pbuf()
    pmx = P.ps("pmx", [128, 8, 128], BF16); b_pmx = P.pbuf()
    pso = [P.ps(f"pso{i}", [128, 512], F32) for i in range(2)]; b_pso = [P.pbuf() for _ in range(2)]
    b_x1s = [P.buf() for _ in range(16)]

    def c1_load(t):
        a, ba = _rot(at, t), _rot(b_at, t)
        y_, by = _rot(yt, t), _rot(b_yt, t)
        x_, bx = _rot(xt, t), _rot(b_xt, t)
        dma(lambda e: e.dma_start(out=a[:].rearrange("p h e -> p (h e)"), in_=attn[t * 128:(t + 1) * 128, :]), writes=[ba])
        dma(lambda e: e.dma_start(out=y_[:], in_=yin[t * 128:(t + 1) * 128, :]), writes=[by])
        dma(lambda e: e.dma_start(out=x_[:], in_=x[t * 128:(t + 1) * 128, :]), writes=[bx])

    c1_load(0)
    for t in range(16):
        if t + 1 < 16:
            c1_load(t + 1)
        a, ba = _rot(at, t), _rot(b_at, t)
        y_, by = _rot(yt, t), _rot(b_yt, t)
        x_, bx = _rot(xt, t), _rot(b_xt, t)
        xo, bxo = _rot(x1t, t), _rot(b_x1t, t)
        op("dve", lambda e: e.reciprocal(out=rl[:], in_=a[:, :, 64]), reads=[ba], writes=[b_rl])
        op("dve", lambda e: e.tensor_tensor(out=A[:], in0=a[:, :, 0:64],
                                            in1=rl[:, :].unsqueeze(2).to_broadcast([128, 8, 64]), op=ALU.mult),
           reads=[ba, b_rl], writes=[b_A])
        op("act", lambda e: e.activation(out=junk[:], in_=A[:].rearrange("p h d -> p (h d)"), func=AF.Square,
                                         accum_out=ss[:, 0:1]),
           reads=[b_A], writes=[b_junk, b_ss])
        op("pool", lambda e: e.tensor_tensor(out=g1[:], in0=y_[:], in1=y_[:], op=ALU.mult), reads=[by], writes=[b_g1])
        op("pool", lambda e: e.tensor_scalar(out=g1[:], in0=g1[:], scalar1=0.044715, scalar2=1.0,
                                             op0=ALU.mult, op1=ALU.add), reads=[b_g1], writes=[b_g1])
        op("pool", lambda e: e.tensor_tensor(out=g2[:], in0=g1[:], in1=y_[:], op=ALU.mult), reads=[b_g1, by], writes=[b_g2])
        op("act", lambda e: e.activation(out=sg[:], in_=g2[:], func=AF.Sigmoid, scale=1.5957691216057308),
           reads=[b_g2], writes=[b_sg])
        op("dve", lambda e: e.tensor_tensor(out=yg[:], in0=y_[:], in1=sg[:], op=ALU.mult), reads=[by, b_sg], writes=[b_yg])
        op("pool", lambda e: e.tensor_tensor(out=ygb[:], in0=y_[:], in1=sg[:], op=ALU.mult), reads=[by, b_sg], writes=[b_ygb])
        for k in range(4):
            op("pe", lambda e: e.transpose(ptr[:, k, :], ygb[:, k * 128:(k + 1) * 128], ident_bf[:]),
               reads=[b_ygb, b_ident], writes=[b_ptr])
        op("act", lambda e: e.activation(out=ygT[:], in_=ptr[:, 0:4, :], func=AF.Copy), reads=[b_ptr], writes=[b_ygT])
        op("pe", lambda e: e.matmul(psg[:], lhsT=ones_bf[0:1, 0:128], rhs=bglu_bf[0:1, :], start=True, stop=False),
           reads=[b_ones_bf, b_bglu], writes=[b_psg])
        for k in range(4):
            op("pe", lambda e: e.matmul(psg[:], lhsT=ygT[:, k, :], rhs=Wglu[:, k, :], start=False, stop=(k == 3)),
               reads=[b_ygT, b_Wglu], writes=[b_psg])
        op("act", lambda e: e.activation(out=sig[:], in_=psg[:], func=AF.Sigmoid), reads=[b_psg], writes=[b_sig])
        op("dve", lambda e: e.tensor_tensor(out=S[:], in0=yg[:], in1=sig[:], op=ALU.mult), reads=[b_yg, b_sig], writes=[b_S])
        op("act", lambda e: e.activation(out=junk[:], in_=S[:], func=AF.Square, accum_out=ss[:, 1:2]),
           reads=[b_S], writes=[b_junk, b_ss])
        op("act", lambda e: e.activation(out=sq[:], in_=ss[:], func=AF.Sqrt, scale=1.0 / 512.0, bias=EPS),
           reads=[b_ss], writes=[b_sq])
        op("dve", lambda e: e.reciprocal(out=rstd[:], in_=sq[:]), reads=[b_sq], writes=[b_rstd])
        op("dve", lambda e: e.scalar_tensor_tensor(out=mixh[:, 0:512], in0=A[:].rearrange("p h d -> p (h d)"),
                                                    scalar=rstd[:, 0:1], in1=Gcat[:, 0:512], op0=ALU.mult, op1=ALU.mult),
           reads=[b_A, b_rstd, b_Gcat], writes=[b_mixh])
        op("dve", lambda e: e.scalar_tensor_tensor(out=mixh[:, 512:1024], in0=S[:], scalar=rstd[:, 1:2],
                                                    in1=Gcat[:, 512:1024], op0=ALU.mult, op1=ALU.mult),
           reads=[b_S, b_rstd, b_Gcat], writes=[b_mixh])
        for k in range(8):
            op("pe", lambda e: e.transpose(pmx[:, k, :], mixh[:, k * 128:(k + 1) * 128], ident_bf[:]),
               reads=[b_mixh, b_ident], writes=[b_pmx])
        op("act", lambda e: e.activation(out=mixT[:], in_=pmx[:], func=AF.Copy), reads=[b_pmx], writes=[b_mixT])
        for hf in range(2):
            for k in range(8):
                op("pe", lambda e: e.matmul(pso[hf][:], lhsT=mixT[:, k, :], rhs=Wout[:, k, hf * 512:(hf + 1) * 512],
                                            start=(k == 0), stop=(k == 7)),
                   reads=[b_mixT, b_Wout], writes=[b_pso[hf]])
            op("dve", lambda e: e.tensor_tensor(out=tt[:, hf * 512:(hf + 1) * 512], in0=pso[hf][:],
                                                in1=G1[:, hf * 512:(hf + 1) * 512], op=ALU.mult),
               reads=[b_pso[hf], b_G1], writes=[b_tt])
        op("pool", lambda e: e.tensor_tensor(out=xo[:], in0=tt[:], in1=x_[:], op=ALU.add), reads=[b_tt, bx], writes=[bxo])
        dma(lambda e: e.dma_start(out=x1s[t * 128:(t + 1) * 128, :], in_=xo[:]), reads=[bxo], writes=[b_x1s[t]])
    P.pop_scope()

    P.push_scope()
    W2 = P.sb("W2", [128, 32, 1024], BF16); b_W2 = [P.buf() for _ in range(8)]
    for i in range(8):
        dma(lambda e: e.dma_start(out=W2[:, 4 * i:4 * i + 4, :],
                                  in_=wfc2[512 * i:512 * (i + 1), :].rearrange("(j p) n -> p j n", p=128)),
            writes=[b_W2[i]], q="pool")
    W1p = [P.sb(f"W1p{i}", [128, 8, 256], BF16) for i in range(2)]; b_W1p = [P.buf() for _ in range(2)]
    hT = P.sb("hT", [128, 32, 512], BF16); b_hT = [P.buf() for _ in range(32)]
    h2T = P.sb("h2T", [128, 8, 512], BF16); b_h2T = P.buf()
    x1g = P.sb("x1g", [128, 4, 1024], F32); b_x1g = [P.buf() for _ in range(4)]
    h2 = P.sb("h2", [128, 1024], BF16); b_h2 = P.buf()
    junk2 = P.sb("junk2", [128, 1024], BF16); b_junk2 = P.buf()
    ssc = P.sb("ssc", [128, 2], F32); b_ssc = P.buf()
    sqc = P.sb("sqc", [128, 2], F32); b_sqc = P.buf()
    rc = P.sb("rc", [128, 2], F32); b_rc = P.buf()
    b1row = P.sb("b1row", [1, 256], BF16); b_b1row = P.buf()
    rl_t = [P.sb(f"rl_t{i}", [128, 512], BF16) for i in range(2)]; b_rl_t = [P.buf() for _ in range(2)]
    t2 = P.sb("t2", [128, 1024], F32); b_t2 = P.buf()
    x2t = t2; b_x2t = b_t2
    ot = [P.sb(f"ot{i}", [128, 1024], F32) for i in range(2)]; b_ot = [P.buf() for _ in range(2)]
    pht = P.ps("pht", [128, 8, 128], BF16); b_pht = P.pbuf()
    psb = P.ps("psb", [128, 512], F32); b_psb = P.pbuf()
    psf = [P.ps(f"psf{i}", [128, 512], F32) for i in range(2)]; b_psf = [P.pbuf() for _ in range(2)]
    pso2 = [P.ps(f"pso2{i}", [128, 512], F32) for i in range(2)]; b_pso2 = [P.pbuf() for _ in range(2)]
    pcount = 0
    ocount = 0
    for g in range(4):
        for i in range(4):
            t = g * 4 + i
            dma(lambda e: e.dma_start(out=x1g[:, i, :], in_=x1s[t * 128:(t + 1) * 128, :]), reads=[b_x1s[t]], writes=[b_x1g[i]])
            op("act", lambda e: e.activation(out=junk2[:], in_=x1g[:, i, :], func=AF.Square, accum_out=ssc[:, 0:1]),
               reads=[b_x1g[i]], writes=[b_junk2, b_ssc])
            op("act", lambda e: e.activation(out=sqc[:, 0:1], in_=ssc[:, 0:1], func=AF.Sqrt, scale=1.0 / 1024.0, bias=EPS),
               reads=[b_ssc], writes=[b_sqc])
            op("dve", lambda e: e.reciprocal(out=rc[:, 0:1], in_=sqc[:, 0:1]), reads=[b_sqc], writes=[b_rc])
            op("dve", lambda e: e.scalar_tensor_tensor(out=h2[:], in0=x1g[:, i, :], scalar=rc[:, 0:1], in1=S2b[:],
                                                        op0=ALU.mult, op1=ALU.mult),
               reads=[b_x1g[i], b_rc, b_S2b], writes=[b_h2])
            for k in range(8):
                op("pe", lambda e: e.transpose(pht[:, k, :], h2[:, k * 128:(k + 1) * 128], ident_bf[:]),
                   reads=[b_h2, b_ident], writes=[b_pht])
            op("act", lambda e: e.activation(out=h2T[:, :, i * 128:(i + 1) * 128], in_=pht[:], func=AF.Copy),
               reads=[b_pht], writes=[b_h2T])
        for pc in range(16):
            w1, bw1 = _rot(W1p, pcount), _rot(b_W1p, pcount)
            pcount += 1
            dma(lambda e: e.dma_start(out=w1[:], in_=wfc1[:, pc * 256:(pc + 1) * 256].rearrange("(k p) n -> p k n", p=128)),
                writes=[bw1], q="pool")
            for k in range(8):
                op("pe", lambda e: e.matmul(psb[0:1, 0:256], lhsT=sh2T[:, 2 * k:2 * k + 1], rhs=w1[:, k, :],
                                            start=(k == 0), stop=(k == 7)),
                   reads=[b_sh2T, bw1], writes=[b_psb])
            op("act", lambda e: e.activation(out=b1row[:], in_=psb[0:1, 0:256], func=AF.Copy), reads=[b_psb], writes=[b_b1row])
            for fc in range(2):
                j = pc * 2 + fc
                pf, bpf = _rot(psf, j), _rot(b_psf, j)
                op("pe", lambda e: e.matmul(pf[:], lhsT=b1row[0:1, fc * 128:(fc + 1) * 128], rhs=ones_bf[0:1, :],
                                            start=True, stop=False),
                   reads=[b_b1row, b_ones_bf], writes=[bpf])
                for k in range(8):
                    op("pe", lambda e: e.matmul(pf[:], lhsT=w1[:, k, fc * 128:(fc + 1) * 128], rhs=h2T[:, k, :],
                                                start=False, stop=(k == 7)),
                       reads=[bw1, b_h2T], writes=[bpf])
                r_, br_ = _rot(rl_t, j), _rot(b_rl_t, j)
                op("act", lambda e: e.activation(out=r_[:], in_=pf[:], func=AF.Relu), reads=[bpf], writes=[br_])
                op("pool", lambda e: e.tensor_tensor(out=hT[:, j, :], in0=r_[:], in1=r_[:], op=ALU.mult),
                   reads=[br_], writes=[b_hT[j]])
        for i in range(4):
            t = g * 4 + i
            for hf in range(2):
                po, bpo = _rot(pso2, ocount), _rot(b_pso2, ocount)
                ocount += 1
                for j in range(32):
                    op("pe", lambda e: e.matmul(po[:], lhsT=hT[:, j, i * 128:(i + 1) * 128],
                                                rhs=W2[:, j, hf * 512:(hf + 1) * 512], start=(j == 0), stop=(j == 31)),
                       reads=[b_hT[j], b_W2[j // 4]], writes=[bpo])
                op("dve", lambda e: e.tensor_tensor(out=t2[:, hf * 512:(hf + 1) * 512], in0=po[:],
                                                    in1=G2[:, hf * 512:(hf + 1) * 512], op=ALU.mult),
                   reads=[bpo, b_G2], writes=[b_t2])
            op("pool", lambda e: e.tensor_tensor(out=x2t[:], in0=t2[:], in1=x1g[:, i, :], op=ALU.add),
               reads=[b_t2, b_x1g[i]], writes=[b_x2t])
            op("act", lambda e: e.activation(out=junk2[:], in_=x2t[:], func=AF.Square, accum_out=ssc[:, 1:2]),
               reads=[b_x2t], writes=[b_junk2, b_ssc])
            op("act", lambda e: e.activation(out=sqc[:, 1:2], in_=ssc[:, 1:2], func=AF.Sqrt, scale=1.0 / 1024.0, bias=EPS),
               reads=[b_ssc], writes=[b_sqc])
            op("dve", lambda e: e.reciprocal(out=rc[:, 1:2], in_=sqc[:, 1:2]), reads=[b_sqc], writes=[b_rc])
            o_, bo_ = _rot(ot, t), _rot(b_ot, t)
            op("dve", lambda e: e.scalar_tensor_tensor(out=o_[:], in0=x2t[:], scalar=rc[:, 1:2], in1=Gf[:],
                                                        op0=ALU.mult, op1=ALU.mult),
               reads=[b_x2t, b_rc, b_Gf], writes=[bo_])
            dma(lambda e: e.dma_start(out=out[t * 128:(t + 1) * 128, :], in_=o_[:]), reads=[bo_])
    P.pop_scope()
    P.finish()
    return nc


def _ident():
    return np.eye(128, dtype=np.float32)


def phase_c_inputs(inp, b, qi, attn_full, y_full):
    T0 = qi * NT_C
    c = inp["c"][b]
    cT = np.ascontiguousarray(c.reshape(8, 128).T)
    cT2 = np.repeat(cT[:, :, None], 2, axis=2).reshape(128, 16)
    rows = np.concatenate([inp["g_attn_out"][0], inp["g_ssm_out"][0], inp["g_mlp"][0], inp["g_final"]])[None, :]
    return {
        "xc": np.ascontiguousarray(inp["x"][b, T0:T0 + NT_C]),
        "attn": np.ascontiguousarray(attn_full[b, T0:T0 + NT_C].reshape(NT_C, 520)),
        "yin": np.ascontiguousarray(y_full[b, T0:T0 + NT_C]),
        "cT2": np.ascontiguousarray(cT2),
        "wada_c": np.ascontiguousarray(inp["w_ada"][0][:, 2048:6144]),
        "bada_c": np.ascontiguousarray(inp["b_ada"][0][None, 2048:6144]),
        "rows_c": np.ascontiguousarray(rows.astype(np.float32)),
        "bglu": np.ascontiguousarray(inp["b_glu"][0][None, :]),
        "wglu": np.ascontiguousarray(inp["w_glu"][0]),
        "wout": np.ascontiguousarray(inp["w_out"][0]),
        "wfc1": np.ascontiguousarray(inp["w_fc1"][0]),
        "wfc2": np.ascontiguousarray(inp["w_fc2"][0]),
        "ident": _ident(),
    }


L_SEQ = 8192
NTILE = 64


def build_phase_ab(nc, with_ssm=True, dsts=None, dbg_tiles=NTILE, dbg_attn=True, dbg_stage=9):
    def din(name, shape, dt=F32):
        return nc.dram_tensor(name, list(shape), dt, kind="ExternalInput").ap()
    xb = din("xb", [L_SEQ, 1024])
    cT2 = din("cT2", [128, 16])
    wada = din("wada_a", [1024, 2048])
    bada = din("bada_a", [1, 2048])
    gmix = din("gmix", [1, 1024])
    win = din("win", [1024, 512])
    cosl = din("cosl", [128, 2048])
    sinl = din("sinl", [128, 2048])
    ind = din("ind", [32, L_SEQ])
    cmask = din("cmask", [128, 2048])
    ident = din("ident", [128, 128])
    if dsts is None:
        attn_o = nc.dram_tensor("attn_o", [L_SEQ, 130], F32, kind="ExternalOutput").ap()
        y_o = nc.dram_tensor("y_o", [L_SEQ, 128], F32, kind="ExternalOutput").ap()
    else:
        attn_o, y_o = dsts["attn"], dsts["y"]

    P = Prog(nc)
    op, dma = P.op, P.dma

    ident_bf = P.sb("ident_bf", [128, 128], BF16); b_ident = P.buf()
    ident_f = P.sb("ident_f", [128, 128], F32); b_identf = P.buf()
    ones_bf = P.sb("ones_bf", [1, 512], BF16); b_ones_bf = P.buf()
    zeros_bf = P.sb("zeros_bf", [1, 512], BF16); b_zeros_bf = P.buf()
    ones_f = P.sb("ones_f", [1, 128], F32); b_ones_f = P.buf()
    U_tok = P.sb("U_tok", [128, 8, 8, 8, 16], BF16); b_U = [P.buf() for _ in range(8)]
    dma(lambda e: e.dma_start(out=ident_bf[:], in_=ident[:, :]), writes=[b_ident], q="pool")
    dma(lambda e: e.dma_start(out=ident_f[:], in_=ident[:, :]), writes=[b_identf])
    op("dve", lambda e: e.memset(ones_f[:], 1.0), writes=[b_ones_f])
    op("dve", lambda e: e.memset(ones_bf[:], 1.0), writes=[b_ones_bf])
    op("dve", lambda e: e.memset(zeros_bf[:], 0.0), writes=[b_zeros_bf])

    P.push_scope()
    QaT = P.sb("QaT", [128, 2, L_SEQ], BF16); b_QaT = [P.buf() for _ in range(NTILE)]
    KaT = P.sb("KaT", [128, 2, L_SEQ], BF16); b_KaT = [P.buf() for _ in range(NTILE)]; b_Kind = P.buf()
    Vaug = P.sb("Vaug", [128, NTILE, 2, 65], BF16); b_V = [P.buf() for _ in range(NTILE)]; b_Vones = P.buf()
    for h in range(2):
        for cch in range(4):
            dma(lambda e: e.dma_start(out=KaT[64:96, h, cch * 2048:(cch + 1) * 2048], in_=ind[:, cch * 2048:(cch + 1) * 2048]),
                writes=[b_Kind], q="pool")
    op("pool", lambda e: e.memset(Vaug[:, :, :, 64:65], 1.0), writes=[b_Vones])

    P.push_scope()
    S1b = P.sb("S1b", [128, 1024], F32); b_S1b = P.buf()
    sh1T = P.sb("sh1T", [128, 16], BF16); b_sh1T = P.buf()
    Win = P.sb("Win", [128, 8, 512], BF16); b_Win = P.buf()
    bin_bf = P.sb("bin_bf", [1, 512], BF16); b_bin = P.buf()
    for k in range(8):
        dma(lambda e: e.dma_start(out=Win[:, k, :], in_=win[k * 128:(k + 1) * 128, :]), writes=[b_Win], q="pool")

    P.push_scope()
    ct = P.sb("ct", [128, 16], F32); b_ct = P.buf()
    sct = P.sb("sct", [128, 16], F32); b_sct = P.buf()
    sgc = P.sb("sgc", [128, 16], F32)
    modrow = P.sb("modrow", [1, 2048], F32); b_modrow = P.buf()
    badas = P.sb("badas", [1, 2048], F32); b_badas = P.buf()
    gmixs = P.sb("gmixs", [1, 1024], F32); b_gmixs = P.buf()
    s1row = P.sb("s1row", [1, 1024], F32); b_s1row = P.buf()
    pieces = [P.sb(f"wpiece{i}", [128, 8, 512], F32) for i in range(2)]
    b_pieces = [P.buf() for _ in range(2)]
    ps_row = [P.ps(f"ps_row{i}", [128, 512], F32) for i in range(2)]
    b_ps_row = [P.pbuf() for _ in range(2)]
    dma(lambda e: e.dma_start(out=ct[:], in_=cT2[:, :]), writes=[b_ct])
    dma(lambda e: e.dma_start(out=badas[:], in_=bada[:, :]), writes=[b_badas])
    dma(lambda e: e.dma_start(out=gmixs[:], in_=gmix[:, :]), writes=[b_gmixs])
    op("act", lambda e: e.activation(out=sgc[:], in_=ct[:], func=AF.Sigmoid), reads=[b_ct], writes=[b_sct])
    op("dve", lambda e: e.tensor_tensor(out=sct[:], in0=sgc[:], in1=ct[:], op=ALU.mult), reads=[b_ct, b_sct], writes=[b_sct])
    for j in range(4):
        pc, bpc = _rot(pieces, j), _rot(b_pieces, j)
        pr, bpr = _rot(ps_row, j), _rot(b_ps_row, j)
        dma(lambda e: e.dma_start(out=pc[:], in_=wada[:, j * 512:(j + 1) * 512].rearrange("(k p) n -> p k n", p=128)),
            writes=[bpc])
        for k in range(8):
            op("pe", lambda e: e.matmul(pr[0:1, :], lhsT=sct[:, 2 * k:2 * k + 1], rhs=pc[:, k, :],
                                        start=(k == 0), stop=(k == 7)),
               reads=[b_sct, bpc], writes=[bpr])
        op("dve", lambda e: e.tensor_tensor(out=modrow[0:1, j * 512:(j + 1) * 512], in0=pr[0:1, :],
                                            in1=badas[0:1, j * 512:(j + 1) * 512], op=ALU.add),
           reads=[bpr, b_badas], writes=[b_modrow])
    op("dve", lambda e: e.scalar_tensor_tensor(out=s1row[:], in0=modrow[0:1, 1024:2048], scalar=1.0,
                                                in1=gmixs[0:1, :], op0=ALU.add, op1=ALU.mult),
       reads=[b_modrow, b_gmixs], writes=[b_s1row])
    for hf in range(2):
        pr, bpr = _rot(ps_row, hf), _rot(b_ps_row, hf)
        op("pe", lambda e: e.matmul(pr[:, :], lhsT=ones_f[0:1, :], rhs=s1row[0:1, hf * 512:(hf + 1) * 512],
                                    start=True, stop=True), reads=[b_ones_f, b_s1row], writes=[bpr])
        op("act", lambda e: e.activation(out=S1b[:, hf * 512:(hf + 1) * 512], in_=pr[:, :], func=AF.Copy),
           reads=[bpr], writes=[b_S1b])
    pr, bpr = ps_row[0], b_ps_row[0]
    for k in range(8):
        op("pe", lambda e: e.matmul(pr[:, 2 * k:2 * k + 2], lhsT=modrow[0:1, k * 128:(k + 1) * 128],
                                    rhs=ones_f[0:1, 0:2], start=True, stop=True),
           reads=[b_ones_f, b_modrow], writes=[bpr])
    op("dve", lambda e: e.tensor_copy(out=sh1T[:], in_=pr[:, 0:16]), reads=[bpr], writes=[b_sh1T])
    pr, bpr = ps_row[1], b_ps_row[1]
    for k in range(8):
        op("pe", lambda e: e.matmul(pr[0:1, :], lhsT=sh1T[:, 2 * k:2 * k + 1], rhs=Win[:, k, :],
                                    start=(k == 0), stop=(k == 7)), reads=[b_sh1T, b_Win], writes=[bpr])
    op("act", lambda e: e.activation(out=bin_bf[:], in_=pr[0:1, :], func=AF.Copy), reads=[bpr], writes=[b_bin])
    P.pop_scope()

    xt = [P.sb(f"xt{i}", [128, 1024], F32) for i in range(3)]; b_xt = [P.buf() for _ in range(3)]
    hb = P.sb("hb", [128, 1024], BF16); b_hb = P.buf()
    junk = P.sb("junk", [128, 1024], BF16); b_junk = P.buf()
    ssx = P.sb("ssx", [128, 1], F32); b_ssx = P.buf()
    sqx = P.sb("sqx", [128, 1], F32); b_sqx = P.buf()
    rx = P.sb("rx", [128, 1], F32); b_rx = P.buf()
    hTs = P.sb("hTs", [128, 8, 1024], BF16); b_hTs = [P.buf() for _ in range(8)]
    cs = [P.sb(f"cs{i}", [128, 8, 32], F32) for i in range(2)]; b_cs = [P.buf() for _ in range(2)]
    sn = [P.sb(f"sn{i}", [128, 8, 32], F32) for i in range(2)]; b_sn = [P.buf() for _ in range(2)]
    qkr = P.sb("qkr", [128, 4, 2, 32], F32); b_qkr = P.buf()
    r1 = P.sb("r1", [128, 4, 32], F32); b_r1 = P.buf()
    r2 = P.sb("r2", [128, 4, 32], F32); b_r2 = P.buf()
    r3 = P.sb("r3", [128, 4, 32], F32); b_r3 = P.buf()
    r4 = P.sb("r4", [128, 4, 32], F32); b_r4 = P.buf()
    ksum = P.sb("ksum", [64, 2, 2], F32); b_ksum = [P.buf(), P.buf()]
    kmT = P.sb("kmT", [64, 2, 32], F32); b_kmT = P.buf()
    qTt = P.sb("qTt", [64, 2, 128], F32); b_qTt = P.buf()
    Gt = P.sb("Gt", [128, 2, 32], F32); b_Gt = P.buf()
    mx8 = P.sb("mx8", [128, 2, 8], F32); b_mx8 = P.buf()
    nm = P.sb("nm", [128, 2, 32], F32); b_nm = P.buf()
    Qtok = P.sb("Qtok", [128, 2, 96], BF16); b_Qtok = P.buf()
    pxt = P.ps("pxt", [128, 8, 128], BF16); b_pxt = P.pbuf()
    pqkv = P.ps("pqkv", [128, 512], F32); b_pqkv = P.pbuf()
    pT = P.ps("pT", [128, 4, 128], F32); b_pT = P.pbuf()
    pg = P.ps("pg", [128, 512], F32); b_pg = P.pbuf()
    pqa = P.ps("pqa", [128, 8, 128], BF16); b_pqa = P.pbuf()
    pu = P.ps("pu", [128, 8, 128], F32); b_pu = P.pbuf()
    op("dve", lambda e: e.memset(Gt[:], -1.0e30), writes=[b_Gt])
    op("dve", lambda e: e.memset(kmT[:], 0.0), writes=[b_kmT])

    def load_x(t):
        x_, bx = _rot(xt, t), _rot(b_xt, t)
        dma(lambda e: e.dma_start(out=x_[:], in_=xb[t * 128:(t + 1) * 128, :]), writes=[bx])

    if dbg_tiles > 0:
        load_x(0)
        load_x(1)
    for t in range(dbg_tiles):
        S_, ti = t // 8, t % 8
        j = t // 2
        if t + 2 < NTILE:
            load_x(t + 2)
        if ti == 0:
            c_, bc_ = _rot(cs, S_), _rot(b_cs, S_)
            s_, bs_ = _rot(sn, S_), _rot(b_sn, S_)
            dma(lambda e: e.dma_start(out=c_[:].rearrange("p i d -> p (i d)"), in_=cosl[:, S_ * 256:(S_ + 1) * 256]), writes=[bc_])
            dma(lambda e: e.dma_start(out=s_[:].rearrange("p i d -> p (i d)"), in_=sinl[:, S_ * 256:(S_ + 1) * 256]), writes=[bs_])
        c_, bc_ = _rot(cs, S_), _rot(b_cs, S_)
        s_, bs_ = _rot(sn, S_), _rot(b_sn, S_)
        x_, bx = _rot(xt, t), _rot(b_xt, t)
        tok = slice(t * 128, (t + 1) * 128)
        op("act", lambda e: e.activation(out=junk[:], in_=x_[:], func=AF.Square, accum_out=ssx[:, 0:1]),
           reads=[bx], writes=[b_junk, b_ssx])
        op("act", lambda e: e.activation(out=sqx[:], in_=ssx[:], func=AF.Sqrt, scale=1.0 / 1024.0, bias=EPS),
           reads=[b_ssx], writes=[b_sqx])
        op("dve", lambda e: e.reciprocal(out=rx[:], in_=sqx[:]), reads=[b_sqx], writes=[b_rx])
        op("dve", lambda e: e.scalar_tensor_tensor(out=hb[:], in0=x_[:], scalar=rx[:, 0:1], in1=S1b[:],
                                                    op0=ALU.mult, op1=ALU.mult),
           reads=[bx, b_rx, b_S1b], writes=[b_hb])
        for k in range(8):
            op("pe", lambda e: e.transpose(pxt[:, k, :], hb[:, k * 128:(k + 1) * 128], ident_bf[:]),
               reads=[b_hb, b_ident], writes=[b_pxt])
        op("act", lambda e: e.activation(out=hTs[:, :, ti * 128:(ti + 1) * 128], in_=pxt[:], func=AF.Copy),
           reads=[b_pxt], writes=[b_hTs[ti]])
        if dbg_stage < 2:
            continue
        op("pe", lambda e: e.matmul(pqkv[:, 0:384], lhsT=ones_bf[0:1, 0:128], rhs=bin_bf[0:1, 0:384], start=True, stop=False),
           reads=[b_ones_bf, b_bin], writes=[b_pqkv])
        for k in range(8):
            op("pe", lambda e: e.matmul(pqkv[:, 0:384], lhsT=hTs[:, k, ti * 128:(ti + 1) * 128], rhs=Win[:, k, 0:384],
                                        start=False, stop=(k == 7)),
               reads=[b_hTs[ti], b_Win], writes=[b_pqkv])
        op("act", lambda e: e.activation(out=Vaug[:, t, :, 0:64], in_=pqkv[:, 256:384].rearrange("p (h d) -> p h d", h=2),
                                         func=AF.Copy), reads=[b_pqkv], writes=[b_V[t]])
        if dbg_stage < 3:
            continue
        pq = pqkv[:, 0:256].rearrange("p (f two d) -> p f two d", f=4, two=2)
        cb = c_[:, ti, :].unsqueeze(1).to_broadcast([128, 4, 32])
        sb_ = s_[:, ti, :].unsqueeze(1).to_broadcast([128, 4, 32])
        import os
        _m = int(os.environ.get("DBG_R", "15"))
        if _m & 1:
            op("dve", lambda e: e.tensor_tensor(out=r1[:], in0=pq[:, :, 0, :], in1=cb, op=ALU.mult), reads=[b_pqkv, bc_], writes=[b_r1])
        if _m & 2:
            op("dve", lambda e: e.tensor_tensor(out=r2[:], in0=pq[:, :, 1, :], in1=sb_, op=ALU.mult), reads=[b_pqkv, bs_], writes=[b_r2])
        if _m & 4:
            op("dve", lambda e: e.tensor_tensor(out=r3[:], in0=pq[:, :, 0, :], in1=sb_, op=ALU.mult), reads=[b_pqkv, bs_], writes=[b_r3])
        if _m & 8:
            op("dve", lambda e: e.tensor_tensor(out=r4[:], in0=pq[:, :, 1, :], in1=cb, op=ALU.mult), reads=[b_pqkv, bc_], writes=[b_r4])
        if dbg_stage == 3:
            continue
        op("pool", lambda e: e.tensor_tensor(out=qkr[:, :, 0, :], in0=r1[:], in1=r2[:], op=ALU.subtract),
           reads=[b_r1, b_r2], writes=[b_qkr])
        op("pool", lambda e: e.tensor_tensor(out=qkr[:, :, 1, :], in0=r3[:], in1=r4[:], op=ALU.add),
           reads=[b_r3, b_r4], writes=[b_qkr])
        qkf = qkr[:].rearrange("p f two d -> p (f two d)")
        if dbg_stage < 4:
            continue
        for h in range(2):
            op("pe", lambda e: e.transpose(pT[0:64, h, :], qkf[:, 128 + h * 64:128 + (h + 1) * 64], ident_f[:]),
               reads=[b_qkr, b_identf], writes=[b_pT])
        if j >= 4:
            for h in range(2):
                op("pe", lambda e: e.transpose(pT[0:64, 2 + h, :], qkf[:, h * 64:(h + 1) * 64], ident_f[:]),
                   reads=[b_qkr, b_identf], writes=[b_pT])
        op("act", lambda e: e.activation(out=KaT[0:64, :, tok], in_=pT[0:64, 0:2, :], func=AF.Copy),
           reads=[b_pT], writes=[b_KaT[t]])
        par = t % 2
        op("dve", lambda e: e.tensor_reduce(out=ksum[:, par, :], in_=pT[0:64, 0:2, :], axis=mybir.AxisListType.X, op=ALU.add),
           reads=[b_pT], writes=[b_ksum[par]])
        if dbg_stage < 5:
            continue
        if j >= 4:
            op("act", lambda e: e.activation(out=qTt[:], in_=pT[0:64, 2:4, :], func=AF.Copy), reads=[b_pT], writes=[b_qTt])
            for h in range(2):
                op("pe", lambda e: e.matmul(pg[:, h * 32:(h + 1) * 32], lhsT=qTt[:, h, :], rhs=kmT[:, h, :], start=True, stop=True),
                   reads=[b_qTt, b_kmT], writes=[b_pg])
            op("dve", lambda e: e.tensor_copy(out=Gt[:, :, 0:j], in_=pg[:, 0:64].rearrange("p (h n) -> p h n", h=2)[:, :, 0:j]),
               reads=[b_pg], writes=[b_Gt])
            for h in range(2):
                op("dve", lambda e: e.max(out=mx8[:, h, :], in_=Gt[:, h, :]), reads=[b_Gt], writes=[b_mx8])
            for h in range(2):
                op("dve", lambda e: e.tensor_scalar(out=nm[:, h, :], in0=Gt[:, h, :], scalar1=mx8[:, h, 2:3], scalar2=1.0,
                                                    op0=ALU.is_ge, op1=ALU.subtract), reads=[b_Gt, b_mx8], writes=[b_nm])
            op("dve", lambda e: e.memset(nm[:, :, j:j + 1], 0.0), writes=[b_nm])
            op("dve", lambda e: e.tensor_scalar(out=Qtok[:, :, 64:96], in0=nm[:], scalar1=-NEGBIG, scalar2=None, op0=ALU.mult),
               reads=[b_nm], writes=[b_Qtok])
        else:
            op("dve", lambda e: e.memset(Qtok[:, :, 64:96], NEGBIG), writes=[b_Qtok])
            op("dve", lambda e: e.memset(Qtok[:, :, 64:64 + j + 1], 0.0), writes=[b_Qtok])
        op("pool", lambda e: e.tensor_scalar(out=Qtok[:, :, 0:64], in0=qkf[:, 0:128].rearrange("p (h d) -> p h d", h=2),
                                             scalar1=0.125, scalar2=None, op0=ALU.mult),
           reads=[b_qkr], writes=[b_Qtok])
        for h in range(2):
            op("pe", lambda e: e.transpose(pqa[0:96, h, :], Qtok[:, h, :], ident_bf[:]), reads=[b_Qtok, b_ident], writes=[b_pqa])
        op("act", lambda e: e.activation(out=QaT[0:96, :, tok], in_=pqa[0:96, 0:2, :], func=AF.Copy),
           reads=[b_pqa], writes=[b_QaT[t]])
        if par == 1:
            op("dve", lambda e: e.tensor_tensor(out=kmT[:, :, j], in0=ksum[:, 0, :], in1=ksum[:, 1, :], op=ALU.add),
               reads=[b_ksum[0], b_ksum[1]], writes=[b_kmT])
        if ti == 7 and with_ssm:
            for s in range(8):
                op("pe", lambda e: e.matmul(pu[:, s, :], lhsT=ones_bf[0:1, 0:128], rhs=bin_bf[0:1, 384:512], start=True, stop=False),
                   reads=[b_ones_bf, b_bin], writes=[b_pu])
                for k in range(8):
                    lhs = hTs[:, k, :].rearrange("p (c s) -> p s c", s=8)[:, s, :]
                    op("pe", lambda e: e.matmul(pu[:, s, :], lhsT=lhs, rhs=Win[:, k, 384:512], start=False, stop=(k == 7)),
                       reads=b_hTs + [b_Win], writes=[b_pu])
            op("act", lambda e: e.activation(out=U_tok[:, S_].rearrange("p g s m -> p s g m"),
                                             in_=pu[:].rearrange("p s (g m) -> p s g m", g=8), func=AF.Copy),
               reads=[b_pu], writes=[b_U[S_]])
    P.pop_scope()

    P.push_scope()
    cm = P.sb("cm", [128, 4, 512], BF16); b_cm = P.buf()
    dma(lambda e: e.dma_start(out=cm[:].rearrange("p a c -> p (a c)"), in_=cmask[:, :]), writes=[b_cm], q="pool")
    Pt = [P.sb(f"Pt{i}", [128, 512], BF16) for i in range(3)]; b_Pt = [P.buf() for _ in range(3)]
    Osb = [P.sb(f"Osb{i}", [128, 4, 65], F32) for i in range(2)]; b_Osb = [P.buf() for _ in range(2)]
    pS = [P.ps(f"pS{i}", [128, 512], F32) for i in range(3)]; b_pS = [P.pbuf() for _ in range(3)]
    pO = [P.ps(f"pO{i}", [128, 512], F32) for i in range(2)]; b_pO = [P.pbuf() for _ in range(2)]
    b_allQ, b_allK, b_allV = b_QaT, b_KaT + [b_Kind], b_V + [b_Vones]
    cnt = 0
    gi = 0
    for h in range(2 if dbg_attn else 0):
        for G in range(16 if dbg_attn is True else dbg_attn):
            po, bpo = _rot(pO, gi), _rot(b_pO, gi)
            ob, bob = _rot(Osb, gi), _rot(b_Osb, gi)
            gi += 1
            nkt = 4 * G + 4
            qs = slice(G * 512, (G + 1) * 512)
            qbufs = b_QaT[4 * G:4 * G + 4]
            op("pe", lambda e: e.matmul(po[:, 0:260], lhsT=zeros_bf[0:1, 0:128], rhs=zeros_bf[0:1, 0:260], start=True, stop=False),
               reads=[b_zeros_bf], writes=[bpo])

            def s_mm(kt, c):
                ps_, bps = _rot(pS, c), _rot(b_pS, c)
                op("pe", lambda e: e.matmul(ps_[:], lhsT=KaT[0:96, h, kt * 128:(kt + 1) * 128], rhs=QaT[0:96, h, qs],
                                            start=True, stop=True),
                   reads=[b_KaT[kt], b_Kind] + qbufs, writes=[bps])
            s_mm(0, cnt)
            for kt in range(nkt):
                c = cnt + kt
                if kt + 1 < nkt:
                    s_mm(kt + 1, c + 1)
                ps_, bps = _rot(pS, c), _rot(b_pS, c)
                p_, bp_ = _rot(Pt, c), _rot(b_Pt, c)
                op("act", lambda e: e.activation(out=p_[:], in_=ps_[:], func=AF.Exp), reads=[bps], writes=[bp_])
                a = kt - 4 * G
                if a >= 0:
                    op("pool", lambda e: e.tensor_tensor(out=p_[:], in0=p_[:], in1=cm[:, a, :], op=ALU.mult),
                       reads=[bp_, b_cm], writes=[bp_])
                for bq in range(4):
                    if a >= 0 and bq < a:
                        continue
                    last = (kt == nkt - 1)
                    op("pe", lambda e: e.matmul(po[:, bq * 65:(bq + 1) * 65], lhsT=p_[:, bq * 128:(bq + 1) * 128],
                                                rhs=Vaug[:, kt, h, :], start=False, stop=last),
                       reads=[bp_, b_V[kt], b_Vones], writes=[bpo])
            cnt += nkt
            op("dve", lambda e: e.tensor_copy(out=ob[:].rearrange("p b e -> p (b e)"), in_=po[:, 0:260]), reads=[bpo], writes=[bob])
            dma(lambda e: e.dma_start(out=attn_o[G * 512:(G + 1) * 512, h * 65:(h + 1) * 65].rearrange("(b p) e -> p b e", p=128),
                                      in_=ob[:]), reads=[bob])
    P.pop_scope()
    P.pop_scope()

    if with_ssm:
        build_ssm(nc, P, U_tok, b_U, y_o, ident_bf, b_ident, ident_f, b_identf)
    P.finish()
    return nc


def rope_tables():
    half = 32
    inv = (10000.0 ** (-np.arange(half, dtype=np.float32) / half)).astype(np.float32)
    ang = np.arange(L_SEQ, dtype=np.float32)[:, None] * inv[None, :]
    cos, sin = np.cos(ang).astype(np.float32), np.sin(ang).astype(np.float32)
    cl = np.ascontiguousarray(cos.reshape(NTILE, 128, half).transpose(1, 0, 2).reshape(128, NTILE * half))
    sl = np.ascontiguousarray(sin.reshape(NTILE, 128, half).transpose(1, 0, 2).reshape(128, NTILE * half))
    return cl, sl


def const_tables():
    ind = np.zeros((32, L_SEQ), np.float32)
    for j in range(32):
        ind[j, j * 256:(j + 1) * 256] = 1.0
    cm = np.zeros((128, 4, 512), np.float32)
    kk = np.arange(128)[:, None]
    qq = np.arange(128)[None, :]
    tri = (kk <= qq).astype(np.float32)
    for a in range(4):
        for bq in range(4):
            if bq > a:
                cm[:, a, bq * 128:(bq + 1) * 128] = 1.0
            elif bq == a:
                cm[:, a, bq * 128:(bq + 1) * 128] = tri
    return ind, cm.reshape(128, 2048)


def phase_ab_inputs(inp, b, r):
    c = inp["c"][b]
    cT = np.ascontiguousarray(c.reshape(8, 128).T)
    cT2 = np.repeat(cT[:, :, None], 2, axis=2).reshape(128, 16)
    w = inp["w_in"][0]
    cols = np.concatenate([np.arange(128 * r, 128 * r + 128), 512 + np.arange(128 * r, 128 * r + 128),
                           1024 + np.arange(128 * r, 128 * r + 128), 1536 + np.arange(128 * r, 128 * r + 128)])
    cl, sl = rope_tables()
    ind, cm = const_tables()
    d = {
        "xb": np.ascontiguousarray(inp["x"][b]),
        "cT2": np.ascontiguousarray(cT2),
        "wada_a": np.ascontiguousarray(inp["w_ada"][0][:, 0:2048]),
        "bada_a": np.ascontiguousarray(inp["b_ada"][0][None, 0:2048]),
        "gmix": np.ascontiguousarray(inp["g_mix"][0][None, :]),
        "win": np.ascontiguousarray(w[:, cols]),
        "cosl": cl, "sinl": sl, "ind": ind, "cmask": cm, "ident": _ident(),
    }
    d.update(ssm_inputs(inp, r))
    return d


KV_F, KV_G, KV_O, KV_E, KV_A, KV_H = 0, 8, 16, 24, 32, 42
NKV = 43
TWO_PI = 2.0 * math.pi
CW1 = 6.28125
CW2 = TWO_PI - CW1


def ssm_kvec():
    kv = np.zeros(NKV, np.float32)
    for s in range(8):
        kv[KV_F + s] = -s
        kv[KV_G + s] = s
        kv[KV_O + s] = s + 1
        kv[KV_E + s] = 7 - s
    for i in range(10):
        kv[KV_A + i] = 8.0 * (2 ** i)
    kv[KV_H] = 0.5
    return kv


def build_ssm(nc, P, U_tok, b_U, y_o, ident_bf, b_ident, ident_f, b_identf):
    def din(name, shape, dt=F32):
        return nc.dram_tensor(name, list(shape), dt, kind="ExternalInput").ap()
    sp_small = din("ssm_small", [128, 8 * 4 + NKV + 2 + 8])
    sp_bc = din("ssm_bc", [128, 4 * 128])
    sp_mat = din("ssm_mat", [128, 256])
    op, dma = P.op, P.dma
    P.push_scope()
    small = P.sb("ssm_small_sb", [128, 8 * 4 + NKV + 2 + 8], F32); b_small = P.buf()
    bc = P.sb("ssm_bc_sb", [128, 4, 8, 16], F32); b_bc = P.buf()
    mats = P.sb("ssm_mat_sb", [128, 2, 128], F32); b_mats = P.buf()
    dma(lambda e: e.dma_start(out=small[:], in_=sp_small[:, :]), writes=[b_small])
    dma(lambda e: e.dma_start(out=bc[:].rearrange("p a g m -> p (a g m)"), in_=sp_bc[:, :]), writes=[b_bc])
    dma(lambda e: e.dma_start(out=mats[:].rearrange("p a n -> p (a n)"), in_=sp_mat[:, :]), writes=[b_mats])
    lamre, lamim, logdt, dsk = small[:, 0:8], small[:, 8:16], small[:, 16:24], small[:, 24:32]
    kvec = small[:, 32:32 + NKV]
    sgn_a, sgn_b = small[:, 32 + NKV:33 + NKV], small[:, 33 + NKV:34 + NKV]
    smask, P2 = mats[:, 0, :], mats[:, 1, :]
    Bs1, Bs2, Cs1, Cs2 = bc[:, 0], bc[:, 1], bc[:, 2], bc[:, 3]

    def T(name, shape, dt=F32):
        return P.sb(name, shape, dt), P.buf()

    def dv(fn, reads, writes):
        return op("dve", fn, reads=reads, writes=writes)

    dt_, b_dt = T("s_dt", [128, 8])
    a_, b_a = T("s_a", [128, 8])
    th, b_th = T("s_th", [128, 8])
    op("act", lambda e: e.activation(out=dt_[:], in_=logdt, func=AF.Exp), reads=[b_small], writes=[b_dt])
    dv(lambda e: e.tensor_tensor(out=a_[:], in0=lamre, in1=dt_[:], op=ALU.mult), [b_small, b_dt], [b_a])
    dv(lambda e: e.tensor_tensor(out=th[:], in0=lamim, in1=dt_[:], op=ALU.mult), [b_small, b_dt], [b_th])
    KA, b_KA = T("s_KA", [128, 8, NKV])
    KT, b_KT = T("s_KT", [128, 8, NKV])
    kb = kvec.unsqueeze(1).to_broadcast([128, 8, NKV])
    dv(lambda e: e.tensor_tensor(out=KA[:], in0=a_[:].unsqueeze(2).to_broadcast([128, 8, NKV]), in1=kb, op=ALU.mult),
       [b_a, b_small], [b_KA])
    dv(lambda e: e.tensor_tensor(out=KT[:], in0=th[:].unsqueeze(2).to_broadcast([128, 8, NKV]), in1=kb, op=ALU.mult),
       [b_th, b_small], [b_KT])
    MAG, b_MAG = T("s_MAG", [128, 8, NKV])
    op("act", lambda e: e.activation(out=MAG[:], in_=KA[:], func=AF.Exp), reads=[b_KA], writes=[b_MAG])
    ni, b_ni = T("s_ni", [128, 8, NKV], I32)
    nf, b_nf = T("s_nf", [128, 8, NKV])
    rr, b_rr = T("s_rr", [128, 8, NKV])
    uu, b_uu = T("s_uu", [128, 8, NKV])
    SIN, b_SIN = T("s_SIN", [128, 8, NKV])
    COS, b_COS = T("s_COS", [128, 8, NKV])

    def sin_of(dst, b_dst, shift):
        dv(lambda e: e.tensor_scalar(out=uu[:], in0=KT[:], scalar1=shift, scalar2=1.0 / TWO_PI, op0=ALU.add, op1=ALU.mult),
           [b_KT], [b_uu])
        dv(lambda e: e.tensor_copy(out=ni[:], in_=uu[:]), [b_uu], [b_ni])
        dv(lambda e: e.tensor_copy(out=nf[:], in_=ni[:]), [b_ni], [b_nf])
        dv(lambda e: e.scalar_tensor_tensor(out=rr[:], in0=nf[:], scalar=-CW1, in1=KT[:], op0=ALU.mult, op1=ALU.add),
           [b_nf, b_KT], [b_rr])
        dv(lambda e: e.scalar_tensor_tensor(out=rr[:], in0=nf[:], scalar=-CW2, in1=rr[:], op0=ALU.mult, op1=ALU.add),
           [b_nf, b_rr], [b_rr])
        dv(lambda e: e.tensor_scalar(out=rr[:], in0=rr[:], scalar1=shift, scalar2=math.pi, op0=ALU.add, op1=ALU.min),
           [b_rr], [b_rr])
        dv(lambda e: e.tensor_scalar(out=rr[:], in0=rr[:], scalar1=-math.pi, scalar2=None, op0=ALU.max), [b_rr], [b_rr])
        op("act", lambda e: e.activation(out=dst[:], in_=rr[:], func=AF.Sin), reads=[b_rr], writes=[b_dst])
    sin_of(SIN, b_SIN, 0.0)
    sin_of(COS, b_COS, math.pi / 2.0)
    PR, b_PR = T("s_PR", [128, 8, NKV])
    PI_, b_PI = T("s_PI", [128, 8, NKV])
    dv(lambda e: e.tensor_tensor(out=PR[:], in0=MAG[:], in1=COS[:], op=ALU.mult), [b_MAG, b_COS], [b_PR])
    dv(lambda e: e.tensor_tensor(out=PI_[:], in0=MAG[:], in1=SIN[:], op=ALU.mult), [b_MAG, b_SIN], [b_PI])
    em1, b_em1 = T("s_em1", [128, 8])
    tq, b_tq = T("s_tq", [128, 8])
    dv(lambda e: e.tensor_scalar(out=tq[:], in0=a_[:], scalar1=0.25, scalar2=1.0, op0=ALU.mult, op1=ALU.add), [b_a], [b_tq])
    dv(lambda e: e.tensor_tensor(out=tq[:], in0=tq[:], in1=a_[:], op=ALU.mult), [b_tq, b_a], [b_tq])
    dv(lambda e: e.tensor_scalar(out=tq[:], in0=tq[:], scalar1=1.0 / 3.0, scalar2=1.0, op0=ALU.mult, op1=ALU.add), [b_tq], [b_tq])
    dv(lambda e: e.tensor_tensor(out=tq[:], in0=tq[:], in1=a_[:], op=ALU.mult), [b_tq, b_a], [b_tq])
    dv(lambda e: e.tensor_scalar(out=tq[:], in0=tq[:], scalar1=0.5, scalar2=1.0, op0=ALU.mult, op1=ALU.add), [b_tq], [b_tq])
    dv(lambda e: e.tensor_tensor(out=em1[:], in0=tq[:], in1=a_[:], op=ALU.mult), [b_tq, b_a], [b_em1])
    cth, sth, shalf = COS[:, :, KV_G + 1], SIN[:, :, KV_G + 1], SIN[:, :, KV_H]
    re1, b_re1 = T("s_re1", [128, 8])
    im1, b_im1 = T("s_im1", [128, 8])
    w1, b_w1 = T("s_w1", [128, 8])
    w2, b_w2 = T("s_w2", [128, 8])
    dv(lambda e: e.tensor_tensor(out=w1[:], in0=shalf, in1=shalf, op=ALU.mult), [b_SIN], [b_w1])
    dv(lambda e: e.tensor_tensor(out=re1[:], in0=em1[:], in1=cth, op=ALU.mult), [b_em1, b_COS], [b_re1])
    dv(lambda e: e.scalar_tensor_tensor(out=re1[:], in0=w1[:], scalar=-2.0, in1=re1[:], op0=ALU.mult, op1=ALU.add),
       [b_w1, b_re1], [b_re1])
    dv(lambda e: e.scalar_tensor_tensor(out=im1[:], in0=em1[:], scalar=1.0, in1=sth, op0=ALU.add, op1=ALU.mult),
       [b_em1, b_SIN], [b_im1])
    den, b_den = T("s_den", [128, 8])
    dv(lambda e: e.tensor_tensor(out=den[:], in0=lamre, in1=lamre, op=ALU.mult), [b_small], [b_den])
    dv(lambda e: e.tensor_tensor(out=w1[:], in0=lamim, in1=lamim, op=ALU.mult), [b_small], [b_w1])
    dv(lambda e: e.tensor_tensor(out=den[:], in0=den[:], in1=w1[:], op=ALU.add), [b_den, b_w1], [b_den])
    dv(lambda e: e.reciprocal(out=den[:], in_=den[:]), [b_den], [b_den])
    cr, b_cr = T("s_cr", [128, 8])
    ci, b_ci = T("s_ci", [128, 8])
    dv(lambda e: e.tensor_tensor(out=w1[:], in0=re1[:], in1=lamre, op=ALU.mult), [b_re1, b_small], [b_w1])
    dv(lambda e: e.tensor_tensor(out=w2[:], in0=im1[:], in1=lamim, op=ALU.mult), [b_im1, b_small], [b_w2])
    dv(lambda e: e.tensor_tensor(out=w1[:], in0=w1[:], in1=w2[:], op=ALU.add), [b_w1, b_w2], [b_w1])
    dv(lambda e: e.tensor_tensor(out=cr[:], in0=w1[:], in1=den[:], op=ALU.mult), [b_w1, b_den], [b_cr])
    dv(lambda e: e.tensor_tensor(out=w1[:], in0=im1[:], in1=lamre, op=ALU.mult), [b_im1, b_small], [b_w1])
    dv(lambda e: e.tensor_tensor(out=w2[:], in0=re1[:], in1=lamim, op=ALU.mult), [b_re1, b_small], [b_w2])
    dv(lambda e: e.tensor_tensor(out=w1[:], in0=w1[:], in1=w2[:], op=ALU.subtract), [b_w1, b_w2], [b_w1])
    dv(lambda e: e.tensor_tensor(out=ci[:], in0=w1[:], in1=den[:], op=ALU.mult), [b_w1, b_den], [b_ci])
    bb1, b_bb1 = T("s_bb1", [128, 8, 16])
    bb2, b_bb2 = T("s_bb2", [128, 8, 16])
    q1, b_q1 = T("s_q1", [128, 8, 16])
    q2, b_q2 = T("s_q2", [128, 8, 16])
    crb = cr[:].unsqueeze(2).to_broadcast([128, 8, 16])
    cib = ci[:].unsqueeze(2).to_broadcast([128, 8, 16])
    dv(lambda e: e.tensor_tensor(out=q1[:], in0=crb, in1=Bs1, op=ALU.mult), [b_cr, b_bc], [b_q1])
    dv(lambda e: e.tensor_tensor(out=q2[:], in0=cib, in1=Bs2, op=ALU.mult), [b_ci, b_bc], [b_q2])
    dv(lambda e: e.scalar_tensor_tensor(out=bb1[:].rearrange("p g m -> p (g m)"), in0=q2[:].rearrange("p g m -> p (g m)"),
                                         scalar=sgn_a, in1=q1[:].rearrange("p g m -> p (g m)"), op0=ALU.mult, op1=ALU.add),
       [b_q1, b_q2, b_small], [b_bb1])
    dv(lambda e: e.tensor_tensor(out=q1[:], in0=crb, in1=Bs2, op=ALU.mult), [b_cr, b_bc], [b_q1])
    dv(lambda e: e.tensor_tensor(out=q2[:], in0=cib, in1=Bs1, op=ALU.mult), [b_ci, b_bc], [b_q2])
    dv(lambda e: e.scalar_tensor_tensor(out=bb2[:].rearrange("p g m -> p (g m)"), in0=q2[:].rearrange("p g m -> p (g m)"),
                                         scalar=sgn_b, in1=q1[:].rearrange("p g m -> p (g m)"), op0=ALU.mult, op1=ALU.add),
       [b_q1, b_q2, b_small], [b_bb2])
    CA, b_CA = T("s_CA", [128, 8, 16])
    CB, b_CB = T("s_CB", [128, 8, 16])
    dv(lambda e: e.tensor_scalar(out=CA[:], in0=Cs1, scalar1=sgn_b, scalar2=None, op0=ALU.mult), [b_bc, b_small], [b_CA])
    dv(lambda e: e.tensor_scalar(out=CB[:], in0=Cs2, scalar1=-1.0, scalar2=None, op0=ALU.mult), [b_bc], [b_CB])
    Fm, b_Fm = T("s_F", [128, 8, 8, 16])
    Fe, b_Fe = T("s_Fe", [128, 8, 8, 16])
    Gm, b_Gm = T("s_G", [128, 8, 8, 16])
    Om, b_Om = T("s_O", [128, 8, 8, 16])
    z1, b_z1 = T("s_z1", [128, 8, 8, 16])
    z2, b_z2 = T("s_z2", [128, 8, 8, 16])

    def outer(dst, bdst, kv0, v1, bv1, v2, bv2, sgn):
        pr = PR[:, :, kv0:kv0 + 8].unsqueeze(3).to_broadcast([128, 8, 8, 16])
        pi = PI_[:, :, kv0:kv0 + 8].unsqueeze(3).to_broadcast([128, 8, 8, 16])
        dv(lambda e: e.tensor_tensor(out=z1[:], in0=pr, in1=v1[:].unsqueeze(2).to_broadcast([128, 8, 8, 16]), op=ALU.mult),
           [b_PR, bv1], [b_z1])
        dv(lambda e: e.tensor_tensor(out=z2[:], in0=pi, in1=v2[:].unsqueeze(2).to_broadcast([128, 8, 8, 16]), op=ALU.mult),
           [b_PI, bv2], [b_z2])
        fl = "p g s m -> p (g s m)"
        if sgn is None:
            dv(lambda e: e.tensor_tensor(out=dst[:].rearrange(fl), in0=z1[:].rearrange(fl), in1=z2[:].rearrange(fl), op=ALU.add),
               [b_z1, b_z2], [bdst])
        else:
            dv(lambda e: e.scalar_tensor_tensor(out=dst[:].rearrange(fl), in0=z2[:].rearrange(fl), scalar=sgn,
                                                 in1=z1[:].rearrange(fl), op0=ALU.mult, op1=ALU.add),
               [b_z1, b_z2, b_small], [bdst])
    outer(Fm, b_Fm, KV_F, bb1, b_bb1, bb2, b_bb2, sgn_a)
    outer(Fe, b_Fe, KV_E, bb1, b_bb1, bb2, b_bb2, sgn_a)
    outer(Gm, b_Gm, KV_G, CA, b_CA, CB, b_CB, None)
    outer(Om, b_Om, KV_O, CA, b_CA, CB, b_CB, None)
    PIA, b_PIA = T("s_PIA", [128, 8, 10])
    dv(lambda e: e.tensor_scalar(out=PIA[:], in0=PI_[:, :, KV_A:KV_A + 10], scalar1=sgn_b, scalar2=None, op0=ALU.mult),
       [b_PI, b_small], [b_PIA])

    Yout = P.sb("s_Yout", [128, 8, 8, 128], F32); b_Yout = P.buf()
    Ug = P.sb("s_Ug", [128, 1024], BF16); b_Ug = P.buf()
    H = P.sb("s_H", [128, 1024], F32); b_H = P.buf()
    Ysb = P.sb("s_Ysb", [128, 1024], F32); b_Ysb = P.buf()
    Mbf = P.sb("s_Mbf", [128, 128], BF16); b_Mbf = P.buf()
    Ebf = P.sb("s_Ebf", [128, 128], BF16); b_Ebf = P.buf()
    mtmp = P.sb("s_mtmp", [128, 128], F32); b_mtmp = P.buf()
    Amat = P.sb("s_Amat", [128, 10, 128], F32); b_Amat = P.buf()
    put = P.ps("s_put", [128, 8, 128], BF16); b_put = P.pbuf()
    pxy = [P.ps(f"s_pxy{i}", [128, 512], F32) for i in range(2)]; b_pxy = [P.pbuf() for _ in range(2)]
    psc = [P.ps(f"s_psc{i}", [128, 512], F32) for i in range(2)]; b_psc = [P.pbuf() for _ in range(2)]
    pme = P.ps("s_pme", [128, 512], F32); b_pme = P.pbuf()
    pyt = P.ps("s_pyt", [128, 8, 128], F32); b_pyt = P.pbuf()
    for g in range(8):
        Fg = Fm[:, g].rearrange("p s m -> p (s m)")
        Feg = Fe[:, g].rearrange("p s m -> p (s m)")
        Gg = Gm[:, g].rearrange("p s m -> p (s m)")
        Og = Om[:, g].rearrange("p s m -> p (s m)")
        op("pe", lambda e: e.matmul(pme[:, 0:128], lhsT=Fg, rhs=Gg, start=True, stop=True), reads=[b_Fm, b_Gm], writes=[b_pme])
        dv(lambda e: e.tensor_tensor(out=mtmp[:], in0=pme[:, 0:128], in1=smask, op=ALU.mult), [b_pme, b_mats], [b_mtmp])
        dv(lambda e: e.scalar_tensor_tensor(out=Mbf[:], in0=ident_f[:], scalar=dsk[:, g:g + 1], in1=mtmp[:],
                                             op0=ALU.mult, op1=ALU.add), [b_identf, b_small, b_mtmp], [b_Mbf])
        op("pe", lambda e: e.transpose(pme[:, 128:256], Feg, ident_f[:]), reads=[b_Fe, b_identf], writes=[b_pme])
        op("act", lambda e: e.activation(out=Ebf[:], in_=pme[:, 128:256], func=AF.Copy), reads=[b_pme], writes=[b_Ebf])
        for i in range(10):
            dv(lambda e: e.tensor_scalar(out=mtmp[:], in0=ident_f[:], scalar1=PR[:, g, KV_A + i:KV_A + i + 1], scalar2=None,
                                         op0=ALU.mult), [b_identf, b_PR], [b_mtmp])
            dv(lambda e: e.scalar_tensor_tensor(out=Amat[:, i, :], in0=P2, scalar=PIA[:, g, i:i + 1], in1=mtmp[:],
                                                 op0=ALU.mult, op1=ALU.add), [b_mats, b_PIA, b_mtmp], [b_Amat])
        for S in range(8):
            op("pe", lambda e: e.transpose(put[:, S, :], U_tok[:, S, g].rearrange("p s m -> p (s m)"), ident_bf[:]),
               reads=[b_U[S], b_ident], writes=[b_put])
        op("act", lambda e: e.activation(out=Ug[:].rearrange("p (S c) -> p S c", S=8), in_=put[:], func=AF.Copy),
           reads=[b_put], writes=[b_Ug])
        for hf in range(2):
            op("pe", lambda e: e.matmul(pxy[hf][:], lhsT=Ebf[:], rhs=Ug[:, hf * 512:(hf + 1) * 512], start=True, stop=True),
               reads=[b_Ebf, b_Ug], writes=[b_pxy[hf]])
            op("act", lambda e: e.activation(out=H[:, hf * 512:(hf + 1) * 512], in_=pxy[hf][:], func=AF.Copy),
               reads=[b_pxy[hf]], writes=[b_H])
        for i in range(10):
            d = 1 << i
            rngs = []
            for hb in range(2):
                lo = max(d, hb * 512)
                hi = (hb + 1) * 512
                if lo < hi:
                    rngs.append((hb, lo, hi))
            for (hb, lo, hi) in rngs:
                op("pe", lambda e: e.matmul(psc[hb][:, lo - hb * 512:hi - hb * 512], lhsT=Amat[:, i, :], rhs=H[:, lo - d:hi - d],
                                            start=True, stop=True), reads=[b_Amat, b_H], writes=[b_psc[hb]])
            for (hb, lo, hi) in rngs:
                dv(lambda e: e.tensor_tensor(out=H[:, lo:hi], in0=psc[hb][:, lo - hb * 512:hi - hb * 512], in1=H[:, lo:hi], op=ALU.add),
                   [b_psc[hb], b_H], [b_H])
        for hf in range(2):
            op("pe", lambda e: e.matmul(pxy[hf][:], lhsT=Mbf[:], rhs=Ug[:, hf * 512:(hf + 1) * 512], start=True, stop=False),
               reads=[b_Mbf, b_Ug], writes=[b_pxy[hf]])
            if hf == 0:
                op("pe", lambda e: e.matmul(pxy[0][:, 1:512], lhsT=Og, rhs=H[:, 0:511], start=False, stop=True),
                   reads=[b_Om, b_H], writes=[b_pxy[0]])
            else:
                op("pe", lambda e: e.matmul(pxy[1][:], lhsT=Og, rhs=H[:, 511:1023], start=False, stop=True),
                   reads=[b_Om, b_H], writes=[b_pxy[1]])
            op("act", lambda e: e.activation(out=Ysb[:, hf * 512:(hf + 1) * 512], in_=pxy[hf][:], func=AF.Copy),
               reads=[b_pxy[hf]], writes=[b_Ysb])
        for S in range(8):
            op("pe", lambda e: e.transpose(pyt[:, S, :], Ysb[:, S * 128:(S + 1) * 128], ident_f[:]),
               reads=[b_Ysb, b_identf], writes=[b_pyt])
        dv(lambda e: e.tensor_copy(out=Yout[:, :, :, g * 16:(g + 1) * 16],
                                   in_=pyt[:].rearrange("p S (t n) -> p S t n", t=8)), [b_pyt], [b_Yout])
    for S in range(8):
        dma(lambda e: e.dma_start(out=y_o[S * 1024:(S + 1) * 1024, :].rearrange("(c t) ch -> c t ch", t=8), in_=Yout[:, S]),
            reads=[b_Yout])
    P.pop_scope()


def ssm_inputs(inp, r):
    gs = slice(8 * r, 8 * r + 8)
    lam_re = inp["lam_re"][0][gs]
    lam_im = inp["lam_im"][0][gs]
    log_dt = inp["log_dt"][0][gs]
    b_re, b_im = inp["b_re"][0][gs], inp["b_im"][0][gs]
    c_re, c_im = inp["c_re"][0][gs], inp["c_im"][0][gs]
    d = inp["d_skip"][0][gs]
    dup = lambda a: np.concatenate([a, a], axis=0)
    small = np.zeros((128, 8 * 4 + NKV + 2 + 8), np.float32)
    small[:, 0:8] = dup(lam_re.T)
    small[:, 8:16] = dup(lam_im.T)
    small[:, 16:24] = np.broadcast_to(log_dt[None, :], (128, 8))
    small[:, 24:32] = np.tile(d.T, (8, 1))
    small[:, 32:32 + NKV] = ssm_kvec()[None, :]
    small[:64, 32 + NKV] = -1.0; small[64:, 32 + NKV] = 1.0
    small[:64, 33 + NKV] = 1.0; small[64:, 33 + NKV] = -1.0
    bre = b_re.transpose(1, 0, 2); bim = b_im.transpose(1, 0, 2)
    cre = c_re.transpose(2, 0, 1); cim = c_im.transpose(2, 0, 1)
    bcv = np.zeros((128, 4, 8, 16), np.float32)
    bcv[:64, 0], bcv[64:, 0] = bre, bim
    bcv[:64, 1], bcv[64:, 1] = bim, bre
    bcv[:64, 2], bcv[64:, 2] = cre, cim
    bcv[:64, 3], bcv[64:, 3] = cim, cre
    s_idx = np.arange(128) // 16
    smask = (s_idx[None, :] >= s_idx[:, None]).astype(np.float32)
    P2 = np.zeros((128, 128), np.float32)
    P2[np.arange(128), (np.arange(128) + 64) % 128] = 1.0
    return {"ssm_small": small, "ssm_bc": np.ascontiguousarray(bcv.reshape(128, 512)),
            "ssm_mat": np.ascontiguousarray(np.concatenate([smask, P2], axis=1))}


_NC_CACHE = {}


def _get_nc(which):
    if which not in _NC_CACHE:
        nc = bass.Bass("TRN2", target_bir_lowering=False)
        if which == "ab":
            build_phase_ab(nc, with_ssm=True)
        else:
            build_phase_c(nc)
        _NC_CACHE[which] = nc
    return _NC_CACHE[which]


def kernel(**inputs):
    inp = {k: np.asarray(v, dtype=np.float32) for k, v in inputs.items()}
    B = inp["x"].shape[0]
    maps1 = [phase_ab_inputs(inp, ci // 4, ci % 4) for ci in range(8)]
    res1 = run_bass_kernel_spmd(_get_nc("ab"), maps1, core_ids=list(range(8))).results
    attn_full = np.zeros((B, L_SEQ, 8, 65), np.float32)
    y_full = np.zeros((B, L_SEQ, 512), np.float32)
    for ci in range(8):
        b, r = ci // 4, ci % 4
        attn_full[b, :, 2 * r:2 * r + 2, :] = res1[ci]["attn_o"].reshape(L_SEQ, 2, 65)
        y_full[b, :, 128 * r:128 * r + 128] = res1[ci]["y_o"]
    maps2 = [phase_c_inputs(inp, ci // 4, ci % 4, attn_full, y_full) for ci in range(8)]
    res2 = run_bass_kernel_spmd(_get_nc("c"), maps2, core_ids=list(range(8))).results
    out = np.zeros((B, L_SEQ, 1024), np.float32)
    for ci in range(8):
        b, qi = ci // 4, ci % 4
        out[b, qi * NT_C:(qi + 1) * NT_C] = res2[ci]["out"]
    return out
```

```python
import math
import numpy as np
from contextlib import ExitStack

import concourse.bass as bass
import concourse.mybir as mybir
from concourse.bass_utils import run_bass_kernel_spmd

F32 = mybir.dt.float32
BF16 = mybir.dt.bfloat16
I32 = mybir.dt.int32
ALU = mybir.AluOpType
AF = mybir.ActivationFunctionType

PHYS = ["pe", "act", "dve", "pool", "sp"]
NDMA = 8
SAME_ENG_WAIT = True
EPS = 1e-6
NEGBIG = -30000.0


class Buf:
    __slots__ = ("name", "last_w", "readers", "excl")

    def __init__(self, name, excl=False):
        self.name = name
        self.last_w = None
        self.readers = []
        self.excl = excl


class Prog:
    def __init__(self, nc):
        self.nc = nc
        self.stack = ExitStack()
        self.semnames = ["pe", "act", "dve", "pool"] + [f"d{i}" for i in range(NDMA)]
        self.cnt = {s: 0 for s in self.semnames}
        self.seen = {e: {s: 0 for s in self.semnames} for e in PHYS}
        self.sems = {s: self.stack.enter_context(nc.semaphore("sem_" + s)) for s in self.semnames}
        self.engobjs = {"pe": nc.tensor, "act": nc.scalar, "dve": nc.vector, "pool": nc.gpsimd, "sp": nc.sync}
        self.pending = {e: {} for e in PHYS}
        self.nbuf = 0
        self.dma_rr = 0
        self.scopes = [self.stack]

    def buf(self, name=None, excl=False):
        self.nbuf += 1
        return Buf(name or f"b{self.nbuf}", excl)

    def pbuf(self):
        return self.buf(excl=True)

    def sb(self, name, shape, dtype):
        return self.scopes[-1].enter_context(self.nc.sbuf_tensor(name, list(shape), dtype))

    def ps(self, name, shape, dtype=F32):
        return self.scopes[-1].enter_context(self.nc.psum_tensor(name, list(shape), dtype))

    def push_scope(self):
        st = ExitStack()
        self.scopes.append(st)
        return st

    def pop_scope(self):
        st = self.scopes.pop()
        st.close()
        self.fence()

    def fence(self):
        for e in PHYS:
            for s in self.semnames:
                if self.cnt[s] > self.seen[e][s]:
                    self.pending[e][s] = self.cnt[s]

    def _op(self, phys, sem, inc, fn, reads, writes, extra_waits=()):
        waits = dict(self.pending[phys])
        self.pending[phys] = {}
        xr = [b for b in reads if b.excl]
        if xr:
            reads = [b for b in reads if not b.excl]
            writes = list(writes) + [b for b in xr if b not in writes]
        for (f, c) in extra_waits:
            waits[f] = max(waits.get(f, 0), c)

        def need(f):
            return f != phys or (SAME_ENG_WAIT and phys != "pe")
        for b in reads:
            if b.last_w is not None:
                f, c = b.last_w
                if need(f):
                    waits[f] = max(waits.get(f, 0), c)
        for b in writes:
            if b.last_w is not None:
                f, c = b.last_w
                if need(f):
                    waits[f] = max(waits.get(f, 0), c)
            for (f, c) in b.readers:
                if need(f):
                    waits[f] = max(waits.get(f, 0), c)
        engobj = self.engobjs[phys]
        for f, c in waits.items():
            if c > self.seen[phys][f]:
                self.seen[phys][f] = c
                engobj.wait_ge(self.sems[f], c)
        self.cnt[sem] += inc
        me = (sem, self.cnt[sem])
        ins = fn(engobj)
        ins.then_inc(self.sems[sem], inc)
        for b in reads:
            b.readers.append(me)
        for b in writes:
            b.last_w = me
            b.readers = []
        return me

    def op(self, eng, fn, reads=(), writes=()):
        return self._op(eng, eng, 1, fn, reads, writes)

    def dma(self, fn, reads=(), writes=(), q="sp"):
        k = self.dma_rr % NDMA
        self.dma_rr += 1
        sem = f"d{k}"
        prev = self.cnt[sem]
        extra = [(sem, prev)] if prev > 0 else []
        return self._op(q, sem, 16, fn, reads, writes, extra_waits=extra)

    def finish(self):
        for i in range(NDMA):
            s = f"d{i}"
            if self.cnt[s] > 0:
                self.nc.sync.wait_ge(self.sems[s], self.cnt[s])
        for s in ["pe", "act", "dve", "pool"]:
            if self.cnt[s] > 0:
                self.nc.sync.wait_ge(self.sems[s], self.cnt[s])
        while len(self.scopes) > 1:
            self.scopes.pop().close()
        self.stack.close()


def _rot(lst, i):
    return lst[i % len(lst)]


NT_C = 2048


def build_phase_c(nc, srcs=None):
    def din(name, shape, dt=F32):
        return nc.dram_tensor(name, list(shape), dt, kind="ExternalInput").ap()
    x = din("xc", [NT_C, 1024])
    if srcs is None:
        attn = din("attn", [NT_C, 520])
        yin = din("yin", [NT_C, 512])
    else:
        attn, yin = srcs["attn"], srcs["y"]
    cT2 = din("cT2", [128, 16])
    wada = din("wada_c", [1024, 4096])
    bada = din("bada_c", [1, 4096])
    rows = din("rows_c", [1, 3072])
    bglu = din("bglu", [1, 512])
    wglu = din("wglu", [512, 512])
    wout = din("wout", [1024, 1024])
    wfc1 = din("wfc1", [1024, 4096])
    wfc2 = din("wfc2", [4096, 1024])
    ident = din("ident", [128, 128])
    out = nc.dram_tensor("out", [NT_C, 1024], F32, kind="ExternalOutput").ap()
    x1s = nc.dram_tensor("x1s", [NT_C, 1024], F32).ap()

    P = Prog(nc)
    op, dma = P.op, P.dma

    ident_bf = P.sb("ident_bf", [128, 128], BF16); b_ident = P.buf()
    ones_f = P.sb("ones_f", [1, 128], F32); b_ones_f = P.buf()
    ones_bf = P.sb("ones_bf", [1, 512], BF16); b_ones_bf = P.buf()
    bglu_bf = P.sb("bglu_bf", [1, 512], BF16); b_bglu = P.buf()
    sh2T = P.sb("sh2T", [128, 16], BF16); b_sh2T = P.buf()
    Gcat = P.sb("Gcat", [128, 1024], F32); b_Gcat = P.buf()
    G1 = P.sb("G1", [128, 1024], F32); b_G1 = P.buf()
    S2b = P.sb("S2b", [128, 1024], F32); b_S2b = P.buf()
    G2 = P.sb("G2", [128, 1024], F32); b_G2 = P.buf()
    Gf = P.sb("Gf", [128, 1024], F32); b_Gf = P.buf()

    dma(lambda e: e.dma_start(out=ident_bf[:], in_=ident[:, :]), writes=[b_ident], q="pool")
    dma(lambda e: e.dma_start(out=bglu_bf[:], in_=bglu[:, :]), writes=[b_bglu], q="pool")
    op("dve", lambda e: e.memset(ones_f[:], 1.0), writes=[b_ones_f])
    op("dve", lambda e: e.memset(ones_bf[:], 1.0), writes=[b_ones_bf])

    P.push_scope()
    ct = P.sb("ct", [128, 16], F32); b_ct = P.buf()
    sct = P.sb("sct", [128, 16], F32); b_sct = P.buf()
    sgc = P.sb("sgc", [128, 16], F32)
    modrow = P.sb("modrow", [1, 4096], F32); b_modrow = P.buf()
    badas = P.sb("badas", [1, 4096], F32); b_badas = P.buf()
    rowss = P.sb("rowss", [1, 3072], F32); b_rowss = P.buf()
    s2row = P.sb("s2row", [1, 1024], F32); b_s2row = P.buf()
    pieces = [P.sb(f"wpiece{i}", [128, 8, 512], F32) for i in range(2)]
    b_pieces = [P.buf() for _ in range(2)]
    ps_row = [P.ps(f"ps_row{i}", [128, 512], F32) for i in range(2)]
    b_ps_row = [P.pbuf() for _ in range(2)]

    dma(lambda e: e.dma_start(out=ct[:], in_=cT2[:, :]), writes=[b_ct])
    dma(lambda e: e.dma_start(out=badas[:], in_=bada[:, :]), writes=[b_badas])
    dma(lambda e: e.dma_start(out=rowss[:], in_=rows[:, :]), writes=[b_rowss])
    op("act", lambda e: e.activation(out=sgc[:], in_=ct[:], func=AF.Sigmoid), reads=[b_ct], writes=[b_sct])
    op("dve", lambda e: e.tensor_tensor(out=sct[:], in0=sgc[:], in1=ct[:], op=ALU.mult), reads=[b_ct, b_sct], writes=[b_sct])
    for j in range(8):
        pc, bpc = _rot(pieces, j), _rot(b_pieces, j)
        pr, bpr = _rot(ps_row, j), _rot(b_ps_row, j)
        dma(lambda e: e.dma_start(out=pc[:], in_=wada[:, j * 512:(j + 1) * 512].rearrange("(k p) n -> p k n", p=128)),
            writes=[bpc])
        for k in range(8):
            op("pe", lambda e: e.matmul(pr[0:1, :], lhsT=sct[:, 2 * k:2 * k + 1], rhs=pc[:, k, :],
                                        start=(k == 0), stop=(k == 7)),
               reads=[b_sct, bpc], writes=[bpr])
        op("dve", lambda e: e.tensor_tensor(out=modrow[0:1, j * 512:(j + 1) * 512], in0=pr[0:1, :],
                                            in1=badas[0:1, j * 512:(j + 1) * 512], op=ALU.add),
           reads=[bpr, b_badas], writes=[b_modrow])
    op("dve", lambda e: e.scalar_tensor_tensor(out=s2row[:], in0=modrow[0:1, 2048:3072], scalar=1.0,
                                                in1=rowss[0:1, 1024:2048], op0=ALU.add, op1=ALU.mult),
       reads=[b_modrow, b_rowss], writes=[b_s2row])
    bc_list = [(Gcat, b_Gcat, rowss, b_rowss, 0), (G1, b_G1, modrow, b_modrow, 0), (S2b, b_S2b, s2row, b_s2row, 0),
               (G2, b_G2, modrow, b_modrow, 3072), (Gf, b_Gf, rowss, b_rowss, 2048)]
    i = 0
    for (dst, bdst, src, bsrc, off) in bc_list:
        for hf in range(2):
            pr, bpr = _rot(ps_row, i), _rot(b_ps_row, i)
            i += 1
            op("pe", lambda e: e.matmul(pr[:, :], lhsT=ones_f[0:1, :], rhs=src[0:1, off + hf * 512: off + (hf + 1) * 512],
                                        start=True, stop=True),
               reads=[b_ones_f, bsrc], writes=[bpr])
            op("act", lambda e: e.activation(out=dst[:, hf * 512:(hf + 1) * 512], in_=pr[:, :], func=AF.Copy),
               reads=[bpr], writes=[bdst])
    pr, bpr = ps_row[0], b_ps_row[0]
    for k in range(8):
        op("pe", lambda e: e.matmul(pr[:, 2 * k:2 * k + 2], lhsT=modrow[0:1, 1024 + k * 128:1024 + (k + 1) * 128],
                                    rhs=ones_f[0:1, 0:2], start=True, stop=True),
           reads=[b_ones_f, b_modrow], writes=[bpr])
    op("dve", lambda e: e.tensor_copy(out=sh2T[:], in_=pr[:, 0:16]), reads=[bpr], writes=[b_sh2T])
    P.pop_scope()

    P.push_scope()
    Wout = P.sb("Wout", [128, 8, 1024], BF16); b_Wout = P.buf()
    Wglu = P.sb("Wglu", [128, 4, 512], BF16); b_Wglu = P.buf()
    for k in range(4):
        dma(lambda e: e.dma_start(out=Wglu[:, k, :], in_=wglu[k * 128:(k + 1) * 128, :]), writes=[b_Wglu], q="pool")
    for k in range(8):
        dma(lambda e: e.dma_start(out=Wout[:, k, :], in_=wout[k * 128:(k + 1) * 128, :]), writes=[b_Wout], q="pool")
    NB = 3

    def dbl(name, shape, dt, n=2):
        return [P.sb(f"{name}{i}", shape, dt) for i in range(n)], [P.buf() for _ in range(n)]

    def dblp(name, shape, dt, n=2):
        return [P.ps(f"{name}{i}", shape, dt) for i in range(n)], [P.pbuf() for _ in range(n)]
    at, b_at = dbl("at", [128, 8, 65], F32, NB)
    yt, b_yt = dbl("yt", [128, 512], F32, NB)
    xt, b_xt = dbl("xt", [128, 1024], F32, NB)
    rl, b_rl = dbl("rl", [128, 8], F32)
    A, b_A = dbl("A", [128, 8, 64], F32)
    junk = P.sb("junk", [128, 512], BF16); b_junk = P.buf()
    ss, b_ss = dbl("ss", [128, 2], F32)
    sq, b_sq = dbl("sq", [128, 2], F32)
    rstd, b_rstd = dbl("rstd", [128, 2], F32)
    g1, b_g1 = dbl("g1", [128, 512], F32)
    g2, b_g2 = dbl("g2", [128, 512], F32)
    sg, b_sg = dbl("sg", [128, 512], F32)
    yg, b_yg = dbl("yg", [128, 512], F32)
    ygb, b_ygb = dbl("ygb", [128, 512], BF16)
    ygT, b_ygT = dbl("ygT", [128, 4, 128], BF16)
    sig, b_sig = dbl("sig", [128, 512], F32)
    S, b_S = dbl("S", [128, 512], F32)
    mixh, b_mixh = dbl("mixh", [128, 1024], BF16)
    mixT, b_mixT = dbl("mixT", [128, 8, 128], BF16)
    tt, b_tt = dbl("tt", [128, 1024], F32)
    x1t, b_x1t = dbl("x1t", [128, 1024], F32)
    ptr, b_ptr = dblp("ptr", [128, 8, 128], BF16)
    psg, b_psg = dblp("psg", [128, 512], F32)
    pmx = P.ps("pmx", [128, 8, 128], BF16); b_pmx = P.pbuf()
    pso = [P.ps(f"pso{i}", [128, 512], F32) for i in range(2)]; b_pso = [P.pbuf() for _ in range(2)]
    b_x1s = [P.buf() for _ in range(16)]

    def c1_load(t):
        a, ba = _rot(at, t), _rot(b_at, t)
        y_, by = _rot(yt, t), _rot(b_yt, t)
        x_, bx = _rot(xt, t), _rot(b_xt, t)
        dma(lambda e: e.dma_start(out=a[:].rearrange("p h e -> p (h e)"), in_=attn[t * 128:(t + 1) * 128, :]), writes=[ba])
        dma(lambda e: e.dma_start(out=y_[:], in_=yin[t * 128:(t + 1) * 128, :]), writes=[by])
        dma(lambda e: e.dma_start(out=x_[:], in_=x[t * 128:(t + 1) * 128, :]), writes=[bx])

    def c1_a(t):
        a, ba = _rot(at, t), _rot(b_at, t)
        y_, by = _rot(yt, t), _rot(b_yt, t)
        i = t % 2
        op("dve", lambda e: e.reciprocal(out=rl[i][:], in_=a[:, :, 64]), reads=[ba], writes=[b_rl[i]])
        op("dve", lambda e: e.tensor_tensor(out=A[i][:], in0=a[:, :, 0:64],
                                            in1=rl[i][:, :].unsqueeze(2).to_broadcast([128, 8, 64]), op=ALU.mult),
           reads=[ba, b_rl[i]], writes=[b_A[i]])
        op("act", lambda e: e.activation(out=junk[:], in_=A[i][:].rearrange("p h d -> p (h d)"), func=AF.Square,
                                         accum_out=ss[i][:, 0:1]),
           reads=[b_A[i]], writes=[b_junk, b_ss[i]])
        op("pool", lambda e: e.tensor_tensor(out=g1[i][:], in0=y_[:], in1=y_[:], op=ALU.mult), reads=[by], writes=[b_g1[i]])
        op("pool", lambda e: e.tensor_scalar(out=g1[i][:], in0=g1[i][:], scalar1=0.044715, scalar2=1.0,
                                             op0=ALU.mult, op1=ALU.add), reads=[b_g1[i]], writes=[b_g1[i]])
        op("pool", lambda e: e.tensor_tensor(out=g2[i][:], in0=g1[i][:], in1=y_[:], op=ALU.mult), reads=[b_g1[i], by], writes=[b_g2[i]])
        op("act", lambda e: e.activation(out=sg[i][:], in_=g2[i][:], func=AF.Sigmoid, scale=1.5957691216057308),
           reads=[b_g2[i]], writes=[b_sg[i]])
        op("dve", lambda e: e.tensor_tensor(out=yg[i][:], in0=y_[:], in1=sg[i][:], op=ALU.mult), reads=[by, b_sg[i]], writes=[b_yg[i]])
        op("pool", lambda e: e.tensor_tensor(out=ygb[i][:], in0=y_[:], in1=sg[i][:], op=ALU.mult), reads=[by, b_sg[i]], writes=[b_ygb[i]])
        for k in range(4):
            op("pe", lambda e: e.transpose(ptr[i][:, k, :], ygb[i][:, k * 128:(k + 1) * 128], ident_bf[:]),
               reads=[b_ygb[i], b_ident], writes=[b_ptr[i]])
        op("act", lambda e: e.activation(out=ygT[i][:], in_=ptr[i][:, 0:4, :], func=AF.Copy), reads=[b_ptr[i]], writes=[b_ygT[i]])
        op("pe", lambda e: e.matmul(psg[i][:], lhsT=ones_bf[0:1, 0:128], rhs=bglu_bf[0:1, :], start=True, stop=False),
           reads=[b_ones_bf, b_bglu], writes=[b_psg[i]])
        for k in range(4):
            op("pe", lambda e: e.matmul(psg[i][:], lhsT=ygT[i][:, k, :], rhs=Wglu[:, k, :], start=False, stop=(k == 3)),
               reads=[b_ygT[i], b_Wglu], writes=[b_psg[i]])

    def c1_b(t):
        x_, bx = _rot(xt, t), _rot(b_xt, t)
        i = t % 2
        op("act", lambda e: e.activation(out=sig[i][:], in_=psg[i][:], func=AF.Sigmoid), reads=[b_psg[i]], writes=[b_sig[i]])
        op("dve", lambda e: e.tensor_tensor(out=S[i][:], in0=yg[i][:], in1=sig[i][:], op=ALU.mult), reads=[b_yg[i], b_sig[i]], writes=[b_S[i]])
        op("act", lambda e: e.activation(out=junk[:], in_=S[i][:], func=AF.Square, accum_out=ss[i][:, 1:2]),
           reads=[b_S[i]], writes=[b_junk, b_ss[i]])
        op("act", lambda e: e.activation(out=sq[i][:], in_=ss[i][:], func=AF.Sqrt, scale=1.0 / 512.0, bias=EPS),
           reads=[b_ss[i]], writes=[b_sq[i]])
        op("dve", lambda e: e.reciprocal(out=rstd[i][:], in_=sq[i][:]), reads=[b_sq[i]], writes=[b_rstd[i]])
        op("dve", lambda e: e.scalar_tensor_tensor(out=mixh[i][:, 0:512], in0=A[i][:].rearrange("p h d -> p (h d)"),
                                                    scalar=rstd[i][:, 0:1], in1=Gcat[:, 0:512], op0=ALU.mult, op1=ALU.mult),
           reads=[b_A[i], b_rstd[i], b_Gcat], writes=[b_mixh[i]])
        op("dve", lambda e: e.scalar_tensor_tensor(out=mixh[i][:, 512:1024], in0=S[i][:], scalar=rstd[i][:, 1:2],
                                                    in1=Gcat[:, 512:1024], op0=ALU.mult, op1=ALU.mult),
           reads=[b_S[i], b_rstd[i], b_Gcat], writes=[b_mixh[i]])
        for k in range(8):
            op("pe", lambda e: e.transpose(pmx[:, k, :], mixh[i][:, k * 128:(k + 1) * 128], ident_bf[:]),
               reads=[b_mixh[i], b_ident], writes=[b_pmx])
        op("act", lambda e: e.activation(out=mixT[i][:], in_=pmx[:], func=AF.Copy), reads=[b_pmx], writes=[b_mixT[i]])
        for hf in range(2):
            for k in range(8):
                op("pe", lambda e: e.matmul(pso[hf][:], lhsT=mixT[i][:, k, :], rhs=Wout[:, k, hf * 512:(hf + 1) * 512],
                                            start=(k == 0), stop=(k == 7)),
                   reads=[b_mixT[i], b_Wout], writes=[b_pso[hf]])
            op("dve", lambda e: e.tensor_tensor(out=tt[i][:, hf * 512:(hf + 1) * 512], in0=pso[hf][:],
                                                in1=G1[:, hf * 512:(hf + 1) * 512], op=ALU.mult),
               reads=[b_pso[hf], b_G1], writes=[b_tt[i]])
        op("pool", lambda e: e.tensor_tensor(out=x1t[i][:], in0=tt[i][:], in1=x_[:], op=ALU.add), reads=[b_tt[i], bx], writes=[b_x1t[i]])
        dma(lambda e: e.dma_start(out=x1s[t * 128:(t + 1) * 128, :], in_=x1t[i][:]), reads=[b_x1t[i]], writes=[b_x1s[t]])

    c1_load(0)
    c1_load(1)
    c1_a(0)
    for t in range(16):
        if t + 2 < 16:
            c1_load(t + 2)
        if t + 1 < 16:
            c1_a(t + 1)
        c1_b(t)
    P.pop_scope()

    P.push_scope()
    W2 = P.sb("W2", [128, 32, 1024], BF16); b_W2 = [P.buf() for _ in range(8)]
    for i in range(8):
        dma(lambda e: e.dma_start(out=W2[:, 4 * i:4 * i + 4, :],
                                  in_=wfc2[512 * i:512 * (i + 1), :].rearrange("(j p) n -> p j n", p=128)),
            writes=[b_W2[i]], q="pool")
    W1p = [P.sb(f"W1p{i}", [128, 8, 256], BF16) for i in range(2)]; b_W1p = [P.buf() for _ in range(2)]
    hT = P.sb("hT", [128, 32, 512], BF16); b_hT = [P.buf() for _ in range(32)]
    h2T = P.sb("h2T", [128, 8, 512], BF16); b_h2T = P.buf()
    x1g = P.sb("x1g", [128, 4, 1024], F32); b_x1g = [P.buf() for _ in range(4)]
    h2 = P.sb("h2", [128, 1024], BF16); b_h2 = P.buf()
    junk2 = P.sb("junk2", [128, 1024], BF16); b_junk2 = P.buf()
    ssc = P.sb("ssc", [128, 2], F32); b_ssc = P.buf()
    sqc = P.sb("sqc", [128, 2], F32); b_sqc = P.buf()
    rc = P.sb("rc", [128, 2], F32); b_rc = P.buf()
    b1row = P.sb("b1row", [1, 256], BF16); b_b1row = P.buf()
    rl_t = [P.sb(f"rl_t{i}", [128, 512], BF16) for i in range(2)]; b_rl_t = [P.buf() for _ in range(2)]
    t2 = P.sb("t2", [128, 1024], F32); b_t2 = P.buf()
    x2t = t2; b_x2t = b_t2
    ot = [P.sb(f"ot{i}", [128, 1024], F32) for i in range(2)]; b_ot = [P.buf() for _ in range(2)]
    pht = P.ps("pht", [128, 8, 128], BF16); b_pht = P.pbuf()
    psb = P.ps("psb", [128, 512], F32); b_psb = P.pbuf()
    psf = [P.ps(f"psf{i}", [128, 512], F32) for i in range(2)]; b_psf = [P.pbuf() for _ in range(2)]
    pso2 = [P.ps(f"pso2{i}", [128, 512], F32) for i in range(2)]; b_pso2 = [P.pbuf() for _ in range(2)]
    pcount = 0
    ocount = 0
    for g in range(4):
        for i in range(4):
            t = g * 4 + i
            dma(lambda e: e.dma_start(out=x1g[:, i, :], in_=x1s[t * 128:(t + 1) * 128, :]), reads=[b_x1s[t]], writes=[b_x1g[i]])
            op("act", lambda e: e.activation(out=junk2[:], in_=x1g[:, i, :], func=AF.Square, accum_out=ssc[:, 0:1]),
               reads=[b_x1g[i]], writes=[b_junk2, b_ssc])
            op("act", lambda e: e.activation(out=sqc[:, 0:1], in_=ssc[:, 0:1], func=AF.Sqrt, scale=1.0 / 1024.0, bias=EPS),
               reads=[b_ssc], writes=[b_sqc])
            op("dve", lambda e: e.reciprocal(out=rc[:, 0:1], in_=sqc[:, 0:1]), reads=[b_sqc], writes=[b_rc])
            op("dve", lambda e: e.scalar_tensor_tensor(out=h2[:], in0=x1g[:, i, :], scalar=rc[:, 0:1], in1=S2b[:],
                                                        op0=ALU.mult, op1=ALU.mult),
               reads=[b_x1g[i], b_rc, b_S2b], writes=[b_h2])
            for k in range(8):
                op("pe", lambda e: e.transpose(pht[:, k, :], h2[:, k * 128:(k + 1) * 128], ident_bf[:]),
                   reads=[b_h2, b_ident], writes=[b_pht])
            op("act", lambda e: e.activation(out=h2T[:, :, i * 128:(i + 1) * 128], in_=pht[:], func=AF.Copy),
               reads=[b_pht], writes=[b_h2T])
        for pc in range(16):
            w1, bw1 = _rot(W1p, pcount), _rot(b_W1p, pcount)
            pcount += 1
            dma(lambda e: e.dma_start(out=w1[:], in_=wfc1[:, pc * 256:(pc + 1) * 256].rearrange("(k p) n -> p k n", p=128)),
                writes=[bw1], q="pool")
            for k in range(8):
                op("pe", lambda e: e.matmul(psb[0:1, 0:256], lhsT=sh2T[:, 2 * k:2 * k + 1], rhs=w1[:, k, :],
                                            start=(k == 0), stop=(k == 7)),
                   reads=[b_sh2T, bw1], writes=[b_psb])
            op("act", lambda e: e.activation(out=b1row[:], in_=psb[0:1, 0:256], func=AF.Copy), reads=[b_psb], writes=[b_b1row])
            for fc in range(2):
                j = pc * 2 + fc
                pf, bpf = _rot(psf, j), _rot(b_psf, j)
                op("pe", lambda e: e.matmul(pf[:], lhsT=b1row[0:1, fc * 128:(fc + 1) * 128], rhs=ones_bf[0:1, :],
                                            start=True, stop=False),
                   reads=[b_b1row, b_ones_bf], writes=[bpf])
                for k in range(8):
                    op("pe", lambda e: e.matmul(pf[:], lhsT=w1[:, k, fc * 128:(fc + 1) * 128], rhs=h2T[:, k, :],
                                                start=False, stop=(k == 7)),
                       reads=[bw1, b_h2T], writes=[bpf])
                r_, br_ = _rot(rl_t, j), _rot(b_rl_t, j)
                op("act", lambda e: e.activation(out=r_[:], in_=pf[:], func=AF.Relu), reads=[bpf], writes=[br_])
                op("pool", lambda e: e.tensor_tensor(out=hT[:, j, :], in0=r_[:], in1=r_[:], op=ALU.mult),
                   reads=[br_], writes=[b_hT[j]])
        for i in range(4):
            t = g * 4 + i
            for hf in range(2):
                po, bpo = _rot(pso2, ocount), _rot(b_pso2, ocount)
                ocount += 1
                for j in range(32):
                    op("pe", lambda e: e.matmul(po[:], lhsT=hT[:, j, i * 128:(i + 1) * 128],
                                                rhs=W2[:, j, hf * 512:(hf + 1) * 512], start=(j == 0), stop=(j == 31)),
                       reads=[b_hT[j], b_W2[j // 4]], writes=[bpo])
                op("dve", lambda e: e.tensor_tensor(out=t2[:, hf * 512:(hf + 1) * 512], in0=po[:],
                                                    in1=G2[:, hf * 512:(hf + 1) * 512], op=ALU.mult),
                   reads=[bpo, b_G2], writes=[b_t2])
            op("pool", lambda e: e.tensor_tensor(out=x2t[:], in0=t2[:], in1=x1g[:, i, :], op=ALU.add),
               reads=[b_t2, b_x1g[i]], writes=[b_x2t])
            op("act", lambda e: e.activation(out=junk2[:], in_=x2t[:], func=AF.Square, accum_out=ssc[:, 1:2]),
               reads=[b_x2t], writes=[b_junk2, b_ssc])
            op("act", lambda e: e.activation(out=sqc[:, 1:2], in_=ssc[:, 1:2], func=AF.Sqrt, scale=1.0 / 1024.0, bias=EPS),
               reads=[b_ssc], writes=[b_sqc])
            op("dve", lambda e: e.reciprocal(out=rc[:, 1:2], in_=sqc[:, 1:2]), reads=[b_sqc], writes=[b_rc])
            o_, bo_ = _rot(ot, t), _rot(b_ot, t)
            op("dve", lambda e: e.scalar_tensor_tensor(out=o_[:], in0=x2t[:], scalar=rc[:, 1:2], in1=Gf[:],
                                                        op0=ALU.mult, op1=ALU.mult),
               reads=[b_x2t, b_rc, b_Gf], writes=[bo_])
            dma(lambda e: e.dma_start(out=out[t * 128:(t + 1) * 128, :], in_=o_[:]), reads=[bo_])
    P.pop_scope()
    P.finish()
    return nc


def _ident():
    return np.eye(128, dtype=np.float32)


def phase_c_inputs(inp, b, qi, attn_full, y_full):
    T0 = qi * NT_C
    c = inp["c"][b]
    cT = np.ascontiguousarray(c.reshape(8, 128).T)
    cT2 = np.repeat(cT[:, :, None], 2, axis=2).reshape(128, 16)
    rows = np.concatenate([inp["g_attn_out"][0], inp["g_ssm_out"][0], inp["g_mlp"][0], inp["g_final"]])[None, :]
    return {
        "xc": np.ascontiguousarray(inp["x"][b, T0:T0 + NT_C]),
        "attn": np.ascontiguousarray(attn_full[b, T0:T0 + NT_C].reshape(NT_C, 520)),
        "yin": np.ascontiguousarray(y_full[b, T0:T0 + NT_C]),
        "cT2": np.ascontiguousarray(cT2),
        "wada_c": np.ascontiguousarray(inp["w_ada"][0][:, 2048:6144]),
        "bada_c": np.ascontiguousarray(inp["b_ada"][0][None, 2048:6144]),
        "rows_c": np.ascontiguousarray(rows.astype(np.float32)),
        "bglu": np.ascontiguousarray(inp["b_glu"][0][None, :]),
        "wglu": np.ascontiguousarray(inp["w_glu"][0]),
        "wout": np.ascontiguousarray(inp["w_out"][0]),
        "wfc1": np.ascontiguousarray(inp["w_fc1"][0]),
        "wfc2": np.ascontiguousarray(inp["w_fc2"][0]),
        "ident": _ident(),
    }


L_SEQ = 8192
NTILE = 64


def build_phase_ab(nc, with_ssm=True, dsts=None, dbg_tiles=NTILE, dbg_attn=True, dbg_stage=9):
    def din(name, shape, dt=F32):
        return nc.dram_tensor(name, list(shape), dt, kind="ExternalInput").ap()
    xb = din("xb", [L_SEQ, 1024])
    cT2 = din("cT2", [128, 16])
    wada = din("wada_a", [1024, 2048])
    bada = din("bada_a", [1, 2048])
    gmix = din("gmix", [1, 1024])
    win = din("win", [1024, 512])
    cosl = din("cosl", [128, 2048])
    sinl = din("sinl", [128, 2048])
    ind = din("ind", [32, L_SEQ])
    cmask = din("cmask", [128, 2048])
    ident = din("ident", [128, 128])
    if dsts is None:
        attn_o = nc.dram_tensor("attn_o", [L_SEQ, 130], F32, kind="ExternalOutput").ap()
        y_o = nc.dram_tensor("y_o", [L_SEQ, 128], F32, kind="ExternalOutput").ap()
    else:
        attn_o, y_o = dsts["attn"], dsts["y"]

    P = Prog(nc)
    op, dma = P.op, P.dma

    ident_bf = P.sb("ident_bf", [128, 128], BF16); b_ident = P.buf()
    ident_f = P.sb("ident_f", [128, 128], F32); b_identf = P.buf()
    ones_bf = P.sb("ones_bf", [1, 512], BF16); b_ones_bf = P.buf()
    zeros_bf = P.sb("zeros_bf", [1, 512], BF16); b_zeros_bf = P.buf()
    ones_f = P.sb("ones_f", [1, 128], F32); b_ones_f = P.buf()
    U_tok = P.sb("U_tok", [128, 8, 8, 8, 16], BF16); b_U = [P.buf() for _ in range(8)]
    dma(lambda e: e.dma_start(out=ident_bf[:], in_=ident[:, :]), writes=[b_ident], q="pool")
    dma(lambda e: e.dma_start(out=ident_f[:], in_=ident[:, :]), writes=[b_identf])
    op("dve", lambda e: e.memset(ones_f[:], 1.0), writes=[b_ones_f])
    op("dve", lambda e: e.memset(ones_bf[:], 1.0), writes=[b_ones_bf])
    op("dve", lambda e: e.memset(zeros_bf[:], 0.0), writes=[b_zeros_bf])

    P.push_scope()
    QaT = P.sb("QaT", [128, 2, L_SEQ], BF16); b_QaT = [P.buf() for _ in range(NTILE)]
    KaT = P.sb("KaT", [128, 2, L_SEQ], BF16); b_KaT = [P.buf() for _ in range(NTILE)]; b_Kind = P.buf()
    Vaug = P.sb("Vaug", [128, NTILE, 2, 65], BF16); b_V = [P.buf() for _ in range(NTILE)]; b_Vones = P.buf()
    for h in range(2):
        for cch in range(4):
            dma(lambda e: e.dma_start(out=KaT[64:96, h, cch * 2048:(cch + 1) * 2048], in_=ind[:, cch * 2048:(cch + 1) * 2048]),
                writes=[b_Kind], q="pool")
    op("pool", lambda e: e.memset(Vaug[:, :, :, 64:65], 1.0), writes=[b_Vones])

    P.push_scope()
    S1b = P.sb("S1b", [128, 1024], F32); b_S1b = P.buf()
    sh1T = P.sb("sh1T", [128, 16], BF16); b_sh1T = P.buf()
    Win = P.sb("Win", [128, 8, 512], BF16); b_Win = P.buf()
    bin_bf = P.sb("bin_bf", [1, 512], BF16); b_bin = P.buf()
    for k in range(8):
        dma(lambda e: e.dma_start(out=Win[:, k, :], in_=win[k * 128:(k + 1) * 128, :]), writes=[b_Win], q="pool")

    P.push_scope()
    ct = P.sb("ct", [128, 16], F32); b_ct = P.buf()
    sct = P.sb("sct", [128, 16], F32); b_sct = P.buf()
    sgc = P.sb("sgc", [128, 16], F32)
    modrow = P.sb("modrow", [1, 2048], F32); b_modrow = P.buf()
    badas = P.sb("badas", [1, 2048], F32); b_badas = P.buf()
    gmixs = P.sb("gmixs", [1, 1024], F32); b_gmixs = P.buf()
    s1row = P.sb("s1row", [1, 1024], F32); b_s1row = P.buf()
    pieces = [P.sb(f"wpiece{i}", [128, 8, 512], F32) for i in range(2)]
    b_pieces = [P.buf() for _ in range(2)]
    ps_row = [P.ps(f"ps_row{i}", [128, 512], F32) for i in range(2)]
    b_ps_row = [P.pbuf() for _ in range(2)]
    dma(lambda e: e.dma_start(out=ct[:], in_=cT2[:, :]), writes=[b_ct])
    dma(lambda e: e.dma_start(out=badas[:], in_=bada[:, :]), writes=[b_badas])
    dma(lambda e: e.dma_start(out=gmixs[:], in_=gmix[:, :]), writes=[b_gmixs])
    op("act", lambda e: e.activation(out=sgc[:], in_=ct[:], func=AF.Sigmoid), reads=[b_ct], writes=[b_sct])
    op("dve", lambda e: e.tensor_tensor(out=sct[:], in0=sgc[:], in1=ct[:], op=ALU.mult), reads=[b_ct, b_sct], writes=[b_sct])
    for j in range(4):
        pc, bpc = _rot(pieces, j), _rot(b_pieces, j)
        pr, bpr = _rot(ps_row, j), _rot(b_ps_row, j)
        dma(lambda e: e.dma_start(out=pc[:], in_=wada[:, j * 512:(j + 1) * 512].rearrange("(k p) n -> p k n", p=128)),
            writes=[bpc])
        for k in range(8):
            op("pe", lambda e: e.matmul(pr[0:1, :], lhsT=sct[:, 2 * k:2 * k + 1], rhs=pc[:, k, :],
                                        start=(k == 0), stop=(k == 7)),
               reads=[b_sct, bpc], writes=[bpr])
        op("dve", lambda e: e.tensor_tensor(out=modrow[0:1, j * 512:(j + 1) * 512], in0=pr[0:1, :],
                                            in1=badas[0:1, j * 512:(j + 1) * 512], op=ALU.add),
           reads=[bpr, b_badas], writes=[b_modrow])
    op("dve", lambda e: e.scalar_tensor_tensor(out=s1row[:], in0=modrow[0:1, 1024:2048], scalar=1.0,
                                                in1=gmixs[0:1, :], op0=ALU.add, op1=ALU.mult),
       reads=[b_modrow, b_gmixs], writes=[b_s1row])
    for hf in range(2):
        pr, bpr = _rot(ps_row, hf), _rot(b_ps_row, hf)
        op("pe", lambda e: e.matmul(pr[:, :], lhsT=ones_f[0:1, :], rhs=s1row[0:1, hf * 512:(hf + 1) * 512],
                                    start=True, stop=True), reads=[b_ones_f, b_s1row], writes=[bpr])
        op("act", lambda e: e.activation(out=S1b[:, hf * 512:(hf + 1) * 512], in_=pr[:, :], func=AF.Copy),
           reads=[bpr], writes=[b_S1b])
    pr, bpr = ps_row[0], b_ps_row[0]
    for k in range(8):
        op("pe", lambda e: e.matmul(pr[:, 2 * k:2 * k + 2], lhsT=modrow[0:1, k * 128:(k + 1) * 128],
                                    rhs=ones_f[0:1, 0:2], start=True, stop=True),
           reads=[b_ones_f, b_modrow], writes=[bpr])
    op("dve", lambda e: e.tensor_copy(out=sh1T[:], in_=pr[:, 0:16]), reads=[bpr], writes=[b_sh1T])
    pr, bpr = ps_row[1], b_ps_row[1]
    for k in range(8):
        op("pe", lambda e: e.matmul(pr[0:1, :], lhsT=sh1T[:, 2 * k:2 * k + 1], rhs=Win[:, k, :],
                                    start=(k == 0), stop=(k == 7)), reads=[b_sh1T, b_Win], writes=[bpr])
    op("act", lambda e: e.activation(out=bin_bf[:], in_=pr[0:1, :], func=AF.Copy), reads=[bpr], writes=[b_bin])
    P.pop_scope()

    xt = [P.sb(f"xt{i}", [128, 1024], F32) for i in range(3)]; b_xt = [P.buf() for _ in range(3)]
    hb = [P.sb(f"hb{i}", [128, 1024], BF16) for i in range(2)]; b_hb = [P.buf() for _ in range(2)]
    junk = P.sb("junk", [128, 1024], BF16); b_junk = P.buf()
    ssx = [P.sb(f"ssx{i}", [128, 1], F32) for i in range(2)]; b_ssx = [P.buf() for _ in range(2)]
    sqx = [P.sb(f"sqx{i}", [128, 1], F32) for i in range(2)]; b_sqx = [P.buf() for _ in range(2)]
    rx = [P.sb(f"rx{i}", [128, 1], F32) for i in range(2)]; b_rx = [P.buf() for _ in range(2)]
    hTs = P.sb("hTs", [128, 8, 1024], BF16); b_hTs = [P.buf() for _ in range(8)]
    cs = [P.sb(f"cs{i}", [128, 8, 32], F32) for i in range(2)]; b_cs = [P.buf() for _ in range(2)]
    sn = [P.sb(f"sn{i}", [128, 8, 32], F32) for i in range(2)]; b_sn = [P.buf() for _ in range(2)]
    qkr = P.sb("qkr", [128, 4, 2, 32], F32); b_qkr = P.buf()
    r1 = P.sb("r1", [128, 4, 32], F32); b_r1 = P.buf()
    r2 = P.sb("r2", [128, 4, 32], F32); b_r2 = P.buf()
    r3 = P.sb("r3", [128, 4, 32], F32); b_r3 = P.buf()
    r4 = P.sb("r4", [128, 4, 32], F32); b_r4 = P.buf()
    ksum = P.sb("ksum", [64, 2, 2], F32); b_ksum = [P.buf(), P.buf()]
    kmT = P.sb("kmT", [64, 2, 32], F32); b_kmT = P.buf()
    qTt = P.sb("qTt", [64, 2, 128], F32); b_qTt = P.buf()
    Gt = P.sb("Gt", [128, 2, 32], F32); b_Gt = P.buf()
    mx8 = P.sb("mx8", [128, 2, 8], F32); b_mx8 = P.buf()
    nm = P.sb("nm", [128, 2, 32], F32); b_nm = P.buf()
    Qtok = P.sb("Qtok", [128, 2, 96], BF16); b_Qtok = P.buf()
    pxt = [P.ps(f"pxt{i}", [128, 8, 128], BF16) for i in range(2)]; b_pxt = [P.pbuf() for _ in range(2)]
    pqkv = [P.ps(f"pqkv{i}", [128, 512], F32) for i in range(2)]; b_pqkv = [P.pbuf() for _ in range(2)]
    pT = P.ps("pT", [128, 4, 128], F32); b_pT = P.pbuf()
    pgq = P.ps("pgq", [128, 512], F32); b_pgq = P.pbuf()
    pu = P.ps("pu", [128, 8, 128], F32); b_pu = P.pbuf()
    pg = pgq
    pqa = pgq[:, 256:512].bitcast(BF16).rearrange("p (h n) -> p h n", h=4)
    b_pg = b_pgq
    b_pqa = b_pgq
    op("dve", lambda e: e.memset(Gt[:], -1.0e30), writes=[b_Gt])
    op("dve", lambda e: e.memset(kmT[:], 0.0), writes=[b_kmT])

    def load_x(t):
        x_, bx = _rot(xt, t), _rot(b_xt, t)
        dma(lambda e: e.dma_start(out=x_[:], in_=xb[t * 128:(t + 1) * 128, :]), writes=[bx])

    def stage_a(t):
        S_, ti = t // 8, t % 8
        x_, bx = _rot(xt, t), _rot(b_xt, t)
        hb_, bhb = _rot(hb, t), _rot(b_hb, t)
        ss_, bss = _rot(ssx, t), _rot(b_ssx, t)
        sq_, bsq = _rot(sqx, t), _rot(b_sqx, t)
        rx_, brx = _rot(rx, t), _rot(b_rx, t)
        px_, bpx = _rot(pxt, t), _rot(b_pxt, t)
        pq_, bpq = _rot(pqkv, t), _rot(b_pqkv, t)
        if ti == 0:
            c_, bc_ = _rot(cs, S_), _rot(b_cs, S_)
            s_, bs_ = _rot(sn, S_), _rot(b_sn, S_)
            dma(lambda e: e.dma_start(out=c_[:].rearrange("p i d -> p (i d)"), in_=cosl[:, S_ * 256:(S_ + 1) * 256]), writes=[bc_])
            dma(lambda e: e.dma_start(out=s_[:].rearrange("p i d -> p (i d)"), in_=sinl[:, S_ * 256:(S_ + 1) * 256]), writes=[bs_])
        op("act", lambda e: e.activation(out=junk[:], in_=x_[:], func=AF.Square, accum_out=ss_[:, 0:1]),
           reads=[bx], writes=[b_junk, bss])
        op("act", lambda e: e.activation(out=sq_[:], in_=ss_[:], func=AF.Sqrt, scale=1.0 / 1024.0, bias=EPS),
           reads=[bss], writes=[bsq])
        op("dve", lambda e: e.reciprocal(out=rx_[:], in_=sq_[:]), reads=[bsq], writes=[brx])
        op("dve", lambda e: e.scalar_tensor_tensor(out=hb_[:], in0=x_[:], scalar=rx_[:, 0:1], in1=S1b[:],
                                                    op0=ALU.mult, op1=ALU.mult),
           reads=[bx, brx, b_S1b], writes=[bhb])
        for k in range(8):
            op("pe", lambda e: e.transpose(px_[:, k, :], hb_[:, k * 128:(k + 1) * 128], ident_bf[:]),
               reads=[bhb, b_ident], writes=[bpx])
        op("act", lambda e: e.activation(out=hTs[:, :, ti * 128:(ti + 1) * 128], in_=px_[:], func=AF.Copy),
           reads=[bpx], writes=[b_hTs[ti]])
        op("pe", lambda e: e.matmul(pq_[:, 0:384], lhsT=ones_bf[0:1, 0:128], rhs=bin_bf[0:1, 0:384], start=True, stop=False),
           reads=[b_ones_bf, b_bin], writes=[bpq])
        for k in range(8):
            op("pe", lambda e: e.matmul(pq_[:, 0:384], lhsT=hTs[:, k, ti * 128:(ti + 1) * 128], rhs=Win[:, k, 0:384],
                                        start=False, stop=(k == 7)),
               reads=[b_hTs[ti], b_Win], writes=[bpq])

    def u_proj(S_):
        for s in range(8):
            op("pe", lambda e: e.matmul(pu[:, s, :], lhsT=ones_bf[0:1, 0:128], rhs=bin_bf[0:1, 384:512], start=True, stop=False),
               reads=[b_ones_bf, b_bin], writes=[b_pu])
            for k in range(8):
                lhs = hTs[:, k, :].rearrange("p (c s) -> p s c", s=8)[:, s, :]
                op("pe", lambda e: e.matmul(pu[:, s, :], lhsT=lhs, rhs=Win[:, k, 384:512], start=False, stop=(k == 7)),
                   reads=b_hTs + [b_Win], writes=[b_pu])
        op("act", lambda e: e.activation(out=U_tok[:, S_].rearrange("p g s m -> p s g m"),
                                         in_=pu[:].rearrange("p s (g m) -> p s g m", g=8), func=AF.Copy),
           reads=[b_pu], writes=[b_U[S_]])

    def stage_b(t):
        S_, ti = t // 8, t % 8
        j = t // 2
        par = t % 2
        tok = slice(t * 128, (t + 1) * 128)
        c_, bc_ = _rot(cs, S_), _rot(b_cs, S_)
        s_, bs_ = _rot(sn, S_), _rot(b_sn, S_)
        pq_, bpq = _rot(pqkv, t), _rot(b_pqkv, t)
        op("act", lambda e: e.activation(out=Vaug[:, t, :, 0:64], in_=pq_[:, 256:384].rearrange("p (h d) -> p h d", h=2),
                                         func=AF.Copy), reads=[bpq], writes=[b_V[t]])
        pq = pq_[:, 0:256].rearrange("p (f two d) -> p f two d", f=4, two=2)
        cb = c_[:, ti, :].unsqueeze(1).to_broadcast([128, 4, 32])
        sb_ = s_[:, ti, :].unsqueeze(1).to_broadcast([128, 4, 32])
        op("dve", lambda e: e.tensor_tensor(out=r1[:], in0=pq[:, :, 0, :], in1=cb, op=ALU.mult), reads=[bpq, bc_], writes=[b_r1])
        op("dve", lambda e: e.tensor_tensor(out=r2[:], in0=pq[:, :, 1, :], in1=sb_, op=ALU.mult), reads=[bpq, bs_], writes=[b_r2])
        op("dve", lambda e: e.tensor_tensor(out=r3[:], in0=pq[:, :, 0, :], in1=sb_, op=ALU.mult), reads=[bpq, bs_], writes=[b_r3])
        op("dve", lambda e: e.tensor_tensor(out=r4[:], in0=pq[:, :, 1, :], in1=cb, op=ALU.mult), reads=[bpq, bc_], writes=[b_r4])
        op("pool", lambda e: e.tensor_tensor(out=qkr[:, :, 0, :], in0=r1[:], in1=r2[:], op=ALU.subtract),
           reads=[b_r1, b_r2], writes=[b_qkr])
        op("pool", lambda e: e.tensor_tensor(out=qkr[:, :, 1, :], in0=r3[:], in1=r4[:], op=ALU.add),
           reads=[b_r3, b_r4], writes=[b_qkr])
        qkf = qkr[:].rearrange("p f two d -> p (f two d)")
        for h in range(2):
            op("pe", lambda e: e.transpose(pT[0:64, h, :], qkf[:, 128 + h * 64:128 + (h + 1) * 64], ident_f[:]),
               reads=[b_qkr, b_identf], writes=[b_pT])
        if j >= 4:
            for h in range(2):
                op("pe", lambda e: e.transpose(pT[0:64, 2 + h, :], qkf[:, h * 64:(h + 1) * 64], ident_f[:]),
                   reads=[b_qkr, b_identf], writes=[b_pT])
        op("act", lambda e: e.activation(out=KaT[0:64, :, tok], in_=pT[0:64, 0:2, :], func=AF.Copy),
           reads=[b_pT], writes=[b_KaT[t]])
        op("dve", lambda e: e.tensor_reduce(out=ksum[:, par, :], in_=pT[0:64, 0:2, :], axis=mybir.AxisListType.X, op=ALU.add),
           reads=[b_pT], writes=[b_ksum[par]])
        if j >= 4:
            op("act", lambda e: e.activation(out=qTt[:], in_=pT[0:64, 2:4, :], func=AF.Copy), reads=[b_pT], writes=[b_qTt])
            for h in range(2):
                op("pe", lambda e: e.matmul(pg[:, h * 32:(h + 1) * 32], lhsT=qTt[:, h, :], rhs=kmT[:, h, :], start=True, stop=True),
                   reads=[b_qTt, b_kmT], writes=[b_pg])
            op("dve", lambda e: e.tensor_copy(out=Gt[:, :, 0:j], in_=pg[:, 0:64].rearrange("p (h n) -> p h n", h=2)[:, :, 0:j]),
               reads=[b_pg], writes=[b_Gt])
            for h in range(2):
                op("dve", lambda e: e.max(out=mx8[:, h, :], in_=Gt[:, h, :]), reads=[b_Gt], writes=[b_mx8])
            for h in range(2):
                op("dve", lambda e: e.tensor_scalar(out=nm[:, h, :], in0=Gt[:, h, :], scalar1=mx8[:, h, 2:3], scalar2=1.0,
                                                    op0=ALU.is_ge, op1=ALU.subtract), reads=[b_Gt, b_mx8], writes=[b_nm])
            op("dve", lambda e: e.memset(nm[:, :, j:j + 1], 0.0), writes=[b_nm])
            op("dve", lambda e: e.tensor_scalar(out=Qtok[:, :, 64:96], in0=nm[:], scalar1=-NEGBIG, scalar2=None, op0=ALU.mult),
               reads=[b_nm], writes=[b_Qtok])
        else:
            op("dve", lambda e: e.memset(Qtok[:, :, 64:96], NEGBIG), writes=[b_Qtok])
            op("dve", lambda e: e.memset(Qtok[:, :, 64:64 + j + 1], 0.0), writes=[b_Qtok])
        op("pool", lambda e: e.tensor_scalar(out=Qtok[:, :, 0:64], in0=qkf[:, 0:128].rearrange("p (h d) -> p h d", h=2),
                                             scalar1=0.125, scalar2=None, op0=ALU.mult),
           reads=[b_qkr], writes=[b_Qtok])
        for h in range(2):
            op("pe", lambda e: e.transpose(pqa[0:96, h, :], Qtok[:, h, :], ident_bf[:]), reads=[b_Qtok, b_ident], writes=[b_pqa])
        op("act", lambda e: e.activation(out=QaT[0:96, :, tok], in_=pqa[0:96, 0:2, :], func=AF.Copy),
           reads=[b_pqa], writes=[b_QaT[t]])
        if par == 1:
            op("dve", lambda e: e.tensor_tensor(out=kmT[:, :, j], in0=ksum[:, 0, :], in1=ksum[:, 1, :], op=ALU.add),
               reads=[b_ksum[0], b_ksum[1]], writes=[b_kmT])

    if dbg_tiles > 0:
        load_x(0)
        load_x(1)
        stage_a(0)
    for t in range(dbg_tiles):
        if t + 2 < NTILE:
            load_x(t + 2)
        if t % 8 == 7 and with_ssm:
            u_proj(t // 8)
        if t + 1 < dbg_tiles:
            stage_a(t + 1)
        stage_b(t)
    P.pop_scope()

    P.push_scope()
    cm = P.sb("cm", [128, 4, 512], BF16); b_cm = P.buf()
    dma(lambda e: e.dma_start(out=cm[:].rearrange("p a c -> p (a c)"), in_=cmask[:, :]), writes=[b_cm], q="pool")
    Pt = [P.sb(f"Pt{i}", [128, 512], BF16) for i in range(3)]; b_Pt = [P.buf() for _ in range(3)]
    Osb = [P.sb(f"Osb{i}", [128, 4, 65], F32) for i in range(2)]; b_Osb = [P.buf() for _ in range(2)]
    pS = [P.ps(f"pS{i}", [128, 512], F32) for i in range(3)]; b_pS = [P.pbuf() for _ in range(3)]
    pO = [P.ps(f"pO{i}", [128, 512], F32) for i in range(2)]; b_pO = [P.pbuf() for _ in range(2)]
    b_allQ, b_allK, b_allV = b_QaT, b_KaT + [b_Kind], b_V + [b_Vones]
    cnt = 0
    gi = 0
    for h in range(2 if dbg_attn else 0):
        for G in range(16 if dbg_attn is True else dbg_attn):
            po, bpo = _rot(pO, gi), _rot(b_pO, gi)
            ob, bob = _rot(Osb, gi), _rot(b_Osb, gi)
            gi += 1
            nkt = 4 * G + 4
            qs = slice(G * 512, (G + 1) * 512)
            qbufs = b_QaT[4 * G:4 * G + 4]
            op("pe", lambda e: e.matmul(po[:, 0:260], lhsT=zeros_bf[0:1, 0:128], rhs=zeros_bf[0:1, 0:260], start=True, stop=False),
               reads=[b_zeros_bf], writes=[bpo])

            def s_mm(kt, c):
                ps_, bps = _rot(pS, c), _rot(b_pS, c)
                op("pe", lambda e: e.matmul(ps_[:], lhsT=KaT[0:96, h, kt * 128:(kt + 1) * 128], rhs=QaT[0:96, h, qs],
                                            start=True, stop=True),
                   reads=[b_KaT[kt], b_Kind] + qbufs, writes=[bps])
            s_mm(0, cnt)
            for kt in range(nkt):
                c = cnt + kt
                if kt + 1 < nkt:
                    s_mm(kt + 1, c + 1)
                ps_, bps = _rot(pS, c), _rot(b_pS, c)
                p_, bp_ = _rot(Pt, c), _rot(b_Pt, c)
                op("act", lambda e: e.activation(out=p_[:], in_=ps_[:], func=AF.Exp), reads=[bps], writes=[bp_])
                a = kt - 4 * G
                if a >= 0:
                    op("pool", lambda e: e.tensor_tensor(out=p_[:], in0=p_[:], in1=cm[:, a, :], op=ALU.mult),
                       reads=[bp_, b_cm], writes=[bp_])
                for bq in range(4):
                    if a >= 0 and bq < a:
                        continue
                    last = (kt == nkt - 1)
                    op("pe", lambda e: e.matmul(po[:, bq * 65:(bq + 1) * 65], lhsT=p_[:, bq * 128:(bq + 1) * 128],
                                                rhs=Vaug[:, kt, h, :], start=False, stop=last),
                       reads=[bp_, b_V[kt], b_Vones], writes=[bpo])
            cnt += nkt
            op("dve", lambda e: e.tensor_copy(out=ob[:].rearrange("p b e -> p (b e)"), in_=po[:, 0:260]), reads=[bpo], writes=[bob])
            dma(lambda e: e.dma_start(out=attn_o[G * 512:(G + 1) * 512, h * 65:(h + 1) * 65].rearrange("(b p) e -> p b e", p=128),
                                      in_=ob[:]), reads=[bob])
    P.pop_scope()
    P.pop_scope()

    if with_ssm:
        build_ssm(nc, P, U_tok, b_U, y_o, ident_bf, b_ident, ident_f, b_identf)
    P.finish()
    return nc


def rope_tables():
    half = 32
    inv = (10000.0 ** (-np.arange(half, dtype=np.float32) / half)).astype(np.float32)
    ang = np.arange(L_SEQ, dtype=np.float32)[:, None] * inv[None, :]
    cos, sin = np.cos(ang).astype(np.float32), np.sin(ang).astype(np.float32)
    cl = np.ascontiguousarray(cos.reshape(NTILE, 128, half).transpose(1, 0, 2).reshape(128, NTILE * half))
    sl = np.ascontiguousarray(sin.reshape(NTILE, 128, half).transpose(1, 0, 2).reshape(128, NTILE * half))
    return cl, sl


def const_tables():
    ind = np.zeros((32, L_SEQ), np.float32)
    for j in range(32):
        ind[j, j * 256:(j + 1) * 256] = 1.0
    cm = np.zeros((128, 4, 512), np.float32)
    kk = np.arange(128)[:, None]
    qq = np.arange(128)[None, :]
    tri = (kk <= qq).astype(np.float32)
    for a in range(4):
        for bq in range(4):
            if bq > a:
                cm[:, a, bq * 128:(bq + 1) * 128] = 1.0
            elif bq == a:
                cm[:, a, bq * 128:(bq + 1) * 128] = tri
    return ind, cm.reshape(128, 2048)


def phase_ab_inputs(inp, b, r):
    c = inp["c"][b]
    cT = np.ascontiguousarray(c.reshape(8, 128).T)
    cT2 = np.repeat(cT[:, :, None], 2, axis=2).reshape(128, 16)
    w = inp["w_in"][0]
    cols = np.concatenate([np.arange(128 * r, 128 * r + 128), 512 + np.arange(128 * r, 128 * r + 128),
                           1024 + np.arange(128 * r, 128 * r + 128), 1536 + np.arange(128 * r, 128 * r + 128)])
    cl, sl = rope_tables()
    ind, cm = const_tables()
    d = {
        "xb": np.ascontiguousarray(inp["x"][b]),
        "cT2": np.ascontiguousarray(cT2),
        "wada_a": np.ascontiguousarray(inp["w_ada"][0][:, 0:2048]),
        "bada_a": np.ascontiguousarray(inp["b_ada"][0][None, 0:2048]),
        "gmix": np.ascontiguousarray(inp["g_mix"][0][None, :]),
        "win": np.ascontiguousarray(w[:, cols]),
        "cosl": cl, "sinl": sl, "ind": ind, "cmask": cm, "ident": _ident(),
    }
    d.update(ssm_inputs(inp, r))
    return d


KV_F, KV_G, KV_O, KV_E, KV_A, KV_H = 0, 8, 16, 24, 32, 42
NKV = 43
TWO_PI = 2.0 * math.pi
CW1 = 6.28125
CW2 = TWO_PI - CW1


def ssm_kvec():
    kv = np.zeros(NKV, np.float32)
    for s in range(8):
        kv[KV_F + s] = -s
        kv[KV_G + s] = s
        kv[KV_O + s] = s + 1
        kv[KV_E + s] = 7 - s
    for i in range(10):
        kv[KV_A + i] = 8.0 * (2 ** i)
    kv[KV_H] = 0.5
    return kv


def build_ssm(nc, P, U_tok, b_U, y_o, ident_bf, b_ident, ident_f, b_identf):
    def din(name, shape, dt=F32):
        return nc.dram_tensor(name, list(shape), dt, kind="ExternalInput").ap()
    sp_small = din("ssm_small", [128, 8 * 4 + NKV + 2 + 8])
    sp_bc = din("ssm_bc", [128, 4 * 128])
    sp_mat = din("ssm_mat", [128, 256])
    op, dma = P.op, P.dma
    P.push_scope()
    small = P.sb("ssm_small_sb", [128, 8 * 4 + NKV + 2 + 8], F32); b_small = P.buf()
    bc = P.sb("ssm_bc_sb", [128, 4, 8, 16], F32); b_bc = P.buf()
    mats = P.sb("ssm_mat_sb", [128, 2, 128], F32); b_mats = P.buf()
    dma(lambda e: e.dma_start(out=small[:], in_=sp_small[:, :]), writes=[b_small])
    dma(lambda e: e.dma_start(out=bc[:].rearrange("p a g m -> p (a g m)"), in_=sp_bc[:, :]), writes=[b_bc])
    dma(lambda e: e.dma_start(out=mats[:].rearrange("p a n -> p (a n)"), in_=sp_mat[:, :]), writes=[b_mats])
    lamre, lamim, logdt, dsk = small[:, 0:8], small[:, 8:16], small[:, 16:24], small[:, 24:32]
    kvec = small[:, 32:32 + NKV]
    sgn_a, sgn_b = small[:, 32 + NKV:33 + NKV], small[:, 33 + NKV:34 + NKV]
    smask, P2 = mats[:, 0, :], mats[:, 1, :]
    Bs1, Bs2, Cs1, Cs2 = bc[:, 0], bc[:, 1], bc[:, 2], bc[:, 3]

    def T(name, shape, dt=F32):
        return P.sb(name, shape, dt), P.buf()

    def dv(fn, reads, writes):
        return op("dve", fn, reads=reads, writes=writes)

    dt_, b_dt = T("s_dt", [128, 8])
    a_, b_a = T("s_a", [128, 8])
    th, b_th = T("s_th", [128, 8])
    op("act", lambda e: e.activation(out=dt_[:], in_=logdt, func=AF.Exp), reads=[b_small], writes=[b_dt])
    dv(lambda e: e.tensor_tensor(out=a_[:], in0=lamre, in1=dt_[:], op=ALU.mult), [b_small, b_dt], [b_a])
    dv(lambda e: e.tensor_tensor(out=th[:], in0=lamim, in1=dt_[:], op=ALU.mult), [b_small, b_dt], [b_th])
    KA, b_KA = T("s_KA", [128, 8, NKV])
    KT, b_KT = T("s_KT", [128, 8, NKV])
    kb = kvec.unsqueeze(1).to_broadcast([128, 8, NKV])
    dv(lambda e: e.tensor_tensor(out=KA[:], in0=a_[:].unsqueeze(2).to_broadcast([128, 8, NKV]), in1=kb, op=ALU.mult),
       [b_a, b_small], [b_KA])
    dv(lambda e: e.tensor_tensor(out=KT[:], in0=th[:].unsqueeze(2).to_broadcast([128, 8, NKV]), in1=kb, op=ALU.mult),
       [b_th, b_small], [b_KT])
    MAG, b_MAG = T("s_MAG", [128, 8, NKV])
    op("act", lambda e: e.activation(out=MAG[:], in_=KA[:], func=AF.Exp), reads=[b_KA], writes=[b_MAG])
    ni, b_ni = T("s_ni", [128, 8, NKV], I32)
    nf, b_nf = T("s_nf", [128, 8, NKV])
    rr, b_rr = T("s_rr", [128, 8, NKV])
    uu, b_uu = T("s_uu", [128, 8, NKV])
    SIN, b_SIN = T("s_SIN", [128, 8, NKV])
    COS, b_COS = T("s_COS", [128, 8, NKV])

    def sin_of(dst, b_dst, shift):
        dv(lambda e: e.tensor_scalar(out=uu[:], in0=KT[:], scalar1=shift, scalar2=1.0 / TWO_PI, op0=ALU.add, op1=ALU.mult),
           [b_KT], [b_uu])
        dv(lambda e: e.tensor_copy(out=ni[:], in_=uu[:]), [b_uu], [b_ni])
        dv(lambda e: e.tensor_copy(out=nf[:], in_=ni[:]), [b_ni], [b_nf])
        dv(lambda e: e.scalar_tensor_tensor(out=rr[:], in0=nf[:], scalar=-CW1, in1=KT[:], op0=ALU.mult, op1=ALU.add),
           [b_nf, b_KT], [b_rr])
        dv(lambda e: e.scalar_tensor_tensor(out=rr[:], in0=nf[:], scalar=-CW2, in1=rr[:], op0=ALU.mult, op1=ALU.add),
           [b_nf, b_rr], [b_rr])
        dv(lambda e: e.tensor_scalar(out=rr[:], in0=rr[:], scalar1=shift, scalar2=math.pi, op0=ALU.add, op1=ALU.min),
           [b_rr], [b_rr])
        dv(lambda e: e.tensor_scalar(out=rr[:], in0=rr[:], scalar1=-math.pi, scalar2=None, op0=ALU.max), [b_rr], [b_rr])
        op("act", lambda e: e.activation(out=dst[:], in_=rr[:], func=AF.Sin), reads=[b_rr], writes=[b_dst])
    sin_of(SIN, b_SIN, 0.0)
    sin_of(COS, b_COS, math.pi / 2.0)
    PR, b_PR = T("s_PR", [128, 8, NKV])
    PI_, b_PI = T("s_PI", [128, 8, NKV])
    dv(lambda e: e.tensor_tensor(out=PR[:], in0=MAG[:], in1=COS[:], op=ALU.mult), [b_MAG, b_COS], [b_PR])
    dv(lambda e: e.tensor_tensor(out=PI_[:], in0=MAG[:], in1=SIN[:], op=ALU.mult), [b_MAG, b_SIN], [b_PI])
    em1, b_em1 = T("s_em1", [128, 8])
    tq, b_tq = T("s_tq", [128, 8])
    dv(lambda e: e.tensor_scalar(out=tq[:], in0=a_[:], scalar1=0.25, scalar2=1.0, op0=ALU.mult, op1=ALU.add), [b_a], [b_tq])
    dv(lambda e: e.tensor_tensor(out=tq[:], in0=tq[:], in1=a_[:], op=ALU.mult), [b_tq, b_a], [b_tq])
    dv(lambda e: e.tensor_scalar(out=tq[:], in0=tq[:], scalar1=1.0 / 3.0, scalar2=1.0, op0=ALU.mult, op1=ALU.add), [b_tq], [b_tq])
    dv(lambda e: e.tensor_tensor(out=tq[:], in0=tq[:], in1=a_[:], op=ALU.mult), [b_tq, b_a], [b_tq])
    dv(lambda e: e.tensor_scalar(out=tq[:], in0=tq[:], scalar1=0.5, scalar2=1.0, op0=ALU.mult, op1=ALU.add), [b_tq], [b_tq])
    dv(lambda e: e.tensor_tensor(out=em1[:], in0=tq[:], in1=a_[:], op=ALU.mult), [b_tq, b_a], [b_em1])
    cth, sth, shalf = COS[:, :, KV_G + 1], SIN[:, :, KV_G + 1], SIN[:, :, KV_H]
    re1, b_re1 = T("s_re1", [128, 8])
    im1, b_im1 = T("s_im1", [128, 8])
    w1, b_w1 = T("s_w1", [128, 8])
    w2, b_w2 = T("s_w2", [128, 8])
    dv(lambda e: e.tensor_tensor(out=w1[:], in0=shalf, in1=shalf, op=ALU.mult), [b_SIN], [b_w1])
    dv(lambda e: e.tensor_tensor(out=re1[:], in0=em1[:], in1=cth, op=ALU.mult), [b_em1, b_COS], [b_re1])
    dv(lambda e: e.scalar_tensor_tensor(out=re1[:], in0=w1[:], scalar=-2.0, in1=re1[:], op0=ALU.mult, op1=ALU.add),
       [b_w1, b_re1], [b_re1])
    dv(lambda e: e.scalar_tensor_tensor(out=im1[:], in0=em1[:], scalar=1.0, in1=sth, op0=ALU.add, op1=ALU.mult),
       [b_em1, b_SIN], [b_im1])
    den, b_den = T("s_den", [128, 8])
    dv(lambda e: e.tensor_tensor(out=den[:], in0=lamre, in1=lamre, op=ALU.mult), [b_small], [b_den])
    dv(lambda e: e.tensor_tensor(out=w1[:], in0=lamim, in1=lamim, op=ALU.mult), [b_small], [b_w1])
    dv(lambda e: e.tensor_tensor(out=den[:], in0=den[:], in1=w1[:], op=ALU.add), [b_den, b_w1], [b_den])
    dv(lambda e: e.reciprocal(out=den[:], in_=den[:]), [b_den], [b_den])
    cr, b_cr = T("s_cr", [128, 8])
    ci, b_ci = T("s_ci", [128, 8])
    dv(lambda e: e.tensor_tensor(out=w1[:], in0=re1[:], in1=lamre, op=ALU.mult), [b_re1, b_small], [b_w1])
    dv(lambda e: e.tensor_tensor(out=w2[:], in0=im1[:], in1=lamim, op=ALU.mult), [b_im1, b_small], [b_w2])
    dv(lambda e: e.tensor_tensor(out=w1[:], in0=w1[:], in1=w2[:], op=ALU.add), [b_w1, b_w2], [b_w1])
    dv(lambda e: e.tensor_tensor(out=cr[:], in0=w1[:], in1=den[:], op=ALU.mult), [b_w1, b_den], [b_cr])
    dv(lambda e: e.tensor_tensor(out=w1[:], in0=im1[:], in1=lamre, op=ALU.mult), [b_im1, b_small], [b_w1])
    dv(lambda e: e.tensor_tensor(out=w2[:], in0=re1[:], in1=lamim, op=ALU.mult), [b_re1, b_small], [b_w2])
    dv(lambda e: e.tensor_tensor(out=w1[:], in0=w1[:], in1=w2[:], op=ALU.subtract), [b_w1, b_w2], [b_w1])
    dv(lambda e: e.tensor_tensor(out=ci[:], in0=w1[:], in1=den[:], op=ALU.mult), [b_w1, b_den], [b_ci])
    bb1, b_bb1 = T("s_bb1", [128, 8, 16])
    bb2, b_bb2 = T("s_bb2", [128, 8, 16])
    q1, b_q1 = T("s_q1", [128, 8, 16])
    q2, b_q2 = T("s_q2", [128, 8, 16])
    crb = cr[:].unsqueeze(2).to_broadcast([128, 8, 16])
    cib = ci[:].unsqueeze(2).to_broadcast([128, 8, 16])
    dv(lambda e: e.tensor_tensor(out=q1[:], in0=crb, in1=Bs1, op=ALU.mult), [b_cr, b_bc], [b_q1])
    dv(lambda e: e.tensor_tensor(out=q2[:], in0=cib, in1=Bs2, op=ALU.mult), [b_ci, b_bc], [b_q2])
    dv(lambda e: e.scalar_tensor_tensor(out=bb1[:].rearrange("p g m -> p (g m)"), in0=q2[:].rearrange("p g m -> p (g m)"),
                                         scalar=sgn_a, in1=q1[:].rearrange("p g m -> p (g m)"), op0=ALU.mult, op1=ALU.add),
       [b_q1, b_q2, b_small], [b_bb1])
    dv(lambda e: e.tensor_tensor(out=q1[:], in0=crb, in1=Bs2, op=ALU.mult), [b_cr, b_bc], [b_q1])
    dv(lambda e: e.tensor_tensor(out=q2[:], in0=cib, in1=Bs1, op=ALU.mult), [b_ci, b_bc], [b_q2])
    dv(lambda e: e.scalar_tensor_tensor(out=bb2[:].rearrange("p g m -> p (g m)"), in0=q2[:].rearrange("p g m -> p (g m)"),
                                         scalar=sgn_b, in1=q1[:].rearrange("p g m -> p (g m)"), op0=ALU.mult, op1=ALU.add),
       [b_q1, b_q2, b_small], [b_bb2])
    CA, b_CA = T("s_CA", [128, 8, 16])
    CB, b_CB = T("s_CB", [128, 8, 16])
    dv(lambda e: e.tensor_scalar(out=CA[:], in0=Cs1, scalar1=sgn_b, scalar2=None, op0=ALU.mult), [b_bc, b_small], [b_CA])
    dv(lambda e: e.tensor_scalar(out=CB[:], in0=Cs2, scalar1=-1.0, scalar2=None, op0=ALU.mult), [b_bc], [b_CB])
    Fm, b_Fm = T("s_F", [128, 8, 8, 16])
    Fe, b_Fe = T("s_Fe", [128, 8, 8, 16])
    Gm, b_Gm = T("s_G", [128, 8, 8, 16])
    Om, b_Om = T("s_O", [128, 8, 8, 16])
    z1, b_z1 = T("s_z1", [128, 8, 8, 16])
    z2, b_z2 = T("s_z2", [128, 8, 8, 16])

    def outer(dst, bdst, kv0, v1, bv1, v2, bv2, sgn):
        pr = PR[:, :, kv0:kv0 + 8].unsqueeze(3).to_broadcast([128, 8, 8, 16])
        pi = PI_[:, :, kv0:kv0 + 8].unsqueeze(3).to_broadcast([128, 8, 8, 16])
        dv(lambda e: e.tensor_tensor(out=z1[:], in0=pr, in1=v1[:].unsqueeze(2).to_broadcast([128, 8, 8, 16]), op=ALU.mult),
           [b_PR, bv1], [b_z1])
        dv(lambda e: e.tensor_tensor(out=z2[:], in0=pi, in1=v2[:].unsqueeze(2).to_broadcast([128, 8, 8, 16]), op=ALU.mult),
           [b_PI, bv2], [b_z2])
        fl = "p g s m -> p (g s m)"
        if sgn is None:
            dv(lambda e: e.tensor_tensor(out=dst[:].rearrange(fl), in0=z1[:].rearrange(fl), in1=z2[:].rearrange(fl), op=ALU.add),
               [b_z1, b_z2], [bdst])
        else:
            dv(lambda e: e.scalar_tensor_tensor(out=dst[:].rearrange(fl), in0=z2[:].rearrange(fl), scalar=sgn,
                                                 in1=z1[:].rearrange(fl), op0=ALU.mult, op1=ALU.add),
               [b_z1, b_z2, b_small], [bdst])
    outer(Fm, b_Fm, KV_F, bb1, b_bb1, bb2, b_bb2, sgn_a)
    outer(Fe, b_Fe, KV_E, bb1, b_bb1, bb2, b_bb2, sgn_a)
    outer(Gm, b_Gm, KV_G, CA, b_CA, CB, b_CB, None)
    outer(Om, b_Om, KV_O, CA, b_CA, CB, b_CB, None)
    PIA, b_PIA = T("s_PIA", [128, 8, 10])
    dv(lambda e: e.tensor_scalar(out=PIA[:], in0=PI_[:, :, KV_A:KV_A + 10], scalar1=sgn_b, scalar2=None, op0=ALU.mult),
       [b_PI, b_small], [b_PIA])

    Yout = P.sb("s_Yout", [128, 8, 8, 128], F32); b_Yout = P.buf()
    Ug = P.sb("s_Ug", [128, 1024], BF16); b_Ug = P.buf()
    H = P.sb("s_H", [128, 1024], F32); b_H = P.buf()
    Ysb = P.sb("s_Ysb", [128, 1024], F32); b_Ysb = P.buf()
    Mbf = P.sb("s_Mbf", [128, 128], BF16); b_Mbf = P.buf()
    Ebf = P.sb("s_Ebf", [128, 128], BF16); b_Ebf = P.buf()
    mtmp = P.sb("s_mtmp", [128, 128], F32); b_mtmp = P.buf()
    Amat = P.sb("s_Amat", [128, 10, 128], F32); b_Amat = P.buf()
    put = P.ps("s_put", [128, 8, 128], BF16); b_put = P.pbuf()
    pxy = [P.ps(f"s_pxy{i}", [128, 512], F32) for i in range(2)]; b_pxy = [P.pbuf() for _ in range(2)]
    psc = [P.ps(f"s_psc{i}", [128, 512], F32) for i in range(2)]; b_psc = [P.pbuf() for _ in range(2)]
    pme = P.ps("s_pme", [128, 512], F32); b_pme = P.pbuf()
    pyt = P.ps("s_pyt", [128, 8, 128], F32); b_pyt = P.pbuf()
    for g in range(8):
        Fg = Fm[:, g].rearrange("p s m -> p (s m)")
        Feg = Fe[:, g].rearrange("p s m -> p (s m)")
        Gg = Gm[:, g].rearrange("p s m -> p (s m)")
        Og = Om[:, g].rearrange("p s m -> p (s m)")
        op("pe", lambda e: e.matmul(pme[:, 0:128], lhsT=Fg, rhs=Gg, start=True, stop=True), reads=[b_Fm, b_Gm], writes=[b_pme])
        dv(lambda e: e.tensor_tensor(out=mtmp[:], in0=pme[:, 0:128], in1=smask, op=ALU.mult), [b_pme, b_mats], [b_mtmp])
        dv(lambda e: e.scalar_tensor_tensor(out=Mbf[:], in0=ident_f[:], scalar=dsk[:, g:g + 1], in1=mtmp[:],
                                             op0=ALU.mult, op1=ALU.add), [b_identf, b_small, b_mtmp], [b_Mbf])
        op("pe", lambda e: e.transpose(pme[:, 128:256], Feg, ident_f[:]), reads=[b_Fe, b_identf], writes=[b_pme])
        op("act", lambda e: e.activation(out=Ebf[:], in_=pme[:, 128:256], func=AF.Copy), reads=[b_pme], writes=[b_Ebf])
        for i in range(10):
            dv(lambda e: e.tensor_scalar(out=mtmp[:], in0=ident_f[:], scalar1=PR[:, g, KV_A + i:KV_A + i + 1], scalar2=None,
                                         op0=ALU.mult), [b_identf, b_PR], [b_mtmp])
            dv(lambda e: e.scalar_tensor_tensor(out=Amat[:, i, :], in0=P2, scalar=PIA[:, g, i:i + 1], in1=mtmp[:],
                                                 op0=ALU.mult, op1=ALU.add), [b_mats, b_PIA, b_mtmp], [b_Amat])
        for S in range(8):
            op("pe", lambda e: e.transpose(put[:, S, :], U_tok[:, S, g].rearrange("p s m -> p (s m)"), ident_bf[:]),
               reads=[b_U[S], b_ident], writes=[b_put])
        op("act", lambda e: e.activation(out=Ug[:].rearrange("p (S c) -> p S c", S=8), in_=put[:], func=AF.Copy),
           reads=[b_put], writes=[b_Ug])
        for hf in range(2):
            op("pe", lambda e: e.matmul(pxy[hf][:], lhsT=Ebf[:], rhs=Ug[:, hf * 512:(hf + 1) * 512], start=True, stop=True),
               reads=[b_Ebf, b_Ug], writes=[b_pxy[hf]])
            op("act", lambda e: e.activation(out=H[:, hf * 512:(hf + 1) * 512], in_=pxy[hf][:], func=AF.Copy),
               reads=[b_pxy[hf]], writes=[b_H])
        for i in range(10):
            d = 1 << i
            rngs = []
            for hb in range(2):
                lo = max(d, hb * 512)
                hi = (hb + 1) * 512
                if lo < hi:
                    rngs.append((hb, lo, hi))
            for (hb, lo, hi) in rngs:
                op("pe", lambda e: e.matmul(psc[hb][:, lo - hb * 512:hi - hb * 512], lhsT=Amat[:, i, :], rhs=H[:, lo - d:hi - d],
                                            start=True, stop=True), reads=[b_Amat, b_H], writes=[b_psc[hb]])
            for (hb, lo, hi) in rngs:
                dv(lambda e: e.tensor_tensor(out=H[:, lo:hi], in0=psc[hb][:, lo - hb * 512:hi - hb * 512], in1=H[:, lo:hi], op=ALU.add),
                   [b_psc[hb], b_H], [b_H])
        for hf in range(2):
            op("pe", lambda e: e.matmul(pxy[hf][:], lhsT=Mbf[:], rhs=Ug[:, hf * 512:(hf + 1) * 512], start=True, stop=False),
               reads=[b_Mbf, b_Ug], writes=[b_pxy[hf]])
            if hf == 0:
                op("pe", lambda e: e.matmul(pxy[0][:, 1:512], lhsT=Og, rhs=H[:, 0:511], start=False, stop=True),
                   reads=[b_Om, b_H], writes=[b_pxy[0]])
            else:
                op("pe", lambda e: e.matmul(pxy[1][:], lhsT=Og, rhs=H[:, 511:1023], start=False, stop=True),
                   reads=[b_Om, b_H], writes=[b_pxy[1]])
            op("act", lambda e: e.activation(out=Ysb[:, hf * 512:(hf + 1) * 512], in_=pxy[hf][:], func=AF.Copy),
               reads=[b_pxy[hf]], writes=[b_Ysb])
        for S in range(8):
            op("pe", lambda e: e.transpose(pyt[:, S, :], Ysb[:, S * 128:(S + 1) * 128], ident_f[:]),
               reads=[b_Ysb, b_identf], writes=[b_pyt])
        dv(lambda e: e.tensor_copy(out=Yout[:, :, :, g * 16:(g + 1) * 16],
                                   in_=pyt[:].rearrange("p S (t n) -> p S t n", t=8)), [b_pyt], [b_Yout])
    for S in range(8):
        dma(lambda e: e.dma_start(out=y_o[S * 1024:(S + 1) * 1024, :].rearrange("(c t) ch -> c t ch", t=8), in_=Yout[:, S]),
            reads=[b_Yout])
    P.pop_scope()


def ssm_inputs(inp, r):
    gs = slice(8 * r, 8 * r + 8)
    lam_re = inp["lam_re"][0][gs]
    lam_im = inp["lam_im"][0][gs]
    log_dt = inp["log_dt"][0][gs]
    b_re, b_im = inp["b_re"][0][gs], inp["b_im"][0][gs]
    c_re, c_im = inp["c_re"][0][gs], inp["c_im"][0][gs]
    d = inp["d_skip"][0][gs]
    dup = lambda a: np.concatenate([a, a], axis=0)
    small = np.zeros((128, 8 * 4 + NKV + 2 + 8), np.float32)
    small[:, 0:8] = dup(lam_re.T)
    small[:, 8:16] = dup(lam_im.T)
    small[:, 16:24] = np.broadcast_to(log_dt[None, :], (128, 8))
    small[:, 24:32] = np.tile(d.T, (8, 1))
    small[:, 32:32 + NKV] = ssm_kvec()[None, :]
    small[:64, 32 + NKV] = -1.0; small[64:, 32 + NKV] = 1.0
    small[:64, 33 + NKV] = 1.0; small[64:, 33 + NKV] = -1.0
    bre = b_re.transpose(1, 0, 2); bim = b_im.transpose(1, 0, 2)
    cre = c_re.transpose(2, 0, 1); cim = c_im.transpose(2, 0, 1)
    bcv = np.zeros((128, 4, 8, 16), np.float32)
    bcv[:64, 0], bcv[64:, 0] = bre, bim
    bcv[:64, 1], bcv[64:, 1] = bim, bre
    bcv[:64, 2], bcv[64:, 2] = cre, cim
    bcv[:64, 3], bcv[64:, 3] = cim, cre
    s_idx = np.arange(128) // 16
    smask = (s_idx[None, :] >= s_idx[:, None]).astype(np.float32)
    P2 = np.zeros((128, 128), np.float32)
    P2[np.arange(128), (np.arange(128) + 64) % 128] = 1.0
    return {"ssm_small": small, "ssm_bc": np.ascontiguousarray(bcv.reshape(128, 512)),
            "ssm_mat": np.ascontiguousarray(np.concatenate([smask, P2], axis=1))}


_NC_CACHE = {}


def _get_nc(which):
    if which not in _NC_CACHE:
        nc = bass.Bass("TRN2", target_bir_lowering=False)
        if which == "ab":
            build_phase_ab(nc, with_ssm=True)
        else:
            build_phase_c(nc)
        _NC_CACHE[which] = nc
    return _NC_CACHE[which]


def kernel(**inputs):
    inp = {k: np.asarray(v, dtype=np.float32) for k, v in inputs.items()}
    B = inp["x"].shape[0]
    maps1 = [phase_ab_inputs(inp, ci // 4, ci % 4) for ci in range(8)]
    res1 = run_bass_kernel_spmd(_get_nc("ab"), maps1, core_ids=list(range(8))).results
    attn_full = np.zeros((B, L_SEQ, 8, 65), np.float32)
    y_full = np.zeros((B, L_SEQ, 512), np.float32)
    for ci in range(8):
        b, r = ci // 4, ci % 4
        attn_full[b, :, 2 * r:2 * r + 2, :] = res1[ci]["attn_o"].reshape(L_SEQ, 2, 65)
        y_full[b, :, 128 * r:128 * r + 128] = res1[ci]["y_o"]
    maps2 = [phase_c_inputs(inp, ci // 4, ci % 4, attn_full, y_full) for ci in range(8)]
    res2 = run_bass_kernel_spmd(_get_nc("c"), maps2, core_ids=list(range(8))).results
    out = np.zeros((B, L_SEQ, 1024), np.float32)
    for ci in range(8):
        b, qi = ci // 4, ci % 4
        out[b, qi * NT_C:(qi + 1) * NT_C] = res2[ci]["out"]
    return out
```

```python
import math
import numpy as np
from contextlib import ExitStack

import concourse.bass as bass
import concourse.mybir as mybir
from concourse.bass_utils import run_bass_kernel_spmd

F32 = mybir.dt.float32
BF16 = mybir.dt.bfloat16
I32 = mybir.dt.int32
ALU = mybir.AluOpType
AF = mybir.ActivationFunctionType

PHYS = ["pe", "act", "dve", "pool", "sp"]
NDMA = 8
SAME_ENG_WAIT = True
EPS = 1e-6
NEGBIG = -30000.0


class Buf:
    __slots__ = ("name", "last_w", "readers", "excl")

    def __init__(self, name, excl=False):
        self.name = name
        self.last_w = None
        self.readers = []
        self.excl = excl


class Prog:
    def __init__(self, nc):
        self.nc = nc
        self.stack = ExitStack()
        self.semnames = ["pe", "act", "dve", "pool"] + [f"d{i}" for i in range(NDMA)]
        self.cnt = {s: 0 for s in self.semnames}
        self.seen = {e: {s: 0 for s in self.semnames} for e in PHYS}
        self.sems = {s: self.stack.enter_context(nc.semaphore("sem_" + s)) for s in self.semnames}
        self.engobjs = {"pe": nc.tensor, "act": nc.scalar, "dve": nc.vector, "pool": nc.gpsimd, "sp": nc.sync}
        self.pending = {e: {} for e in PHYS}
        self.nbuf = 0
        self.dma_rr = 0
        self.scopes = [self.stack]

    def buf(self, name=None, excl=False):
        self.nbuf += 1
        return Buf(name or f"b{self.nbuf}", excl)

    def pbuf(self):
        return self.buf(excl=True)

    def sb(self, name, shape, dtype):
        return self.scopes[-1].enter_context(self.nc.sbuf_tensor(name, list(shape), dtype))

    def ps(self, name, shape, dtype=F32):
        return self.scopes[-1].enter_context(self.nc.psum_tensor(name, list(shape), dtype))

    def push_scope(self):
        st = ExitStack()
        self.scopes.append(st)
        return st

    def pop_scope(self):
        st = self.scopes.pop()
        st.close()
        self.fence()

    def fence(self):
        for e in PHYS:
            for s in self.semnames:
                if self.cnt[s] > self.seen[e][s]:
                    self.pending[e][s] = self.cnt[s]

    def _op(self, phys, sem, inc, fn, reads, writes, extra_waits=()):
        waits = dict(self.pending[phys])
        self.pending[phys] = {}
        xr = [b for b in reads if b.excl]
        if xr:
            reads = [b for b in reads if not b.excl]
            writes = list(writes) + [b for b in xr if b not in writes]
        for (f, c) in extra_waits:
            waits[f] = max(waits.get(f, 0), c)

        def need(f):
            return f != phys or (SAME_ENG_WAIT and phys != "pe")
        for b in reads:
            if b.last_w is not None:
                f, c = b.last_w
                if need(f):
                    waits[f] = max(waits.get(f, 0), c)
        for b in writes:
            if b.last_w is not None:
                f, c = b.last_w
                if need(f):
                    waits[f] = max(waits.get(f, 0), c)
            for (f, c) in b.readers:
                if need(f):
                    waits[f] = max(waits.get(f, 0), c)
        engobj = self.engobjs[phys]
        for f, c in waits.items():
            if c > self.seen[phys][f]:
                self.seen[phys][f] = c
                engobj.wait_ge(self.sems[f], c)
        self.cnt[sem] += inc
        me = (sem, self.cnt[sem])
        ins = fn(engobj)
        ins.then_inc(self.sems[sem], inc)
        for b in reads:
            b.readers.append(me)
        for b in writes:
            b.last_w = me
            b.readers = []
        return me

    def op(self, eng, fn, reads=(), writes=()):
        return self._op(eng, eng, 1, fn, reads, writes)

    def dma(self, fn, reads=(), writes=(), q="sp"):
        k = self.dma_rr % NDMA
        self.dma_rr += 1
        sem = f"d{k}"
        prev = self.cnt[sem]
        extra = [(sem, prev)] if prev > 0 else []
        return self._op(q, sem, 16, fn, reads, writes, extra_waits=extra)

    def finish(self):
        for i in range(NDMA):
            s = f"d{i}"
            if self.cnt[s] > 0:
                self.nc.sync.wait_ge(self.sems[s], self.cnt[s])
        for s in ["pe", "act", "dve", "pool"]:
            if self.cnt[s] > 0:
                self.nc.sync.wait_ge(self.sems[s], self.cnt[s])
        while len(self.scopes) > 1:
            self.scopes.pop().close()
        self.stack.close()


def _rot(lst, i):
    return lst[i % len(lst)]


NT_C = 2048


def build_phase_c(nc, srcs=None):
    def din(name, shape, dt=F32):
        return nc.dram_tensor(name, list(shape), dt, kind="ExternalInput").ap()
    x = din("xc", [NT_C, 1024])
    if srcs is None:
        attn = din("attn", [NT_C, 520])
        yin = din("yin", [NT_C, 512])
    else:
        attn, yin = srcs["attn"], srcs["y"]
    cT2 = din("cT2", [128, 16])
    wada = din("wada_c", [1024, 4096])
    bada = din("bada_c", [1, 4096])
    rows = din("rows_c", [1, 3072])
    bglu = din("bglu", [1, 512])
    wglu = din("wglu", [512, 512])
    wout = din("wout", [1024, 1024])
    wfc1 = din("wfc1", [1024, 4096])
    wfc2 = din("wfc2", [4096, 1024])
    ident = din("ident", [128, 128])
    out = nc.dram_tensor("out", [NT_C, 1024], F32, kind="ExternalOutput").ap()
    x1s = nc.dram_tensor("x1s", [NT_C, 1024], F32).ap()

    P = Prog(nc)
    op, dma = P.op, P.dma

    ident_bf = P.sb("ident_bf", [128, 128], BF16); b_ident = P.buf()
    ones_f = P.sb("ones_f", [1, 128], F32); b_ones_f = P.buf()
    ones_bf = P.sb("ones_bf", [1, 512], BF16); b_ones_bf = P.buf()
    bglu_bf = P.sb("bglu_bf", [1, 512], BF16); b_bglu = P.buf()
    sh2T = P.sb("sh2T", [128, 16], BF16); b_sh2T = P.buf()
    Gcat = P.sb("Gcat", [128, 1024], F32); b_Gcat = P.buf()
    G1 = P.sb("G1", [128, 1024], F32); b_G1 = P.buf()
    S2b = P.sb("S2b", [128, 1024], F32); b_S2b = P.buf()
    G2 = P.sb("G2", [128, 1024], F32); b_G2 = P.buf()
    Gf = P.sb("Gf", [128, 1024], F32); b_Gf = P.buf()

    dma(lambda e: e.dma_start(out=ident_bf[:], in_=ident[:, :]), writes=[b_ident], q="pool")
    dma(lambda e: e.dma_start(out=bglu_bf[:], in_=bglu[:, :]), writes=[b_bglu], q="pool")
    op("dve", lambda e: e.memset(ones_f[:], 1.0), writes=[b_ones_f])
    op("dve", lambda e: e.memset(ones_bf[:], 1.0), writes=[b_ones_bf])

    P.push_scope()
    ct = P.sb("ct", [128, 16], F32); b_ct = P.buf()
    sct = P.sb("sct", [128, 16], F32); b_sct = P.buf()
    sgc = P.sb("sgc", [128, 16], F32)
    modrow = P.sb("modrow", [1, 4096], F32); b_modrow = P.buf()
    badas = P.sb("badas", [1, 4096], F32); b_badas = P.buf()
    rowss = P.sb("rowss", [1, 3072], F32); b_rowss = P.buf()
    s2row = P.sb("s2row", [1, 1024], F32); b_s2row = P.buf()
    pieces = [P.sb(f"wpiece{i}", [128, 8, 512], F32) for i in range(2)]
    b_pieces = [P.buf() for _ in range(2)]
    ps_row = [P.ps(f"ps_row{i}", [128, 512], F32) for i in range(2)]
    b_ps_row = [P.pbuf() for _ in range(2)]

    dma(lambda e: e.dma_start(out=ct[:], in_=cT2[:, :]), writes=[b_ct])
    dma(lambda e: e.dma_start(out=badas[:], in_=bada[:, :]), writes=[b_badas])
    dma(lambda e: e.dma_start(out=rowss[:], in_=rows[:, :]), writes=[b_rowss])
    op("act", lambda e: e.activation(out=sgc[:], in_=ct[:], func=AF.Sigmoid), reads=[b_ct], writes=[b_sct])
    op("dve", lambda e: e.tensor_tensor(out=sct[:], in0=sgc[:], in1=ct[:], op=ALU.mult), reads=[b_ct, b_sct], writes=[b_sct])
    for j in range(8):
        pc, bpc = _rot(pieces, j), _rot(b_pieces, j)
        pr, bpr = _rot(ps_row, j), _rot(b_ps_row, j)
        dma(lambda e: e.dma_start(out=pc[:], in_=wada[:, j * 512:(j + 1) * 512].rearrange("(k p) n -> p k n", p=128)),
            writes=[bpc])
        for k in range(8):
            op("pe", lambda e: e.matmul(pr[0:1, :], lhsT=sct[:, 2 * k:2 * k + 1], rhs=pc[:, k, :],
                                        start=(k == 0), stop=(k == 7)),
               reads=[b_sct, bpc], writes=[bpr])
        op("dve", lambda e: e.tensor_tensor(out=modrow[0:1, j * 512:(j + 1) * 512], in0=pr[0:1, :],
                                            in1=badas[0:1, j * 512:(j + 1) * 512], op=ALU.add),
           reads=[bpr, b_badas], writes=[b_modrow])
    op("dve", lambda e: e.scalar_tensor_tensor(out=s2row[:], in0=modrow[0:1, 2048:3072], scalar=1.0,
                                                in1=rowss[0:1, 1024:2048], op0=ALU.add, op1=ALU.mult),
       reads=[b_modrow, b_rowss], writes=[b_s2row])
    bc_list = [(Gcat, b_Gcat, rowss, b_rowss, 0), (G1, b_G1, modrow, b_modrow, 0), (S2b, b_S2b, s2row, b_s2row, 0),
               (G2, b_G2, modrow, b_modrow, 3072), (Gf, b_Gf, rowss, b_rowss, 2048)]
    i = 0
    for (dst, bdst, src, bsrc, off) in bc_list:
        for hf in range(2):
            pr, bpr = _rot(ps_row, i), _rot(b_ps_row, i)
            i += 1
            op("pe", lambda e: e.matmul(pr[:, :], lhsT=ones_f[0:1, :], rhs=src[0:1, off + hf * 512: off + (hf + 1) * 512],
                                        start=True, stop=True),
               reads=[b_ones_f, bsrc], writes=[bpr])
            op("act", lambda e: e.activation(out=dst[:, hf * 512:(hf + 1) * 512], in_=pr[:, :], func=AF.Copy),
               reads=[bpr], writes=[bdst])
    pr, bpr = ps_row[0], b_ps_row[0]
    for k in range(8):
        op("pe", lambda e: e.matmul(pr[:, 2 * k:2 * k + 2], lhsT=modrow[0:1, 1024 + k * 128:1024 + (k + 1) * 128],
                                    rhs=ones_f[0:1, 0:2], start=True, stop=True),
           reads=[b_ones_f, b_modrow], writes=[bpr])
    op("dve", lambda e: e.tensor_copy(out=sh2T[:], in_=pr[:, 0:16]), reads=[bpr], writes=[b_sh2T])
    P.pop_scope()

    P.push_scope()
    Wout = P.sb("Wout", [128, 8, 1024], BF16); b_Wout = P.buf()
    Wglu = P.sb("Wglu", [128, 4, 512], BF16); b_Wglu = P.buf()
    for k in range(4):
        dma(lambda e: e.dma_start(out=Wglu[:, k, :], in_=wglu[k * 128:(k + 1) * 128, :]), writes=[b_Wglu], q="pool")
    for k in range(8):
        dma(lambda e: e.dma_start(out=Wout[:, k, :], in_=wout[k * 128:(k + 1) * 128, :]), writes=[b_Wout], q="pool")
    NB = 3

    def dbl(name, shape, dt, n=2):
        return [P.sb(f"{name}{i}", shape, dt) for i in range(n)], [P.buf() for _ in range(n)]

    def dblp(name, shape, dt, n=2):
        return [P.ps(f"{name}{i}", shape, dt) for i in range(n)], [P.pbuf() for _ in range(n)]
    at, b_at = dbl("at", [128, 8, 65], F32, NB)
    yt, b_yt = dbl("yt", [128, 512], F32, NB)
    xt, b_xt = dbl("xt", [128, 1024], F32, NB)
    rl, b_rl = dbl("rl", [128, 8], F32)
    A, b_A = dbl("A", [128, 8, 64], F32)
    junk = P.sb("junk", [128, 512], BF16); b_junk = P.buf()
    ss, b_ss = dbl("ss", [128, 2], F32)
    sq, b_sq = dbl("sq", [128, 2], F32)
    rstd, b_rstd = dbl("rstd", [128, 2], F32)
    g1, b_g1 = dbl("g1", [128, 512], F32)
    g2, b_g2 = dbl("g2", [128, 512], F32)
    sg, b_sg = dbl("sg", [128, 512], F32)
    yg, b_yg = dbl("yg", [128, 512], F32)
    ygb, b_ygb = dbl("ygb", [128, 512], BF16)
    ygT, b_ygT = dbl("ygT", [128, 4, 128], BF16)
    sig, b_sig = dbl("sig", [128, 512], F32)
    S, b_S = dbl("S", [128, 512], F32)
    mixh, b_mixh = dbl("mixh", [128, 1024], BF16)
    mixT, b_mixT = dbl("mixT", [128, 8, 128], BF16)
    tt, b_tt = dbl("tt", [128, 1024], F32)
    x1t, b_x1t = dbl("x1t", [128, 1024], F32)
    ptr, b_ptr = dblp("ptr", [128, 8, 128], BF16)
    psg, b_psg = dblp("psg", [128, 512], F32)
    pmx = P.ps("pmx", [128, 8, 128], BF16); b_pmx = P.pbuf()
    pso = [P.ps(f"pso{i}", [128, 512], F32) for i in range(2)]; b_pso = [P.pbuf() for _ in range(2)]
    b_x1s = [P.buf() for _ in range(16)]

    def c1_load(t):
        a, ba = _rot(at, t), _rot(b_at, t)
        y_, by = _rot(yt, t), _rot(b_yt, t)
        x_, bx = _rot(xt, t), _rot(b_xt, t)
        dma(lambda e: e.dma_start(out=a[:].rearrange("p h e -> p (h e)"), in_=attn[t * 128:(t + 1) * 128, :]), writes=[ba])
        dma(lambda e: e.dma_start(out=y_[:], in_=yin[t * 128:(t + 1) * 128, :]), writes=[by])
        dma(lambda e: e.dma_start(out=x_[:], in_=x[t * 128:(t + 1) * 128, :]), writes=[bx])

    def c1_a(t):
        a, ba = _rot(at, t), _rot(b_at, t)
        y_, by = _rot(yt, t), _rot(b_yt, t)
        i = t % 2
        op("dve", lambda e: e.reciprocal(out=rl[i][:], in_=a[:, :, 64]), reads=[ba], writes=[b_rl[i]])
        op("dve", lambda e: e.tensor_tensor(out=A[i][:], in0=a[:, :, 0:64],
                                            in1=rl[i][:, :].unsqueeze(2).to_broadcast([128, 8, 64]), op=ALU.mult),
           reads=[ba, b_rl[i]], writes=[b_A[i]])
        op("act", lambda e: e.activation(out=junk[:], in_=A[i][:].rearrange("p h d -> p (h d)"), func=AF.Square,
                                         accum_out=ss[i][:, 0:1]),
           reads=[b_A[i]], writes=[b_junk, b_ss[i]])
        op("pool", lambda e: e.tensor_tensor(out=g1[i][:], in0=y_[:], in1=y_[:], op=ALU.mult), reads=[by], writes=[b_g1[i]])
        op("pool", lambda e: e.tensor_scalar(out=g1[i][:], in0=g1[i][:], scalar1=0.044715, scalar2=1.0,
                                             op0=ALU.mult, op1=ALU.add), reads=[b_g1[i]], writes=[b_g1[i]])
        op("pool", lambda e: e.tensor_tensor(out=g2[i][:], in0=g1[i][:], in1=y_[:], op=ALU.mult), reads=[b_g1[i], by], writes=[b_g2[i]])
        op("act", lambda e: e.activation(out=sg[i][:], in_=g2[i][:], func=AF.Sigmoid, scale=1.5957691216057308),
           reads=[b_g2[i]], writes=[b_sg[i]])
        op("dve", lambda e: e.tensor_tensor(out=yg[i][:], in0=y_[:], in1=sg[i][:], op=ALU.mult), reads=[by, b_sg[i]], writes=[b_yg[i]])
        op("pool", lambda e: e.tensor_tensor(out=ygb[i][:], in0=y_[:], in1=sg[i][:], op=ALU.mult), reads=[by, b_sg[i]], writes=[b_ygb[i]])
        for k in range(4):
            op("pe", lambda e: e.transpose(ptr[i][:, k, :], ygb[i][:, k * 128:(k + 1) * 128], ident_bf[:]),
               reads=[b_ygb[i], b_ident], writes=[b_ptr[i]])
        op("act", lambda e: e.activation(out=ygT[i][:], in_=ptr[i][:, 0:4, :], func=AF.Copy), reads=[b_ptr[i]], writes=[b_ygT[i]])
        op("pe", lambda e: e.matmul(psg[i][:], lhsT=ones_bf[0:1, 0:128], rhs=bglu_bf[0:1, :], start=True, stop=False),
           reads=[b_ones_bf, b_bglu], writes=[b_psg[i]])
        for k in range(4):
            op("pe", lambda e: e.matmul(psg[i][:], lhsT=ygT[i][:, k, :], rhs=Wglu[:, k, :], start=False, stop=(k == 3)),
               reads=[b_ygT[i], b_Wglu], writes=[b_psg[i]])

    def c1_b(t):
        x_, bx = _rot(xt, t), _rot(b_xt, t)
        i = t % 2
        op("act", lambda e: e.activation(out=sig[i][:], in_=psg[i][:], func=AF.Sigmoid), reads=[b_psg[i]], writes=[b_sig[i]])
        op("dve", lambda e: e.tensor_tensor(out=S[i][:], in0=yg[i][:], in1=sig[i][:], op=ALU.mult), reads=[b_yg[i], b_sig[i]], writes=[b_S[i]])
        op("act", lambda e: e.activation(out=junk[:], in_=S[i][:], func=AF.Square, accum_out=ss[i][:, 1:2]),
           reads=[b_S[i]], writes=[b_junk, b_ss[i]])
        op("act", lambda e: e.activation(out=sq[i][:], in_=ss[i][:], func=AF.Sqrt, scale=1.0 / 512.0, bias=EPS),
           reads=[b_ss[i]], writes=[b_sq[i]])
        op("dve", lambda e: e.reciprocal(out=rstd[i][:], in_=sq[i][:]), reads=[b_sq[i]], writes=[b_rstd[i]])
        op("dve", lambda e: e.scalar_tensor_tensor(out=mixh[i][:, 0:512], in0=A[i][:].rearrange("p h d -> p (h d)"),
                                                    scalar=rstd[i][:, 0:1], in1=Gcat[:, 0:512], op0=ALU.mult, op1=ALU.mult),
           reads=[b_A[i], b_rstd[i], b_Gcat], writes=[b_mixh[i]])
        op("dve", lambda e: e.scalar_tensor_tensor(out=mixh[i][:, 512:1024], in0=S[i][:], scalar=rstd[i][:, 1:2],
                                                    in1=Gcat[:, 512:1024], op0=ALU.mult, op1=ALU.mult),
           reads=[b_S[i], b_rstd[i], b_Gcat], writes=[b_mixh[i]])
        for k in range(8):
            op("pe", lambda e: e.transpose(pmx[:, k, :], mixh[i][:, k * 128:(k + 1) * 128], ident_bf[:]),
               reads=[b_mixh[i], b_ident], writes=[b_pmx])
        op("act", lambda e: e.activation(out=mixT[i][:], in_=pmx[:], func=AF.Copy), reads=[b_pmx], writes=[b_mixT[i]])
        for hf in range(2):
            for k in range(8):
                op("pe", lambda e: e.matmul(pso[hf][:], lhsT=mixT[i][:, k, :], rhs=Wout[:, k, hf * 512:(hf + 1) * 512],
                                            start=(k == 0), stop=(k == 7)),
                   reads=[b_mixT[i], b_Wout], writes=[b_pso[hf]])
            op("dve", lambda e: e.tensor_tensor(out=tt[i][:, hf * 512:(hf + 1) * 512], in0=pso[hf][:],
                                                in1=G1[:, hf * 512:(hf + 1) * 512], op=ALU.mult),
               reads=[b_pso[hf], b_G1], writes=[b_tt[i]])
        op("pool", lambda e: e.tensor_tensor(out=x1t[i][:], in0=tt[i][:], in1=x_[:], op=ALU.add), reads=[b_tt[i], bx], writes=[b_x1t[i]])
        dma(lambda e: e.dma_start(out=x1s[t * 128:(t + 1) * 128, :], in_=x1t[i][:]), reads=[b_x1t[i]], writes=[b_x1s[t]])

    c1_load(0)
    c1_load(1)
    c1_a(0)
    for t in range(16):
        if t + 2 < 16:
            c1_load(t + 2)
        if t + 1 < 16:
            c1_a(t + 1)
        c1_b(t)
    P.pop_scope()

    P.push_scope()
    W2 = P.sb("W2", [128, 32, 1024], BF16); b_W2 = [P.buf() for _ in range(8)]
    for i in range(8):
        dma(lambda e: e.dma_start(out=W2[:, 4 * i:4 * i + 4, :],
                                  in_=wfc2[512 * i:512 * (i + 1), :].rearrange("(j p) n -> p j n", p=128)),
            writes=[b_W2[i]], q="pool")
    W1p = [P.sb(f"W1p{i}", [128, 8, 256], BF16) for i in range(2)]; b_W1p = [P.buf() for _ in range(2)]
    hT = P.sb("hT", [128, 32, 512], BF16); b_hT = [P.buf() for _ in range(32)]
    h2T = P.sb("h2T", [128, 8, 512], BF16); b_h2T = P.buf()
    x1g = P.sb("x1g", [128, 4, 1024], F32); b_x1g = [P.buf() for _ in range(4)]
    h2 = P.sb("h2", [128, 1024], BF16); b_h2 = P.buf()
    junk2 = P.sb("junk2", [128, 1024], BF16); b_junk2 = P.buf()
    ssc = P.sb("ssc", [128, 2], F32); b_ssc = P.buf()
    sqc = P.sb("sqc", [128, 2], F32); b_sqc = P.buf()
    rc = P.sb("rc", [128, 2], F32); b_rc = P.buf()
    b1row = P.sb("b1row", [1, 256], BF16); b_b1row = P.buf()
    rl_t = [P.sb(f"rl_t{i}", [128, 512], BF16) for i in range(2)]; b_rl_t = [P.buf() for _ in range(2)]
    t2 = P.sb("t2", [128, 1024], F32); b_t2 = P.buf()
    x2t = t2; b_x2t = b_t2
    ot = [P.sb(f"ot{i}", [128, 1024], F32) for i in range(2)]; b_ot = [P.buf() for _ in range(2)]
    pht = P.ps("pht", [128, 8, 128], BF16); b_pht = P.pbuf()
    psb = P.ps("psb", [128, 512], F32); b_psb = P.pbuf()
    psf = [P.ps(f"psf{i}", [128, 512], F32) for i in range(2)]; b_psf = [P.pbuf() for _ in range(2)]
    pso2 = [P.ps(f"pso2{i}", [128, 512], F32) for i in range(2)]; b_pso2 = [P.pbuf() for _ in range(2)]
    pcount = 0
    ocount = 0
    for g in range(4):
        for i in range(4):
            t = g * 4 + i
            dma(lambda e: e.dma_start(out=x1g[:, i, :], in_=x1s[t * 128:(t + 1) * 128, :]), reads=[b_x1s[t]], writes=[b_x1g[i]])
            op("act", lambda e: e.activation(out=junk2[:], in_=x1g[:, i, :], func=AF.Square, accum_out=ssc[:, 0:1]),
               reads=[b_x1g[i]], writes=[b_junk2, b_ssc])
            op("act", lambda e: e.activation(out=sqc[:, 0:1], in_=ssc[:, 0:1], func=AF.Sqrt, scale=1.0 / 1024.0, bias=EPS),
               reads=[b_ssc], writes=[b_sqc])
            op("dve", lambda e: e.reciprocal(out=rc[:, 0:1], in_=sqc[:, 0:1]), reads=[b_sqc], writes=[b_rc])
            op("dve", lambda e: e.scalar_tensor_tensor(out=h2[:], in0=x1g[:, i, :], scalar=rc[:, 0:1], in1=S2b[:],
                                                        op0=ALU.mult, op1=ALU.mult),
               reads=[b_x1g[i], b_rc, b_S2b], writes=[b_h2])
            for k in range(8):
                op("pe", lambda e: e.transpose(pht[:, k, :], h2[:, k * 128:(k + 1) * 128], ident_bf[:]),
                   reads=[b_h2, b_ident], writes=[b_pht])
            op("act", lambda e: e.activation(out=h2T[:, :, i * 128:(i + 1) * 128], in_=pht[:], func=AF.Copy),
               reads=[b_pht], writes=[b_h2T])
        for pc in range(16):
            w1, bw1 = _rot(W1p, pcount), _rot(b_W1p, pcount)
            pcount += 1
            dma(lambda e: e.dma_start(out=w1[:], in_=wfc1[:, pc * 256:(pc + 1) * 256].rearrange("(k p) n -> p k n", p=128)),
                writes=[bw1], q="pool")
            for k in range(8):
                op("pe", lambda e: e.matmul(psb[0:1, 0:256], lhsT=sh2T[:, 2 * k:2 * k + 1], rhs=w1[:, k, :],
                                            start=(k == 0), stop=(k == 7)),
                   reads=[b_sh2T, bw1], writes=[b_psb])
            op("act", lambda e: e.activation(out=b1row[:], in_=psb[0:1, 0:256], func=AF.Copy), reads=[b_psb], writes=[b_b1row])
            for fc in range(2):
                j = pc * 2 + fc
                pf, bpf = _rot(psf, j), _rot(b_psf, j)
                op("pe", lambda e: e.matmul(pf[:], lhsT=b1row[0:1, fc * 128:(fc + 1) * 128], rhs=ones_bf[0:1, :],
                                            start=True, stop=False),
                   reads=[b_b1row, b_ones_bf], writes=[bpf])
                for k in range(8):
                    op("pe", lambda e: e.matmul(pf[:], lhsT=w1[:, k, fc * 128:(fc + 1) * 128], rhs=h2T[:, k, :],
                                                start=False, stop=(k == 7)),
                       reads=[bw1, b_h2T], writes=[bpf])
                r_, br_ = _rot(rl_t, j), _rot(b_rl_t, j)
                op("act", lambda e: e.activation(out=r_[:], in_=pf[:], func=AF.Relu), reads=[bpf], writes=[br_])
                op("pool", lambda e: e.tensor_tensor(out=hT[:, j, :], in0=r_[:], in1=r_[:], op=ALU.mult),
                   reads=[br_], writes=[b_hT[j]])
        for i in range(4):
            t = g * 4 + i
            for hf in range(2):
                po, bpo = _rot(pso2, ocount), _rot(b_pso2, ocount)
                ocount += 1
                for j in range(32):
                    op("pe", lambda e: e.matmul(po[:], lhsT=hT[:, j, i * 128:(i + 1) * 128],
                                                rhs=W2[:, j, hf * 512:(hf + 1) * 512], start=(j == 0), stop=(j == 31)),
                       reads=[b_hT[j], b_W2[j // 4]], writes=[bpo])
                op("dve", lambda e: e.tensor_tensor(out=t2[:, hf * 512:(hf + 1) * 512], in0=po[:],
                                                    in1=G2[:, hf * 512:(hf + 1) * 512], op=ALU.mult),
                   reads=[bpo, b_G2], writes=[b_t2])
            op("pool", lambda e: e.tensor_tensor(out=x2t[:], in0=t2[:], in1=x1g[:, i, :], op=ALU.add),
               reads=[b_t2, b_x1g[i]], writes=[b_x2t])
            op("act", lambda e: e.activation(out=junk2[:], in_=x2t[:], func=AF.Square, accum_out=ssc[:, 1:2]),
               reads=[b_x2t], writes=[b_junk2, b_ssc])
            op("act", lambda e: e.activation(out=sqc[:, 1:2], in_=ssc[:, 1:2], func=AF.Sqrt, scale=1.0 / 1024.0, bias=EPS),
               reads=[b_ssc], writes=[b_sqc])
            op("dve", lambda e: e.reciprocal(out=rc[:, 1:2], in_=sqc[:, 1:2]), reads=[b_sqc], writes=[b_rc])
            o_, bo_ = _rot(ot, t), _rot(b_ot, t)
            op("dve", lambda e: e.scalar_tensor_tensor(out=o_[:], in0=x2t[:], scalar=rc[:, 1:2], in1=Gf[:],
                                                        op0=ALU.mult, op1=ALU.mult),
               reads=[b_x2t, b_rc, b_Gf], writes=[bo_])
            dma(lambda e: e.dma_start(out=out[t * 128:(t + 1) * 128, :], in_=o_[:]), reads=[bo_])
    P.pop_scope()
    P.finish()
    return nc


def _ident():
    return np.eye(128, dtype=np.float32)


def phase_c_inputs(inp, b, qi, attn_full, y_full):
    T0 = qi * NT_C
    c = inp["c"][b]
    cT = np.ascontiguousarray(c.reshape(8, 128).T)
    cT2 = np.repeat(cT[:, :, None], 2, axis=2).reshape(128, 16)
    rows = np.concatenate([inp["g_attn_out"][0], inp["g_ssm_out"][0], inp["g_mlp"][0], inp["g_final"]])[None, :]
    return {
        "xc": np.ascontiguousarray(inp["x"][b, T0:T0 + NT_C]),
        "attn": np.ascontiguousarray(attn_full[b, T0:T0 + NT_C].reshape(NT_C, 520)),
        "yin": np.ascontiguousarray(y_full[b, T0:T0 + NT_C]),
        "cT2": np.ascontiguousarray(cT2),
        "wada_c": np.ascontiguousarray(inp["w_ada"][0][:, 2048:6144]),
        "bada_c": np.ascontiguousarray(inp["b_ada"][0][None, 2048:6144]),
        "rows_c": np.ascontiguousarray(rows.astype(np.float32)),
        "bglu": np.ascontiguousarray(inp["b_glu"][0][None, :]),
        "wglu": np.ascontiguousarray(inp["w_glu"][0]),
        "wout": np.ascontiguousarray(inp["w_out"][0]),
        "wfc1": np.ascontiguousarray(inp["w_fc1"][0]),
        "wfc2": np.ascontiguousarray(inp["w_fc2"][0]),
        "ident": _ident(),
    }


L_SEQ = 8192
NTILE = 64


def build_phase_ab(nc, with_ssm=True, dsts=None, dbg_tiles=NTILE, dbg_attn=True, dbg_stage=9):
    def din(name, shape, dt=F32):
        return nc.dram_tensor(name, list(shape), dt, kind="ExternalInput").ap()
    xb = din("xb", [L_SEQ, 1024])
    cT2 = din("cT2", [128, 16])
    wada = din("wada_a", [1024, 2048])
    bada = din("bada_a", [1, 2048])
    gmix = din("gmix", [1, 1024])
    win = din("win", [1024, 512])
    cosl = din("cosl", [128, 2048])
    sinl = din("sinl", [128, 2048])
    ind = din("ind", [32, L_SEQ])
    cmask = din("cmask", [128, 2048])
    ident = din("ident", [128, 128])
    if dsts is None:
        attn_o = nc.dram_tensor("attn_o", [130, L_SEQ], F32, kind="ExternalOutput").ap()
        y_o = nc.dram_tensor("y_o", [L_SEQ, 128], F32, kind="ExternalOutput").ap()
    else:
        attn_o, y_o = dsts["attn"], dsts["y"]

    P = Prog(nc)
    op, dma = P.op, P.dma

    ident_bf = P.sb("ident_bf", [128, 128], BF16); b_ident = P.buf()
    ident_f = P.sb("ident_f", [128, 128], F32); b_identf = P.buf()
    ones_bf = P.sb("ones_bf", [1, 512], BF16); b_ones_bf = P.buf()
    zeros_bf = P.sb("zeros_bf", [1, 512], BF16); b_zeros_bf = P.buf()
    ones_f = P.sb("ones_f", [1, 128], F32); b_ones_f = P.buf()
    U_tok = P.sb("U_tok", [128, 8, 8, 8, 16], BF16); b_U = [P.buf() for _ in range(8)]
    dma(lambda e: e.dma_start(out=ident_bf[:], in_=ident[:, :]), writes=[b_ident], q="pool")
    dma(lambda e: e.dma_start(out=ident_f[:], in_=ident[:, :]), writes=[b_identf])
    op("dve", lambda e: e.memset(ones_f[:], 1.0), writes=[b_ones_f])
    op("dve", lambda e: e.memset(ones_bf[:], 1.0), writes=[b_ones_bf])
    op("dve", lambda e: e.memset(zeros_bf[:], 0.0), writes=[b_zeros_bf])

    P.push_scope()
    QaT = P.sb("QaT", [128, 2, L_SEQ], BF16); b_QaT = [P.buf() for _ in range(NTILE)]
    KaT = P.sb("KaT", [128, 2, L_SEQ], BF16); b_KaT = [P.buf() for _ in range(NTILE)]; b_Kind = P.buf()
    Vaug = P.sb("Vaug", [128, NTILE, 2, 65], BF16); b_V = [P.buf() for _ in range(NTILE)]; b_Vones = P.buf()
    for h in range(2):
        for cch in range(4):
            dma(lambda e: e.dma_start(out=KaT[64:96, h, cch * 2048:(cch + 1) * 2048], in_=ind[:, cch * 2048:(cch + 1) * 2048]),
                writes=[b_Kind], q="pool")
    op("pool", lambda e: e.memset(Vaug[:, :, :, 64:65], 1.0), writes=[b_Vones])

    P.push_scope()
    S1b = P.sb("S1b", [128, 1024], F32); b_S1b = P.buf()
    sh1T = P.sb("sh1T", [128, 16], BF16); b_sh1T = P.buf()
    Win = P.sb("Win", [128, 8, 512], BF16); b_Win = P.buf()
    bin_bf = P.sb("bin_bf", [1, 512], BF16); b_bin = P.buf()
    for k in range(8):
        dma(lambda e: e.dma_start(out=Win[:, k, :], in_=win[k * 128:(k + 1) * 128, :]), writes=[b_Win], q="pool")

    P.push_scope()
    ct = P.sb("ct", [128, 16], F32); b_ct = P.buf()
    sct = P.sb("sct", [128, 16], F32); b_sct = P.buf()
    sgc = P.sb("sgc", [128, 16], F32)
    modrow = P.sb("modrow", [1, 2048], F32); b_modrow = P.buf()
    badas = P.sb("badas", [1, 2048], F32); b_badas = P.buf()
    gmixs = P.sb("gmixs", [1, 1024], F32); b_gmixs = P.buf()
    s1row = P.sb("s1row", [1, 1024], F32); b_s1row = P.buf()
    pieces = [P.sb(f"wpiece{i}", [128, 8, 512], F32) for i in range(2)]
    b_pieces = [P.buf() for _ in range(2)]
    ps_row = [P.ps(f"ps_row{i}", [128, 512], F32) for i in range(2)]
    b_ps_row = [P.pbuf() for _ in range(2)]
    dma(lambda e: e.dma_start(out=ct[:], in_=cT2[:, :]), writes=[b_ct])
    dma(lambda e: e.dma_start(out=badas[:], in_=bada[:, :]), writes=[b_badas])
    dma(lambda e: e.dma_start(out=gmixs[:], in_=gmix[:, :]), writes=[b_gmixs])
    op("act", lambda e: e.activation(out=sgc[:], in_=ct[:], func=AF.Sigmoid), reads=[b_ct], writes=[b_sct])
    op("dve", lambda e: e.tensor_tensor(out=sct[:], in0=sgc[:], in1=ct[:], op=ALU.mult), reads=[b_ct, b_sct], writes=[b_sct])
    for j in range(4):
        pc, bpc = _rot(pieces, j), _rot(b_pieces, j)
        pr, bpr = _rot(ps_row, j), _rot(b_ps_row, j)
        dma(lambda e: e.dma_start(out=pc[:], in_=wada[:, j * 512:(j + 1) * 512].rearrange("(k p) n -> p k n", p=128)),
            writes=[bpc])
        for k in range(8):
            op("pe", lambda e: e.matmul(pr[0:1, :], lhsT=sct[:, 2 * k:2 * k + 1], rhs=pc[:, k, :],
                                        start=(k == 0), stop=(k == 7)),
               reads=[b_sct, bpc], writes=[bpr])
        op("dve", lambda e: e.tensor_tensor(out=modrow[0:1, j * 512:(j + 1) * 512], in0=pr[0:1, :],
                                            in1=badas[0:1, j * 512:(j + 1) * 512], op=ALU.add),
           reads=[bpr, b_badas], writes=[b_modrow])
    op("dve", lambda e: e.scalar_tensor_tensor(out=s1row[:], in0=modrow[0:1, 1024:2048], scalar=1.0,
                                                in1=gmixs[0:1, :], op0=ALU.add, op1=ALU.mult),
       reads=[b_modrow, b_gmixs], writes=[b_s1row])
    for hf in range(2):
        pr, bpr = _rot(ps_row, hf), _rot(b_ps_row, hf)
        op("pe", lambda e: e.matmul(pr[:, :], lhsT=ones_f[0:1, :], rhs=s1row[0:1, hf * 512:(hf + 1) * 512],
                                    start=True, stop=True), reads=[b_ones_f, b_s1row], writes=[bpr])
        op("act", lambda e: e.activation(out=S1b[:, hf * 512:(hf + 1) * 512], in_=pr[:, :], func=AF.Copy),
           reads=[bpr], writes=[b_S1b])
    pr, bpr = ps_row[0], b_ps_row[0]
    for k in range(8):
        op("pe", lambda e: e.matmul(pr[:, 2 * k:2 * k + 2], lhsT=modrow[0:1, k * 128:(k + 1) * 128],
                                    rhs=ones_f[0:1, 0:2], start=True, stop=True),
           reads=[b_ones_f, b_modrow], writes=[bpr])
    op("dve", lambda e: e.tensor_copy(out=sh1T[:], in_=pr[:, 0:16]), reads=[bpr], writes=[b_sh1T])
    pr, bpr = ps_row[1], b_ps_row[1]
    for k in range(8):
        op("pe", lambda e: e.matmul(pr[0:1, :], lhsT=sh1T[:, 2 * k:2 * k + 1], rhs=Win[:, k, :],
                                    start=(k == 0), stop=(k == 7)), reads=[b_sh1T, b_Win], writes=[bpr])
    op("act", lambda e: e.activation(out=bin_bf[:], in_=pr[0:1, :], func=AF.Copy), reads=[bpr], writes=[b_bin])
    P.pop_scope()

    xt = [P.sb(f"xt{i}", [128, 1024], F32) for i in range(3)]; b_xt = [P.buf() for _ in range(3)]
    hb = [P.sb(f"hb{i}", [128, 1024], BF16) for i in range(2)]; b_hb = [P.buf() for _ in range(2)]
    junk = P.sb("junk", [128, 1024], BF16); b_junk = P.buf()
    ssx = [P.sb(f"ssx{i}", [128, 1], F32) for i in range(2)]; b_ssx = [P.buf() for _ in range(2)]
    sqx = [P.sb(f"sqx{i}", [128, 1], F32) for i in range(2)]; b_sqx = [P.buf() for _ in range(2)]
    rx = [P.sb(f"rx{i}", [128, 1], F32) for i in range(2)]; b_rx = [P.buf() for _ in range(2)]
    hTs = P.sb("hTs", [128, 8, 1024], BF16); b_hTs = [P.buf() for _ in range(8)]
    cs = [P.sb(f"cs{i}", [128, 8, 32], F32) for i in range(2)]; b_cs = [P.buf() for _ in range(2)]
    sn = [P.sb(f"sn{i}", [128, 8, 32], F32) for i in range(2)]; b_sn = [P.buf() for _ in range(2)]
    qkr = P.sb("qkr", [128, 4, 2, 32], F32); b_qkr = P.buf()
    r1 = P.sb("r1", [128, 4, 32], F32); b_r1 = P.buf()
    r2 = P.sb("r2", [128, 4, 32], F32); b_r2 = P.buf()
    r3 = P.sb("r3", [128, 4, 32], F32); b_r3 = P.buf()
    r4 = P.sb("r4", [128, 4, 32], F32); b_r4 = P.buf()
    ksum = P.sb("ksum", [64, 2, 2], F32); b_ksum = [P.buf(), P.buf()]
    kmT = P.sb("kmT", [64, 2, 32], F32); b_kmT = P.buf()
    qTt = P.sb("qTt", [64, 2, 128], F32); b_qTt = P.buf()
    Gt = P.sb("Gt", [128, 2, 32], F32); b_Gt = P.buf()
    mx8 = P.sb("mx8", [128, 2, 8], F32); b_mx8 = P.buf()
    nm = P.sb("nm", [128, 2, 32], F32); b_nm = P.buf()
    Qtok = P.sb("Qtok", [128, 2, 96], BF16); b_Qtok = P.buf()
    pxt = [P.ps(f"pxt{i}", [128, 8, 128], BF16) for i in range(2)]; b_pxt = [P.pbuf() for _ in range(2)]
    pqkv = [P.ps(f"pqkv{i}", [128, 512], F32) for i in range(2)]; b_pqkv = [P.pbuf() for _ in range(2)]
    pT = P.ps("pT", [128, 4, 128], F32); b_pT = P.pbuf()
    pgq = P.ps("pgq", [128, 512], F32); b_pgq = P.pbuf()
    pu = P.ps("pu", [128, 8, 128], F32); b_pu = P.pbuf()
    pg = pgq
    pqa = pgq[:, 256:512].bitcast(BF16).rearrange("p (h n) -> p h n", h=4)
    b_pg = b_pgq
    b_pqa = b_pgq
    op("dve", lambda e: e.memset(Gt[:], -1.0e30), writes=[b_Gt])
    op("dve", lambda e: e.memset(kmT[:], 0.0), writes=[b_kmT])

    def load_x(t):
        x_, bx = _rot(xt, t), _rot(b_xt, t)
        dma(lambda e: e.dma_start(out=x_[:], in_=xb[t * 128:(t + 1) * 128, :]), writes=[bx])

    def stage_a(t):
        S_, ti = t // 8, t % 8
        x_, bx = _rot(xt, t), _rot(b_xt, t)
        hb_, bhb = _rot(hb, t), _rot(b_hb, t)
        ss_, bss = _rot(ssx, t), _rot(b_ssx, t)
        sq_, bsq = _rot(sqx, t), _rot(b_sqx, t)
        rx_, brx = _rot(rx, t), _rot(b_rx, t)
        px_, bpx = _rot(pxt, t), _rot(b_pxt, t)
        pq_, bpq = _rot(pqkv, t), _rot(b_pqkv, t)
        if ti == 0:
            c_, bc_ = _rot(cs, S_), _rot(b_cs, S_)
            s_, bs_ = _rot(sn, S_), _rot(b_sn, S_)
            dma(lambda e: e.dma_start(out=c_[:].rearrange("p i d -> p (i d)"), in_=cosl[:, S_ * 256:(S_ + 1) * 256]), writes=[bc_])
            dma(lambda e: e.dma_start(out=s_[:].rearrange("p i d -> p (i d)"), in_=sinl[:, S_ * 256:(S_ + 1) * 256]), writes=[bs_])
        op("act", lambda e: e.activation(out=junk[:], in_=x_[:], func=AF.Square, accum_out=ss_[:, 0:1]),
           reads=[bx], writes=[b_junk, bss])
        op("act", lambda e: e.activation(out=sq_[:], in_=ss_[:], func=AF.Sqrt, scale=1.0 / 1024.0, bias=EPS),
           reads=[bss], writes=[bsq])
        op("dve", lambda e: e.reciprocal(out=rx_[:], in_=sq_[:]), reads=[bsq], writes=[brx])
        op("dve", lambda e: e.scalar_tensor_tensor(out=hb_[:], in0=x_[:], scalar=rx_[:, 0:1], in1=S1b[:],
                                                    op0=ALU.mult, op1=ALU.mult),
           reads=[bx, brx, b_S1b], writes=[bhb])
        for k in range(8):
            op("pe", lambda e: e.transpose(px_[:, k, :], hb_[:, k * 128:(k + 1) * 128], ident_bf[:]),
               reads=[bhb, b_ident], writes=[bpx])
        op("act", lambda e: e.activation(out=hTs[:, :, ti * 128:(ti + 1) * 128], in_=px_[:], func=AF.Copy),
           reads=[bpx], writes=[b_hTs[ti]])
        op("pe", lambda e: e.matmul(pq_[:, 0:384], lhsT=ones_bf[0:1, 0:128], rhs=bin_bf[0:1, 0:384], start=True, stop=False),
           reads=[b_ones_bf, b_bin], writes=[bpq])
        for k in range(8):
            op("pe", lambda e: e.matmul(pq_[:, 0:384], lhsT=hTs[:, k, ti * 128:(ti + 1) * 128], rhs=Win[:, k, 0:384],
                                        start=False, stop=(k == 7)),
               reads=[b_hTs[ti], b_Win], writes=[bpq])

    def u_proj(S_):
        for s in range(8):
            op("pe", lambda e: e.matmul(pu[:, s, :], lhsT=ones_bf[0:1, 0:128], rhs=bin_bf[0:1, 384:512], start=True, stop=False),
               reads=[b_ones_bf, b_bin], writes=[b_pu])
            for k in range(8):
                lhs = hTs[:, k, :].rearrange("p (c s) -> p s c", s=8)[:, s, :]
                op("pe", lambda e: e.matmul(pu[:, s, :], lhsT=lhs, rhs=Win[:, k, 384:512], start=False, stop=(k == 7)),
                   reads=b_hTs + [b_Win], writes=[b_pu])
        op("act", lambda e: e.activation(out=U_tok[:, S_].rearrange("p g s m -> p s g m"),
                                         in_=pu[:].rearrange("p s (g m) -> p s g m", g=8), func=AF.Copy),
           reads=[b_pu], writes=[b_U[S_]])

    def stage_b(t):
        S_, ti = t // 8, t % 8
        j = t // 2
        par = t % 2
        tok = slice(t * 128, (t + 1) * 128)
        c_, bc_ = _rot(cs, S_), _rot(b_cs, S_)
        s_, bs_ = _rot(sn, S_), _rot(b_sn, S_)
        pq_, bpq = _rot(pqkv, t), _rot(b_pqkv, t)
        op("act", lambda e: e.activation(out=Vaug[:, t, :, 0:64], in_=pq_[:, 256:384].rearrange("p (h d) -> p h d", h=2),
                                         func=AF.Copy), reads=[bpq], writes=[b_V[t]])
        pq = pq_[:, 0:256].rearrange("p (f two d) -> p f two d", f=4, two=2)
        cb = c_[:, ti, :].unsqueeze(1).to_broadcast([128, 4, 32])
        sb_ = s_[:, ti, :].unsqueeze(1).to_broadcast([128, 4, 32])
        op("dve", lambda e: e.tensor_tensor(out=r1[:], in0=pq[:, :, 0, :], in1=cb, op=ALU.mult), reads=[bpq, bc_], writes=[b_r1])
        op("dve", lambda e: e.tensor_tensor(out=r2[:], in0=pq[:, :, 1, :], in1=sb_, op=ALU.mult), reads=[bpq, bs_], writes=[b_r2])
        op("dve", lambda e: e.tensor_tensor(out=r3[:], in0=pq[:, :, 0, :], in1=sb_, op=ALU.mult), reads=[bpq, bs_], writes=[b_r3])
        op("dve", lambda e: e.tensor_tensor(out=r4[:], in0=pq[:, :, 1, :], in1=cb, op=ALU.mult), reads=[bpq, bc_], writes=[b_r4])
        op("pool", lambda e: e.tensor_tensor(out=qkr[:, :, 0, :], in0=r1[:], in1=r2[:], op=ALU.subtract),
           reads=[b_r1, b_r2], writes=[b_qkr])
        op("pool", lambda e: e.tensor_tensor(out=qkr[:, :, 1, :], in0=r3[:], in1=r4[:], op=ALU.add),
           reads=[b_r3, b_r4], writes=[b_qkr])
        qkf = qkr[:].rearrange("p f two d -> p (f two d)")
        for h in range(2):
            op("pe", lambda e: e.transpose(pT[0:64, h, :], qkf[:, 128 + h * 64:128 + (h + 1) * 64], ident_f[:]),
               reads=[b_qkr, b_identf], writes=[b_pT])
        if j >= 4:
            for h in range(2):
                op("pe", lambda e: e.transpose(pT[0:64, 2 + h, :], qkf[:, h * 64:(h + 1) * 64], ident_f[:]),
                   reads=[b_qkr, b_identf], writes=[b_pT])
        op("act", lambda e: e.activation(out=KaT[0:64, :, tok], in_=pT[0:64, 0:2, :], func=AF.Copy),
           reads=[b_pT], writes=[b_KaT[t]])
        op("dve", lambda e: e.tensor_reduce(out=ksum[:, par, :], in_=pT[0:64, 0:2, :], axis=mybir.AxisListType.X, op=ALU.add),
           reads=[b_pT], writes=[b_ksum[par]])
        if j >= 4:
            op("act", lambda e: e.activation(out=qTt[:], in_=pT[0:64, 2:4, :], func=AF.Copy), reads=[b_pT], writes=[b_qTt])
            for h in range(2):
                op("pe", lambda e: e.matmul(pg[:, h * 32:(h + 1) * 32], lhsT=qTt[:, h, :], rhs=kmT[:, h, :], start=True, stop=True),
                   reads=[b_qTt, b_kmT], writes=[b_pg])
            op("dve", lambda e: e.tensor_copy(out=Gt[:, :, 0:j], in_=pg[:, 0:64].rearrange("p (h n) -> p h n", h=2)[:, :, 0:j]),
               reads=[b_pg], writes=[b_Gt])
            for h in range(2):
                op("dve", lambda e: e.max(out=mx8[:, h, :], in_=Gt[:, h, :]), reads=[b_Gt], writes=[b_mx8])
            for h in range(2):
                op("dve", lambda e: e.tensor_scalar(out=nm[:, h, :], in0=Gt[:, h, :], scalar1=mx8[:, h, 2:3], scalar2=1.0,
                                                    op0=ALU.is_ge, op1=ALU.subtract), reads=[b_Gt, b_mx8], writes=[b_nm])
            op("dve", lambda e: e.memset(nm[:, :, j:j + 1], 0.0), writes=[b_nm])
            op("dve", lambda e: e.tensor_scalar(out=Qtok[:, :, 64:96], in0=nm[:], scalar1=-NEGBIG, scalar2=None, op0=ALU.mult),
               reads=[b_nm], writes=[b_Qtok])
        else:
            op("dve", lambda e: e.memset(Qtok[:, :, 64:96], NEGBIG), writes=[b_Qtok])
            op("dve", lambda e: e.memset(Qtok[:, :, 64:64 + j + 1], 0.0), writes=[b_Qtok])
        op("pool", lambda e: e.tensor_scalar(out=Qtok[:, :, 0:64], in0=qkf[:, 0:128].rearrange("p (h d) -> p h d", h=2),
                                             scalar1=0.125, scalar2=None, op0=ALU.mult),
           reads=[b_qkr], writes=[b_Qtok])
        for h in range(2):
            op("pe", lambda e: e.transpose(pqa[0:96, h, :], Qtok[:, h, :], ident_bf[:]), reads=[b_Qtok, b_ident], writes=[b_pqa])
        op("act", lambda e: e.activation(out=QaT[0:96, :, tok], in_=pqa[0:96, 0:2, :], func=AF.Copy),
           reads=[b_pqa], writes=[b_QaT[t]])
        if par == 1:
            op("dve", lambda e: e.tensor_tensor(out=kmT[:, :, j], in0=ksum[:, 0, :], in1=ksum[:, 1, :], op=ALU.add),
               reads=[b_ksum[0], b_ksum[1]], writes=[b_kmT])

    if dbg_tiles > 0:
        load_x(0)
        load_x(1)
        stage_a(0)
    for t in range(dbg_tiles):
        if t + 2 < NTILE:
            load_x(t + 2)
        if t % 8 == 7 and with_ssm:
            u_proj(t // 8)
        if t + 1 < dbg_tiles:
            stage_a(t + 1)
        stage_b(t)
    P.pop_scope()

    P.push_scope()
    cm = P.sb("cm", [128, 4, 512], BF16); b_cm = P.buf()
    dma(lambda e: e.dma_start(out=cm[:].rearrange("p a c -> p (a c)"), in_=cmask[:, :]), writes=[b_cm], q="pool")
    Pt = [P.sb(f"Pt{i}", [128, 512], BF16) for i in range(3)]; b_Pt = [P.buf() for _ in range(3)]
    Osb = [P.sb(f"Osb{i}", [65, 512], F32) for i in range(2)]; b_Osb = [P.buf() for _ in range(2)]
    pS = [P.ps(f"pS{i}", [128, 512], F32) for i in range(3)]; b_pS = [P.pbuf() for _ in range(3)]
    pO = [P.ps(f"pO{i}", [128, 512], F32) for i in range(2)]; b_pO = [P.pbuf() for _ in range(2)]
    cnt = 0
    gi = 0
    for h in range(2 if dbg_attn else 0):
        for G in range(16 if dbg_attn is True else dbg_attn):
            po, bpo = _rot(pO, gi), _rot(b_pO, gi)
            ob, bob = _rot(Osb, gi), _rot(b_Osb, gi)
            gi += 1
            nkt = 4 * G + 4
            qs = slice(G * 512, (G + 1) * 512)
            qbufs = b_QaT[4 * G:4 * G + 4]

            def s_mm(kt, c):
                ps_, bps = _rot(pS, c), _rot(b_pS, c)
                op("pe", lambda e: e.matmul(ps_[:], lhsT=KaT[0:96, h, kt * 128:(kt + 1) * 128], rhs=QaT[0:96, h, qs],
                                            start=True, stop=True),
                   reads=[b_KaT[kt], b_Kind] + qbufs, writes=[bps])
            s_mm(0, cnt)
            for kt in range(nkt):
                c = cnt + kt
                if kt + 1 < nkt:
                    s_mm(kt + 1, c + 1)
                ps_, bps = _rot(pS, c), _rot(b_pS, c)
                p_, bp_ = _rot(Pt, c), _rot(b_Pt, c)
                op("act", lambda e: e.activation(out=p_[:], in_=ps_[:], func=AF.Exp), reads=[bps], writes=[bp_])
                a = kt - 4 * G
                c0 = 0
                if a >= 0:
                    op("pool", lambda e: e.tensor_tensor(out=p_[:], in0=p_[:], in1=cm[:, a, :], op=ALU.mult),
                       reads=[bp_, b_cm], writes=[bp_])
                    c0 = a * 128
                op("pe", lambda e: e.matmul(po[0:65, c0:512], lhsT=Vaug[:, kt, h, :], rhs=p_[:, c0:512],
                                            start=(kt == 0), stop=(kt == nkt - 1)),
                   reads=[bp_, b_V[kt], b_Vones], writes=[bpo])
            cnt += nkt
            op("dve", lambda e: e.tensor_copy(out=ob[:], in_=po[0:65, :]), reads=[bpo], writes=[bob])
            dma(lambda e: e.dma_start(out=attn_o[h * 65:(h + 1) * 65, G * 512:(G + 1) * 512], in_=ob[:]), reads=[bob])
    P.pop_scope()
    P.pop_scope()

    if with_ssm:
        build_ssm(nc, P, U_tok, b_U, y_o, ident_bf, b_ident, ident_f, b_identf)
    P.finish()
    return nc


def rope_tables():
    half = 32
    inv = (10000.0 ** (-np.arange(half, dtype=np.float32) / half)).astype(np.float32)
    ang = np.arange(L_SEQ, dtype=np.float32)[:, None] * inv[None, :]
    cos, sin = np.cos(ang).astype(np.float32), np.sin(ang).astype(np.float32)
    cl = np.ascontiguousarray(cos.reshape(NTILE, 128, half).transpose(1, 0, 2).reshape(128, NTILE * half))
    sl = np.ascontiguousarray(sin.reshape(NTILE, 128, half).transpose(1, 0, 2).reshape(128, NTILE * half))
    return cl, sl


def const_tables():
    ind = np.zeros((32, L_SEQ), np.float32)
    for j in range(32):
        ind[j, j * 256:(j + 1) * 256] = 1.0
    cm = np.zeros((128, 4, 512), np.float32)
    kk = np.arange(128)[:, None]
    qq = np.arange(128)[None, :]
    tri = (kk <= qq).astype(np.float32)
    for a in range(4):
        for bq in range(4):
            if bq > a:
                cm[:, a, bq * 128:(bq + 1) * 128] = 1.0
            elif bq == a:
                cm[:, a, bq * 128:(bq + 1) * 128] = tri
    return ind, cm.reshape(128, 2048)


def phase_ab_inputs(inp, b, r):
    c = inp["c"][b]
    cT = np.ascontiguousarray(c.reshape(8, 128).T)
    cT2 = np.repeat(cT[:, :, None], 2, axis=2).reshape(128, 16)
    w = inp["w_in"][0]
    cols = np.concatenate([np.arange(128 * r, 128 * r + 128), 512 + np.arange(128 * r, 128 * r + 128),
                           1024 + np.arange(128 * r, 128 * r + 128), 1536 + np.arange(128 * r, 128 * r + 128)])
    cl, sl = rope_tables()
    ind, cm = const_tables()
    d = {
        "xb": np.ascontiguousarray(inp["x"][b]),
        "cT2": np.ascontiguousarray(cT2),
        "wada_a": np.ascontiguousarray(inp["w_ada"][0][:, 0:2048]),
        "bada_a": np.ascontiguousarray(inp["b_ada"][0][None, 0:2048]),
        "gmix": np.ascontiguousarray(inp["g_mix"][0][None, :]),
        "win": np.ascontiguousarray(w[:, cols]),
        "cosl": cl, "sinl": sl, "ind": ind, "cmask": cm, "ident": _ident(),
    }
    d.update(ssm_inputs(inp, r))
    return d


KV_F, KV_G, KV_O, KV_E, KV_A, KV_H = 0, 8, 16, 24, 32, 42
NKV = 43
TWO_PI = 2.0 * math.pi
CW1 = 6.28125
CW2 = TWO_PI - CW1


def ssm_kvec():
    kv = np.zeros(NKV, np.float32)
    for s in range(8):
        kv[KV_F + s] = -s
        kv[KV_G + s] = s
        kv[KV_O + s] = s + 1
        kv[KV_E + s] = 7 - s
    for i in range(10):
        kv[KV_A + i] = 8.0 * (2 ** i)
    kv[KV_H] = 0.5
    return kv


def build_ssm(nc, P, U_tok, b_U, y_o, ident_bf, b_ident, ident_f, b_identf):
    def din(name, shape, dt=F32):
        return nc.dram_tensor(name, list(shape), dt, kind="ExternalInput").ap()
    sp_small = din("ssm_small", [128, 8 * 4 + NKV + 2 + 8])
    sp_bc = din("ssm_bc", [128, 4 * 128])
    sp_mat = din("ssm_mat", [128, 256])
    op, dma = P.op, P.dma
    P.push_scope()
    small = P.sb("ssm_small_sb", [128, 8 * 4 + NKV + 2 + 8], F32); b_small = P.buf()
    bc = P.sb("ssm_bc_sb", [128, 4, 8, 16], F32); b_bc = P.buf()
    mats = P.sb("ssm_mat_sb", [128, 2, 128], F32); b_mats = P.buf()
    dma(lambda e: e.dma_start(out=small[:], in_=sp_small[:, :]), writes=[b_small])
    dma(lambda e: e.dma_start(out=bc[:].rearrange("p a g m -> p (a g m)"), in_=sp_bc[:, :]), writes=[b_bc])
    dma(lambda e: e.dma_start(out=mats[:].rearrange("p a n -> p (a n)"), in_=sp_mat[:, :]), writes=[b_mats])
    lamre, lamim, logdt, dsk = small[:, 0:8], small[:, 8:16], small[:, 16:24], small[:, 24:32]
    kvec = small[:, 32:32 + NKV]
    sgn_a, sgn_b = small[:, 32 + NKV:33 + NKV], small[:, 33 + NKV:34 + NKV]
    smask, P2 = mats[:, 0, :], mats[:, 1, :]
    Bs1, Bs2, Cs1, Cs2 = bc[:, 0], bc[:, 1], bc[:, 2], bc[:, 3]

    def T(name, shape, dt=F32):
        return P.sb(name, shape, dt), P.buf()

    def dv(fn, reads, writes):
        return op("dve", fn, reads=reads, writes=writes)

    dt_, b_dt = T("s_dt", [128, 8])
    a_, b_a = T("s_a", [128, 8])
    th, b_th = T("s_th", [128, 8])
    op("act", lambda e: e.activation(out=dt_[:], in_=logdt, func=AF.Exp), reads=[b_small], writes=[b_dt])
    dv(lambda e: e.tensor_tensor(out=a_[:], in0=lamre, in1=dt_[:], op=ALU.mult), [b_small, b_dt], [b_a])
    dv(lambda e: e.tensor_tensor(out=th[:], in0=lamim, in1=dt_[:], op=ALU.mult), [b_small, b_dt], [b_th])
    KA, b_KA = T("s_KA", [128, 8, NKV])
    KT, b_KT = T("s_KT", [128, 8, NKV])
    kb = kvec.unsqueeze(1).to_broadcast([128, 8, NKV])
    dv(lambda e: e.tensor_tensor(out=KA[:], in0=a_[:].unsqueeze(2).to_broadcast([128, 8, NKV]), in1=kb, op=ALU.mult),
       [b_a, b_small], [b_KA])
    dv(lambda e: e.tensor_tensor(out=KT[:], in0=th[:].unsqueeze(2).to_broadcast([128, 8, NKV]), in1=kb, op=ALU.mult),
       [b_th, b_small], [b_KT])
    MAG, b_MAG = T("s_MAG", [128, 8, NKV])
    op("act", lambda e: e.activation(out=MAG[:], in_=KA[:], func=AF.Exp), reads=[b_KA], writes=[b_MAG])
    ni, b_ni = T("s_ni", [128, 8, NKV], I32)
    nf, b_nf = T("s_nf", [128, 8, NKV])
    rr, b_rr = T("s_rr", [128, 8, NKV])
    uu, b_uu = T("s_uu", [128, 8, NKV])
    SIN, b_SIN = T("s_SIN", [128, 8, NKV])
    COS, b_COS = T("s_COS", [128, 8, NKV])

    def sin_of(dst, b_dst, shift):
        dv(lambda e: e.tensor_scalar(out=uu[:], in0=KT[:], scalar1=shift, scalar2=1.0 / TWO_PI, op0=ALU.add, op1=ALU.mult),
           [b_KT], [b_uu])
        dv(lambda e: e.tensor_copy(out=ni[:], in_=uu[:]), [b_uu], [b_ni])
        dv(lambda e: e.tensor_copy(out=nf[:], in_=ni[:]), [b_ni], [b_nf])
        dv(lambda e: e.scalar_tensor_tensor(out=rr[:], in0=nf[:], scalar=-CW1, in1=KT[:], op0=ALU.mult, op1=ALU.add),
           [b_nf, b_KT], [b_rr])
        dv(lambda e: e.scalar_tensor_tensor(out=rr[:], in0=nf[:], scalar=-CW2, in1=rr[:], op0=ALU.mult, op1=ALU.add),
           [b_nf, b_rr], [b_rr])
        dv(lambda e: e.tensor_scalar(out=rr[:], in0=rr[:], scalar1=shift, scalar2=math.pi, op0=ALU.add, op1=ALU.min),
           [b_rr], [b_rr])
        dv(lambda e: e.tensor_scalar(out=rr[:], in0=rr[:], scalar1=-math.pi, scalar2=None, op0=ALU.max), [b_rr], [b_rr])
        op("act", lambda e: e.activation(out=dst[:], in_=rr[:], func=AF.Sin), reads=[b_rr], writes=[b_dst])
    sin_of(SIN, b_SIN, 0.0)
    sin_of(COS, b_COS, math.pi / 2.0)
    PR, b_PR = T("s_PR", [128, 8, NKV])
    PI_, b_PI = T("s_PI", [128, 8, NKV])
    dv(lambda e: e.tensor_tensor(out=PR[:], in0=MAG[:], in1=COS[:], op=ALU.mult), [b_MAG, b_COS], [b_PR])
    dv(lambda e: e.tensor_tensor(out=PI_[:], in0=MAG[:], in1=SIN[:], op=ALU.mult), [b_MAG, b_SIN], [b_PI])
    em1, b_em1 = T("s_em1", [128, 8])
    tq, b_tq = T("s_tq", [128, 8])
    dv(lambda e: e.tensor_scalar(out=tq[:], in0=a_[:], scalar1=0.25, scalar2=1.0, op0=ALU.mult, op1=ALU.add), [b_a], [b_tq])
    dv(lambda e: e.tensor_tensor(out=tq[:], in0=tq[:], in1=a_[:], op=ALU.mult), [b_tq, b_a], [b_tq])
    dv(lambda e: e.tensor_scalar(out=tq[:], in0=tq[:], scalar1=1.0 / 3.0, scalar2=1.0, op0=ALU.mult, op1=ALU.add), [b_tq], [b_tq])
    dv(lambda e: e.tensor_tensor(out=tq[:], in0=tq[:], in1=a_[:], op=ALU.mult), [b_tq, b_a], [b_tq])
    dv(lambda e: e.tensor_scalar(out=tq[:], in0=tq[:], scalar1=0.5, scalar2=1.0, op0=ALU.mult, op1=ALU.add), [b_tq], [b_tq])
    dv(lambda e: e.tensor_tensor(out=em1[:], in0=tq[:], in1=a_[:], op=ALU.mult), [b_tq, b_a], [b_em1])
    cth, sth, shalf = COS[:, :, KV_G + 1], SIN[:, :, KV_G + 1], SIN[:, :, KV_H]
    re1, b_re1 = T("s_re1", [128, 8])
    im1, b_im1 = T("s_im1", [128, 8])
    w1, b_w1 = T("s_w1", [128, 8])
    w2, b_w2 = T("s_w2", [128, 8])
    dv(lambda e: e.tensor_tensor(out=w1[:], in0=shalf, in1=shalf, op=ALU.mult), [b_SIN], [b_w1])
    dv(lambda e: e.tensor_tensor(out=re1[:], in0=em1[:], in1=cth, op=ALU.mult), [b_em1, b_COS], [b_re1])
    dv(lambda e: e.scalar_tensor_tensor(out=re1[:], in0=w1[:], scalar=-2.0, in1=re1[:], op0=ALU.mult, op1=ALU.add),
       [b_w1, b_re1], [b_re1])
    dv(lambda e: e.scalar_tensor_tensor(out=im1[:], in0=em1[:], scalar=1.0, in1=sth, op0=ALU.add, op1=ALU.mult),
       [b_em1, b_SIN], [b_im1])
    den, b_den = T("s_den", [128, 8])
    dv(lambda e: e.tensor_tensor(out=den[:], in0=lamre, in1=lamre, op=ALU.mult), [b_small], [b_den])
    dv(lambda e: e.tensor_tensor(out=w1[:], in0=lamim, in1=lamim, op=ALU.mult), [b_small], [b_w1])
    dv(lambda e: e.tensor_tensor(out=den[:], in0=den[:], in1=w1[:], op=ALU.add), [b_den, b_w1], [b_den])
    dv(lambda e: e.reciprocal(out=den[:], in_=den[:]), [b_den], [b_den])
    cr, b_cr = T("s_cr", [128, 8])
    ci, b_ci = T("s_ci", [128, 8])
    dv(lambda e: e.tensor_tensor(out=w1[:], in0=re1[:], in1=lamre, op=ALU.mult), [b_re1, b_small], [b_w1])
    dv(lambda e: e.tensor_tensor(out=w2[:], in0=im1[:], in1=lamim, op=ALU.mult), [b_im1, b_small], [b_w2])
    dv(lambda e: e.tensor_tensor(out=w1[:], in0=w1[:], in1=w2[:], op=ALU.add), [b_w1, b_w2], [b_w1])
    dv(lambda e: e.tensor_tensor(out=cr[:], in0=w1[:], in1=den[:], op=ALU.mult), [b_w1, b_den], [b_cr])
    dv(lambda e: e.tensor_tensor(out=w1[:], in0=im1[:], in1=lamre, op=ALU.mult), [b_im1, b_small], [b_w1])
    dv(lambda e: e.tensor_tensor(out=w2[:], in0=re1[:], in1=lamim, op=ALU.mult), [b_re1, b_small], [b_w2])
    dv(lambda e: e.tensor_tensor(out=w1[:], in0=w1[:], in1=w2[:], op=ALU.subtract), [b_w1, b_w2], [b_w1])
    dv(lambda e: e.tensor_tensor(out=ci[:], in0=w1[:], in1=den[:], op=ALU.mult), [b_w1, b_den], [b_ci])
    bb1, b_bb1 = T("s_bb1", [128, 8, 16])
    bb2, b_bb2 = T("s_bb2", [128, 8, 16])
    q1, b_q1 = T("s_q1", [128, 8, 16])
    q2, b_q2 = T("s_q2", [128, 8, 16])
    crb = cr[:].unsqueeze(2).to_broadcast([128, 8, 16])
    cib = ci[:].unsqueeze(2).to_broadcast([128, 8, 16])
    dv(lambda e: e.tensor_tensor(out=q1[:], in0=crb, in1=Bs1, op=ALU.mult), [b_cr, b_bc], [b_q1])
    dv(lambda e: e.tensor_tensor(out=q2[:], in0=cib, in1=Bs2, op=ALU.mult), [b_ci, b_bc], [b_q2])
    dv(lambda e: e.scalar_tensor_tensor(out=bb1[:].rearrange("p g m -> p (g m)"), in0=q2[:].rearrange("p g m -> p (g m)"),
                                         scalar=sgn_a, in1=q1[:].rearrange("p g m -> p (g m)"), op0=ALU.mult, op1=ALU.add),
       [b_q1, b_q2, b_small], [b_bb1])
    dv(lambda e: e.tensor_tensor(out=q1[:], in0=crb, in1=Bs2, op=ALU.mult), [b_cr, b_bc], [b_q1])
    dv(lambda e: e.tensor_tensor(out=q2[:], in0=cib, in1=Bs1, op=ALU.mult), [b_ci, b_bc], [b_q2])
    dv(lambda e: e.scalar_tensor_tensor(out=bb2[:].rearrange("p g m -> p (g m)"), in0=q2[:].rearrange("p g m -> p (g m)"),
                                         scalar=sgn_b, in1=q1[:].rearrange("p g m -> p (g m)"), op0=ALU.mult, op1=ALU.add),
       [b_q1, b_q2, b_small], [b_bb2])
    CA, b_CA = T("s_CA", [128, 8, 16])
    CB, b_CB = T("s_CB", [128, 8, 16])
    dv(lambda e: e.tensor_scalar(out=CA[:], in0=Cs1, scalar1=sgn_b, scalar2=None, op0=ALU.mult), [b_bc, b_small], [b_CA])
    dv(lambda e: e.tensor_scalar(out=CB[:], in0=Cs2, scalar1=-1.0, scalar2=None, op0=ALU.mult), [b_bc], [b_CB])
    Fm, b_Fm = T("s_F", [128, 8, 8, 16])
    Fe, b_Fe = T("s_Fe", [128, 8, 8, 16])
    Gm, b_Gm = T("s_G", [128, 8, 8, 16])
    Om, b_Om = T("s_O", [128, 8, 8, 16])
    z1, b_z1 = T("s_z1", [128, 8, 8, 16])
    z2, b_z2 = T("s_z2", [128, 8, 8, 16])

    def outer(dst, bdst, kv0, v1, bv1, v2, bv2, sgn):
        pr = PR[:, :, kv0:kv0 + 8].unsqueeze(3).to_broadcast([128, 8, 8, 16])
        pi = PI_[:, :, kv0:kv0 + 8].unsqueeze(3).to_broadcast([128, 8, 8, 16])
        dv(lambda e: e.tensor_tensor(out=z1[:], in0=pr, in1=v1[:].unsqueeze(2).to_broadcast([128, 8, 8, 16]), op=ALU.mult),
           [b_PR, bv1], [b_z1])
        dv(lambda e: e.tensor_tensor(out=z2[:], in0=pi, in1=v2[:].unsqueeze(2).to_broadcast([128, 8, 8, 16]), op=ALU.mult),
           [b_PI, bv2], [b_z2])
        fl = "p g s m -> p (g s m)"
        if sgn is None:
            dv(lambda e: e.tensor_tensor(out=dst[:].rearrange(fl), in0=z1[:].rearrange(fl), in1=z2[:].rearrange(fl), op=ALU.add),
               [b_z1, b_z2], [bdst])
        else:
            dv(lambda e: e.scalar_tensor_tensor(out=dst[:].rearrange(fl), in0=z2[:].rearrange(fl), scalar=sgn,
                                                 in1=z1[:].rearrange(fl), op0=ALU.mult, op1=ALU.add),
               [b_z1, b_z2, b_small], [bdst])
    outer(Fm, b_Fm, KV_F, bb1, b_bb1, bb2, b_bb2, sgn_a)
    outer(Fe, b_Fe, KV_E, bb1, b_bb1, bb2, b_bb2, sgn_a)
    outer(Gm, b_Gm, KV_G, CA, b_CA, CB, b_CB, None)
    outer(Om, b_Om, KV_O, CA, b_CA, CB, b_CB, None)
    PIA, b_PIA = T("s_PIA", [128, 8, 10])
    dv(lambda e: e.tensor_scalar(out=PIA[:], in0=PI_[:, :, KV_A:KV_A + 10], scalar1=sgn_b, scalar2=None, op0=ALU.mult),
       [b_PI, b_small], [b_PIA])

    Yout = P.sb("s_Yout", [128, 8, 8, 128], F32); b_Yout = P.buf()
    Ug = P.sb("s_Ug", [128, 1024], BF16); b_Ug = P.buf()
    H = P.sb("s_H", [128, 1024], F32); b_H = P.buf()
    Ysb = P.sb("s_Ysb", [128, 1024], F32); b_Ysb = P.buf()
    Mbf = P.sb("s_Mbf", [128, 128], BF16); b_Mbf = P.buf()
    Ebf = P.sb("s_Ebf", [128, 128], BF16); b_Ebf = P.buf()
    mtmp = P.sb("s_mtmp", [128, 128], F32); b_mtmp = P.buf()
    Amat = P.sb("s_Amat", [128, 10, 128], F32); b_Amat = P.buf()
    put = P.ps("s_put", [128, 8, 128], BF16); b_put = P.pbuf()
    pxy = [P.ps(f"s_pxy{i}", [128, 512], F32) for i in range(2)]; b_pxy = [P.pbuf() for _ in range(2)]
    psc = [P.ps(f"s_psc{i}", [128, 512], F32) for i in range(2)]; b_psc = [P.pbuf() for _ in range(2)]
    pme = P.ps("s_pme", [128, 512], F32); b_pme = P.pbuf()
    pyt = P.ps("s_pyt", [128, 8, 128], F32); b_pyt = P.pbuf()
    for g in range(8):
        Fg = Fm[:, g].rearrange("p s m -> p (s m)")
        Feg = Fe[:, g].rearrange("p s m -> p (s m)")
        Gg = Gm[:, g].rearrange("p s m -> p (s m)")
        Og = Om[:, g].rearrange("p s m -> p (s m)")
        op("pe", lambda e: e.matmul(pme[:, 0:128], lhsT=Fg, rhs=Gg, start=True, stop=True), reads=[b_Fm, b_Gm], writes=[b_pme])
        dv(lambda e: e.tensor_tensor(out=mtmp[:], in0=pme[:, 0:128], in1=smask, op=ALU.mult), [b_pme, b_mats], [b_mtmp])
        dv(lambda e: e.scalar_tensor_tensor(out=Mbf[:], in0=ident_f[:], scalar=dsk[:, g:g + 1], in1=mtmp[:],
                                             op0=ALU.mult, op1=ALU.add), [b_identf, b_small, b_mtmp], [b_Mbf])
        op("pe", lambda e: e.transpose(pme[:, 128:256], Feg, ident_f[:]), reads=[b_Fe, b_identf], writes=[b_pme])
        op("act", lambda e: e.activation(out=Ebf[:], in_=pme[:, 128:256], func=AF.Copy), reads=[b_pme], writes=[b_Ebf])
        for i in range(10):
            dv(lambda e: e.tensor_scalar(out=mtmp[:], in0=ident_f[:], scalar1=PR[:, g, KV_A + i:KV_A + i + 1], scalar2=None,
                                         op0=ALU.mult), [b_identf, b_PR], [b_mtmp])
            dv(lambda e: e.scalar_tensor_tensor(out=Amat[:, i, :], in0=P2, scalar=PIA[:, g, i:i + 1], in1=mtmp[:],
                                                 op0=ALU.mult, op1=ALU.add), [b_mats, b_PIA, b_mtmp], [b_Amat])
        for S in range(8):
            op("pe", lambda e: e.transpose(put[:, S, :], U_tok[:, S, g].rearrange("p s m -> p (s m)"), ident_bf[:]),
               reads=[b_U[S], b_ident], writes=[b_put])
        op("act", lambda e: e.activation(out=Ug[:].rearrange("p (S c) -> p S c", S=8), in_=put[:], func=AF.Copy),
           reads=[b_put], writes=[b_Ug])
        for hf in range(2):
            op("pe", lambda e: e.matmul(pxy[hf][:], lhsT=Ebf[:], rhs=Ug[:, hf * 512:(hf + 1) * 512], start=True, stop=True),
               reads=[b_Ebf, b_Ug], writes=[b_pxy[hf]])
            op("act", lambda e: e.activation(out=H[:, hf * 512:(hf + 1) * 512], in_=pxy[hf][:], func=AF.Copy),
               reads=[b_pxy[hf]], writes=[b_H])
        for i in range(10):
            d = 1 << i
            rngs = []
            for hb in range(2):
                lo = max(d, hb * 512)
                hi = (hb + 1) * 512
                if lo < hi:
                    rngs.append((hb, lo, hi))
            for (hb, lo, hi) in rngs:
                op("pe", lambda e: e.matmul(psc[hb][:, lo - hb * 512:hi - hb * 512], lhsT=Amat[:, i, :], rhs=H[:, lo - d:hi - d],
                                            start=True, stop=True), reads=[b_Amat, b_H], writes=[b_psc[hb]])
            for (hb, lo, hi) in rngs:
                dv(lambda e: e.tensor_tensor(out=H[:, lo:hi], in0=psc[hb][:, lo - hb * 512:hi - hb * 512], in1=H[:, lo:hi], op=ALU.add),
                   [b_psc[hb], b_H], [b_H])
        for hf in range(2):
            op("pe", lambda e: e.matmul(pxy[hf][:], lhsT=Mbf[:], rhs=Ug[:, hf * 512:(hf + 1) * 512], start=True, stop=False),
               reads=[b_Mbf, b_Ug], writes=[b_pxy[hf]])
            if hf == 0:
                op("pe", lambda e: e.matmul(pxy[0][:, 1:512], lhsT=Og, rhs=H[:, 0:511], start=False, stop=True),
                   reads=[b_Om, b_H], writes=[b_pxy[0]])
            else:
                op("pe", lambda e: e.matmul(pxy[1][:], lhsT=Og, rhs=H[:, 511:1023], start=False, stop=True),
                   reads=[b_Om, b_H], writes=[b_pxy[1]])
            op("act", lambda e: e.activation(out=Ysb[:, hf * 512:(hf + 1) * 512], in_=pxy[hf][:], func=AF.Copy),
               reads=[b_pxy[hf]], writes=[b_Ysb])
        for S in range(8):
            op("pe", lambda e: e.transpose(pyt[:, S, :], Ysb[:, S * 128:(S + 1) * 128], ident_f[:]),
               reads=[b_Ysb, b_identf], writes=[b_pyt])
        dv(lambda e: e.tensor_copy(out=Yout[:, :, :, g * 16:(g + 1) * 16],
                                   in_=pyt[:].rearrange("p S (t n) -> p S t n", t=8)), [b_pyt], [b_Yout])
    for S in range(8):
        dma(lambda e: e.dma_start(out=y_o[S * 1024:(S + 1) * 1024, :].rearrange("(c t) ch -> c t ch", t=8), in_=Yout[:, S]),
            reads=[b_Yout])
    P.pop_scope()


def ssm_inputs(inp, r):
    gs = slice(8 * r, 8 * r + 8)
    lam_re = inp["lam_re"][0][gs]
    lam_im = inp["lam_im"][0][gs]
    log_dt = inp["log_dt"][0][gs]
    b_re, b_im = inp["b_re"][0][gs], inp["b_im"][0][gs]
    c_re, c_im = inp["c_re"][0][gs], inp["c_im"][0][gs]
    d = inp["d_skip"][0][gs]
    dup = lambda a: np.concatenate([a, a], axis=0)
    small = np.zeros((128, 8 * 4 + NKV + 2 + 8), np.float32)
    small[:, 0:8] = dup(lam_re.T)
    small[:, 8:16] = dup(lam_im.T)
    small[:, 16:24] = np.broadcast_to(log_dt[None, :], (128, 8))
    small[:, 24:32] = np.tile(d.T, (8, 1))
    small[:, 32:32 + NKV] = ssm_kvec()[None, :]
    small[:64, 32 + NKV] = -1.0; small[64:, 32 + NKV] = 1.0
    small[:64, 33 + NKV] = 1.0; small[64:, 33 + NKV] = -1.0
    bre = b_re.transpose(1, 0, 2); bim = b_im.transpose(1, 0, 2)
    cre = c_re.transpose(2, 0, 1); cim = c_im.transpose(2, 0, 1)
    bcv = np.zeros((128, 4, 8, 16), np.float32)
    bcv[:64, 0], bcv[64:, 0] = bre, bim
    bcv[:64, 1], bcv[64:, 1] = bim, bre
    bcv[:64, 2], bcv[64:, 2] = cre, cim
    bcv[:64, 3], bcv[64:, 3] = cim, cre
    s_idx = np.arange(128) // 16
    smask = (s_idx[None, :] >= s_idx[:, None]).astype(np.float32)
    P2 = np.zeros((128, 128), np.float32)
    P2[np.arange(128), (np.arange(128) + 64) % 128] = 1.0
    return {"ssm_small": small, "ssm_bc": np.ascontiguousarray(bcv.reshape(128, 512)),
            "ssm_mat": np.ascontiguousarray(np.concatenate([smask, P2], axis=1))}


_NC_CACHE = {}


def _get_nc(which):
    if which not in _NC_CACHE:
        nc = bass.Bass("TRN2", target_bir_lowering=False)
        if which == "ab":
            build_phase_ab(nc, with_ssm=True)
        else:
            build_phase_c(nc)
        _NC_CACHE[which] = nc
    return _NC_CACHE[which]


def kernel(**inputs):
    inp = {k: np.asarray(v, dtype=np.float32) for k, v in inputs.items()}
    B = inp["x"].shape[0]
    maps1 = [phase_ab_inputs(inp, ci // 4, ci % 4) for ci in range(8)]
    res1 = run_bass_kernel_spmd(_get_nc("ab"), maps1, core_ids=list(range(8))).results
    attn_full = np.zeros((B, L_SEQ, 8, 65), np.float32)
    y_full = np.zeros((B, L_SEQ, 512), np.float32)
    for ci in range(8):
        b, r = ci // 4, ci % 4
        attn_full[b, :, 2 * r:2 * r + 2, :] = res1[ci]["attn_o"].reshape(2, 65, L_SEQ).transpose(2, 0, 1)
        y_full[b, :, 128 * r:128 * r + 128] = res1[ci]["y_o"]
    maps2 = [phase_c_inputs(inp, ci // 4, ci % 4, attn_full, y_full) for ci in range(8)]
    res2 = run_bass_kernel_spmd(_get_nc("c"), maps2, core_ids=list(range(8))).results
    out = np.zeros((B, L_SEQ, 1024), np.float32)
    for ci in range(8):
        b, qi = ci // 4, ci % 4
        out[b, qi * NT_C:(qi + 1) * NT_C] = res2[ci]["out"]
    return out
```

```python
import math
import numpy as np
from contextlib import ExitStack

import concourse.bass as bass
import concourse.mybir as mybir
from concourse.bass_utils import run_bass_kernel_spmd

F32 = mybir.dt.float32
BF16 = mybir.dt.bfloat16
I32 = mybir.dt.int32
ALU = mybir.AluOpType
AF = mybir.ActivationFunctionType

PHYS = ["pe", "act", "dve", "pool", "sp"]
NDMA = 8
SAME_ENG_WAIT = True
EPS = 1e-6
NEGBIG = -30000.0


class Buf:
    __slots__ = ("name", "last_w", "readers", "excl")

    def __init__(self, name, excl=False):
        self.name = name
        self.last_w = None
        self.readers = []
        self.excl = excl


class Prog:
    def __init__(self, nc):
        self.nc = nc
        self.stack = ExitStack()
        self.semnames = ["pe", "act", "dve", "pool"] + [f"d{i}" for i in range(NDMA)]
        self.cnt = {s: 0 for s in self.semnames}
        self.seen = {e: {s: 0 for s in self.semnames} for e in PHYS}
        self.sems = {s: self.stack.enter_context(nc.semaphore("sem_" + s)) for s in self.semnames}
        self.engobjs = {"pe": nc.tensor, "act": nc.scalar, "dve": nc.vector, "pool": nc.gpsimd, "sp": nc.sync}
        self.pending = {e: {} for e in PHYS}
        self.nbuf = 0
        self.dma_rr = 0
        self.scopes = [self.stack]

    def buf(self, name=None, excl=False):
        self.nbuf += 1
        return Buf(name or f"b{self.nbuf}", excl)

    def pbuf(self):
        return self.buf(excl=True)

    def sb(self, name, shape, dtype):
        return self.scopes[-1].enter_context(self.nc.sbuf_tensor(name, list(shape), dtype))

    def ps(self, name, shape, dtype=F32):
        return self.scopes[-1].enter_context(self.nc.psum_tensor(name, list(shape), dtype))

    def push_scope(self):
        st = ExitStack()
        self.scopes.append(st)
        return st

    def pop_scope(self):
        st = self.scopes.pop()
        st.close()
        self.fence()

    def fence(self):
        for e in PHYS:
            for s in self.semnames:
                if self.cnt[s] > self.seen[e][s]:
                    self.pending[e][s] = self.cnt[s]

    def _op(self, phys, sem, inc, fn, reads, writes, extra_waits=()):
        waits = dict(self.pending[phys])
        self.pending[phys] = {}
        xr = [b for b in reads if b.excl]
        if xr:
            reads = [b for b in reads if not b.excl]
            writes = list(writes) + [b for b in xr if b not in writes]
        for (f, c) in extra_waits:
            waits[f] = max(waits.get(f, 0), c)

        def need(f):
            return f != phys or (SAME_ENG_WAIT and phys != "pe")
        for b in reads:
            if b.last_w is not None:
                f, c = b.last_w
                if need(f):
                    waits[f] = max(waits.get(f, 0), c)
        for b in writes:
            if b.last_w is not None:
                f, c = b.last_w
                if need(f):
                    waits[f] = max(waits.get(f, 0), c)
            for (f, c) in b.readers:
                if need(f):
                    waits[f] = max(waits.get(f, 0), c)
        engobj = self.engobjs[phys]
        for f, c in waits.items():
            if c > self.seen[phys][f]:
                self.seen[phys][f] = c
                engobj.wait_ge(self.sems[f], c)
        self.cnt[sem] += inc
        me = (sem, self.cnt[sem])
        ins = fn(engobj)
        ins.then_inc(self.sems[sem], inc)
        for b in reads:
            b.readers.append(me)
        for b in writes:
            b.last_w = me
            b.readers = []
        return me

    def op(self, eng, fn, reads=(), writes=()):
        return self._op(eng, eng, 1, fn, reads, writes)

    def dma(self, fn, reads=(), writes=(), q="sp"):
        k = self.dma_rr % NDMA
        self.dma_rr += 1
        sem = f"d{k}"
        prev = self.cnt[sem]
        extra = [(sem, prev)] if prev > 0 else []
        return self._op(q, sem, 16, fn, reads, writes, extra_waits=extra)

    def finish(self):
        for i in range(NDMA):
            s = f"d{i}"
            if self.cnt[s] > 0:
                self.nc.sync.wait_ge(self.sems[s], self.cnt[s])
        for s in ["pe", "act", "dve", "pool"]:
            if self.cnt[s] > 0:
                self.nc.sync.wait_ge(self.sems[s], self.cnt[s])
        while len(self.scopes) > 1:
            self.scopes.pop().close()
        self.stack.close()


def _rot(lst, i):
    return lst[i % len(lst)]


NT_C = 2048


def build_phase_c(nc, srcs=None):
    def din(name, shape, dt=F32):
        return nc.dram_tensor(name, list(shape), dt, kind="ExternalInput").ap()
    x = din("xc", [NT_C, 1024])
    if srcs is None:
        attn = din("attn", [NT_C, 520])
        yin = din("yin", [NT_C, 512])
    else:
        attn, yin = srcs["attn"], srcs["y"]
    cT2 = din("cT2", [128, 16])
    wada = din("wada_c", [1024, 4096])
    bada = din("bada_c", [1, 4096])
    rows = din("rows_c", [1, 3072])
    bglu = din("bglu", [1, 512])
    wglu = din("wglu", [512, 512])
    wout = din("wout", [1024, 1024])
    wfc1 = din("wfc1", [1024, 4096])
    wfc2 = din("wfc2", [4096, 1024])
    ident = din("ident", [128, 128])
    out = nc.dram_tensor("out", [NT_C, 1024], F32, kind="ExternalOutput").ap()
    x1s = nc.dram_tensor("x1s", [NT_C, 1024], F32).ap()

    P = Prog(nc)
    op, dma = P.op, P.dma

    ident_bf = P.sb("ident_bf", [128, 128], BF16); b_ident = P.buf()
    ones_f = P.sb("ones_f", [1, 128], F32); b_ones_f = P.buf()
    ones_bf = P.sb("ones_bf", [1, 512], BF16); b_ones_bf = P.buf()
    bglu_bf = P.sb("bglu_bf", [1, 512], BF16); b_bglu = P.buf()
    sh2T = P.sb("sh2T", [128, 16], BF16); b_sh2T = P.buf()
    Gcat = P.sb("Gcat", [128, 1024], F32); b_Gcat = P.buf()
    G1 = P.sb("G1", [128, 1024], F32); b_G1 = P.buf()
    S2b = P.sb("S2b", [128, 1024], F32); b_S2b = P.buf()
    G2 = P.sb("G2", [128, 1024], F32); b_G2 = P.buf()
    Gf = P.sb("Gf", [128, 1024], F32); b_Gf = P.buf()

    ident_f = P.sb("ident_f", [128, 128], F32); b_identf = P.buf()
    bglu_f = P.sb("bglu_f", [1, 512], F32); b_bgluf = P.buf()
    dma(lambda e: e.dma_start(out=ident_f[:], in_=ident[:, :]), writes=[b_identf])
    dma(lambda e: e.dma_start(out=bglu_f[:], in_=bglu[:, :]), writes=[b_bgluf])
    op("dve", lambda e: e.tensor_copy(out=ident_bf[:], in_=ident_f[:]), reads=[b_identf], writes=[b_ident])
    op("dve", lambda e: e.tensor_copy(out=bglu_bf[:], in_=bglu_f[:]), reads=[b_bgluf], writes=[b_bglu])
    op("dve", lambda e: e.memset(ones_f[:], 1.0), writes=[b_ones_f])
    op("dve", lambda e: e.memset(ones_bf[:], 1.0), writes=[b_ones_bf])

    P.push_scope()
    ct = P.sb("ct", [128, 16], F32); b_ct = P.buf()
    sct = P.sb("sct", [128, 16], F32); b_sct = P.buf()
    sgc = P.sb("sgc", [128, 16], F32)
    modrow = P.sb("modrow", [1, 4096], F32); b_modrow = P.buf()
    badas = P.sb("badas", [1, 4096], F32); b_badas = P.buf()
    rowss = P.sb("rowss", [1, 3072], F32); b_rowss = P.buf()
    s2row = P.sb("s2row", [1, 1024], F32); b_s2row = P.buf()
    pieces = [P.sb(f"wpiece{i}", [128, 8, 512], F32) for i in range(2)]
    b_pieces = [P.buf() for _ in range(2)]
    ps_row = [P.ps(f"ps_row{i}", [128, 512], F32) for i in range(2)]
    b_ps_row = [P.pbuf() for _ in range(2)]

    dma(lambda e: e.dma_start(out=ct[:], in_=cT2[:, :]), writes=[b_ct])
    dma(lambda e: e.dma_start(out=badas[:], in_=bada[:, :]), writes=[b_badas])
    dma(lambda e: e.dma_start(out=rowss[:], in_=rows[:, :]), writes=[b_rowss])
    op("act", lambda e: e.activation(out=sgc[:], in_=ct[:], func=AF.Sigmoid), reads=[b_ct], writes=[b_sct])
    op("dve", lambda e: e.tensor_tensor(out=sct[:], in0=sgc[:], in1=ct[:], op=ALU.mult), reads=[b_ct, b_sct], writes=[b_sct])
    for j in range(8):
        pc, bpc = _rot(pieces, j), _rot(b_pieces, j)
        pr, bpr = _rot(ps_row, j), _rot(b_ps_row, j)
        dma(lambda e: e.dma_start(out=pc[:], in_=wada[:, j * 512:(j + 1) * 512].rearrange("(k p) n -> p k n", p=128)),
            writes=[bpc])
        for k in range(8):
            op("pe", lambda e: e.matmul(pr[0:1, :], lhsT=sct[:, 2 * k:2 * k + 1], rhs=pc[:, k, :],
                                        start=(k == 0), stop=(k == 7)),
               reads=[b_sct, bpc], writes=[bpr])
        op("dve", lambda e: e.tensor_tensor(out=modrow[0:1, j * 512:(j + 1) * 512], in0=pr[0:1, :],
                                            in1=badas[0:1, j * 512:(j + 1) * 512], op=ALU.add),
           reads=[bpr, b_badas], writes=[b_modrow])
    op("dve", lambda e: e.scalar_tensor_tensor(out=s2row[:], in0=modrow[0:1, 2048:3072], scalar=1.0,
                                                in1=rowss[0:1, 1024:2048], op0=ALU.add, op1=ALU.mult),
       reads=[b_modrow, b_rowss], writes=[b_s2row])
    bc_list = [(Gcat, b_Gcat, rowss, b_rowss, 0), (G1, b_G1, modrow, b_modrow, 0), (S2b, b_S2b, s2row, b_s2row, 0),
               (G2, b_G2, modrow, b_modrow, 3072), (Gf, b_Gf, rowss, b_rowss, 2048)]
    i = 0
    for (dst, bdst, src, bsrc, off) in bc_list:
        for hf in range(2):
            pr, bpr = _rot(ps_row, i), _rot(b_ps_row, i)
            i += 1
            op("pe", lambda e: e.matmul(pr[:, :], lhsT=ones_f[0:1, :], rhs=src[0:1, off + hf * 512: off + (hf + 1) * 512],
                                        start=True, stop=True),
               reads=[b_ones_f, bsrc], writes=[bpr])
            op("act", lambda e: e.activation(out=dst[:, hf * 512:(hf + 1) * 512], in_=pr[:, :], func=AF.Copy),
               reads=[bpr], writes=[bdst])
    pr, bpr = ps_row[0], b_ps_row[0]
    for k in range(8):
        op("pe", lambda e: e.matmul(pr[:, 2 * k:2 * k + 2], lhsT=modrow[0:1, 1024 + k * 128:1024 + (k + 1) * 128],
                                    rhs=ones_f[0:1, 0:2], start=True, stop=True),
           reads=[b_ones_f, b_modrow], writes=[bpr])
    op("dve", lambda e: e.tensor_copy(out=sh2T[:], in_=pr[:, 0:16]), reads=[bpr], writes=[b_sh2T])
    P.pop_scope()

    P.push_scope()
    Wout = P.sb("Wout", [128, 8, 1024], BF16); b_Wout = P.buf()
    Wglu = P.sb("Wglu", [128, 4, 512], BF16); b_Wglu = P.buf()
    stg = [P.sb(f"stgc1_{i}", [128, 2048], F32) for i in range(2)]; b_stg = [P.buf() for _ in range(2)]
    dma(lambda e: e.dma_start(out=stg[0][:].rearrange("p (k n) -> p k n", k=4), in_=wglu.rearrange("(k p) n -> p k n", p=128)),
        writes=[b_stg[0]])
    op("pool", lambda e: e.tensor_copy(out=Wglu[:].rearrange("p k n -> p (k n)"), in_=stg[0][:]), reads=[b_stg[0]], writes=[b_Wglu])
    for kk in range(4):
        sg_, bsg_ = _rot(stg, kk + 1), _rot(b_stg, kk + 1)
        dma(lambda e: e.dma_start(out=sg_[:].rearrange("p (k n) -> p k n", k=2),
                                  in_=wout[kk * 256:(kk + 1) * 256, :].rearrange("(k p) n -> p k n", p=128)), writes=[bsg_])
        op("pool" if kk % 2 else "dve", lambda e: e.tensor_copy(out=Wout[:, 2 * kk:2 * kk + 2, :].rearrange("p k n -> p (k n)"), in_=sg_[:]),
           reads=[bsg_], writes=[b_Wout])
    NB = 3

    def dbl(name, shape, dt, n=2):
        return [P.sb(f"{name}{i}", shape, dt) for i in range(n)], [P.buf() for _ in range(n)]

    def dblp(name, shape, dt, n=2):
        return [P.ps(f"{name}{i}", shape, dt) for i in range(n)], [P.pbuf() for _ in range(n)]
    at, b_at = dbl("at", [128, 8, 65], F32, NB)
    yt, b_yt = dbl("yt", [128, 512], F32, NB)
    xt, b_xt = dbl("xt", [128, 1024], F32, NB)
    rl, b_rl = dbl("rl", [128, 8], F32)
    A, b_A = dbl("A", [128, 8, 64], F32)
    junk = P.sb("junk", [128, 512], BF16); b_junk = P.buf()
    ss, b_ss = dbl("ss", [128, 2], F32)
    sq, b_sq = dbl("sq", [128, 2], F32)
    rstd, b_rstd = dbl("rstd", [128, 2], F32)
    g1, b_g1 = dbl("g1", [128, 512], F32)
    g2, b_g2 = dbl("g2", [128, 512], F32)
    sg, b_sg = dbl("sg", [128, 512], F32)
    yg, b_yg = dbl("yg", [128, 512], F32)
    ygb, b_ygb = dbl("ygb", [128, 512], BF16)
    ygT, b_ygT = dbl("ygT", [128, 4, 128], BF16)
    sig, b_sig = dbl("sig", [128, 512], F32)
    S, b_S = dbl("S", [128, 512], F32)
    mixh, b_mixh = dbl("mixh", [128, 1024], BF16)
    mixT, b_mixT = dbl("mixT", [128, 8, 128], BF16)
    tt, b_tt = dbl("tt", [128, 1024], F32)
    x1t, b_x1t = dbl("x1t", [128, 1024], F32)
    ptr, b_ptr = dblp("ptr", [128, 8, 128], BF16)
    psg, b_psg = dblp("psg", [128, 512], F32)
    pmx = P.ps("pmx", [128, 8, 128], BF16); b_pmx = P.pbuf()
    pso = [P.ps(f"pso{i}", [128, 512], F32) for i in range(2)]; b_pso = [P.pbuf() for _ in range(2)]
    b_x1s = [P.buf() for _ in range(16)]

    def c1_load(t):
        a, ba = _rot(at, t), _rot(b_at, t)
        y_, by = _rot(yt, t), _rot(b_yt, t)
        x_, bx = _rot(xt, t), _rot(b_xt, t)
        dma(lambda e: e.dma_start(out=a[:].rearrange("p h e -> p (h e)"), in_=attn[t * 128:(t + 1) * 128, :]), writes=[ba])
        dma(lambda e: e.dma_start(out=y_[:], in_=yin[t * 128:(t + 1) * 128, :]), writes=[by])
        dma(lambda e: e.dma_start(out=x_[:], in_=x[t * 128:(t + 1) * 128, :]), writes=[bx])

    def c1_a(t):
        a, ba = _rot(at, t), _rot(b_at, t)
        y_, by = _rot(yt, t), _rot(b_yt, t)
        i = t % 2
        op("dve", lambda e: e.reciprocal(out=rl[i][:], in_=a[:, :, 64]), reads=[ba], writes=[b_rl[i]])
        op("dve", lambda e: e.tensor_tensor(out=A[i][:], in0=a[:, :, 0:64],
                                            in1=rl[i][:, :].unsqueeze(2).to_broadcast([128, 8, 64]), op=ALU.mult),
           reads=[ba, b_rl[i]], writes=[b_A[i]])
        op("act", lambda e: e.activation(out=junk[:], in_=A[i][:].rearrange("p h d -> p (h d)"), func=AF.Square,
                                         accum_out=ss[i][:, 0:1]),
           reads=[b_A[i]], writes=[b_junk, b_ss[i]])
        op("pool", lambda e: e.tensor_tensor(out=g1[i][:], in0=y_[:], in1=y_[:], op=ALU.mult), reads=[by], writes=[b_g1[i]])
        op("pool", lambda e: e.tensor_scalar(out=g1[i][:], in0=g1[i][:], scalar1=0.044715, scalar2=1.0,
                                             op0=ALU.mult, op1=ALU.add), reads=[b_g1[i]], writes=[b_g1[i]])
        op("pool", lambda e: e.tensor_tensor(out=g2[i][:], in0=g1[i][:], in1=y_[:], op=ALU.mult), reads=[b_g1[i], by], writes=[b_g2[i]])
        op("act", lambda e: e.activation(out=sg[i][:], in_=g2[i][:], func=AF.Sigmoid, scale=1.5957691216057308),
           reads=[b_g2[i]], writes=[b_sg[i]])
        op("dve", lambda e: e.tensor_tensor(out=yg[i][:], in0=y_[:], in1=sg[i][:], op=ALU.mult), reads=[by, b_sg[i]], writes=[b_yg[i]])
        op("pool", lambda e: e.tensor_tensor(out=ygb[i][:], in0=y_[:], in1=sg[i][:], op=ALU.mult), reads=[by, b_sg[i]], writes=[b_ygb[i]])
        for k in range(4):
            op("pe", lambda e: e.transpose(ptr[i][:, k, :], ygb[i][:, k * 128:(k + 1) * 128], ident_bf[:]),
               reads=[b_ygb[i], b_ident], writes=[b_ptr[i]])
        op("act", lambda e: e.activation(out=ygT[i][:], in_=ptr[i][:, 0:4, :], func=AF.Copy), reads=[b_ptr[i]], writes=[b_ygT[i]])
        op("pe", lambda e: e.matmul(psg[i][:], lhsT=ones_bf[0:1, 0:128], rhs=bglu_bf[0:1, :], start=True, stop=False),
           reads=[b_ones_bf, b_bglu], writes=[b_psg[i]])
        for k in range(4):
            op("pe", lambda e: e.matmul(psg[i][:], lhsT=ygT[i][:, k, :], rhs=Wglu[:, k, :], start=False, stop=(k == 3)),
               reads=[b_ygT[i], b_Wglu], writes=[b_psg[i]])

    def c1_b(t):
        x_, bx = _rot(xt, t), _rot(b_xt, t)
        i = t % 2
        op("act", lambda e: e.activation(out=sig[i][:], in_=psg[i][:], func=AF.Sigmoid), reads=[b_psg[i]], writes=[b_sig[i]])
        op("dve", lambda e: e.tensor_tensor(out=S[i][:], in0=yg[i][:], in1=sig[i][:], op=ALU.mult), reads=[b_yg[i], b_sig[i]], writes=[b_S[i]])
        op("act", lambda e: e.activation(out=junk[:], in_=S[i][:], func=AF.Square, accum_out=ss[i][:, 1:2]),
           reads=[b_S[i]], writes=[b_junk, b_ss[i]])
        op("act", lambda e: e.activation(out=sq[i][:], in_=ss[i][:], func=AF.Sqrt, scale=1.0 / 512.0, bias=EPS),
           reads=[b_ss[i]], writes=[b_sq[i]])
        op("dve", lambda e: e.reciprocal(out=rstd[i][:], in_=sq[i][:]), reads=[b_sq[i]], writes=[b_rstd[i]])
        op("dve", lambda e: e.scalar_tensor_tensor(out=mixh[i][:, 0:512], in0=A[i][:].rearrange("p h d -> p (h d)"),
                                                    scalar=rstd[i][:, 0:1], in1=Gcat[:, 0:512], op0=ALU.mult, op1=ALU.mult),
           reads=[b_A[i], b_rstd[i], b_Gcat], writes=[b_mixh[i]])
        op("dve", lambda e: e.scalar_tensor_tensor(out=mixh[i][:, 512:1024], in0=S[i][:], scalar=rstd[i][:, 1:2],
                                                    in1=Gcat[:, 512:1024], op0=ALU.mult, op1=ALU.mult),
           reads=[b_S[i], b_rstd[i], b_Gcat], writes=[b_mixh[i]])
        for k in range(8):
            op("pe", lambda e: e.transpose(pmx[:, k, :], mixh[i][:, k * 128:(k + 1) * 128], ident_bf[:]),
               reads=[b_mixh[i], b_ident], writes=[b_pmx])
        op("act", lambda e: e.activation(out=mixT[i][:], in_=pmx[:], func=AF.Copy), reads=[b_pmx], writes=[b_mixT[i]])
        for hf in range(2):
            for k in range(8):
                op("pe", lambda e: e.matmul(pso[hf][:], lhsT=mixT[i][:, k, :], rhs=Wout[:, k, hf * 512:(hf + 1) * 512],
                                            start=(k == 0), stop=(k == 7)),
                   reads=[b_mixT[i], b_Wout], writes=[b_pso[hf]])
            op("dve", lambda e: e.tensor_tensor(out=tt[i][:, hf * 512:(hf + 1) * 512], in0=pso[hf][:],
                                                in1=G1[:, hf * 512:(hf + 1) * 512], op=ALU.mult),
               reads=[b_pso[hf], b_G1], writes=[b_tt[i]])
        op("pool", lambda e: e.tensor_tensor(out=x1t[i][:], in0=tt[i][:], in1=x_[:], op=ALU.add), reads=[b_tt[i], bx], writes=[b_x1t[i]])
        dma(lambda e: e.dma_start(out=x1s[t * 128:(t + 1) * 128, :], in_=x1t[i][:]), reads=[b_x1t[i]], writes=[b_x1s[t]])

    c1_load(0)
    c1_load(1)
    c1_a(0)
    for t in range(16):
        if t + 2 < 16:
            c1_load(t + 2)
        if t + 1 < 16:
            c1_a(t + 1)
        c1_b(t)
    P.pop_scope()

    P.push_scope()
    W2 = P.sb("W2", [128, 32, 1024], BF16); b_W2 = [P.buf() for _ in range(8)]
    stg2 = [P.sb(f"stgc2_{i}", [128, 2048], F32) for i in range(2)]; b_stg2 = [P.buf() for _ in range(2)]
    scount = 0
    for i in range(16):
        sg_, bsg_ = _rot(stg2, scount), _rot(b_stg2, scount)
        scount += 1
        dma(lambda e: e.dma_start(out=sg_[:].rearrange("p (j n) -> p j n", j=2),
                                  in_=wfc2[256 * i:256 * (i + 1), :].rearrange("(j p) n -> p j n", p=128)), writes=[bsg_])
        op("dve", lambda e: e.tensor_copy(out=W2[:, 2 * i:2 * i + 2, :].rearrange("p j n -> p (j n)"), in_=sg_[:]),
           reads=[bsg_], writes=[b_W2[i // 2]])
    W1p = [P.sb(f"W1p{i}", [128, 8, 256], BF16) for i in range(2)]; b_W1p = [P.buf() for _ in range(2)]
    hT = P.sb("hT", [128, 32, 512], BF16); b_hT = [P.buf() for _ in range(32)]
    h2T = P.sb("h2T", [128, 8, 512], BF16); b_h2T = P.buf()
    x1g = P.sb("x1g", [128, 4, 1024], F32); b_x1g = [P.buf() for _ in range(4)]
    h2 = [P.sb(f"h2_{i}", [128, 1024], BF16) for i in range(2)]; b_h2 = [P.buf() for _ in range(2)]
    junk2 = P.sb("junk2", [128, 1024], BF16); b_junk2 = P.buf()
    ssc = P.sb("ssc", [128, 2], F32); b_ssc = P.buf()
    sqc = P.sb("sqc", [128, 2], F32); b_sqc = P.buf()
    rc = P.sb("rc", [128, 2], F32); b_rc = P.buf()
    ssp = [P.sb(f"ssp{i}", [128, 1], F32) for i in range(2)]; b_ssp = [P.buf() for _ in range(2)]
    sqp = [P.sb(f"sqp{i}", [128, 1], F32) for i in range(2)]; b_sqp = [P.buf() for _ in range(2)]
    rcp = [P.sb(f"rcp{i}", [128, 1], F32) for i in range(2)]; b_rcp = [P.buf() for _ in range(2)]
    b1T = P.sb("b1T", [128, 32], F32); b_b1T = P.buf()
    rl_t = [P.sb(f"rl_t{i}", [128, 512], BF16) for i in range(2)]; b_rl_t = [P.buf() for _ in range(2)]
    t2 = P.sb("t2", [128, 1024], F32); b_t2 = P.buf()
    x2t = t2; b_x2t = b_t2
    ot = [P.sb(f"ot{i}", [128, 1024], F32) for i in range(1)]; b_ot = [P.buf() for _ in range(1)]
    pht = [P.ps(f"pht{i}", [128, 8, 128], BF16) for i in range(2)]; b_pht = [P.pbuf() for _ in range(2)]
    psb = P.ps("psb", [128, 512], F32); b_psb = P.pbuf()
    psf = [P.ps(f"psf{i}", [128, 512], F32) for i in range(2)]; b_psf = [P.pbuf() for _ in range(2)]
    pso2 = [P.ps(f"pso2{i}", [128, 512], F32) for i in range(2)]; b_pso2 = [P.pbuf() for _ in range(2)]
    pcount = 0
    ocount = 0
    for g in range(4):
        for i in range(4):
            t = g * 4 + i
            q_ = i % 2
            dma(lambda e: e.dma_start(out=x1g[:, i, :], in_=x1s[t * 128:(t + 1) * 128, :]), reads=[b_x1s[t]], writes=[b_x1g[i]])
            op("act", lambda e: e.activation(out=junk2[:], in_=x1g[:, i, :], func=AF.Square, accum_out=ssp[q_][:, 0:1]),
               reads=[b_x1g[i]], writes=[b_junk2, b_ssp[q_]])
            op("act", lambda e: e.activation(out=sqp[q_][:], in_=ssp[q_][:], func=AF.Sqrt, scale=1.0 / 1024.0, bias=EPS),
               reads=[b_ssp[q_]], writes=[b_sqp[q_]])
            op("dve", lambda e: e.reciprocal(out=rcp[q_][:], in_=sqp[q_][:]), reads=[b_sqp[q_]], writes=[b_rcp[q_]])
            op("dve", lambda e: e.scalar_tensor_tensor(out=h2[q_][:], in0=x1g[:, i, :], scalar=rcp[q_][:, 0:1], in1=S2b[:],
                                                        op0=ALU.mult, op1=ALU.mult),
               reads=[b_x1g[i], b_rcp[q_], b_S2b], writes=[b_h2[q_]])
            for k in range(8):
                op("pe", lambda e: e.transpose(pht[q_][:, k, :], h2[q_][:, k * 128:(k + 1) * 128], ident_bf[:]),
                   reads=[b_h2[q_], b_ident], writes=[b_pht[q_]])
            op("act", lambda e: e.activation(out=h2T[:, :, i * 128:(i + 1) * 128], in_=pht[q_][:], func=AF.Copy),
               reads=[b_pht[q_]], writes=[b_h2T])
        for pc in range(16):
            w1, bw1 = _rot(W1p, pcount), _rot(b_W1p, pcount)
            pcount += 1
            sg_, bsg_ = _rot(stg2, scount), _rot(b_stg2, scount)
            scount += 1
            dma(lambda e: e.dma_start(out=sg_[:].rearrange("p (k n) -> p k n", k=8),
                                      in_=wfc1[:, pc * 256:(pc + 1) * 256].rearrange("(k p) n -> p k n", p=128)), writes=[bsg_])
            op("dve", lambda e: e.tensor_copy(out=w1[:].rearrange("p k n -> p (k n)"), in_=sg_[:]), reads=[bsg_], writes=[bw1])
            for fc in range(2):
                j = pc * 2 + fc
                if g == 0:
                    for k in range(8):
                        op("pe", lambda e: e.matmul(psb[:, 0:2], lhsT=w1[:, k, fc * 128:(fc + 1) * 128], rhs=sh2T[:, 2 * k:2 * k + 2],
                                                    start=(k == 0), stop=(k == 7)),
                           reads=[b_sh2T, bw1], writes=[b_psb])
                    op("dve", lambda e: e.tensor_copy(out=b1T[:, j:j + 1], in_=psb[:, 0:1]), reads=[b_psb], writes=[b_b1T])
                pf, bpf = _rot(psf, j), _rot(b_psf, j)
                for k in range(8):
                    op("pe", lambda e: e.matmul(pf[:], lhsT=w1[:, k, fc * 128:(fc + 1) * 128], rhs=h2T[:, k, :],
                                                start=(k == 0), stop=(k == 7)),
                       reads=[bw1, b_h2T], writes=[bpf])
                r_, br_ = _rot(rl_t, j), _rot(b_rl_t, j)
                op("act", lambda e: e.activation(out=r_[:], in_=pf[:], func=AF.Relu, bias=b1T[:, j:j + 1]),
                   reads=[bpf, b_b1T], writes=[br_])
                op("pool", lambda e: e.tensor_tensor(out=hT[:, j, :], in0=r_[:], in1=r_[:], op=ALU.mult),
                   reads=[br_], writes=[b_hT[j]])
        for i in range(4):
            t = g * 4 + i
            for hf in range(2):
                po, bpo = _rot(pso2, ocount), _rot(b_pso2, ocount)
                ocount += 1
                for j in range(32):
                    op("pe", lambda e: e.matmul(po[:], lhsT=hT[:, j, i * 128:(i + 1) * 128],
                                                rhs=W2[:, j, hf * 512:(hf + 1) * 512], start=(j == 0), stop=(j == 31)),
                       reads=[b_hT[j], b_W2[j // 4]], writes=[bpo])
                op("dve", lambda e: e.tensor_tensor(out=t2[:, hf * 512:(hf + 1) * 512], in0=po[:],
                                                    in1=G2[:, hf * 512:(hf + 1) * 512], op=ALU.mult),
                   reads=[bpo, b_G2], writes=[b_t2])
            op("pool", lambda e: e.tensor_tensor(out=x2t[:], in0=t2[:], in1=x1g[:, i, :], op=ALU.add),
               reads=[b_t2, b_x1g[i]], writes=[b_x2t])
            op("act", lambda e: e.activation(out=junk2[:], in_=x2t[:], func=AF.Square, accum_out=ssc[:, 1:2]),
               reads=[b_x2t], writes=[b_junk2, b_ssc])
            op("act", lambda e: e.activation(out=sqc[:, 1:2], in_=ssc[:, 1:2], func=AF.Sqrt, scale=1.0 / 1024.0, bias=EPS),
               reads=[b_ssc], writes=[b_sqc])
            op("dve", lambda e: e.reciprocal(out=rc[:, 1:2], in_=sqc[:, 1:2]), reads=[b_sqc], writes=[b_rc])
            o_, bo_ = _rot(ot, t), _rot(b_ot, t)
            op("dve", lambda e: e.scalar_tensor_tensor(out=o_[:], in0=x2t[:], scalar=rc[:, 1:2], in1=Gf[:],
                                                        op0=ALU.mult, op1=ALU.mult),
               reads=[b_x2t, b_rc, b_Gf], writes=[bo_])
            dma(lambda e: e.dma_start(out=out[t * 128:(t + 1) * 128, :], in_=o_[:]), reads=[bo_])
    P.pop_scope()
    P.finish()
    return nc


def _ident():
    return np.eye(128, dtype=np.float32)


def phase_c_inputs(inp, b, qi, attn_full, y_full):
    T0 = qi * NT_C
    c = inp["c"][b]
    cT = np.ascontiguousarray(c.reshape(8, 128).T)
    cT2 = np.repeat(cT[:, :, None], 2, axis=2).reshape(128, 16)
    rows = np.concatenate([inp["g_attn_out"][0], inp["g_ssm_out"][0], inp["g_mlp"][0], inp["g_final"]])[None, :]
    return {
        "xc": np.ascontiguousarray(inp["x"][b, T0:T0 + NT_C]),
        "attn": np.ascontiguousarray(attn_full[b, T0:T0 + NT_C].reshape(NT_C, 520)),
        "yin": np.ascontiguousarray(y_full[b, T0:T0 + NT_C]),
        "cT2": np.ascontiguousarray(cT2),
        "wada_c": np.ascontiguousarray(inp["w_ada"][0][:, 2048:6144]),
        "bada_c": np.ascontiguousarray(inp["b_ada"][0][None, 2048:6144]),
        "rows_c": np.ascontiguousarray(rows.astype(np.float32)),
        "bglu": np.ascontiguousarray(inp["b_glu"][0][None, :]),
        "wglu": np.ascontiguousarray(inp["w_glu"][0]),
        "wout": np.ascontiguousarray(inp["w_out"][0]),
        "wfc1": np.ascontiguousarray(inp["w_fc1"][0]),
        "wfc2": np.ascontiguousarray(inp["w_fc2"][0]),
        "ident": _ident(),
    }


L_SEQ = 8192
NTILE = 64


def build_phase_ab(nc, with_ssm=True, dsts=None, dbg_tiles=NTILE, dbg_attn=True, dbg_stage=9):
    def din(name, shape, dt=F32):
        return nc.dram_tensor(name, list(shape), dt, kind="ExternalInput").ap()
    xb = din("xb", [L_SEQ, 1024])
    cT2 = din("cT2", [128, 16])
    wada = din("wada_a", [1024, 2048])
    bada = din("bada_a", [1, 2048])
    gmix = din("gmix", [1, 1024])
    win = din("win", [1024, 512])
    cosl = din("cosl", [128, 2048])
    sinl = din("sinl", [128, 2048])
    ind = din("ind", [32, L_SEQ])
    cmask = din("cmask", [128, 2048])
    ident = din("ident", [128, 128])
    if dsts is None:
        attn_o = nc.dram_tensor("attn_o", [130, L_SEQ], F32, kind="ExternalOutput").ap()
        y_o = nc.dram_tensor("y_o", [L_SEQ, 128], F32, kind="ExternalOutput").ap()
    else:
        attn_o, y_o = dsts["attn"], dsts["y"]

    P = Prog(nc)
    op, dma = P.op, P.dma

    ident_bf = P.sb("ident_bf", [128, 128], BF16); b_ident = P.buf()
    ident_f = P.sb("ident_f", [128, 128], F32); b_identf = P.buf()
    ones_bf = P.sb("ones_bf", [1, 512], BF16); b_ones_bf = P.buf()
    zeros_bf = P.sb("zeros_bf", [1, 512], BF16); b_zeros_bf = P.buf()
    ones_f = P.sb("ones_f", [1, 128], F32); b_ones_f = P.buf()
    U_tok = P.sb("U_tok", [128, 8, 8, 8, 16], BF16); b_U = [P.buf() for _ in range(8)]
    dma(lambda e: e.dma_start(out=ident_f[:], in_=ident[:, :]), writes=[b_identf])
    op("dve", lambda e: e.tensor_copy(out=ident_bf[:], in_=ident_f[:]), reads=[b_identf], writes=[b_ident])
    op("dve", lambda e: e.memset(ones_f[:], 1.0), writes=[b_ones_f])
    op("dve", lambda e: e.memset(ones_bf[:], 1.0), writes=[b_ones_bf])
    op("dve", lambda e: e.memset(zeros_bf[:], 0.0), writes=[b_zeros_bf])

    P.push_scope()
    QaT = P.sb("QaT", [128, 2, L_SEQ], BF16); b_QaT = [P.buf() for _ in range(NTILE)]
    KaT = P.sb("KaT", [128, 2, L_SEQ], BF16); b_KaT = [P.buf() for _ in range(NTILE)]; b_Kind = P.buf()
    Vaug = P.sb("Vaug", [128, NTILE, 2, 65], BF16); b_V = [P.buf() for _ in range(NTILE)]; b_Vones = P.buf()
    P.push_scope()
    stgI = [P.sb(f"stgI{i}", [128, 2048], F32) for i in range(2)]; b_stgI = [P.buf() for _ in range(2)]
    for cch in range(4):
        sg_, bsg_ = _rot(stgI, cch), _rot(b_stgI, cch)
        dma(lambda e: e.dma_start(out=sg_[64:96, :], in_=ind[:, cch * 2048:(cch + 1) * 2048]), writes=[bsg_])
        for h in range(2):
            op("pool" if h else "dve", lambda e: e.tensor_copy(out=KaT[64:96, h, cch * 2048:(cch + 1) * 2048], in_=sg_[64:96, :]),
               reads=[bsg_], writes=[b_Kind])
    P.pop_scope()
    op("pool", lambda e: e.memset(Vaug[:, :, :, 64:65], 1.0), writes=[b_Vones])

    P.push_scope()
    S1b = P.sb("S1b", [128, 1024], F32); b_S1b = P.buf()
    sh1T = P.sb("sh1T", [128, 16], BF16); b_sh1T = P.buf()
    Win = P.sb("Win", [128, 8, 512], BF16); b_Win = P.buf()
    bin_bf = P.sb("bin_bf", [1, 512], BF16); b_bin = P.buf()
    P.push_scope()
    stgW = [P.sb(f"stgW{i}", [128, 2048], F32) for i in range(2)]; b_stgW = [P.buf() for _ in range(2)]
    for kk in range(2):
        sg_, bsg_ = stgW[kk], b_stgW[kk]
        dma(lambda e: e.dma_start(out=sg_[:].rearrange("p (k n) -> p k n", k=4),
                                  in_=win[kk * 512:(kk + 1) * 512, :].rearrange("(k p) n -> p k n", p=128)), writes=[bsg_])
        op("pool" if kk else "dve", lambda e: e.tensor_copy(out=Win[:, 4 * kk:4 * kk + 4, :].rearrange("p k n -> p (k n)"), in_=sg_[:]),
           reads=[bsg_], writes=[b_Win])
    P.pop_scope()

    P.push_scope()
    ct = P.sb("ct", [128, 16], F32); b_ct = P.buf()
    sct = P.sb("sct", [128, 16], F32); b_sct = P.buf()
    sgc = P.sb("sgc", [128, 16], F32)
    modrow = P.sb("modrow", [1, 2048], F32); b_modrow = P.buf()
    badas = P.sb("badas", [1, 2048], F32); b_badas = P.buf()
    gmixs = P.sb("gmixs", [1, 1024], F32); b_gmixs = P.buf()
    s1row = P.sb("s1row", [1, 1024], F32); b_s1row = P.buf()
    pieces = [P.sb(f"wpiece{i}", [128, 8, 512], F32) for i in range(2)]
    b_pieces = [P.buf() for _ in range(2)]
    ps_row = [P.ps(f"ps_row{i}", [128, 512], F32) for i in range(2)]
    b_ps_row = [P.pbuf() for _ in range(2)]
    dma(lambda e: e.dma_start(out=ct[:], in_=cT2[:, :]), writes=[b_ct])
    dma(lambda e: e.dma_start(out=badas[:], in_=bada[:, :]), writes=[b_badas])
    dma(lambda e: e.dma_start(out=gmixs[:], in_=gmix[:, :]), writes=[b_gmixs])
    op("act", lambda e: e.activation(out=sgc[:], in_=ct[:], func=AF.Sigmoid), reads=[b_ct], writes=[b_sct])
    op("dve", lambda e: e.tensor_tensor(out=sct[:], in0=sgc[:], in1=ct[:], op=ALU.mult), reads=[b_ct, b_sct], writes=[b_sct])
    for j in range(4):
        pc, bpc = _rot(pieces, j), _rot(b_pieces, j)
        pr, bpr = _rot(ps_row, j), _rot(b_ps_row, j)
        dma(lambda e: e.dma_start(out=pc[:], in_=wada[:, j * 512:(j + 1) * 512].rearrange("(k p) n -> p k n", p=128)),
            writes=[bpc])
        for k in range(8):
            op("pe", lambda e: e.matmul(pr[0:1, :], lhsT=sct[:, 2 * k:2 * k + 1], rhs=pc[:, k, :],
                                        start=(k == 0), stop=(k == 7)),
               reads=[b_sct, bpc], writes=[bpr])
        op("dve", lambda e: e.tensor_tensor(out=modrow[0:1, j * 512:(j + 1) * 512], in0=pr[0:1, :],
                                            in1=badas[0:1, j * 512:(j + 1) * 512], op=ALU.add),
           reads=[bpr, b_badas], writes=[b_modrow])
    op("dve", lambda e: e.scalar_tensor_tensor(out=s1row[:], in0=modrow[0:1, 1024:2048], scalar=1.0,
                                                in1=gmixs[0:1, :], op0=ALU.add, op1=ALU.mult),
       reads=[b_modrow, b_gmixs], writes=[b_s1row])
    for hf in range(2):
        pr, bpr = _rot(ps_row, hf), _rot(b_ps_row, hf)
        op("pe", lambda e: e.matmul(pr[:, :], lhsT=ones_f[0:1, :], rhs=s1row[0:1, hf * 512:(hf + 1) * 512],
                                    start=True, stop=True), reads=[b_ones_f, b_s1row], writes=[bpr])
        op("act", lambda e: e.activation(out=S1b[:, hf * 512:(hf + 1) * 512], in_=pr[:, :], func=AF.Copy),
           reads=[bpr], writes=[b_S1b])
    pr, bpr = ps_row[0], b_ps_row[0]
    for k in range(8):
        op("pe", lambda e: e.matmul(pr[:, 2 * k:2 * k + 2], lhsT=modrow[0:1, k * 128:(k + 1) * 128],
                                    rhs=ones_f[0:1, 0:2], start=True, stop=True),
           reads=[b_ones_f, b_modrow], writes=[bpr])
    op("dve", lambda e: e.tensor_copy(out=sh1T[:], in_=pr[:, 0:16]), reads=[bpr], writes=[b_sh1T])
    pr, bpr = ps_row[1], b_ps_row[1]
    for k in range(8):
        op("pe", lambda e: e.matmul(pr[0:1, :], lhsT=sh1T[:, 2 * k:2 * k + 1], rhs=Win[:, k, :],
                                    start=(k == 0), stop=(k == 7)), reads=[b_sh1T, b_Win], writes=[bpr])
    op("act", lambda e: e.activation(out=bin_bf[:], in_=pr[0:1, :], func=AF.Copy), reads=[bpr], writes=[b_bin])
    P.pop_scope()

    xt = [P.sb(f"xt{i}", [128, 1024], F32) for i in range(3)]; b_xt = [P.buf() for _ in range(3)]
    hb = [P.sb(f"hb{i}", [128, 1024], BF16) for i in range(2)]; b_hb = [P.buf() for _ in range(2)]
    junk = P.sb("junk", [128, 1024], BF16); b_junk = P.buf()
    ssx = [P.sb(f"ssx{i}", [128, 1], F32) for i in range(2)]; b_ssx = [P.buf() for _ in range(2)]
    sqx = [P.sb(f"sqx{i}", [128, 1], F32) for i in range(2)]; b_sqx = [P.buf() for _ in range(2)]
    rx = [P.sb(f"rx{i}", [128, 1], F32) for i in range(2)]; b_rx = [P.buf() for _ in range(2)]
    hTs = P.sb("hTs", [128, 8, 1024], BF16); b_hTs = [P.buf() for _ in range(8)]
    cs = [P.sb(f"cs{i}", [128, 8, 32], F32) for i in range(2)]; b_cs = [P.buf() for _ in range(2)]
    sn = [P.sb(f"sn{i}", [128, 8, 32], F32) for i in range(2)]; b_sn = [P.buf() for _ in range(2)]
    qkr = P.sb("qkr", [128, 4, 2, 32], F32); b_qkr = P.buf()
    r1 = P.sb("r1", [128, 4, 32], F32); b_r1 = P.buf()
    r2 = P.sb("r2", [128, 4, 32], F32); b_r2 = P.buf()
    r3 = P.sb("r3", [128, 4, 32], F32); b_r3 = P.buf()
    r4 = P.sb("r4", [128, 4, 32], F32); b_r4 = P.buf()
    ksum = P.sb("ksum", [64, 2, 2], F32); b_ksum = [P.buf(), P.buf()]
    kmT = P.sb("kmT", [64, 2, 32], F32); b_kmT = P.buf()
    qTt = P.sb("qTt", [64, 2, 128], F32); b_qTt = P.buf()
    Gt = P.sb("Gt", [128, 2, 32], F32); b_Gt = P.buf()
    mx8 = P.sb("mx8", [128, 2, 8], F32); b_mx8 = P.buf()
    nm = P.sb("nm", [128, 2, 32], F32); b_nm = P.buf()
    Qtok = P.sb("Qtok", [128, 2, 96], BF16); b_Qtok = P.buf()
    pxt = [P.ps(f"pxt{i}", [128, 8, 128], BF16) for i in range(2)]; b_pxt = [P.pbuf() for _ in range(2)]
    pqkv = [P.ps(f"pqkv{i}", [128, 512], F32) for i in range(2)]; b_pqkv = [P.pbuf() for _ in range(2)]
    pT = P.ps("pT", [128, 4, 128], F32); b_pT = P.pbuf()
    pgq = P.ps("pgq", [128, 512], F32); b_pgq = P.pbuf()
    pu = P.ps("pu", [128, 8, 128], F32); b_pu = P.pbuf()
    pg = pgq
    pqa = pgq[:, 256:512].bitcast(BF16).rearrange("p (h n) -> p h n", h=4)
    b_pg = b_pgq
    b_pqa = b_pgq
    op("dve", lambda e: e.memset(Gt[:], -1.0e30), writes=[b_Gt])
    op("dve", lambda e: e.memset(kmT[:], 0.0), writes=[b_kmT])

    def load_x(t):
        x_, bx = _rot(xt, t), _rot(b_xt, t)
        dma(lambda e: e.dma_start(out=x_[:], in_=xb[t * 128:(t + 1) * 128, :]), writes=[bx])

    def stage_a(t):
        S_, ti = t // 8, t % 8
        x_, bx = _rot(xt, t), _rot(b_xt, t)
        hb_, bhb = _rot(hb, t), _rot(b_hb, t)
        ss_, bss = _rot(ssx, t), _rot(b_ssx, t)
        sq_, bsq = _rot(sqx, t), _rot(b_sqx, t)
        rx_, brx = _rot(rx, t), _rot(b_rx, t)
        px_, bpx = _rot(pxt, t), _rot(b_pxt, t)
        pq_, bpq = _rot(pqkv, t), _rot(b_pqkv, t)
        if ti == 0:
            c_, bc_ = _rot(cs, S_), _rot(b_cs, S_)
            s_, bs_ = _rot(sn, S_), _rot(b_sn, S_)
            dma(lambda e: e.dma_start(out=c_[:].rearrange("p i d -> p (i d)"), in_=cosl[:, S_ * 256:(S_ + 1) * 256]), writes=[bc_])
            dma(lambda e: e.dma_start(out=s_[:].rearrange("p i d -> p (i d)"), in_=sinl[:, S_ * 256:(S_ + 1) * 256]), writes=[bs_])
        op("act", lambda e: e.activation(out=junk[:], in_=x_[:], func=AF.Square, accum_out=ss_[:, 0:1]),
           reads=[bx], writes=[b_junk, bss])
        op("act", lambda e: e.activation(out=sq_[:], in_=ss_[:], func=AF.Sqrt, scale=1.0 / 1024.0, bias=EPS),
           reads=[bss], writes=[bsq])
        op("dve", lambda e: e.reciprocal(out=rx_[:], in_=sq_[:]), reads=[bsq], writes=[brx])
        op("dve", lambda e: e.scalar_tensor_tensor(out=hb_[:], in0=x_[:], scalar=rx_[:, 0:1], in1=S1b[:],
                                                    op0=ALU.mult, op1=ALU.mult),
           reads=[bx, brx, b_S1b], writes=[bhb])
        for k in range(8):
            op("pe", lambda e: e.transpose(px_[:, k, :], hb_[:, k * 128:(k + 1) * 128], ident_bf[:]),
               reads=[bhb, b_ident], writes=[bpx])
        op("act", lambda e: e.activation(out=hTs[:, :, ti * 128:(ti + 1) * 128], in_=px_[:], func=AF.Copy),
           reads=[bpx], writes=[b_hTs[ti]])
        op("pe", lambda e: e.matmul(pq_[:, 0:384], lhsT=ones_bf[0:1, 0:128], rhs=bin_bf[0:1, 0:384], start=True, stop=False),
           reads=[b_ones_bf, b_bin], writes=[bpq])
        for k in range(8):
            op("pe", lambda e: e.matmul(pq_[:, 0:384], lhsT=hTs[:, k, ti * 128:(ti + 1) * 128], rhs=Win[:, k, 0:384],
                                        start=False, stop=(k == 7)),
               reads=[b_hTs[ti], b_Win], writes=[bpq])

    def u_proj(S_):
        for s in range(8):
            op("pe", lambda e: e.matmul(pu[:, s, :], lhsT=ones_bf[0:1, 0:128], rhs=bin_bf[0:1, 384:512], start=True, stop=False),
               reads=[b_ones_bf, b_bin], writes=[b_pu])
            for k in range(8):
                lhs = hTs[:, k, :].rearrange("p (c s) -> p s c", s=8)[:, s, :]
                op("pe", lambda e: e.matmul(pu[:, s, :], lhsT=lhs, rhs=Win[:, k, 384:512], start=False, stop=(k == 7)),
                   reads=b_hTs + [b_Win], writes=[b_pu])
        op("act", lambda e: e.activation(out=U_tok[:, S_].rearrange("p g s m -> p s g m"),
                                         in_=pu[:].rearrange("p s (g m) -> p s g m", g=8), func=AF.Copy),
           reads=[b_pu], writes=[b_U[S_]])

    def stage_b(t):
        S_, ti = t // 8, t % 8
        j = t // 2
        par = t % 2
        tok = slice(t * 128, (t + 1) * 128)
        c_, bc_ = _rot(cs, S_), _rot(b_cs, S_)
        s_, bs_ = _rot(sn, S_), _rot(b_sn, S_)
        pq_, bpq = _rot(pqkv, t), _rot(b_pqkv, t)
        op("act", lambda e: e.activation(out=Vaug[:, t, :, 0:64], in_=pq_[:, 256:384].rearrange("p (h d) -> p h d", h=2),
                                         func=AF.Copy), reads=[bpq], writes=[b_V[t]])
        pq = pq_[:, 0:256].rearrange("p (f two d) -> p f two d", f=4, two=2)
        cb = c_[:, ti, :].unsqueeze(1).to_broadcast([128, 4, 32])
        sb_ = s_[:, ti, :].unsqueeze(1).to_broadcast([128, 4, 32])
        op("dve", lambda e: e.tensor_tensor(out=r1[:], in0=pq[:, :, 0, :], in1=cb, op=ALU.mult), reads=[bpq, bc_], writes=[b_r1])
        op("dve", lambda e: e.tensor_tensor(out=r2[:], in0=pq[:, :, 1, :], in1=sb_, op=ALU.mult), reads=[bpq, bs_], writes=[b_r2])
        op("dve", lambda e: e.tensor_tensor(out=r3[:], in0=pq[:, :, 0, :], in1=sb_, op=ALU.mult), reads=[bpq, bs_], writes=[b_r3])
        op("dve", lambda e: e.tensor_tensor(out=r4[:], in0=pq[:, :, 1, :], in1=cb, op=ALU.mult), reads=[bpq, bc_], writes=[b_r4])
        op("pool", lambda e: e.tensor_tensor(out=qkr[:, :, 0, :], in0=r1[:], in1=r2[:], op=ALU.subtract),
           reads=[b_r1, b_r2], writes=[b_qkr])
        op("pool", lambda e: e.tensor_tensor(out=qkr[:, :, 1, :], in0=r3[:], in1=r4[:], op=ALU.add),
           reads=[b_r3, b_r4], writes=[b_qkr])
        qkf = qkr[:].rearrange("p f two d -> p (f two d)")
        for h in range(2):
            op("pe", lambda e: e.transpose(pT[0:64, h, :], qkf[:, 128 + h * 64:128 + (h + 1) * 64], ident_f[:]),
               reads=[b_qkr, b_identf], writes=[b_pT])
        if j >= 4:
            for h in range(2):
                op("pe", lambda e: e.transpose(pT[0:64, 2 + h, :], qkf[:, h * 64:(h + 1) * 64], ident_f[:]),
                   reads=[b_qkr, b_identf], writes=[b_pT])
        op("act", lambda e: e.activation(out=KaT[0:64, :, tok], in_=pT[0:64, 0:2, :], func=AF.Copy),
           reads=[b_pT], writes=[b_KaT[t]])
        op("dve", lambda e: e.tensor_reduce(out=ksum[:, par, :], in_=pT[0:64, 0:2, :], axis=mybir.AxisListType.X, op=ALU.add),
           reads=[b_pT], writes=[b_ksum[par]])
        if j >= 4:
            op("act", lambda e: e.activation(out=qTt[:], in_=pT[0:64, 2:4, :], func=AF.Copy), reads=[b_pT], writes=[b_qTt])
            for h in range(2):
                op("pe", lambda e: e.matmul(pg[:, h * 32:(h + 1) * 32], lhsT=qTt[:, h, :], rhs=kmT[:, h, :], start=True, stop=True),
                   reads=[b_qTt, b_kmT], writes=[b_pg])
            op("dve", lambda e: e.tensor_copy(out=Gt[:, :, 0:j], in_=pg[:, 0:64].rearrange("p (h n) -> p h n", h=2)[:, :, 0:j]),
               reads=[b_pg], writes=[b_Gt])
            for h in range(2):
                op("dve", lambda e: e.max(out=mx8[:, h, :], in_=Gt[:, h, :]), reads=[b_Gt], writes=[b_mx8])
            for h in range(2):
                op("dve", lambda e: e.tensor_scalar(out=nm[:, h, :], in0=Gt[:, h, :], scalar1=mx8[:, h, 2:3], scalar2=1.0,
                                                    op0=ALU.is_ge, op1=ALU.subtract), reads=[b_Gt, b_mx8], writes=[b_nm])
            op("dve", lambda e: e.memset(nm[:, :, j:j + 1], 0.0), writes=[b_nm])
            op("dve", lambda e: e.tensor_scalar(out=Qtok[:, :, 64:96], in0=nm[:], scalar1=-NEGBIG, scalar2=None, op0=ALU.mult),
               reads=[b_nm], writes=[b_Qtok])
        else:
            op("dve", lambda e: e.memset(Qtok[:, :, 64:96], NEGBIG), writes=[b_Qtok])
            op("dve", lambda e: e.memset(Qtok[:, :, 64:64 + j + 1], 0.0), writes=[b_Qtok])
        op("pool", lambda e: e.tensor_scalar(out=Qtok[:, :, 0:64], in0=qkf[:, 0:128].rearrange("p (h d) -> p h d", h=2),
                                             scalar1=0.125, scalar2=None, op0=ALU.mult),
           reads=[b_qkr], writes=[b_Qtok])
        for h in range(2):
            op("pe", lambda e: e.transpose(pqa[0:96, h, :], Qtok[:, h, :], ident_bf[:]), reads=[b_Qtok, b_ident], writes=[b_pqa])
        op("act", lambda e: e.activation(out=QaT[0:96, :, tok], in_=pqa[0:96, 0:2, :], func=AF.Copy),
           reads=[b_pqa], writes=[b_QaT[t]])
        if par == 1:
            op("dve", lambda e: e.tensor_tensor(out=kmT[:, :, j], in0=ksum[:, 0, :], in1=ksum[:, 1, :], op=ALU.add),
               reads=[b_ksum[0], b_ksum[1]], writes=[b_kmT])

    if dbg_tiles > 0:
        load_x(0)
        load_x(1)
        stage_a(0)
    for t in range(dbg_tiles):
        if t + 2 < NTILE:
            load_x(t + 2)
        if t % 8 == 7 and with_ssm:
            u_proj(t // 8)
        if t + 1 < dbg_tiles:
            stage_a(t + 1)
        stage_b(t)
    P.pop_scope()

    P.push_scope()
    cm = P.sb("cm", [128, 4, 512], BF16); b_cm = P.buf()
    P.push_scope()
    stgM = P.sb("stgM", [128, 2048], F32); b_stgM = P.buf()
    dma(lambda e: e.dma_start(out=stgM[:], in_=cmask[:, :]), writes=[b_stgM])
    op("dve", lambda e: e.tensor_copy(out=cm[:].rearrange("p a c -> p (a c)"), in_=stgM[:]), reads=[b_stgM], writes=[b_cm])
    P.pop_scope()
    Pt = [P.sb(f"Pt{i}", [128, 512], BF16) for i in range(3)]; b_Pt = [P.buf() for _ in range(3)]
    Osb = [P.sb(f"Osb{i}", [65, 512], F32) for i in range(2)]; b_Osb = [P.buf() for _ in range(2)]
    pS = [P.ps(f"pS{i}", [128, 512], F32) for i in range(3)]; b_pS = [P.pbuf() for _ in range(3)]
    pO = [P.ps(f"pO{i}", [128, 512], F32) for i in range(2)]; b_pO = [P.pbuf() for _ in range(2)]
    cnt = 0
    gi = 0
    for h in range(2 if dbg_attn else 0):
        for G in range(16 if dbg_attn is True else dbg_attn):
            po, bpo = _rot(pO, gi), _rot(b_pO, gi)
            ob, bob = _rot(Osb, gi), _rot(b_Osb, gi)
            gi += 1
            nkt = 4 * G + 4
            qs = slice(G * 512, (G + 1) * 512)
            qbufs = b_QaT[4 * G:4 * G + 4]

            def s_mm(kt, c):
                ps_, bps = _rot(pS, c), _rot(b_pS, c)
                op("pe", lambda e: e.matmul(ps_[:], lhsT=KaT[0:96, h, kt * 128:(kt + 1) * 128], rhs=QaT[0:96, h, qs],
                                            start=True, stop=True),
                   reads=[b_KaT[kt], b_Kind] + qbufs, writes=[bps])
            s_mm(0, cnt)
            for kt in range(nkt):
                c = cnt + kt
                if kt + 1 < nkt:
                    s_mm(kt + 1, c + 1)
                ps_, bps = _rot(pS, c), _rot(b_pS, c)
                p_, bp_ = _rot(Pt, c), _rot(b_Pt, c)
                op("act", lambda e: e.activation(out=p_[:], in_=ps_[:], func=AF.Exp), reads=[bps], writes=[bp_])
                a = kt - 4 * G
                c0 = 0
                if a >= 0:
                    op("pool", lambda e: e.tensor_tensor(out=p_[:], in0=p_[:], in1=cm[:, a, :], op=ALU.mult),
                       reads=[bp_, b_cm], writes=[bp_])
                    c0 = a * 128
                op("pe", lambda e: e.matmul(po[0:65, c0:512], lhsT=Vaug[:, kt, h, :], rhs=p_[:, c0:512],
                                            start=(kt == 0), stop=(kt == nkt - 1)),
                   reads=[bp_, b_V[kt], b_Vones], writes=[bpo])
            cnt += nkt
            op("dve", lambda e: e.tensor_copy(out=ob[:], in_=po[0:65, :]), reads=[bpo], writes=[bob])
            dma(lambda e: e.dma_start(out=attn_o[h * 65:(h + 1) * 65, G * 512:(G + 1) * 512], in_=ob[:]), reads=[bob])
    P.pop_scope()
    P.pop_scope()

    if with_ssm:
        build_ssm(nc, P, U_tok, b_U, y_o, ident_bf, b_ident, ident_f, b_identf)
    P.finish()
    return nc


def rope_tables():
    half = 32
    inv = (10000.0 ** (-np.arange(half, dtype=np.float32) / half)).astype(np.float32)
    ang = np.arange(L_SEQ, dtype=np.float32)[:, None] * inv[None, :]
    cos, sin = np.cos(ang).astype(np.float32), np.sin(ang).astype(np.float32)
    cl = np.ascontiguousarray(cos.reshape(NTILE, 128, half).transpose(1, 0, 2).reshape(128, NTILE * half))
    sl = np.ascontiguousarray(sin.reshape(NTILE, 128, half).transpose(1, 0, 2).reshape(128, NTILE * half))
    return cl, sl


def const_tables():
    ind = np.zeros((32, L_SEQ), np.float32)
    for j in range(32):
        ind[j, j * 256:(j + 1) * 256] = 1.0
    cm = np.zeros((128, 4, 512), np.float32)
    kk = np.arange(128)[:, None]
    qq = np.arange(128)[None, :]
    tri = (kk <= qq).astype(np.float32)
    for a in range(4):
        for bq in range(4):
            if bq > a:
                cm[:, a, bq * 128:(bq + 1) * 128] = 1.0
            elif bq == a:
                cm[:, a, bq * 128:(bq + 1) * 128] = tri
    return ind, cm.reshape(128, 2048)


def phase_ab_inputs(inp, b, r):
    c = inp["c"][b]
    cT = np.ascontiguousarray(c.reshape(8, 128).T)
    cT2 = np.repeat(cT[:, :, None], 2, axis=2).reshape(128, 16)
    w = inp["w_in"][0]
    cols = np.concatenate([np.arange(128 * r, 128 * r + 128), 512 + np.arange(128 * r, 128 * r + 128),
                           1024 + np.arange(128 * r, 128 * r + 128), 1536 + np.arange(128 * r, 128 * r + 128)])
    cl, sl = rope_tables()
    ind, cm = const_tables()
    d = {
        "xb": np.ascontiguousarray(inp["x"][b]),
        "cT2": np.ascontiguousarray(cT2),
        "wada_a": np.ascontiguousarray(inp["w_ada"][0][:, 0:2048]),
        "bada_a": np.ascontiguousarray(inp["b_ada"][0][None, 0:2048]),
        "gmix": np.ascontiguousarray(inp["g_mix"][0][None, :]),
        "win": np.ascontiguousarray(w[:, cols]),
        "cosl": cl, "sinl": sl, "ind": ind, "cmask": cm, "ident": _ident(),
    }
    d.update(ssm_inputs(inp, r))
    return d


KV_F, KV_G, KV_O, KV_E, KV_A, KV_H = 0, 8, 16, 24, 32, 42
NKV = 43
TWO_PI = 2.0 * math.pi
CW1 = 6.28125
CW2 = TWO_PI - CW1


def ssm_kvec():
    kv = np.zeros(NKV, np.float32)
    for s in range(8):
        kv[KV_F + s] = -s
        kv[KV_G + s] = s
        kv[KV_O + s] = s + 1
        kv[KV_E + s] = 7 - s
    for i in range(10):
        kv[KV_A + i] = 8.0 * (2 ** i)
    kv[KV_H] = 0.5
    return kv


def build_ssm(nc, P, U_tok, b_U, y_o, ident_bf, b_ident, ident_f, b_identf):
    def din(name, shape, dt=F32):
        return nc.dram_tensor(name, list(shape), dt, kind="ExternalInput").ap()
    sp_small = din("ssm_small", [128, 8 * 4 + NKV + 2 + 8])
    sp_bc = din("ssm_bc", [128, 4 * 128])
    sp_mat = din("ssm_mat", [128, 256])
    op, dma = P.op, P.dma
    P.push_scope()
    small = P.sb("ssm_small_sb", [128, 8 * 4 + NKV + 2 + 8], F32); b_small = P.buf()
    bc = P.sb("ssm_bc_sb", [128, 4, 8, 16], F32); b_bc = P.buf()
    mats = P.sb("ssm_mat_sb", [128, 2, 128], F32); b_mats = P.buf()
    dma(lambda e: e.dma_start(out=small[:], in_=sp_small[:, :]), writes=[b_small])
    dma(lambda e: e.dma_start(out=bc[:].rearrange("p a g m -> p (a g m)"), in_=sp_bc[:, :]), writes=[b_bc])
    dma(lambda e: e.dma_start(out=mats[:].rearrange("p a n -> p (a n)"), in_=sp_mat[:, :]), writes=[b_mats])
    lamre, lamim, logdt, dsk = small[:, 0:8], small[:, 8:16], small[:, 16:24], small[:, 24:32]
    kvec = small[:, 32:32 + NKV]
    sgn_a, sgn_b = small[:, 32 + NKV:33 + NKV], small[:, 33 + NKV:34 + NKV]
    smask, P2 = mats[:, 0, :], mats[:, 1, :]
    Bs1, Bs2, Cs1, Cs2 = bc[:, 0], bc[:, 1], bc[:, 2], bc[:, 3]

    def T(name, shape, dt=F32):
        return P.sb(name, shape, dt), P.buf()

    def dv(fn, reads, writes):
        return op("dve", fn, reads=reads, writes=writes)

    dt_, b_dt = T("s_dt", [128, 8])
    a_, b_a = T("s_a", [128, 8])
    th, b_th = T("s_th", [128, 8])
    op("act", lambda e: e.activation(out=dt_[:], in_=logdt, func=AF.Exp), reads=[b_small], writes=[b_dt])
    dv(lambda e: e.tensor_tensor(out=a_[:], in0=lamre, in1=dt_[:], op=ALU.mult), [b_small, b_dt], [b_a])
    dv(lambda e: e.tensor_tensor(out=th[:], in0=lamim, in1=dt_[:], op=ALU.mult), [b_small, b_dt], [b_th])
    KA, b_KA = T("s_KA", [128, 8, NKV])
    KT, b_KT = T("s_KT", [128, 8, NKV])
    kb = kvec.unsqueeze(1).to_broadcast([128, 8, NKV])
    dv(lambda e: e.tensor_tensor(out=KA[:], in0=a_[:].unsqueeze(2).to_broadcast([128, 8, NKV]), in1=kb, op=ALU.mult),
       [b_a, b_small], [b_KA])
    dv(lambda e: e.tensor_tensor(out=KT[:], in0=th[:].unsqueeze(2).to_broadcast([128, 8, NKV]), in1=kb, op=ALU.mult),
       [b_th, b_small], [b_KT])
    MAG, b_MAG = T("s_MAG", [128, 8, NKV])
    op("act", lambda e: e.activation(out=MAG[:], in_=KA[:], func=AF.Exp), reads=[b_KA], writes=[b_MAG])
    ni, b_ni = T("s_ni", [128, 8, NKV], I32)
    nf, b_nf = T("s_nf", [128, 8, NKV])
    rr, b_rr = T("s_rr", [128, 8, NKV])
    uu, b_uu = T("s_uu", [128, 8, NKV])
    SIN, b_SIN = T("s_SIN", [128, 8, NKV])
    COS, b_COS = T("s_COS", [128, 8, NKV])

    def sin_of(dst, b_dst, shift):
        dv(lambda e: e.tensor_scalar(out=uu[:], in0=KT[:], scalar1=shift, scalar2=1.0 / TWO_PI, op0=ALU.add, op1=ALU.mult),
           [b_KT], [b_uu])
        dv(lambda e: e.tensor_copy(out=ni[:], in_=uu[:]), [b_uu], [b_ni])
        dv(lambda e: e.tensor_copy(out=nf[:], in_=ni[:]), [b_ni], [b_nf])
        dv(lambda e: e.scalar_tensor_tensor(out=rr[:], in0=nf[:], scalar=-CW1, in1=KT[:], op0=ALU.mult, op1=ALU.add),
           [b_nf, b_KT], [b_rr])
        dv(lambda e: e.scalar_tensor_tensor(out=rr[:], in0=nf[:], scalar=-CW2, in1=rr[:], op0=ALU.mult, op1=ALU.add),
           [b_nf, b_rr], [b_rr])
        dv(lambda e: e.tensor_scalar(out=rr[:], in0=rr[:], scalar1=shift, scalar2=math.pi, op0=ALU.add, op1=ALU.min),
           [b_rr], [b_rr])
        dv(lambda e: e.tensor_scalar(out=rr[:], in0=rr[:], scalar1=-math.pi, scalar2=None, op0=ALU.max), [b_rr], [b_rr])
        op("act", lambda e: e.activation(out=dst[:], in_=rr[:], func=AF.Sin), reads=[b_rr], writes=[b_dst])
    sin_of(SIN, b_SIN, 0.0)
    sin_of(COS, b_COS, math.pi / 2.0)
    PR, b_PR = T("s_PR", [128, 8, NKV])
    PI_, b_PI = T("s_PI", [128, 8, NKV])
    dv(lambda e: e.tensor_tensor(out=PR[:], in0=MAG[:], in1=COS[:], op=ALU.mult), [b_MAG, b_COS], [b_PR])
    dv(lambda e: e.tensor_tensor(out=PI_[:], in0=MAG[:], in1=SIN[:], op=ALU.mult), [b_MAG, b_SIN], [b_PI])
    em1, b_em1 = T("s_em1", [128, 8])
    tq, b_tq = T("s_tq", [128, 8])
    dv(lambda e: e.tensor_scalar(out=tq[:], in0=a_[:], scalar1=0.25, scalar2=1.0, op0=ALU.mult, op1=ALU.add), [b_a], [b_tq])
    dv(lambda e: e.tensor_tensor(out=tq[:], in0=tq[:], in1=a_[:], op=ALU.mult), [b_tq, b_a], [b_tq])
    dv(lambda e: e.tensor_scalar(out=tq[:], in0=tq[:], scalar1=1.0 / 3.0, scalar2=1.0, op0=ALU.mult, op1=ALU.add), [b_tq], [b_tq])
    dv(lambda e: e.tensor_tensor(out=tq[:], in0=tq[:], in1=a_[:], op=ALU.mult), [b_tq, b_a], [b_tq])
    dv(lambda e: e.tensor_scalar(out=tq[:], in0=tq[:], scalar1=0.5, scalar2=1.0, op0=ALU.mult, op1=ALU.add), [b_tq], [b_tq])
    dv(lambda e: e.tensor_tensor(out=em1[:], in0=tq[:], in1=a_[:], op=ALU.mult), [b_tq, b_a], [b_em1])
    cth, sth, shalf = COS[:, :, KV_G + 1], SIN[:, :, KV_G + 1], SIN[:, :, KV_H]
    re1, b_re1 = T("s_re1", [128, 8])
    im1, b_im1 = T("s_im1", [128, 8])
    w1, b_w1 = T("s_w1", [128, 8])
    w2, b_w2 = T("s_w2", [128, 8])
    dv(lambda e: e.tensor_tensor(out=w1[:], in0=shalf, in1=shalf, op=ALU.mult), [b_SIN], [b_w1])
    dv(lambda e: e.tensor_tensor(out=re1[:], in0=em1[:], in1=cth, op=ALU.mult), [b_em1, b_COS], [b_re1])
    dv(lambda e: e.scalar_tensor_tensor(out=re1[:], in0=w1[:], scalar=-2.0, in1=re1[:], op0=ALU.mult, op1=ALU.add),
       [b_w1, b_re1], [b_re1])
    dv(lambda e: e.scalar_tensor_tensor(out=im1[:], in0=em1[:], scalar=1.0, in1=sth, op0=ALU.add, op1=ALU.mult),
       [b_em1, b_SIN], [b_im1])
    den, b_den = T("s_den", [128, 8])
    dv(lambda e: e.tensor_tensor(out=den[:], in0=lamre, in1=lamre, op=ALU.mult), [b_small], [b_den])
    dv(lambda e: e.tensor_tensor(out=w1[:], in0=lamim, in1=lamim, op=ALU.mult), [b_small], [b_w1])
    dv(lambda e: e.tensor_tensor(out=den[:], in0=den[:], in1=w1[:], op=ALU.add), [b_den, b_w1], [b_den])
    dv(lambda e: e.reciprocal(out=den[:], in_=den[:]), [b_den], [b_den])
    cr, b_cr = T("s_cr", [128, 8])
    ci, b_ci = T("s_ci", [128, 8])
    dv(lambda e: e.tensor_tensor(out=w1[:], in0=re1[:], in1=lamre, op=ALU.mult), [b_re1, b_small], [b_w1])
    dv(lambda e: e.tensor_tensor(out=w2[:], in0=im1[:], in1=lamim, op=ALU.mult), [b_im1, b_small], [b_w2])
    dv(lambda e: e.tensor_tensor(out=w1[:], in0=w1[:], in1=w2[:], op=ALU.add), [b_w1, b_w2], [b_w1])
    dv(lambda e: e.tensor_tensor(out=cr[:], in0=w1[:], in1=den[:], op=ALU.mult), [b_w1, b_den], [b_cr])
    dv(lambda e: e.tensor_tensor(out=w1[:], in0=im1[:], in1=lamre, op=ALU.mult), [b_im1, b_small], [b_w1])
    dv(lambda e: e.tensor_tensor(out=w2[:], in0=re1[:], in1=lamim, op=ALU.mult), [b_re1, b_small], [b_w2])
    dv(lambda e: e.tensor_tensor(out=w1[:], in0=w1[:], in1=w2[:], op=ALU.subtract), [b_w1, b_w2], [b_w1])
    dv(lambda e: e.tensor_tensor(out=ci[:], in0=w1[:], in1=den[:], op=ALU.mult), [b_w1, b_den], [b_ci])
    bb1, b_bb1 = T("s_bb1", [128, 8, 16])
    bb2, b_bb2 = T("s_bb2", [128, 8, 16])
    q1, b_q1 = T("s_q1", [128, 8, 16])
    q2, b_q2 = T("s_q2", [128, 8, 16])
    crb = cr[:].unsqueeze(2).to_broadcast([128, 8, 16])
    cib = ci[:].unsqueeze(2).to_broadcast([128, 8, 16])
    dv(lambda e: e.tensor_tensor(out=q1[:], in0=crb, in1=Bs1, op=ALU.mult), [b_cr, b_bc], [b_q1])
    dv(lambda e: e.tensor_tensor(out=q2[:], in0=cib, in1=Bs2, op=ALU.mult), [b_ci, b_bc], [b_q2])
    dv(lambda e: e.scalar_tensor_tensor(out=bb1[:].rearrange("p g m -> p (g m)"), in0=q2[:].rearrange("p g m -> p (g m)"),
                                         scalar=sgn_a, in1=q1[:].rearrange("p g m -> p (g m)"), op0=ALU.mult, op1=ALU.add),
       [b_q1, b_q2, b_small], [b_bb1])
    dv(lambda e: e.tensor_tensor(out=q1[:], in0=crb, in1=Bs2, op=ALU.mult), [b_cr, b_bc], [b_q1])
    dv(lambda e: e.tensor_tensor(out=q2[:], in0=cib, in1=Bs1, op=ALU.mult), [b_ci, b_bc], [b_q2])
    dv(lambda e: e.scalar_tensor_tensor(out=bb2[:].rearrange("p g m -> p (g m)"), in0=q2[:].rearrange("p g m -> p (g m)"),
                                         scalar=sgn_b, in1=q1[:].rearrange("p g m -> p (g m)"), op0=ALU.mult, op1=ALU.add),
       [b_q1, b_q2, b_small], [b_bb2])
    CA, b_CA = T("s_CA", [128, 8, 16])
    CB, b_CB = T("s_CB", [128, 8, 16])
    dv(lambda e: e.tensor_scalar(out=CA[:], in0=Cs1, scalar1=sgn_b, scalar2=None, op0=ALU.mult), [b_bc, b_small], [b_CA])
    dv(lambda e: e.tensor_scalar(out=CB[:], in0=Cs2, scalar1=-1.0, scalar2=None, op0=ALU.mult), [b_bc], [b_CB])
    Fm, b_Fm = T("s_F", [128, 8, 8, 16])
    Fe, b_Fe = T("s_Fe", [128, 8, 8, 16])
    Gm, b_Gm = T("s_G", [128, 8, 8, 16])
    Om, b_Om = T("s_O", [128, 8, 8, 16])
    z1, b_z1 = T("s_z1", [128, 8, 8, 16])
    z2, b_z2 = T("s_z2", [128, 8, 8, 16])

    def outer(dst, bdst, kv0, v1, bv1, v2, bv2, sgn):
        pr = PR[:, :, kv0:kv0 + 8].unsqueeze(3).to_broadcast([128, 8, 8, 16])
        pi = PI_[:, :, kv0:kv0 + 8].unsqueeze(3).to_broadcast([128, 8, 8, 16])
        dv(lambda e: e.tensor_tensor(out=z1[:], in0=pr, in1=v1[:].unsqueeze(2).to_broadcast([128, 8, 8, 16]), op=ALU.mult),
           [b_PR, bv1], [b_z1])
        dv(lambda e: e.tensor_tensor(out=z2[:], in0=pi, in1=v2[:].unsqueeze(2).to_broadcast([128, 8, 8, 16]), op=ALU.mult),
           [b_PI, bv2], [b_z2])
        fl = "p g s m -> p (g s m)"
        if sgn is None:
            dv(lambda e: e.tensor_tensor(out=dst[:].rearrange(fl), in0=z1[:].rearrange(fl), in1=z2[:].rearrange(fl), op=ALU.add),
               [b_z1, b_z2], [bdst])
        else:
            dv(lambda e: e.scalar_tensor_tensor(out=dst[:].rearrange(fl), in0=z2[:].rearrange(fl), scalar=sgn,
                                                 in1=z1[:].rearrange(fl), op0=ALU.mult, op1=ALU.add),
               [b_z1, b_z2, b_small], [bdst])
    outer(Fm, b_Fm, KV_F, bb1, b_bb1, bb2, b_bb2, sgn_a)
    outer(Fe, b_Fe, KV_E, bb1, b_bb1, bb2, b_bb2, sgn_a)
    outer(Gm, b_Gm, KV_G, CA, b_CA, CB, b_CB, None)
    outer(Om, b_Om, KV_O, CA, b_CA, CB, b_CB, None)
    PIA, b_PIA = T("s_PIA", [128, 8, 10])
    dv(lambda e: e.tensor_scalar(out=PIA[:], in0=PI_[:, :, KV_A:KV_A + 10], scalar1=sgn_b, scalar2=None, op0=ALU.mult),
       [b_PI, b_small], [b_PIA])

    Yout = P.sb("s_Yout", [128, 8, 8, 128], F32); b_Yout = P.buf()
    Ug = P.sb("s_Ug", [128, 1024], BF16); b_Ug = P.buf()
    H = P.sb("s_H", [128, 1024], BF16); b_H = P.buf()
    Ysb = P.sb("s_Ysb", [128, 1024], F32); b_Ysb = P.buf()
    Mbf = P.sb("s_Mbf", [128, 128], BF16); b_Mbf = P.buf()
    Ebf = P.sb("s_Ebf", [128, 128], BF16); b_Ebf = P.buf()
    mtmp = P.sb("s_mtmp", [128, 128], F32); b_mtmp = P.buf()
    Amat = P.sb("s_Amat", [128, 10, 128], BF16); b_Amat = P.buf()
    Obf = P.sb("s_Obf", [128, 128], BF16); b_Obf = P.buf()
    put = P.ps("s_put", [128, 8, 128], BF16); b_put = P.pbuf()
    pxy = [P.ps(f"s_pxy{i}", [128, 512], F32) for i in range(2)]; b_pxy = [P.pbuf() for _ in range(2)]
    psc = [P.ps(f"s_psc{i}", [128, 512], F32) for i in range(2)]; b_psc = [P.pbuf() for _ in range(2)]
    pme = P.ps("s_pme", [128, 512], F32); b_pme = P.pbuf()
    pyt = P.ps("s_pyt", [128, 8, 128], F32); b_pyt = P.pbuf()
    for g in range(8):
        Fg = Fm[:, g].rearrange("p s m -> p (s m)")
        Feg = Fe[:, g].rearrange("p s m -> p (s m)")
        Gg = Gm[:, g].rearrange("p s m -> p (s m)")
        Og = Om[:, g].rearrange("p s m -> p (s m)")
        op("pe", lambda e: e.matmul(pme[:, 0:128], lhsT=Fg, rhs=Gg, start=True, stop=True), reads=[b_Fm, b_Gm], writes=[b_pme])
        dv(lambda e: e.tensor_tensor(out=mtmp[:], in0=pme[:, 0:128], in1=smask, op=ALU.mult), [b_pme, b_mats], [b_mtmp])
        dv(lambda e: e.scalar_tensor_tensor(out=Mbf[:], in0=ident_f[:], scalar=dsk[:, g:g + 1], in1=mtmp[:],
                                             op0=ALU.mult, op1=ALU.add), [b_identf, b_small, b_mtmp], [b_Mbf])
        op("pe", lambda e: e.transpose(pme[:, 128:256], Feg, ident_f[:]), reads=[b_Fe, b_identf], writes=[b_pme])
        op("act", lambda e: e.activation(out=Ebf[:], in_=pme[:, 128:256], func=AF.Copy), reads=[b_pme], writes=[b_Ebf])
        op("act", lambda e: e.activation(out=Obf[:], in_=Og, func=AF.Copy), reads=[b_Om], writes=[b_Obf])
        for i in range(10):
            dv(lambda e: e.tensor_scalar(out=mtmp[:], in0=ident_f[:], scalar1=PR[:, g, KV_A + i:KV_A + i + 1], scalar2=None,
                                         op0=ALU.mult), [b_identf, b_PR], [b_mtmp])
            dv(lambda e: e.scalar_tensor_tensor(out=Amat[:, i, :], in0=P2, scalar=PIA[:, g, i:i + 1], in1=mtmp[:],
                                                 op0=ALU.mult, op1=ALU.add), [b_mats, b_PIA, b_mtmp], [b_Amat])
        for S in range(8):
            op("pe", lambda e: e.transpose(put[:, S, :], U_tok[:, S, g].rearrange("p s m -> p (s m)"), ident_bf[:]),
               reads=[b_U[S], b_ident], writes=[b_put])
        op("act", lambda e: e.activation(out=Ug[:].rearrange("p (S c) -> p S c", S=8), in_=put[:], func=AF.Copy),
           reads=[b_put], writes=[b_Ug])
        for hf in range(2):
            op("pe", lambda e: e.matmul(pxy[hf][:], lhsT=Ebf[:], rhs=Ug[:, hf * 512:(hf + 1) * 512], start=True, stop=True),
               reads=[b_Ebf, b_Ug], writes=[b_pxy[hf]])
            op("act", lambda e: e.activation(out=H[:, hf * 512:(hf + 1) * 512], in_=pxy[hf][:], func=AF.Copy),
               reads=[b_pxy[hf]], writes=[b_H])
        for i in range(10):
            d = 1 << i
            rngs = []
            for hb in range(2):
                lo = max(d, hb * 512)
                hi = (hb + 1) * 512
                if lo < hi:
                    rngs.append((hb, lo, hi))
            for (hb, lo, hi) in rngs:
                op("pe", lambda e: e.matmul(psc[hb][:, lo - hb * 512:hi - hb * 512], lhsT=Amat[:, i, :], rhs=H[:, lo - d:hi - d],
                                            start=True, stop=True), reads=[b_Amat, b_H], writes=[b_psc[hb]])
            for (hb, lo, hi) in rngs:
                dv(lambda e: e.tensor_tensor(out=H[:, lo:hi], in0=psc[hb][:, lo - hb * 512:hi - hb * 512], in1=H[:, lo:hi], op=ALU.add),
                   [b_psc[hb], b_H], [b_H])
        for hf in range(2):
            op("pe", lambda e: e.matmul(pxy[hf][:], lhsT=Mbf[:], rhs=Ug[:, hf * 512:(hf + 1) * 512], start=True, stop=False),
               reads=[b_Mbf, b_Ug], writes=[b_pxy[hf]])
            if hf == 0:
                op("pe", lambda e: e.matmul(pxy[0][:, 1:512], lhsT=Obf[:], rhs=H[:, 0:511], start=False, stop=True),
                   reads=[b_Obf, b_H], writes=[b_pxy[0]])
            else:
                op("pe", lambda e: e.matmul(pxy[1][:], lhsT=Obf[:], rhs=H[:, 511:1023], start=False, stop=True),
                   reads=[b_Obf, b_H], writes=[b_pxy[1]])
            op("act", lambda e: e.activation(out=Ysb[:, hf * 512:(hf + 1) * 512], in_=pxy[hf][:], func=AF.Copy),
               reads=[b_pxy[hf]], writes=[b_Ysb])
        for S in range(8):
            op("pe", lambda e: e.transpose(pyt[:, S, :], Ysb[:, S * 128:(S + 1) * 128], ident_f[:]),
               reads=[b_Ysb, b_identf], writes=[b_pyt])
        dv(lambda e: e.tensor_copy(out=Yout[:, :, :, g * 16:(g + 1) * 16],
                                   in_=pyt[:].rearrange("p S (t n) -> p S t n", t=8)), [b_pyt], [b_Yout])
    for S in range(8):
        dma(lambda e: e.dma_start(out=y_o[S * 1024:(S + 1) * 1024, :].rearrange("(c t) ch -> c t ch", t=8), in_=Yout[:, S]),
            reads=[b_Yout])
    P.pop_scope()


def ssm_inputs(inp, r):
    gs = slice(8 * r, 8 * r + 8)
    lam_re = inp["lam_re"][0][gs]
    lam_im = inp["lam_im"][0][gs]
    log_dt = inp["log_dt"][0][gs]
    b_re, b_im = inp["b_re"][0][gs], inp["b_im"][0][gs]
    c_re, c_im = inp["c_re"][0][gs], inp["c_im"][0][gs]
    d = inp["d_skip"][0][gs]
    dup = lambda a: np.concatenate([a, a], axis=0)
    small = np.zeros((128, 8 * 4 + NKV + 2 + 8), np.float32)
    small[:, 0:8] = dup(lam_re.T)
    small[:, 8:16] = dup(lam_im.T)
    small[:, 16:24] = np.broadcast_to(log_dt[None, :], (128, 8))
    small[:, 24:32] = np.tile(d.T, (8, 1))
    small[:, 32:32 + NKV] = ssm_kvec()[None, :]
    small[:64, 32 + NKV] = -1.0; small[64:, 32 + NKV] = 1.0
    small[:64, 33 + NKV] = 1.0; small[64:, 33 + NKV] = -1.0
    bre = b_re.transpose(1, 0, 2); bim = b_im.transpose(1, 0, 2)
    cre = c_re.transpose(2, 0, 1); cim = c_im.transpose(2, 0, 1)
    bcv = np.zeros((128, 4, 8, 16), np.float32)
    bcv[:64, 0], bcv[64:, 0] = bre, bim
    bcv[:64, 1], bcv[64:, 1] = bim, bre
    bcv[:64, 2], bcv[64:, 2] = cre, cim
    bcv[:64, 3], bcv[64:, 3] = cim, cre
    s_idx = np.arange(128) // 16
    smask = (s_idx[None, :] >= s_idx[:, None]).astype(np.float32)
    P2 = np.zeros((128, 128), np.float32)
    P2[np.arange(128), (np.arange(128) + 64) % 128] = 1.0
    return {"ssm_small": small, "ssm_bc": np.ascontiguousarray(bcv.reshape(128, 512)),
            "ssm_mat": np.ascontiguousarray(np.concatenate([smask, P2], axis=1))}


_NC_CACHE = {}


def _get_nc(which):
    if which not in _NC_CACHE:
        nc = bass.Bass("TRN2", target_bir_lowering=False)
        if which == "ab":
            build_phase_ab(nc, with_ssm=True)
        else:
            build_phase_c(nc)
        _NC_CACHE[which] = nc
    return _NC_CACHE[which]


def kernel(**inputs):
    inp = {k: np.asarray(v, dtype=np.float32) for k, v in inputs.items()}
    B = inp["x"].shape[0]
    maps1 = [phase_ab_inputs(inp, ci // 4, ci % 4) for ci in range(8)]
    res1 = run_bass_kernel_spmd(_get_nc("ab"), maps1, core_ids=list(range(8))).results
    attn_full = np.zeros((B, L_SEQ, 8, 65), np.float32)
    y_full = np.zeros((B, L_SEQ, 512), np.float32)
    for ci in range(8):
        b, r = ci // 4, ci % 4
        attn_full[b, :, 2 * r:2 * r + 2, :] = res1[ci]["attn_o"].reshape(2, 65, L_SEQ).transpose(2, 0, 1)
        y_full[b, :, 128 * r:128 * r + 128] = res1[ci]["y_o"]
    maps2 = [phase_c_inputs(inp, ci // 4, ci % 4, attn_full, y_full) for ci in range(8)]
    res2 = run_bass_kernel_spmd(_get_nc("c"), maps2, core_ids=list(range(8))).results
    out = np.zeros((B, L_SEQ, 1024), np.float32)
    for ci in range(8):
        b, qi = ci // 4, ci % 4
        out[b, qi * NT_C:(qi + 1) * NT_C] = res2[ci]["out"]
    return out
```

```python
import math
import numpy as np
from contextlib import ExitStack

import concourse.bass as bass
import concourse.mybir as mybir
from concourse.bass_utils import run_bass_kernel_spmd

F32 = mybir.dt.float32
BF16 = mybir.dt.bfloat16
I32 = mybir.dt.int32
ALU = mybir.AluOpType
AF = mybir.ActivationFunctionType

PHYS = ["pe", "act", "dve", "pool", "sp"]
NDMA = 8
SAME_ENG_WAIT = True
EPS = 1e-6
NEGBIG = -30000.0


class Buf:
    __slots__ = ("name", "last_w", "readers", "excl")

    def __init__(self, name, excl=False):
        self.name = name
        self.last_w = None
        self.readers = []
        self.excl = excl


class Prog:
    def __init__(self, nc):
        self.nc = nc
        self.stack = ExitStack()
        self.semnames = ["pe", "act", "dve", "pool"] + [f"d{i}" for i in range(NDMA)]
        self.cnt = {s: 0 for s in self.semnames}
        self.seen = {e: {s: 0 for s in self.semnames} for e in PHYS}
        self.sems = {s: self.stack.enter_context(nc.semaphore("sem_" + s)) for s in self.semnames}
        self.engobjs = {"pe": nc.tensor, "act": nc.scalar, "dve": nc.vector, "pool": nc.gpsimd, "sp": nc.sync}
        self.pending = {e: {} for e in PHYS}
        self.nbuf = 0
        self.dma_rr = 0
        self.scopes = [self.stack]

    def buf(self, name=None, excl=False):
        self.nbuf += 1
        return Buf(name or f"b{self.nbuf}", excl)

    def pbuf(self):
        return self.buf(excl=True)

    def sb(self, name, shape, dtype):
        return self.scopes[-1].enter_context(self.nc.sbuf_tensor(name, list(shape), dtype))

    def ps(self, name, shape, dtype=F32):
        return self.scopes[-1].enter_context(self.nc.psum_tensor(name, list(shape), dtype))

    def push_scope(self):
        st = ExitStack()
        self.scopes.append(st)
        return st

    def pop_scope(self):
        st = self.scopes.pop()
        st.close()
        self.fence()

    def fence(self):
        for e in PHYS:
            for s in self.semnames:
                if self.cnt[s] > self.seen[e][s]:
                    self.pending[e][s] = self.cnt[s]

    def _op(self, phys, sem, inc, fn, reads, writes, extra_waits=()):
        waits = dict(self.pending[phys])
        self.pending[phys] = {}
        xr = [b for b in reads if b.excl]
        if xr:
            reads = [b for b in reads if not b.excl]
            writes = list(writes) + [b for b in xr if b not in writes]
        for (f, c) in extra_waits:
            waits[f] = max(waits.get(f, 0), c)

        def need(f):
            return f != phys or (SAME_ENG_WAIT and phys != "pe")
        for b in reads:
            if b.last_w is not None:
                f, c = b.last_w
                if need(f):
                    waits[f] = max(waits.get(f, 0), c)
        for b in writes:
            if b.last_w is not None:
                f, c = b.last_w
                if need(f):
                    waits[f] = max(waits.get(f, 0), c)
            for (f, c) in b.readers:
                if need(f):
                    waits[f] = max(waits.get(f, 0), c)
        engobj = self.engobjs[phys]
        for f, c in waits.items():
            if c > self.seen[phys][f]:
                self.seen[phys][f] = c
                engobj.wait_ge(self.sems[f], c)
        self.cnt[sem] += inc
        me = (sem, self.cnt[sem])
        ins = fn(engobj)
        ins.then_inc(self.sems[sem], inc)
        for b in reads:
            b.readers.append(me)
        for b in writes:
            b.last_w = me
            b.readers = []
        return me

    def op(self, eng, fn, reads=(), writes=()):
        return self._op(eng, eng, 1, fn, reads, writes)

    def dma(self, fn, reads=(), writes=(), q="sp"):
        k = self.dma_rr % NDMA
        self.dma_rr += 1
        sem = f"d{k}"
        prev = self.cnt[sem]
        extra = [(sem, prev)] if prev > 0 else []
        return self._op(q, sem, 16, fn, reads, writes, extra_waits=extra)

    def finish(self):
        for i in range(NDMA):
            s = f"d{i}"
            if self.cnt[s] > 0:
                self.nc.sync.wait_ge(self.sems[s], self.cnt[s])
        for s in ["pe", "act", "dve", "pool"]:
            if self.cnt[s] > 0:
                self.nc.sync.wait_ge(self.sems[s], self.cnt[s])
        while len(self.scopes) > 1:
            self.scopes.pop().close()
        self.stack.close()


def _rot(lst, i):
    return lst[i % len(lst)]


NT_C = 2048


def build_phase_c(nc, srcs=None):
    def din(name, shape, dt=F32):
        return nc.dram_tensor(name, list(shape), dt, kind="ExternalInput").ap()
    x = din("xc", [NT_C, 1024])
    if srcs is None:
        attn = din("attn", [NT_C, 520])
        yin = din("yin", [NT_C, 512])
    else:
        attn, yin = srcs["attn"], srcs["y"]
    cT2 = din("cT2", [128, 16])
    wada = din("wada_c", [1024, 4096])
    bada = din("bada_c", [1, 4096])
    rows = din("rows_c", [1, 3072])
    bglu = din("bglu", [1, 512])
    wglu = din("wglu", [512, 512])
    wout = din("wout", [1024, 1024])
    wfc1 = din("wfc1", [1024, 4096])
    wfc2 = din("wfc2", [4096, 1024])
    ident = din("ident", [128, 128])
    out = nc.dram_tensor("out", [NT_C, 1024], F32, kind="ExternalOutput").ap()
    x1s = nc.dram_tensor("x1s", [NT_C, 1024], F32).ap()

    P = Prog(nc)
    op, dma = P.op, P.dma

    ident_bf = P.sb("ident_bf", [128, 128], BF16); b_ident = P.buf()
    ones_f = P.sb("ones_f", [1, 128], F32); b_ones_f = P.buf()
    ones_bf = P.sb("ones_bf", [1, 512], BF16); b_ones_bf = P.buf()
    bglu_bf = P.sb("bglu_bf", [1, 512], BF16); b_bglu = P.buf()
    sh2T = P.sb("sh2T", [128, 16], BF16); b_sh2T = P.buf()
    Gcat = P.sb("Gcat", [128, 1024], F32); b_Gcat = P.buf()
    G1 = P.sb("G1", [128, 1024], F32); b_G1 = P.buf()
    S2b = P.sb("S2b", [128, 1024], F32); b_S2b = P.buf()
    G2 = P.sb("G2", [128, 1024], F32); b_G2 = P.buf()
    Gf = P.sb("Gf", [128, 1024], F32); b_Gf = P.buf()

    ident_f = P.sb("ident_f", [128, 128], F32); b_identf = P.buf()
    bglu_f = P.sb("bglu_f", [1, 512], F32); b_bgluf = P.buf()
    dma(lambda e: e.dma_start(out=ident_f[:], in_=ident[:, :]), writes=[b_identf])
    dma(lambda e: e.dma_start(out=bglu_f[:], in_=bglu[:, :]), writes=[b_bgluf])
    op("dve", lambda e: e.tensor_copy(out=ident_bf[:], in_=ident_f[:]), reads=[b_identf], writes=[b_ident])
    op("dve", lambda e: e.tensor_copy(out=bglu_bf[:], in_=bglu_f[:]), reads=[b_bgluf], writes=[b_bglu])
    op("dve", lambda e: e.memset(ones_f[:], 1.0), writes=[b_ones_f])
    op("dve", lambda e: e.memset(ones_bf[:], 1.0), writes=[b_ones_bf])

    P.push_scope()
    ct = P.sb("ct", [128, 16], F32); b_ct = P.buf()
    sct = P.sb("sct", [128, 16], F32); b_sct = P.buf()
    sgc = P.sb("sgc", [128, 16], F32)
    modrow = P.sb("modrow", [1, 4096], F32); b_modrow = P.buf()
    badas = P.sb("badas", [1, 4096], F32); b_badas = P.buf()
    rowss = P.sb("rowss", [1, 3072], F32); b_rowss = P.buf()
    s2row = P.sb("s2row", [1, 1024], F32); b_s2row = P.buf()
    pieces = [P.sb(f"wpiece{i}", [128, 8, 512], F32) for i in range(2)]
    b_pieces = [P.buf() for _ in range(2)]
    ps_row = [P.ps(f"ps_row{i}", [128, 512], F32) for i in range(2)]
    b_ps_row = [P.pbuf() for _ in range(2)]

    dma(lambda e: e.dma_start(out=ct[:], in_=cT2[:, :]), writes=[b_ct])
    dma(lambda e: e.dma_start(out=badas[:], in_=bada[:, :]), writes=[b_badas])
    dma(lambda e: e.dma_start(out=rowss[:], in_=rows[:, :]), writes=[b_rowss])
    op("act", lambda e: e.activation(out=sgc[:], in_=ct[:], func=AF.Sigmoid), reads=[b_ct], writes=[b_sct])
    op("dve", lambda e: e.tensor_tensor(out=sct[:], in0=sgc[:], in1=ct[:], op=ALU.mult), reads=[b_ct, b_sct], writes=[b_sct])
    for j in range(8):
        pc, bpc = _rot(pieces, j), _rot(b_pieces, j)
        pr, bpr = _rot(ps_row, j), _rot(b_ps_row, j)
        dma(lambda e: e.dma_start(out=pc[:], in_=wada[:, j * 512:(j + 1) * 512].rearrange("(k p) n -> p k n", p=128)),
            writes=[bpc])
        for k in range(8):
            op("pe", lambda e: e.matmul(pr[0:1, :], lhsT=sct[:, 2 * k:2 * k + 1], rhs=pc[:, k, :],
                                        start=(k == 0), stop=(k == 7)),
               reads=[b_sct, bpc], writes=[bpr])
        op("dve", lambda e: e.tensor_tensor(out=modrow[0:1, j * 512:(j + 1) * 512], in0=pr[0:1, :],
                                            in1=badas[0:1, j * 512:(j + 1) * 512], op=ALU.add),
           reads=[bpr, b_badas], writes=[b_modrow])
    op("dve", lambda e: e.scalar_tensor_tensor(out=s2row[:], in0=modrow[0:1, 2048:3072], scalar=1.0,
                                                in1=rowss[0:1, 1024:2048], op0=ALU.add, op1=ALU.mult),
       reads=[b_modrow, b_rowss], writes=[b_s2row])
    bc_list = [(Gcat, b_Gcat, rowss, b_rowss, 0), (G1, b_G1, modrow, b_modrow, 0), (S2b, b_S2b, s2row, b_s2row, 0),
               (G2, b_G2, modrow, b_modrow, 3072), (Gf, b_Gf, rowss, b_rowss, 2048)]
    i = 0
    for (dst, bdst, src, bsrc, off) in bc_list:
        for hf in range(2):
            pr, bpr = _rot(ps_row, i), _rot(b_ps_row, i)
            i += 1
            op("pe", lambda e: e.matmul(pr[:, :], lhsT=ones_f[0:1, :], rhs=src[0:1, off + hf * 512: off + (hf + 1) * 512],
                                        start=True, stop=True),
               reads=[b_ones_f, bsrc], writes=[bpr])
            op("act", lambda e: e.activation(out=dst[:, hf * 512:(hf + 1) * 512], in_=pr[:, :], func=AF.Copy),
               reads=[bpr], writes=[bdst])
    pr, bpr = ps_row[0], b_ps_row[0]
    for k in range(8):
        op("pe", lambda e: e.matmul(pr[:, 2 * k:2 * k + 2], lhsT=modrow[0:1, 1024 + k * 128:1024 + (k + 1) * 128],
                                    rhs=ones_f[0:1, 0:2], start=True, stop=True),
           reads=[b_ones_f, b_modrow], writes=[bpr])
    op("dve", lambda e: e.tensor_copy(out=sh2T[:], in_=pr[:, 0:16]), reads=[bpr], writes=[b_sh2T])
    P.pop_scope()

    P.push_scope()
    Wout = P.sb("Wout", [128, 8, 1024], BF16); b_Wout = P.buf()
    Wglu = P.sb("Wglu", [128, 4, 512], BF16); b_Wglu = P.buf()
    stg = [P.sb(f"stgc1_{i}", [128, 2048], F32) for i in range(2)]; b_stg = [P.buf() for _ in range(2)]
    dma(lambda e: e.dma_start(out=stg[0][:].rearrange("p (k n) -> p k n", k=4), in_=wglu.rearrange("(k p) n -> p k n", p=128)),
        writes=[b_stg[0]])
    op("pool", lambda e: e.tensor_copy(out=Wglu[:].rearrange("p k n -> p (k n)"), in_=stg[0][:]), reads=[b_stg[0]], writes=[b_Wglu])
    for kk in range(4):
        sg_, bsg_ = _rot(stg, kk + 1), _rot(b_stg, kk + 1)
        dma(lambda e: e.dma_start(out=sg_[:].rearrange("p (k n) -> p k n", k=2),
                                  in_=wout[kk * 256:(kk + 1) * 256, :].rearrange("(k p) n -> p k n", p=128)), writes=[bsg_])
        op("pool" if kk % 2 else "dve", lambda e: e.tensor_copy(out=Wout[:, 2 * kk:2 * kk + 2, :].rearrange("p k n -> p (k n)"), in_=sg_[:]),
           reads=[bsg_], writes=[b_Wout])
    NB = 3

    def dbl(name, shape, dt, n=2):
        return [P.sb(f"{name}{i}", shape, dt) for i in range(n)], [P.buf() for _ in range(n)]

    def dblp(name, shape, dt, n=2):
        return [P.ps(f"{name}{i}", shape, dt) for i in range(n)], [P.pbuf() for _ in range(n)]
    at, b_at = dbl("at", [128, 8, 65], F32, NB)
    yt, b_yt = dbl("yt", [128, 512], F32, NB)
    xt, b_xt = dbl("xt", [128, 1024], F32, NB)
    rl, b_rl = dbl("rl", [128, 8], F32)
    A, b_A = dbl("A", [128, 8, 64], F32)
    junk = P.sb("junk", [128, 512], BF16); b_junk = P.buf()
    ss, b_ss = dbl("ss", [128, 2], F32)
    sq, b_sq = dbl("sq", [128, 2], F32)
    rstd, b_rstd = dbl("rstd", [128, 2], F32)
    g1, b_g1 = dbl("g1", [128, 512], F32)
    g2, b_g2 = dbl("g2", [128, 512], F32)
    sg, b_sg = dbl("sg", [128, 512], F32)
    yg, b_yg = dbl("yg", [128, 512], F32)
    ygb, b_ygb = dbl("ygb", [128, 512], BF16)
    ygT, b_ygT = dbl("ygT", [128, 4, 128], BF16)
    sig, b_sig = dbl("sig", [128, 512], F32)
    S, b_S = dbl("S", [128, 512], F32)
    mixh, b_mixh = dbl("mixh", [128, 1024], BF16)
    mixT, b_mixT = dbl("mixT", [128, 8, 128], BF16)
    tt, b_tt = dbl("tt", [128, 1024], F32)
    x1t, b_x1t = dbl("x1t", [128, 1024], F32)
    ptr, b_ptr = dblp("ptr", [128, 8, 128], BF16)
    psg, b_psg = dblp("psg", [128, 512], F32)
    pmx = P.ps("pmx", [128, 8, 128], BF16); b_pmx = P.pbuf()
    pso = [P.ps(f"pso{i}", [128, 512], F32) for i in range(2)]; b_pso = [P.pbuf() for _ in range(2)]
    b_x1s = [P.buf() for _ in range(16)]

    def c1_load(t):
        a, ba = _rot(at, t), _rot(b_at, t)
        y_, by = _rot(yt, t), _rot(b_yt, t)
        x_, bx = _rot(xt, t), _rot(b_xt, t)
        dma(lambda e: e.dma_start(out=a[:].rearrange("p h e -> p (h e)"), in_=attn[t * 128:(t + 1) * 128, :]), writes=[ba])
        dma(lambda e: e.dma_start(out=y_[:], in_=yin[t * 128:(t + 1) * 128, :]), writes=[by])
        dma(lambda e: e.dma_start(out=x_[:], in_=x[t * 128:(t + 1) * 128, :]), writes=[bx])

    def c1_a(t):
        a, ba = _rot(at, t), _rot(b_at, t)
        y_, by = _rot(yt, t), _rot(b_yt, t)
        i = t % 2
        op("dve", lambda e: e.reciprocal(out=rl[i][:], in_=a[:, :, 64]), reads=[ba], writes=[b_rl[i]])
        yield
        op("dve", lambda e: e.tensor_tensor(out=A[i][:], in0=a[:, :, 0:64],
                                            in1=rl[i][:, :].unsqueeze(2).to_broadcast([128, 8, 64]), op=ALU.mult),
           reads=[ba, b_rl[i]], writes=[b_A[i]])
        yield
        op("act", lambda e: e.activation(out=junk[:], in_=A[i][:].rearrange("p h d -> p (h d)"), func=AF.Square,
                                         accum_out=ss[i][:, 0:1]),
           reads=[b_A[i]], writes=[b_junk, b_ss[i]])
        yield
        op("pool", lambda e: e.tensor_tensor(out=g1[i][:], in0=y_[:], in1=y_[:], op=ALU.mult), reads=[by], writes=[b_g1[i]])
        yield
        op("pool", lambda e: e.tensor_scalar(out=g1[i][:], in0=g1[i][:], scalar1=0.044715, scalar2=1.0,
                                             op0=ALU.mult, op1=ALU.add), reads=[b_g1[i]], writes=[b_g1[i]])
        yield
        op("pool", lambda e: e.tensor_tensor(out=g2[i][:], in0=g1[i][:], in1=y_[:], op=ALU.mult), reads=[b_g1[i], by], writes=[b_g2[i]])
        yield
        op("act", lambda e: e.activation(out=sg[i][:], in_=g2[i][:], func=AF.Sigmoid, scale=1.5957691216057308),
           reads=[b_g2[i]], writes=[b_sg[i]])
        yield
        op("dve", lambda e: e.tensor_tensor(out=yg[i][:], in0=y_[:], in1=sg[i][:], op=ALU.mult), reads=[by, b_sg[i]], writes=[b_yg[i]])
        yield
        op("pool", lambda e: e.tensor_tensor(out=ygb[i][:], in0=y_[:], in1=sg[i][:], op=ALU.mult), reads=[by, b_sg[i]], writes=[b_ygb[i]])
        yield
        for k in range(4):
            op("pe", lambda e: e.transpose(ptr[i][:, k, :], ygb[i][:, k * 128:(k + 1) * 128], ident_bf[:]),
               reads=[b_ygb[i], b_ident], writes=[b_ptr[i]])
            yield
        op("act", lambda e: e.activation(out=ygT[i][:], in_=ptr[i][:, 0:4, :], func=AF.Copy), reads=[b_ptr[i]], writes=[b_ygT[i]])
        yield
        op("pe", lambda e: e.matmul(psg[i][:], lhsT=ones_bf[0:1, 0:128], rhs=bglu_bf[0:1, :], start=True, stop=False),
           reads=[b_ones_bf, b_bglu], writes=[b_psg[i]])
        yield
        for k in range(4):
            op("pe", lambda e: e.matmul(psg[i][:], lhsT=ygT[i][:, k, :], rhs=Wglu[:, k, :], start=False, stop=(k == 3)),
               reads=[b_ygT[i], b_Wglu], writes=[b_psg[i]])
            yield

    def c1_b(t):
        x_, bx = _rot(xt, t), _rot(b_xt, t)
        i = t % 2
        op("act", lambda e: e.activation(out=sig[i][:], in_=psg[i][:], func=AF.Sigmoid), reads=[b_psg[i]], writes=[b_sig[i]])
        yield
        op("dve", lambda e: e.tensor_tensor(out=S[i][:], in0=yg[i][:], in1=sig[i][:], op=ALU.mult), reads=[b_yg[i], b_sig[i]], writes=[b_S[i]])
        yield
        op("act", lambda e: e.activation(out=junk[:], in_=S[i][:], func=AF.Square, accum_out=ss[i][:, 1:2]),
           reads=[b_S[i]], writes=[b_junk, b_ss[i]])
        yield
        op("act", lambda e: e.activation(out=sq[i][:], in_=ss[i][:], func=AF.Sqrt, scale=1.0 / 512.0, bias=EPS),
           reads=[b_ss[i]], writes=[b_sq[i]])
        yield
        op("dve", lambda e: e.reciprocal(out=rstd[i][:], in_=sq[i][:]), reads=[b_sq[i]], writes=[b_rstd[i]])
        yield
        op("dve", lambda e: e.scalar_tensor_tensor(out=mixh[i][:, 0:512], in0=A[i][:].rearrange("p h d -> p (h d)"),
                                                    scalar=rstd[i][:, 0:1], in1=Gcat[:, 0:512], op0=ALU.mult, op1=ALU.mult),
           reads=[b_A[i], b_rstd[i], b_Gcat], writes=[b_mixh[i]])
        yield
        op("dve", lambda e: e.scalar_tensor_tensor(out=mixh[i][:, 512:1024], in0=S[i][:], scalar=rstd[i][:, 1:2],
                                                    in1=Gcat[:, 512:1024], op0=ALU.mult, op1=ALU.mult),
           reads=[b_S[i], b_rstd[i], b_Gcat], writes=[b_mixh[i]])
        yield
        for k in range(8):
            op("pe", lambda e: e.transpose(pmx[:, k, :], mixh[i][:, k * 128:(k + 1) * 128], ident_bf[:]),
               reads=[b_mixh[i], b_ident], writes=[b_pmx])
            yield
        op("act", lambda e: e.activation(out=mixT[i][:], in_=pmx[:], func=AF.Copy), reads=[b_pmx], writes=[b_mixT[i]])
        yield
        for hf in range(2):
            for k in range(8):
                op("pe", lambda e: e.matmul(pso[hf][:], lhsT=mixT[i][:, k, :], rhs=Wout[:, k, hf * 512:(hf + 1) * 512],
                                            start=(k == 0), stop=(k == 7)),
                   reads=[b_mixT[i], b_Wout], writes=[b_pso[hf]])
                yield
            op("dve", lambda e: e.tensor_tensor(out=tt[i][:, hf * 512:(hf + 1) * 512], in0=pso[hf][:],
                                                in1=G1[:, hf * 512:(hf + 1) * 512], op=ALU.mult),
               reads=[b_pso[hf], b_G1], writes=[b_tt[i]])
            yield
        op("pool", lambda e: e.tensor_tensor(out=x1t[i][:], in0=tt[i][:], in1=x_[:], op=ALU.add), reads=[b_tt[i], bx], writes=[b_x1t[i]])
        yield
        dma(lambda e: e.dma_start(out=x1s[t * 128:(t + 1) * 128, :], in_=x1t[i][:]), reads=[b_x1t[i]], writes=[b_x1s[t]])
        yield

    def lockstep(*gens):
        gens = [g for g in gens if g is not None]
        while gens:
            alive = []
            for g in gens:
                try:
                    next(g)
                    alive.append(g)
                except StopIteration:
                    pass
            gens = alive

    c1_load(0)
    c1_load(1)
    lockstep(c1_a(0))
    for t in range(16):
        if t + 2 < 16:
            c1_load(t + 2)
        lockstep(c1_a(t + 1) if t + 1 < 16 else None, c1_b(t))
    P.pop_scope()

    P.push_scope()
    W2 = P.sb("W2", [128, 32, 1024], BF16); b_W2 = [P.buf() for _ in range(8)]
    stg2 = [P.sb(f"stgc2_{i}", [128, 2048], F32) for i in range(2)]; b_stg2 = [P.buf() for _ in range(2)]
    scount = 0
    for i in range(16):
        sg_, bsg_ = _rot(stg2, scount), _rot(b_stg2, scount)
        scount += 1
        dma(lambda e: e.dma_start(out=sg_[:].rearrange("p (j n) -> p j n", j=2),
                                  in_=wfc2[256 * i:256 * (i + 1), :].rearrange("(j p) n -> p j n", p=128)), writes=[bsg_])
        op("dve", lambda e: e.tensor_copy(out=W2[:, 2 * i:2 * i + 2, :].rearrange("p j n -> p (j n)"), in_=sg_[:]),
           reads=[bsg_], writes=[b_W2[i // 2]])
    W1p = [P.sb(f"W1p{i}", [128, 8, 256], BF16) for i in range(2)]; b_W1p = [P.buf() for _ in range(2)]
    hT = P.sb("hT", [128, 32, 512], BF16); b_hT = [P.buf() for _ in range(32)]
    h2T = P.sb("h2T", [128, 8, 512], BF16); b_h2T = P.buf()
    x1g = P.sb("x1g", [128, 4, 1024], F32); b_x1g = [P.buf() for _ in range(4)]
    h2 = [P.sb(f"h2_{i}", [128, 1024], BF16) for i in range(2)]; b_h2 = [P.buf() for _ in range(2)]
    junk2 = P.sb("junk2", [128, 1024], BF16); b_junk2 = P.buf()
    ssc = P.sb("ssc", [128, 2], F32); b_ssc = P.buf()
    sqc = P.sb("sqc", [128, 2], F32); b_sqc = P.buf()
    rc = P.sb("rc", [128, 2], F32); b_rc = P.buf()
    ssp = [P.sb(f"ssp{i}", [128, 1], F32) for i in range(2)]; b_ssp = [P.buf() for _ in range(2)]
    sqp = [P.sb(f"sqp{i}", [128, 1], F32) for i in range(2)]; b_sqp = [P.buf() for _ in range(2)]
    rcp = [P.sb(f"rcp{i}", [128, 1], F32) for i in range(2)]; b_rcp = [P.buf() for _ in range(2)]
    b1T = P.sb("b1T", [128, 32], F32); b_b1T = P.buf()
    rl_t = [P.sb(f"rl_t{i}", [128, 512], BF16) for i in range(2)]; b_rl_t = [P.buf() for _ in range(2)]
    t2 = P.sb("t2", [128, 1024], F32); b_t2 = P.buf()
    x2t = t2; b_x2t = b_t2
    ot = [P.sb(f"ot{i}", [128, 1024], F32) for i in range(1)]; b_ot = [P.buf() for _ in range(1)]
    pht = [P.ps(f"pht{i}", [128, 8, 128], BF16) for i in range(2)]; b_pht = [P.pbuf() for _ in range(2)]
    psb = P.ps("psb", [128, 512], F32); b_psb = P.pbuf()
    psf = [P.ps(f"psf{i}", [128, 512], F32) for i in range(2)]; b_psf = [P.pbuf() for _ in range(2)]
    pso2 = [P.ps(f"pso2{i}", [128, 512], F32) for i in range(2)]; b_pso2 = [P.pbuf() for _ in range(2)]
    pcount = 0
    ocount = 0
    for g in range(4):
        for i in range(4):
            t = g * 4 + i
            q_ = i % 2
            dma(lambda e: e.dma_start(out=x1g[:, i, :], in_=x1s[t * 128:(t + 1) * 128, :]), reads=[b_x1s[t]], writes=[b_x1g[i]])
            op("act", lambda e: e.activation(out=junk2[:], in_=x1g[:, i, :], func=AF.Square, accum_out=ssp[q_][:, 0:1]),
               reads=[b_x1g[i]], writes=[b_junk2, b_ssp[q_]])
            op("act", lambda e: e.activation(out=sqp[q_][:], in_=ssp[q_][:], func=AF.Sqrt, scale=1.0 / 1024.0, bias=EPS),
               reads=[b_ssp[q_]], writes=[b_sqp[q_]])
            op("dve", lambda e: e.reciprocal(out=rcp[q_][:], in_=sqp[q_][:]), reads=[b_sqp[q_]], writes=[b_rcp[q_]])
            op("dve", lambda e: e.scalar_tensor_tensor(out=h2[q_][:], in0=x1g[:, i, :], scalar=rcp[q_][:, 0:1], in1=S2b[:],
                                                        op0=ALU.mult, op1=ALU.mult),
               reads=[b_x1g[i], b_rcp[q_], b_S2b], writes=[b_h2[q_]])
            for k in range(8):
                op("pe", lambda e: e.transpose(pht[q_][:, k, :], h2[q_][:, k * 128:(k + 1) * 128], ident_bf[:]),
                   reads=[b_h2[q_], b_ident], writes=[b_pht[q_]])
            op("act", lambda e: e.activation(out=h2T[:, :, i * 128:(i + 1) * 128], in_=pht[q_][:], func=AF.Copy),
               reads=[b_pht[q_]], writes=[b_h2T])
        for pc in range(16):
            w1, bw1 = _rot(W1p, pcount), _rot(b_W1p, pcount)
            pcount += 1
            sg_, bsg_ = _rot(stg2, scount), _rot(b_stg2, scount)
            scount += 1
            dma(lambda e: e.dma_start(out=sg_[:].rearrange("p (k n) -> p k n", k=8),
                                      in_=wfc1[:, pc * 256:(pc + 1) * 256].rearrange("(k p) n -> p k n", p=128)), writes=[bsg_])
            op("dve", lambda e: e.tensor_copy(out=w1[:].rearrange("p k n -> p (k n)"), in_=sg_[:]), reads=[bsg_], writes=[bw1])
            for fc in range(2):
                j = pc * 2 + fc
                if g == 0:
                    for k in range(8):
                        op("pe", lambda e: e.matmul(psb[:, 0:2], lhsT=w1[:, k, fc * 128:(fc + 1) * 128], rhs=sh2T[:, 2 * k:2 * k + 2],
                                                    start=(k == 0), stop=(k == 7)),
                           reads=[b_sh2T, bw1], writes=[b_psb])
                    op("dve", lambda e: e.tensor_copy(out=b1T[:, j:j + 1], in_=psb[:, 0:1]), reads=[b_psb], writes=[b_b1T])
                pf, bpf = _rot(psf, j), _rot(b_psf, j)
                for k in range(8):
                    op("pe", lambda e: e.matmul(pf[:], lhsT=w1[:, k, fc * 128:(fc + 1) * 128], rhs=h2T[:, k, :],
                                                start=(k == 0), stop=(k == 7)),
                       reads=[bw1, b_h2T], writes=[bpf])
                r_, br_ = _rot(rl_t, j), _rot(b_rl_t, j)
                op("act", lambda e: e.activation(out=r_[:], in_=pf[:], func=AF.Relu, bias=b1T[:, j:j + 1]),
                   reads=[bpf, b_b1T], writes=[br_])
                op("pool", lambda e: e.tensor_tensor(out=hT[:, j, :], in0=r_[:], in1=r_[:], op=ALU.mult),
                   reads=[br_], writes=[b_hT[j]])
        for i in range(4):
            t = g * 4 + i
            for hf in range(2):
                po, bpo = _rot(pso2, ocount), _rot(b_pso2, ocount)
                ocount += 1
                for j in range(32):
                    op("pe", lambda e: e.matmul(po[:], lhsT=hT[:, j, i * 128:(i + 1) * 128],
                                                rhs=W2[:, j, hf * 512:(hf + 1) * 512], start=(j == 0), stop=(j == 31)),
                       reads=[b_hT[j], b_W2[j // 4]], writes=[bpo])
                op("dve", lambda e: e.tensor_tensor(out=t2[:, hf * 512:(hf + 1) * 512], in0=po[:],
                                                    in1=G2[:, hf * 512:(hf + 1) * 512], op=ALU.mult),
                   reads=[bpo, b_G2], writes=[b_t2])
            op("pool", lambda e: e.tensor_tensor(out=x2t[:], in0=t2[:], in1=x1g[:, i, :], op=ALU.add),
               reads=[b_t2, b_x1g[i]], writes=[b_x2t])
            op("act", lambda e: e.activation(out=junk2[:], in_=x2t[:], func=AF.Square, accum_out=ssc[:, 1:2]),
               reads=[b_x2t], writes=[b_junk2, b_ssc])
            op("act", lambda e: e.activation(out=sqc[:, 1:2], in_=ssc[:, 1:2], func=AF.Sqrt, scale=1.0 / 1024.0, bias=EPS),
               reads=[b_ssc], writes=[b_sqc])
            op("dve", lambda e: e.reciprocal(out=rc[:, 1:2], in_=sqc[:, 1:2]), reads=[b_sqc], writes=[b_rc])
            o_, bo_ = _rot(ot, t), _rot(b_ot, t)
            op("dve", lambda e: e.scalar_tensor_tensor(out=o_[:], in0=x2t[:], scalar=rc[:, 1:2], in1=Gf[:],
                                                        op0=ALU.mult, op1=ALU.mult),
               reads=[b_x2t, b_rc, b_Gf], writes=[bo_])
            dma(lambda e: e.dma_start(out=out[t * 128:(t + 1) * 128, :], in_=o_[:]), reads=[bo_])
    P.pop_scope()
    P.finish()
    return nc


def _ident():
    return np.eye(128, dtype=np.float32)


def phase_c_inputs(inp, b, qi, attn_full, y_full):
    T0 = qi * NT_C
    c = inp["c"][b]
    cT = np.ascontiguousarray(c.reshape(8, 128).T)
    cT2 = np.repeat(cT[:, :, None], 2, axis=2).reshape(128, 16)
    rows = np.concatenate([inp["g_attn_out"][0], inp["g_ssm_out"][0], inp["g_mlp"][0], inp["g_final"]])[None, :]
    return {
        "xc": np.ascontiguousarray(inp["x"][b, T0:T0 + NT_C]),
        "attn": np.ascontiguousarray(attn_full[b, T0:T0 + NT_C].reshape(NT_C, 520)),
        "yin": np.ascontiguousarray(y_full[b, T0:T0 + NT_C]),
        "cT2": np.ascontiguousarray(cT2),
        "wada_c": np.ascontiguousarray(inp["w_ada"][0][:, 2048:6144]),
        "bada_c": np.ascontiguousarray(inp["b_ada"][0][None, 2048:6144]),
        "rows_c": np.ascontiguousarray(rows.astype(np.float32)),
        "bglu": np.ascontiguousarray(inp["b_glu"][0][None, :]),
        "wglu": np.ascontiguousarray(inp["w_glu"][0]),
        "wout": np.ascontiguousarray(inp["w_out"][0]),
        "wfc1": np.ascontiguousarray(inp["w_fc1"][0]),
        "wfc2": np.ascontiguousarray(inp["w_fc2"][0]),
        "ident": _ident(),
    }


L_SEQ = 8192
NTILE = 64


def build_phase_ab(nc, with_ssm=True, dsts=None, dbg_tiles=NTILE, dbg_attn=True, dbg_stage=9):
    def din(name, shape, dt=F32):
        return nc.dram_tensor(name, list(shape), dt, kind="ExternalInput").ap()
    xb = din("xb", [L_SEQ, 1024])
    cT2 = din("cT2", [128, 16])
    wada = din("wada_a", [1024, 2048])
    bada = din("bada_a", [1, 2048])
    gmix = din("gmix", [1, 1024])
    win = din("win", [1024, 512])
    cosl = din("cosl", [128, 2048])
    sinl = din("sinl", [128, 2048])
    ind = din("ind", [32, L_SEQ])
    cmask = din("cmask", [128, 2048])
    ident = din("ident", [128, 128])
    if dsts is None:
        attn_o = nc.dram_tensor("attn_o", [130, L_SEQ], F32, kind="ExternalOutput").ap()
        y_o = nc.dram_tensor("y_o", [L_SEQ, 128], F32, kind="ExternalOutput").ap()
    else:
        attn_o, y_o = dsts["attn"], dsts["y"]

    P = Prog(nc)
    op, dma = P.op, P.dma

    ident_bf = P.sb("ident_bf", [128, 128], BF16); b_ident = P.buf()
    ident_f = P.sb("ident_f", [128, 128], F32); b_identf = P.buf()
    ones_bf = P.sb("ones_bf", [1, 512], BF16); b_ones_bf = P.buf()
    zeros_bf = P.sb("zeros_bf", [1, 512], BF16); b_zeros_bf = P.buf()
    ones_f = P.sb("ones_f", [1, 128], F32); b_ones_f = P.buf()
    U_tok = P.sb("U_tok", [128, 8, 8, 8, 16], BF16); b_U = [P.buf() for _ in range(8)]
    dma(lambda e: e.dma_start(out=ident_f[:], in_=ident[:, :]), writes=[b_identf])
    op("dve", lambda e: e.tensor_copy(out=ident_bf[:], in_=ident_f[:]), reads=[b_identf], writes=[b_ident])
    op("dve", lambda e: e.memset(ones_f[:], 1.0), writes=[b_ones_f])
    op("dve", lambda e: e.memset(ones_bf[:], 1.0), writes=[b_ones_bf])
    op("dve", lambda e: e.memset(zeros_bf[:], 0.0), writes=[b_zeros_bf])

    P.push_scope()
    QaT = P.sb("QaT", [128, 2, L_SEQ], BF16); b_QaT = [P.buf() for _ in range(NTILE)]
    KaT = P.sb("KaT", [128, 2, L_SEQ], BF16); b_KaT = [P.buf() for _ in range(NTILE)]; b_Kind = P.buf()
    Vaug = P.sb("Vaug", [128, NTILE, 2, 65], BF16); b_V = [P.buf() for _ in range(NTILE)]; b_Vones = P.buf()
    P.push_scope()
    stgI = [P.sb(f"stgI{i}", [128, 2048], F32) for i in range(2)]; b_stgI = [P.buf() for _ in range(2)]
    for cch in range(4):
        sg_, bsg_ = _rot(stgI, cch), _rot(b_stgI, cch)
        dma(lambda e: e.dma_start(out=sg_[64:96, :], in_=ind[:, cch * 2048:(cch + 1) * 2048]), writes=[bsg_])
        for h in range(2):
            op("pool" if h else "dve", lambda e: e.tensor_copy(out=KaT[64:96, h, cch * 2048:(cch + 1) * 2048], in_=sg_[64:96, :]),
               reads=[bsg_], writes=[b_Kind])
    P.pop_scope()
    op("pool", lambda e: e.memset(Vaug[:, :, :, 64:65], 1.0), writes=[b_Vones])

    P.push_scope()
    S1b = P.sb("S1b", [128, 1024], F32); b_S1b = P.buf()
    sh1T = P.sb("sh1T", [128, 16], BF16); b_sh1T = P.buf()
    Win = P.sb("Win", [128, 8, 512], BF16); b_Win = P.buf()
    bin_bf = P.sb("bin_bf", [1, 512], BF16); b_bin = P.buf()
    P.push_scope()
    stgW = [P.sb(f"stgW{i}", [128, 2048], F32) for i in range(2)]; b_stgW = [P.buf() for _ in range(2)]
    for kk in range(2):
        sg_, bsg_ = stgW[kk], b_stgW[kk]
        dma(lambda e: e.dma_start(out=sg_[:].rearrange("p (k n) -> p k n", k=4),
                                  in_=win[kk * 512:(kk + 1) * 512, :].rearrange("(k p) n -> p k n", p=128)), writes=[bsg_])
        op("pool" if kk else "dve", lambda e: e.tensor_copy(out=Win[:, 4 * kk:4 * kk + 4, :].rearrange("p k n -> p (k n)"), in_=sg_[:]),
           reads=[bsg_], writes=[b_Win])
    P.pop_scope()

    P.push_scope()
    ct = P.sb("ct", [128, 16], F32); b_ct = P.buf()
    sct = P.sb("sct", [128, 16], F32); b_sct = P.buf()
    sgc = P.sb("sgc", [128, 16], F32)
    modrow = P.sb("modrow", [1, 2048], F32); b_modrow = P.buf()
    badas = P.sb("badas", [1, 2048], F32); b_badas = P.buf()
    gmixs = P.sb("gmixs", [1, 1024], F32); b_gmixs = P.buf()
    s1row = P.sb("s1row", [1, 1024], F32); b_s1row = P.buf()
    pieces = [P.sb(f"wpiece{i}", [128, 8, 512], F32) for i in range(2)]
    b_pieces = [P.buf() for _ in range(2)]
    ps_row = [P.ps(f"ps_row{i}", [128, 512], F32) for i in range(2)]
    b_ps_row = [P.pbuf() for _ in range(2)]
    dma(lambda e: e.dma_start(out=ct[:], in_=cT2[:, :]), writes=[b_ct])
    dma(lambda e: e.dma_start(out=badas[:], in_=bada[:, :]), writes=[b_badas])
    dma(lambda e: e.dma_start(out=gmixs[:], in_=gmix[:, :]), writes=[b_gmixs])
    op("act", lambda e: e.activation(out=sgc[:], in_=ct[:], func=AF.Sigmoid), reads=[b_ct], writes=[b_sct])
    op("dve", lambda e: e.tensor_tensor(out=sct[:], in0=sgc[:], in1=ct[:], op=ALU.mult), reads=[b_ct, b_sct], writes=[b_sct])
    for j in range(4):
        pc, bpc = _rot(pieces, j), _rot(b_pieces, j)
        pr, bpr = _rot(ps_row, j), _rot(b_ps_row, j)
        dma(lambda e: e.dma_start(out=pc[:], in_=wada[:, j * 512:(j + 1) * 512].rearrange("(k p) n -> p k n", p=128)),
            writes=[bpc])
        for k in range(8):
            op("pe", lambda e: e.matmul(pr[0:1, :], lhsT=sct[:, 2 * k:2 * k + 1], rhs=pc[:, k, :],
                                        start=(k == 0), stop=(k == 7)),
               reads=[b_sct, bpc], writes=[bpr])
        op("dve", lambda e: e.tensor_tensor(out=modrow[0:1, j * 512:(j + 1) * 512], in0=pr[0:1, :],
                                            in1=badas[0:1, j * 512:(j + 1) * 512], op=ALU.add),
           reads=[bpr, b_badas], writes=[b_modrow])
    op("dve", lambda e: e.scalar_tensor_tensor(out=s1row[:], in0=modrow[0:1, 1024:2048], scalar=1.0,
                                                in1=gmixs[0:1, :], op0=ALU.add, op1=ALU.mult),
       reads=[b_modrow, b_gmixs], writes=[b_s1row])
    for hf in range(2):
        pr, bpr = _rot(ps_row, hf), _rot(b_ps_row, hf)
        op("pe", lambda e: e.matmul(pr[:, :], lhsT=ones_f[0:1, :], rhs=s1row[0:1, hf * 512:(hf + 1) * 512],
                                    start=True, stop=True), reads=[b_ones_f, b_s1row], writes=[bpr])
        op("act", lambda e: e.activation(out=S1b[:, hf * 512:(hf + 1) * 512], in_=pr[:, :], func=AF.Copy),
           reads=[bpr], writes=[b_S1b])
    pr, bpr = ps_row[0], b_ps_row[0]
    for k in range(8):
        op("pe", lambda e: e.matmul(pr[:, 2 * k:2 * k + 2], lhsT=modrow[0:1, k * 128:(k + 1) * 128],
                                    rhs=ones_f[0:1, 0:2], start=True, stop=True),
           reads=[b_ones_f, b_modrow], writes=[bpr])
    op("dve", lambda e: e.tensor_copy(out=sh1T[:], in_=pr[:, 0:16]), reads=[bpr], writes=[b_sh1T])
    pr, bpr = ps_row[1], b_ps_row[1]
    for k in range(8):
        op("pe", lambda e: e.matmul(pr[0:1, :], lhsT=sh1T[:, 2 * k:2 * k + 1], rhs=Win[:, k, :],
                                    start=(k == 0), stop=(k == 7)), reads=[b_sh1T, b_Win], writes=[bpr])
    op("act", lambda e: e.activation(out=bin_bf[:], in_=pr[0:1, :], func=AF.Copy), reads=[bpr], writes=[b_bin])
    P.pop_scope()

    def dbl(name, shape, dt, n=2):
        return [P.sb(f"{name}{i}", shape, dt) for i in range(n)], [P.buf() for _ in range(n)]

    def dblp(name, shape, dt, n=2):
        return [P.ps(f"{name}{i}", shape, dt) for i in range(n)], [P.pbuf() for _ in range(n)]
    xt, b_xt = dbl("xt", [128, 1024], F32, 4)
    hb, b_hb = dbl("hb", [128, 1024], BF16)
    junk = P.sb("junk", [128, 1024], BF16); b_junk = P.buf()
    ssx, b_ssx = dbl("ssx", [128, 1], F32)
    sqx, b_sqx = dbl("sqx", [128, 1], F32)
    rx, b_rx = dbl("rx", [128, 1], F32)
    hTs = P.sb("hTs", [128, 8, 1024], BF16); b_hTs = [P.buf() for _ in range(8)]
    cs, b_cs = dbl("cs", [128, 8, 32], F32)
    sn, b_sn = dbl("sn", [128, 8, 32], F32)
    qkr, b_qkr = dbl("qkr", [128, 4, 2, 32], F32)
    r1, b_r1 = dbl("r1", [128, 4, 32], F32)
    r2, b_r2 = dbl("r2", [128, 4, 32], F32)
    r3, b_r3 = dbl("r3", [128, 4, 32], F32)
    r4, b_r4 = dbl("r4", [128, 4, 32], F32)
    ksum = P.sb("ksum", [64, 2, 2], F32); b_ksum = [P.buf(), P.buf()]
    kmT = P.sb("kmT", [64, 2, 32], F32); b_kmT = P.buf()
    qTt, b_qTt = dbl("qTt", [64, 2, 128], F32)
    Gt, b_Gt = dbl("Gt", [128, 2, 32], F32)
    mx8, b_mx8 = dbl("mx8", [128, 2, 8], F32)
    nm, b_nm = dbl("nm", [128, 2, 32], F32)
    Qtok, b_Qtok = dbl("Qtok", [128, 2, 96], BF16)
    pxt = P.ps("pxt", [128, 8, 128], BF16); b_pxt = P.pbuf()
    pqkv, b_pqkv = dblp("pqkv", [128, 512], F32)
    pT, b_pT = dblp("pT", [128, 4, 128], F32)
    pgq, b_pgq = dblp("pgq", [128, 512], F32)
    pu = P.ps("pu", [128, 4, 128], F32); b_pu = P.pbuf()
    pqa = [pgq[i][:, 256:512].bitcast(BF16).rearrange("p (h n) -> p h n", h=4) for i in range(2)]
    for i in range(2):
        op("dve", lambda e: e.memset(Gt[i][:], -1.0e30), writes=[b_Gt[i]])
    op("dve", lambda e: e.memset(kmT[:], 0.0), writes=[b_kmT])

    def load_x(t):
        x_, bx = _rot(xt, t), _rot(b_xt, t)
        dma(lambda e: e.dma_start(out=x_[:], in_=xb[t * 128:(t + 1) * 128, :]), writes=[bx])

    def stage_a(t):
        S_, ti = t // 8, t % 8
        i = t % 2
        x_, bx = _rot(xt, t), _rot(b_xt, t)
        pq_, bpq = pqkv[i], b_pqkv[i]
        if ti == 0:
            c_, bc_ = _rot(cs, S_), _rot(b_cs, S_)
            s_, bs_ = _rot(sn, S_), _rot(b_sn, S_)
            dma(lambda e: e.dma_start(out=c_[:].rearrange("p i d -> p (i d)"), in_=cosl[:, S_ * 256:(S_ + 1) * 256]), writes=[bc_])
            dma(lambda e: e.dma_start(out=s_[:].rearrange("p i d -> p (i d)"), in_=sinl[:, S_ * 256:(S_ + 1) * 256]), writes=[bs_])
        op("act", lambda e: e.activation(out=junk[:], in_=x_[:], func=AF.Square, accum_out=ssx[i][:, 0:1]),
           reads=[bx], writes=[b_junk, b_ssx[i]])
        yield
        op("act", lambda e: e.activation(out=sqx[i][:], in_=ssx[i][:], func=AF.Sqrt, scale=1.0 / 1024.0, bias=EPS),
           reads=[b_ssx[i]], writes=[b_sqx[i]])
        yield
        op("dve", lambda e: e.reciprocal(out=rx[i][:], in_=sqx[i][:]), reads=[b_sqx[i]], writes=[b_rx[i]])
        yield
        op("dve", lambda e: e.scalar_tensor_tensor(out=hb[i][:], in0=x_[:], scalar=rx[i][:, 0:1], in1=S1b[:],
                                                    op0=ALU.mult, op1=ALU.mult),
           reads=[bx, b_rx[i], b_S1b], writes=[b_hb[i]])
        yield
        for k in range(8):
            op("pe", lambda e: e.transpose(pxt[:, k, :], hb[i][:, k * 128:(k + 1) * 128], ident_bf[:]),
               reads=[b_hb[i], b_ident], writes=[b_pxt])
        yield
        op("act", lambda e: e.activation(out=hTs[:, :, ti * 128:(ti + 1) * 128], in_=pxt[:], func=AF.Copy),
           reads=[b_pxt], writes=[b_hTs[ti]])
        yield
        op("pe", lambda e: e.matmul(pq_[:, 0:384], lhsT=ones_bf[0:1, 0:128], rhs=bin_bf[0:1, 0:384], start=True, stop=False),
           reads=[b_ones_bf, b_bin], writes=[bpq])
        for k in range(8):
            op("pe", lambda e: e.matmul(pq_[:, 0:384], lhsT=hTs[:, k, ti * 128:(ti + 1) * 128], rhs=Win[:, k, 0:384],
                                        start=False, stop=(k == 7)),
               reads=[b_hTs[ti], b_Win], writes=[bpq])
        yield

    def u_proj(S_):
        for half in range(2):
            for s4 in range(4):
                s = half * 4 + s4
                op("pe", lambda e: e.matmul(pu[:, s4, :], lhsT=ones_bf[0:1, 0:128], rhs=bin_bf[0:1, 384:512], start=True, stop=False),
                   reads=[b_ones_bf, b_bin], writes=[b_pu])
                for k in range(8):
                    lhs = hTs[:, k, :].rearrange("p (c s) -> p s c", s=8)[:, s, :]
                    op("pe", lambda e: e.matmul(pu[:, s4, :], lhsT=lhs, rhs=Win[:, k, 384:512], start=False, stop=(k == 7)),
                       reads=b_hTs + [b_Win], writes=[b_pu])
            op("act", lambda e: e.activation(out=U_tok[:, S_].rearrange("p g s m -> p s g m")[:, half * 4:half * 4 + 4],
                                             in_=pu[:].rearrange("p s (g m) -> p s g m", g=8), func=AF.Copy),
               reads=[b_pu], writes=[b_U[S_]])

    def stage_b1(t):
        S_, ti = t // 8, t % 8
        i = t % 2
        c_, bc_ = _rot(cs, S_), _rot(b_cs, S_)
        s_, bs_ = _rot(sn, S_), _rot(b_sn, S_)
        pq_, bpq = pqkv[i], b_pqkv[i]
        pq = pq_[:, 0:256].rearrange("p (f two d) -> p f two d", f=4, two=2)
        cb = c_[:, ti, :].unsqueeze(1).to_broadcast([128, 4, 32])
        sb_ = s_[:, ti, :].unsqueeze(1).to_broadcast([128, 4, 32])
        op("dve", lambda e: e.tensor_tensor(out=r1[i][:], in0=pq[:, :, 0, :], in1=cb, op=ALU.mult), reads=[bpq, bc_], writes=[b_r1[i]])
        yield
        op("dve", lambda e: e.tensor_tensor(out=r2[i][:], in0=pq[:, :, 1, :], in1=sb_, op=ALU.mult), reads=[bpq, bs_], writes=[b_r2[i]])
        yield
        op("dve", lambda e: e.tensor_tensor(out=r3[i][:], in0=pq[:, :, 0, :], in1=sb_, op=ALU.mult), reads=[bpq, bs_], writes=[b_r3[i]])
        yield
        op("dve", lambda e: e.tensor_tensor(out=r4[i][:], in0=pq[:, :, 1, :], in1=cb, op=ALU.mult), reads=[bpq, bc_], writes=[b_r4[i]])
        yield
        op("act", lambda e: e.activation(out=Vaug[:, t, :, 0:64], in_=pq_[:, 256:384].rearrange("p (h d) -> p h d", h=2),
                                         func=AF.Copy), reads=[bpq], writes=[b_V[t]])
        yield

    def stage_b2(t):
        i = t % 2
        j = t // 2
        par = t % 2
        tok = slice(t * 128, (t + 1) * 128)
        pT_, bpT = pT[i], b_pT[i]
        pg_, bpg = pgq[i], b_pgq[i]
        pqa_ = pqa[i]
        op("pool", lambda e: e.tensor_tensor(out=qkr[i][:, :, 0, :], in0=r1[i][:], in1=r2[i][:], op=ALU.subtract),
           reads=[b_r1[i], b_r2[i]], writes=[b_qkr[i]])
        yield
        op("pool", lambda e: e.tensor_tensor(out=qkr[i][:, :, 1, :], in0=r3[i][:], in1=r4[i][:], op=ALU.add),
           reads=[b_r3[i], b_r4[i]], writes=[b_qkr[i]])
        yield
        qkf = qkr[i][:].rearrange("p f two d -> p (f two d)")
        for h in range(2):
            op("pe", lambda e: e.transpose(pT_[0:64, h, :], qkf[:, 128 + h * 64:128 + (h + 1) * 64], ident_f[:]),
               reads=[b_qkr[i], b_identf], writes=[bpT])
        if j >= 4:
            for h in range(2):
                op("pe", lambda e: e.transpose(pT_[0:64, 2 + h, :], qkf[:, h * 64:(h + 1) * 64], ident_f[:]),
                   reads=[b_qkr[i], b_identf], writes=[bpT])
        yield
        op("act", lambda e: e.activation(out=KaT[0:64, :, tok], in_=pT_[0:64, 0:2, :], func=AF.Copy),
           reads=[bpT], writes=[b_KaT[t]])
        yield
        op("dve", lambda e: e.tensor_reduce(out=ksum[:, par, :], in_=pT_[0:64, 0:2, :], axis=mybir.AxisListType.X, op=ALU.add),
           reads=[bpT], writes=[b_ksum[par]])
        yield
        if j >= 4:
            op("act", lambda e: e.activation(out=qTt[i][:], in_=pT_[0:64, 2:4, :], func=AF.Copy), reads=[bpT], writes=[b_qTt[i]])
            yield
            for h in range(2):
                op("pe", lambda e: e.matmul(pg_[:, h * 32:(h + 1) * 32], lhsT=qTt[i][:, h, :], rhs=kmT[:, h, :], start=True, stop=True),
                   reads=[b_qTt[i], b_kmT], writes=[bpg])
            yield
            op("dve", lambda e: e.tensor_copy(out=Gt[i][:, :, 0:j], in_=pg_[:, 0:64].rearrange("p (h n) -> p h n", h=2)[:, :, 0:j]),
               reads=[bpg], writes=[b_Gt[i]])
            yield
            for h in range(2):
                op("dve", lambda e: e.max(out=mx8[i][:, h, :], in_=Gt[i][:, h, :]), reads=[b_Gt[i]], writes=[b_mx8[i]])
                yield
            for h in range(2):
                op("dve", lambda e: e.tensor_scalar(out=nm[i][:, h, :], in0=Gt[i][:, h, :], scalar1=mx8[i][:, h, 2:3], scalar2=1.0,
                                                    op0=ALU.is_ge, op1=ALU.subtract), reads=[b_Gt[i], b_mx8[i]], writes=[b_nm[i]])
                yield
            op("dve", lambda e: e.memset(nm[i][:, :, j:j + 1], 0.0), writes=[b_nm[i]])
            yield
            op("dve", lambda e: e.tensor_scalar(out=Qtok[i][:, :, 64:96], in0=nm[i][:], scalar1=-NEGBIG, scalar2=None, op0=ALU.mult),
               reads=[b_nm[i]], writes=[b_Qtok[i]])
            yield
        else:
            op("dve", lambda e: e.memset(Qtok[i][:, :, 64:96], NEGBIG), writes=[b_Qtok[i]])
            yield
            op("dve", lambda e: e.memset(Qtok[i][:, :, 64:64 + j + 1], 0.0), writes=[b_Qtok[i]])
            yield
        op("pool", lambda e: e.tensor_scalar(out=Qtok[i][:, :, 0:64], in0=qkf[:, 0:128].rearrange("p (h d) -> p h d", h=2),
                                             scalar1=0.125, scalar2=None, op0=ALU.mult),
           reads=[b_qkr[i]], writes=[b_Qtok[i]])
        yield
        for h in range(2):
            op("pe", lambda e: e.transpose(pqa_[0:96, h, :], Qtok[i][:, h, :], ident_bf[:]), reads=[b_Qtok[i], b_ident], writes=[bpg])
        yield
        op("act", lambda e: e.activation(out=QaT[0:96, :, tok], in_=pqa_[0:96, 0:2, :], func=AF.Copy),
           reads=[bpg], writes=[b_QaT[t]])
        yield
        if par == 1:
            op("dve", lambda e: e.tensor_tensor(out=kmT[:, :, j], in0=ksum[:, 0, :], in1=ksum[:, 1, :], op=ALU.add),
               reads=[b_ksum[0], b_ksum[1]], writes=[b_kmT])
            yield

    def lockstep(*gens):
        gens = [g for g in gens if g is not None]
        while gens:
            alive = []
            for g in gens:
                try:
                    next(g)
                    alive.append(g)
                except StopIteration:
                    pass
            gens = alive

    import os as _os
    if _os.environ.get("SEQ_LOCK"):
        def lockstep(*gens):
            for g in gens:
                if g is not None:
                    for _ in g:
                        pass
    ntl = dbg_tiles
    for t in range(min(4, ntl)):
        load_x(t)
    if ntl > 0:
        lockstep(stage_a(0))
        if ntl > 1:
            lockstep(stage_a(1))
    for t in range(0, ntl, 2):
        lockstep(stage_b1(t), stage_b1(t + 1) if t + 1 < ntl else None)
        if (t + 2) % 8 == 0 and with_ssm:
            u_proj(t // 8)
        for tt_ in (t + 4, t + 5):
            if tt_ < ntl:
                load_x(tt_)
        def chain(*gs):
            for g_ in gs:
                if g_ is not None:
                    yield from g_
        lockstep(chain(stage_a(t + 2) if t + 2 < ntl else None, stage_a(t + 3) if t + 3 < ntl else None),
                 stage_b2(t), stage_b2(t + 1) if t + 1 < ntl else None)
    P.pop_scope()

    P.push_scope()
    cm = P.sb("cm", [128, 4, 512], BF16); b_cm = P.buf()
    P.push_scope()
    stgM = P.sb("stgM", [128, 2048], F32); b_stgM = P.buf()
    dma(lambda e: e.dma_start(out=stgM[:], in_=cmask[:, :]), writes=[b_stgM])
    op("dve", lambda e: e.tensor_copy(out=cm[:].rearrange("p a c -> p (a c)"), in_=stgM[:]), reads=[b_stgM], writes=[b_cm])
    P.pop_scope()
    Pt = [P.sb(f"Pt{i}", [128, 512], BF16) for i in range(3)]; b_Pt = [P.buf() for _ in range(3)]
    Osb = [P.sb(f"Osb{i}", [65, 512], F32) for i in range(2)]; b_Osb = [P.buf() for _ in range(2)]
    pS = [P.ps(f"pS{i}", [128, 512], F32) for i in range(3)]; b_pS = [P.pbuf() for _ in range(3)]
    pO = [P.ps(f"pO{i}", [128, 512], F32) for i in range(2)]; b_pO = [P.pbuf() for _ in range(2)]
    cnt = 0
    gi = 0
    for h in range(2 if dbg_attn else 0):
        for G in range(16 if dbg_attn is True else dbg_attn):
            po, bpo = _rot(pO, gi), _rot(b_pO, gi)
            ob, bob = _rot(Osb, gi), _rot(b_Osb, gi)
            gi += 1
            nkt = 4 * G + 4
            qs = slice(G * 512, (G + 1) * 512)
            qbufs = b_QaT[4 * G:4 * G + 4]

            def s_mm(kt, c):
                ps_, bps = _rot(pS, c), _rot(b_pS, c)
                op("pe", lambda e: e.matmul(ps_[:], lhsT=KaT[0:96, h, kt * 128:(kt + 1) * 128], rhs=QaT[0:96, h, qs],
                                            start=True, stop=True),
                   reads=[b_KaT[kt], b_Kind] + qbufs, writes=[bps])
            s_mm(0, cnt)
            for kt in range(nkt):
                c = cnt + kt
                if kt + 1 < nkt:
                    s_mm(kt + 1, c + 1)
                ps_, bps = _rot(pS, c), _rot(b_pS, c)
                p_, bp_ = _rot(Pt, c), _rot(b_Pt, c)
                op("act", lambda e: e.activation(out=p_[:], in_=ps_[:], func=AF.Exp), reads=[bps], writes=[bp_])
                a = kt - 4 * G
                c0 = 0
                if a >= 0:
                    op("pool", lambda e: e.tensor_tensor(out=p_[:], in0=p_[:], in1=cm[:, a, :], op=ALU.mult),
                       reads=[bp_, b_cm], writes=[bp_])
                    c0 = a * 128
                op("pe", lambda e: e.matmul(po[0:65, c0:512], lhsT=Vaug[:, kt, h, :], rhs=p_[:, c0:512],
                                            start=(kt == 0), stop=(kt == nkt - 1)),
                   reads=[bp_, b_V[kt], b_Vones], writes=[bpo])
            cnt += nkt
            op("dve", lambda e: e.tensor_copy(out=ob[:], in_=po[0:65, :]), reads=[bpo], writes=[bob])
            dma(lambda e: e.dma_start(out=attn_o[h * 65:(h + 1) * 65, G * 512:(G + 1) * 512], in_=ob[:]), reads=[bob])
    P.pop_scope()
    P.pop_scope()

    if with_ssm:
        build_ssm(nc, P, U_tok, b_U, y_o, ident_bf, b_ident, ident_f, b_identf)
    P.finish()
    return nc


def rope_tables():
    half = 32
    inv = (10000.0 ** (-np.arange(half, dtype=np.float32) / half)).astype(np.float32)
    ang = np.arange(L_SEQ, dtype=np.float32)[:, None] * inv[None, :]
    cos, sin = np.cos(ang).astype(np.float32), np.sin(ang).astype(np.float32)
    cl = np.ascontiguousarray(cos.reshape(NTILE, 128, half).transpose(1, 0, 2).reshape(128, NTILE * half))
    sl = np.ascontiguousarray(sin.reshape(NTILE, 128, half).transpose(1, 0, 2).reshape(128, NTILE * half))
    return cl, sl


def const_tables():
    ind = np.zeros((32, L_SEQ), np.float32)
    for j in range(32):
        ind[j, j * 256:(j + 1) * 256] = 1.0
    cm = np.zeros((128, 4, 512), np.float32)
    kk = np.arange(128)[:, None]
    qq = np.arange(128)[None, :]
    tri = (kk <= qq).astype(np.float32)
    for a in range(4):
        for bq in range(4):
            if bq > a:
                cm[:, a, bq * 128:(bq + 1) * 128] = 1.0
            elif bq == a:
                cm[:, a, bq * 128:(bq + 1) * 128] = tri
    return ind, cm.reshape(128, 2048)


def phase_ab_inputs(inp, b, r):
    c = inp["c"][b]
    cT = np.ascontiguousarray(c.reshape(8, 128).T)
    cT2 = np.repeat(cT[:, :, None], 2, axis=2).reshape(128, 16)
    w = inp["w_in"][0]
    cols = np.concatenate([np.arange(128 * r, 128 * r + 128), 512 + np.arange(128 * r, 128 * r + 128),
                           1024 + np.arange(128 * r, 128 * r + 128), 1536 + np.arange(128 * r, 128 * r + 128)])
    cl, sl = rope_tables()
    ind, cm = const_tables()
    d = {
        "xb": np.ascontiguousarray(inp["x"][b]),
        "cT2": np.ascontiguousarray(cT2),
        "wada_a": np.ascontiguousarray(inp["w_ada"][0][:, 0:2048]),
        "bada_a": np.ascontiguousarray(inp["b_ada"][0][None, 0:2048]),
        "gmix": np.ascontiguousarray(inp["g_mix"][0][None, :]),
        "win": np.ascontiguousarray(w[:, cols]),
        "cosl": cl, "sinl": sl, "ind": ind, "cmask": cm, "ident": _ident(),
    }
    d.update(ssm_inputs(inp, r))
    return d


KV_F, KV_G, KV_O, KV_E, KV_A, KV_H = 0, 8, 16, 24, 32, 42
NKV = 43
TWO_PI = 2.0 * math.pi
CW1 = 6.28125
CW2 = TWO_PI - CW1


def ssm_kvec():
    kv = np.zeros(NKV, np.float32)
    for s in range(8):
        kv[KV_F + s] = -s
        kv[KV_G + s] = s
        kv[KV_O + s] = s + 1
        kv[KV_E + s] = 7 - s
    for i in range(10):
        kv[KV_A + i] = 8.0 * (2 ** i)
    kv[KV_H] = 0.5
    return kv


def build_ssm(nc, P, U_tok, b_U, y_o, ident_bf, b_ident, ident_f, b_identf):
    def din(name, shape, dt=F32):
        return nc.dram_tensor(name, list(shape), dt, kind="ExternalInput").ap()
    sp_small = din("ssm_small", [128, 8 * 4 + NKV + 2 + 8])
    sp_bc = din("ssm_bc", [128, 4 * 128])
    sp_mat = din("ssm_mat", [128, 256])
    op, dma = P.op, P.dma
    P.push_scope()
    small = P.sb("ssm_small_sb", [128, 8 * 4 + NKV + 2 + 8], F32); b_small = P.buf()
    bc = P.sb("ssm_bc_sb", [128, 4, 8, 16], F32); b_bc = P.buf()
    mats = P.sb("ssm_mat_sb", [128, 2, 128], F32); b_mats = P.buf()
    dma(lambda e: e.dma_start(out=small[:], in_=sp_small[:, :]), writes=[b_small])
    dma(lambda e: e.dma_start(out=bc[:].rearrange("p a g m -> p (a g m)"), in_=sp_bc[:, :]), writes=[b_bc])
    dma(lambda e: e.dma_start(out=mats[:].rearrange("p a n -> p (a n)"), in_=sp_mat[:, :]), writes=[b_mats])
    lamre, lamim, logdt, dsk = small[:, 0:8], small[:, 8:16], small[:, 16:24], small[:, 24:32]
    kvec = small[:, 32:32 + NKV]
    sgn_a, sgn_b = small[:, 32 + NKV:33 + NKV], small[:, 33 + NKV:34 + NKV]
    smask, P2 = mats[:, 0, :], mats[:, 1, :]
    Bs1, Bs2, Cs1, Cs2 = bc[:, 0], bc[:, 1], bc[:, 2], bc[:, 3]

    def T(name, shape, dt=F32):
        return P.sb(name, shape, dt), P.buf()

    def dv(fn, reads, writes):
        return op("dve", fn, reads=reads, writes=writes)

    dt_, b_dt = T("s_dt", [128, 8])
    a_, b_a = T("s_a", [128, 8])
    th, b_th = T("s_th", [128, 8])
    op("act", lambda e: e.activation(out=dt_[:], in_=logdt, func=AF.Exp), reads=[b_small], writes=[b_dt])
    dv(lambda e: e.tensor_tensor(out=a_[:], in0=lamre, in1=dt_[:], op=ALU.mult), [b_small, b_dt], [b_a])
    dv(lambda e: e.tensor_tensor(out=th[:], in0=lamim, in1=dt_[:], op=ALU.mult), [b_small, b_dt], [b_th])
    KA, b_KA = T("s_KA", [128, 8, NKV])
    KT, b_KT = T("s_KT", [128, 8, NKV])
    kb = kvec.unsqueeze(1).to_broadcast([128, 8, NKV])
    dv(lambda e: e.tensor_tensor(out=KA[:], in0=a_[:].unsqueeze(2).to_broadcast([128, 8, NKV]), in1=kb, op=ALU.mult),
       [b_a, b_small], [b_KA])
    dv(lambda e: e.tensor_tensor(out=KT[:], in0=th[:].unsqueeze(2).to_broadcast([128, 8, NKV]), in1=kb, op=ALU.mult),
       [b_th, b_small], [b_KT])
    MAG, b_MAG = T("s_MAG", [128, 8, NKV])
    op("act", lambda e: e.activation(out=MAG[:], in_=KA[:], func=AF.Exp), reads=[b_KA], writes=[b_MAG])
    ni, b_ni = T("s_ni", [128, 8, NKV], I32)
    nf, b_nf = T("s_nf", [128, 8, NKV])
    rr, b_rr = T("s_rr", [128, 8, NKV])
    uu, b_uu = T("s_uu", [128, 8, NKV])
    SIN, b_SIN = T("s_SIN", [128, 8, NKV])
    COS, b_COS = T("s_COS", [128, 8, NKV])

    def sin_of(dst, b_dst, shift):
        dv(lambda e: e.tensor_scalar(out=uu[:], in0=KT[:], scalar1=shift, scalar2=1.0 / TWO_PI, op0=ALU.add, op1=ALU.mult),
           [b_KT], [b_uu])
        dv(lambda e: e.tensor_copy(out=ni[:], in_=uu[:]), [b_uu], [b_ni])
        dv(lambda e: e.tensor_copy(out=nf[:], in_=ni[:]), [b_ni], [b_nf])
        dv(lambda e: e.scalar_tensor_tensor(out=rr[:], in0=nf[:], scalar=-CW1, in1=KT[:], op0=ALU.mult, op1=ALU.add),
           [b_nf, b_KT], [b_rr])
        dv(lambda e: e.scalar_tensor_tensor(out=rr[:], in0=nf[:], scalar=-CW2, in1=rr[:], op0=ALU.mult, op1=ALU.add),
           [b_nf, b_rr], [b_rr])
        dv(lambda e: e.tensor_scalar(out=rr[:], in0=rr[:], scalar1=shift, scalar2=math.pi, op0=ALU.add, op1=ALU.min),
           [b_rr], [b_rr])
        dv(lambda e: e.tensor_scalar(out=rr[:], in0=rr[:], scalar1=-math.pi, scalar2=None, op0=ALU.max), [b_rr], [b_rr])
        op("act", lambda e: e.activation(out=dst[:], in_=rr[:], func=AF.Sin), reads=[b_rr], writes=[b_dst])
    sin_of(SIN, b_SIN, 0.0)
    sin_of(COS, b_COS, math.pi / 2.0)
    PR, b_PR = T("s_PR", [128, 8, NKV])
    PI_, b_PI = T("s_PI", [128, 8, NKV])
    dv(lambda e: e.tensor_tensor(out=PR[:], in0=MAG[:], in1=COS[:], op=ALU.mult), [b_MAG, b_COS], [b_PR])
    dv(lambda e: e.tensor_tensor(out=PI_[:], in0=MAG[:], in1=SIN[:], op=ALU.mult), [b_MAG, b_SIN], [b_PI])
    em1, b_em1 = T("s_em1", [128, 8])
    tq, b_tq = T("s_tq", [128, 8])
    dv(lambda e: e.tensor_scalar(out=tq[:], in0=a_[:], scalar1=0.25, scalar2=1.0, op0=ALU.mult, op1=ALU.add), [b_a], [b_tq])
    dv(lambda e: e.tensor_tensor(out=tq[:], in0=tq[:], in1=a_[:], op=ALU.mult), [b_tq, b_a], [b_tq])
    dv(lambda e: e.tensor_scalar(out=tq[:], in0=tq[:], scalar1=1.0 / 3.0, scalar2=1.0, op0=ALU.mult, op1=ALU.add), [b_tq], [b_tq])
    dv(lambda e: e.tensor_tensor(out=tq[:], in0=tq[:], in1=a_[:], op=ALU.mult), [b_tq, b_a], [b_tq])
    dv(lambda e: e.tensor_scalar(out=tq[:], in0=tq[:], scalar1=0.5, scalar2=1.0, op0=ALU.mult, op1=ALU.add), [b_tq], [b_tq])
    dv(lambda e: e.tensor_tensor(out=em1[:], in0=tq[:], in1=a_[:], op=ALU.mult), [b_tq, b_a], [b_em1])
    cth, sth, shalf = COS[:, :, KV_G + 1], SIN[:, :, KV_G + 1], SIN[:, :, KV_H]
    re1, b_re1 = T("s_re1", [128, 8])
    im1, b_im1 = T("s_im1", [128, 8])
    w1, b_w1 = T("s_w1", [128, 8])
    w2, b_w2 = T("s_w2", [128, 8])
    dv(lambda e: e.tensor_tensor(out=w1[:], in0=shalf, in1=shalf, op=ALU.mult), [b_SIN], [b_w1])
    dv(lambda e: e.tensor_tensor(out=re1[:], in0=em1[:], in1=cth, op=ALU.mult), [b_em1, b_COS], [b_re1])
    dv(lambda e: e.scalar_tensor_tensor(out=re1[:], in0=w1[:], scalar=-2.0, in1=re1[:], op0=ALU.mult, op1=ALU.add),
       [b_w1, b_re1], [b_re1])
    dv(lambda e: e.scalar_tensor_tensor(out=im1[:], in0=em1[:], scalar=1.0, in1=sth, op0=ALU.add, op1=ALU.mult),
       [b_em1, b_SIN], [b_im1])
    den, b_den = T("s_den", [128, 8])
    dv(lambda e: e.tensor_tensor(out=den[:], in0=lamre, in1=lamre, op=ALU.mult), [b_small], [b_den])
    dv(lambda e: e.tensor_tensor(out=w1[:], in0=lamim, in1=lamim, op=ALU.mult), [b_small], [b_w1])
    dv(lambda e: e.tensor_tensor(out=den[:], in0=den[:], in1=w1[:], op=ALU.add), [b_den, b_w1], [b_den])
    dv(lambda e: e.reciprocal(out=den[:], in_=den[:]), [b_den], [b_den])
    cr, b_cr = T("s_cr", [128, 8])
    ci, b_ci = T("s_ci", [128, 8])
    dv(lambda e: e.tensor_tensor(out=w1[:], in0=re1[:], in1=lamre, op=ALU.mult), [b_re1, b_small], [b_w1])
    dv(lambda e: e.tensor_tensor(out=w2[:], in0=im1[:], in1=lamim, op=ALU.mult), [b_im1, b_small], [b_w2])
    dv(lambda e: e.tensor_tensor(out=w1[:], in0=w1[:], in1=w2[:], op=ALU.add), [b_w1, b_w2], [b_w1])
    dv(lambda e: e.tensor_tensor(out=cr[:], in0=w1[:], in1=den[:], op=ALU.mult), [b_w1, b_den], [b_cr])
    dv(lambda e: e.tensor_tensor(out=w1[:], in0=im1[:], in1=lamre, op=ALU.mult), [b_im1, b_small], [b_w1])
    dv(lambda e: e.tensor_tensor(out=w2[:], in0=re1[:], in1=lamim, op=ALU.mult), [b_re1, b_small], [b_w2])
    dv(lambda e: e.tensor_tensor(out=w1[:], in0=w1[:], in1=w2[:], op=ALU.subtract), [b_w1, b_w2], [b_w1])
    dv(lambda e: e.tensor_tensor(out=ci[:], in0=w1[:], in1=den[:], op=ALU.mult), [b_w1, b_den], [b_ci])
    bb1, b_bb1 = T("s_bb1", [128, 8, 16])
    bb2, b_bb2 = T("s_bb2", [128, 8, 16])
    q1, b_q1 = T("s_q1", [128, 8, 16])
    q2, b_q2 = T("s_q2", [128, 8, 16])
    crb = cr[:].unsqueeze(2).to_broadcast([128, 8, 16])
    cib = ci[:].unsqueeze(2).to_broadcast([128, 8, 16])
    dv(lambda e: e.tensor_tensor(out=q1[:], in0=crb, in1=Bs1, op=ALU.mult), [b_cr, b_bc], [b_q1])
    dv(lambda e: e.tensor_tensor(out=q2[:], in0=cib, in1=Bs2, op=ALU.mult), [b_ci, b_bc], [b_q2])
    dv(lambda e: e.scalar_tensor_tensor(out=bb1[:].rearrange("p g m -> p (g m)"), in0=q2[:].rearrange("p g m -> p (g m)"),
                                         scalar=sgn_a, in1=q1[:].rearrange("p g m -> p (g m)"), op0=ALU.mult, op1=ALU.add),
       [b_q1, b_q2, b_small], [b_bb1])
    dv(lambda e: e.tensor_tensor(out=q1[:], in0=crb, in1=Bs2, op=ALU.mult), [b_cr, b_bc], [b_q1])
    dv(lambda e: e.tensor_tensor(out=q2[:], in0=cib, in1=Bs1, op=ALU.mult), [b_ci, b_bc], [b_q2])
    dv(lambda e: e.scalar_tensor_tensor(out=bb2[:].rearrange("p g m -> p (g m)"), in0=q2[:].rearrange("p g m -> p (g m)"),
                                         scalar=sgn_b, in1=q1[:].rearrange("p g m -> p (g m)"), op0=ALU.mult, op1=ALU.add),
       [b_q1, b_q2, b_small], [b_bb2])
    CA, b_CA = T("s_CA", [128, 8, 16])
    CB, b_CB = T("s_CB", [128, 8, 16])
    dv(lambda e: e.tensor_scalar(out=CA[:], in0=Cs1, scalar1=sgn_b, scalar2=None, op0=ALU.mult), [b_bc, b_small], [b_CA])
    dv(lambda e: e.tensor_scalar(out=CB[:], in0=Cs2, scalar1=-1.0, scalar2=None, op0=ALU.mult), [b_bc], [b_CB])
    Fm, b_Fm = T("s_F", [128, 8, 8, 16])
    Fe, b_Fe = T("s_Fe", [128, 8, 8, 16])
    Gm, b_Gm = T("s_G", [128, 8, 8, 16])
    Om, b_Om = T("s_O", [128, 8, 8, 16])
    z1, b_z1 = T("s_z1", [128, 8, 8, 16])
    z2, b_z2 = T("s_z2", [128, 8, 8, 16])

    def outer(dst, bdst, kv0, v1, bv1, v2, bv2, sgn):
        pr = PR[:, :, kv0:kv0 + 8].unsqueeze(3).to_broadcast([128, 8, 8, 16])
        pi = PI_[:, :, kv0:kv0 + 8].unsqueeze(3).to_broadcast([128, 8, 8, 16])
        dv(lambda e: e.tensor_tensor(out=z1[:], in0=pr, in1=v1[:].unsqueeze(2).to_broadcast([128, 8, 8, 16]), op=ALU.mult),
           [b_PR, bv1], [b_z1])
        dv(lambda e: e.tensor_tensor(out=z2[:], in0=pi, in1=v2[:].unsqueeze(2).to_broadcast([128, 8, 8, 16]), op=ALU.mult),
           [b_PI, bv2], [b_z2])
        fl = "p g s m -> p (g s m)"
        if sgn is None:
            dv(lambda e: e.tensor_tensor(out=dst[:].rearrange(fl), in0=z1[:].rearrange(fl), in1=z2[:].rearrange(fl), op=ALU.add),
               [b_z1, b_z2], [bdst])
        else:
            dv(lambda e: e.scalar_tensor_tensor(out=dst[:].rearrange(fl), in0=z2[:].rearrange(fl), scalar=sgn,
                                                 in1=z1[:].rearrange(fl), op0=ALU.mult, op1=ALU.add),
               [b_z1, b_z2, b_small], [bdst])
    outer(Fm, b_Fm, KV_F, bb1, b_bb1, bb2, b_bb2, sgn_a)
    outer(Fe, b_Fe, KV_E, bb1, b_bb1, bb2, b_bb2, sgn_a)
    outer(Gm, b_Gm, KV_G, CA, b_CA, CB, b_CB, None)
    outer(Om, b_Om, KV_O, CA, b_CA, CB, b_CB, None)
    PIA, b_PIA = T("s_PIA", [128, 8, 10])
    dv(lambda e: e.tensor_scalar(out=PIA[:], in0=PI_[:, :, KV_A:KV_A + 10], scalar1=sgn_b, scalar2=None, op0=ALU.mult),
       [b_PI, b_small], [b_PIA])

    Yout = P.sb("s_Yout", [128, 8, 8, 128], F32); b_Yout = P.buf()
    Ug = P.sb("s_Ug", [128, 1024], BF16); b_Ug = P.buf()
    H = P.sb("s_H", [128, 1024], BF16); b_H = P.buf()
    Ysb = P.sb("s_Ysb", [128, 1024], F32); b_Ysb = P.buf()
    Mbf = P.sb("s_Mbf", [128, 128], BF16); b_Mbf = P.buf()
    Ebf = P.sb("s_Ebf", [128, 128], BF16); b_Ebf = P.buf()
    mtmp = P.sb("s_mtmp", [128, 128], F32); b_mtmp = P.buf()
    Amat = P.sb("s_Amat", [128, 10, 128], BF16); b_Amat = P.buf()
    Obf = P.sb("s_Obf", [128, 128], BF16); b_Obf = P.buf()
    put = P.ps("s_put", [128, 8, 128], BF16); b_put = P.pbuf()
    pxy = [P.ps(f"s_pxy{i}", [128, 512], F32) for i in range(2)]; b_pxy = [P.pbuf() for _ in range(2)]
    psc = [P.ps(f"s_psc{i}", [128, 512], F32) for i in range(2)]; b_psc = [P.pbuf() for _ in range(2)]
    pme = P.ps("s_pme", [128, 512], F32); b_pme = P.pbuf()
    pyt = P.ps("s_pyt", [128, 8, 128], F32); b_pyt = P.pbuf()
    for g in range(8):
        Fg = Fm[:, g].rearrange("p s m -> p (s m)")
        Feg = Fe[:, g].rearrange("p s m -> p (s m)")
        Gg = Gm[:, g].rearrange("p s m -> p (s m)")
        Og = Om[:, g].rearrange("p s m -> p (s m)")
        op("pe", lambda e: e.matmul(pme[:, 0:128], lhsT=Fg, rhs=Gg, start=True, stop=True), reads=[b_Fm, b_Gm], writes=[b_pme])
        dv(lambda e: e.tensor_tensor(out=mtmp[:], in0=pme[:, 0:128], in1=smask, op=ALU.mult), [b_pme, b_mats], [b_mtmp])
        dv(lambda e: e.scalar_tensor_tensor(out=Mbf[:], in0=ident_f[:], scalar=dsk[:, g:g + 1], in1=mtmp[:],
                                             op0=ALU.mult, op1=ALU.add), [b_identf, b_small, b_mtmp], [b_Mbf])
        op("pe", lambda e: e.transpose(pme[:, 128:256], Feg, ident_f[:]), reads=[b_Fe, b_identf], writes=[b_pme])
        op("act", lambda e: e.activation(out=Ebf[:], in_=pme[:, 128:256], func=AF.Copy), reads=[b_pme], writes=[b_Ebf])
        op("act", lambda e: e.activation(out=Obf[:], in_=Og, func=AF.Copy), reads=[b_Om], writes=[b_Obf])
        for i in range(10):
            dv(lambda e: e.tensor_scalar(out=mtmp[:], in0=ident_f[:], scalar1=PR[:, g, KV_A + i:KV_A + i + 1], scalar2=None,
                                         op0=ALU.mult), [b_identf, b_PR], [b_mtmp])
            dv(lambda e: e.scalar_tensor_tensor(out=Amat[:, i, :], in0=P2, scalar=PIA[:, g, i:i + 1], in1=mtmp[:],
                                                 op0=ALU.mult, op1=ALU.add), [b_mats, b_PIA, b_mtmp], [b_Amat])
        for S in range(8):
            op("pe", lambda e: e.transpose(put[:, S, :], U_tok[:, S, g].rearrange("p s m -> p (s m)"), ident_bf[:]),
               reads=[b_U[S], b_ident], writes=[b_put])
        op("act", lambda e: e.activation(out=Ug[:].rearrange("p (S c) -> p S c", S=8), in_=put[:], func=AF.Copy),
           reads=[b_put], writes=[b_Ug])
        for hf in range(2):
            op("pe", lambda e: e.matmul(pxy[hf][:], lhsT=Ebf[:], rhs=Ug[:, hf * 512:(hf + 1) * 512], start=True, stop=True),
               reads=[b_Ebf, b_Ug], writes=[b_pxy[hf]])
            op("act", lambda e: e.activation(out=H[:, hf * 512:(hf + 1) * 512], in_=pxy[hf][:], func=AF.Copy),
               reads=[b_pxy[hf]], writes=[b_H])
        for i in range(10):
            d = 1 << i
            rngs = []
            for hb in range(2):
                lo = max(d, hb * 512)
                hi = (hb + 1) * 512
                if lo < hi:
                    rngs.append((hb, lo, hi))
            for (hb, lo, hi) in rngs:
                op("pe", lambda e: e.matmul(psc[hb][:, lo - hb * 512:hi - hb * 512], lhsT=Amat[:, i, :], rhs=H[:, lo - d:hi - d],
                                            start=True, stop=True), reads=[b_Amat, b_H], writes=[b_psc[hb]])
            for (hb, lo, hi) in rngs:
                dv(lambda e: e.tensor_tensor(out=H[:, lo:hi], in0=psc[hb][:, lo - hb * 512:hi - hb * 512], in1=H[:, lo:hi], op=ALU.add),
                   [b_psc[hb], b_H], [b_H])
        for hf in range(2):
            op("pe", lambda e: e.matmul(pxy[hf][:], lhsT=Mbf[:], rhs=Ug[:, hf * 512:(hf + 1) * 512], start=True, stop=False),
               reads=[b_Mbf, b_Ug], writes=[b_pxy[hf]])
            if hf == 0:
                op("pe", lambda e: e.matmul(pxy[0][:, 1:512], lhsT=Obf[:], rhs=H[:, 0:511], start=False, stop=True),
                   reads=[b_Obf, b_H], writes=[b_pxy[0]])
            else:
                op("pe", lambda e: e.matmul(pxy[1][:], lhsT=Obf[:], rhs=H[:, 511:1023], start=False, stop=True),
                   reads=[b_Obf, b_H], writes=[b_pxy[1]])
            op("act", lambda e: e.activation(out=Ysb[:, hf * 512:(hf + 1) * 512], in_=pxy[hf][:], func=AF.Copy),
               reads=[b_pxy[hf]], writes=[b_Ysb])
        for S in range(8):
            op("pe", lambda e: e.transpose(pyt[:, S, :], Ysb[:, S * 128:(S + 1) * 128], ident_f[:]),
               reads=[b_Ysb, b_identf], writes=[b_pyt])
        dv(lambda e: e.tensor_copy(out=Yout[:, :, :, g * 16:(g + 1) * 16],
                                   in_=pyt[:].rearrange("p S (t n) -> p S t n", t=8)), [b_pyt], [b_Yout])
    for S in range(8):
        dma(lambda e: e.dma_start(out=y_o[S * 1024:(S + 1) * 1024, :].rearrange("(c t) ch -> c t ch", t=8), in_=Yout[:, S]),
            reads=[b_Yout])
    P.pop_scope()


def ssm_inputs(inp, r):
    gs = slice(8 * r, 8 * r + 8)
    lam_re = inp["lam_re"][0][gs]
    lam_im = inp["lam_im"][0][gs]
    log_dt = inp["log_dt"][0][gs]
    b_re, b_im = inp["b_re"][0][gs], inp["b_im"][0][gs]
    c_re, c_im = inp["c_re"][0][gs], inp["c_im"][0][gs]
    d = inp["d_skip"][0][gs]
    dup = lambda a: np.concatenate([a, a], axis=0)
    small = np.zeros((128, 8 * 4 + NKV + 2 + 8), np.float32)
    small[:, 0:8] = dup(lam_re.T)
    small[:, 8:16] = dup(lam_im.T)
    small[:, 16:24] = np.broadcast_to(log_dt[None, :], (128, 8))
    small[:, 24:32] = np.tile(d.T, (8, 1))
    small[:, 32:32 + NKV] = ssm_kvec()[None, :]
    small[:64, 32 + NKV] = -1.0; small[64:, 32 + NKV] = 1.0
    small[:64, 33 + NKV] = 1.0; small[64:, 33 + NKV] = -1.0
    bre = b_re.transpose(1, 0, 2); bim = b_im.transpose(1, 0, 2)
    cre = c_re.transpose(2, 0, 1); cim = c_im.transpose(2, 0, 1)
    bcv = np.zeros((128, 4, 8, 16), np.float32)
    bcv[:64, 0], bcv[64:, 0] = bre, bim
    bcv[:64, 1], bcv[64:, 1] = bim, bre
    bcv[:64, 2], bcv[64:, 2] = cre, cim
    bcv[:64, 3], bcv[64:, 3] = cim, cre
    s_idx = np.arange(128) // 16
    smask = (s_idx[None, :] >= s_idx[:, None]).astype(np.float32)
    P2 = np.zeros((128, 128), np.float32)
    P2[np.arange(128), (np.arange(128) + 64) % 128] = 1.0
    return {"ssm_small": small, "ssm_bc": np.ascontiguousarray(bcv.reshape(128, 512)),
            "ssm_mat": np.ascontiguousarray(np.concatenate([smask, P2], axis=1))}


_NC_CACHE = {}


def _get_nc(which):
    if which not in _NC_CACHE:
        nc = bass.Bass("TRN2", target_bir_lowering=False)
        if which == "ab":
            build_phase_ab(nc, with_ssm=True)
        else:
            build_phase_c(nc)
        _NC_CACHE[which] = nc
    return _NC_CACHE[which]


def kernel(**inputs):
    inp = {k: np.asarray(v, dtype=np.float32) for k, v in inputs.items()}
    B = inp["x"].shape[0]
    maps1 = [phase_ab_inputs(inp, ci // 4, ci % 4) for ci in range(8)]
    res1 = run_bass_kernel_spmd(_get_nc("ab"), maps1, core_ids=list(range(8))).results
    attn_full = np.zeros((B, L_SEQ, 8, 65), np.float32)
    y_full = np.zeros((B, L_SEQ, 512), np.float32)
    for ci in range(8):
        b, r = ci // 4, ci % 4
        attn_full[b, :, 2 * r:2 * r + 2, :] = res1[ci]["attn_o"].reshape(2, 65, L_SEQ).transpose(2, 0, 1)
        y_full[b, :, 128 * r:128 * r + 128] = res1[ci]["y_o"]
    maps2 = [phase_c_inputs(inp, ci // 4, ci % 4, attn_full, y_full) for ci in range(8)]
    res2 = run_bass_kernel_spmd(_get_nc("c"), maps2, core_ids=list(range(8))).results
    out = np.zeros((B, L_SEQ, 1024), np.float32)
    for ci in range(8):
        b, qi = ci // 4, ci % 4
        out[b, qi * NT_C:(qi + 1) * NT_C] = res2[ci]["out"]
    return out
```

```python
import math
import numpy as np
from contextlib import ExitStack

import concourse.bass as bass
import concourse.mybir as mybir
from concourse.bass_utils import run_bass_kernel_spmd

F32 = mybir.dt.float32
BF16 = mybir.dt.bfloat16
I32 = mybir.dt.int32
ALU = mybir.AluOpType
AF = mybir.ActivationFunctionType

PHYS = ["pe", "act", "dve", "pool", "sp"]
NDMA = 8
SAME_ENG_WAIT = True
EPS = 1e-6
NEGBIG = -30000.0


class Buf:
    __slots__ = ("name", "last_w", "readers", "excl")

    def __init__(self, name, excl=False):
        self.name = name
        self.last_w = None
        self.readers = []
        self.excl = excl


class Prog:
    def __init__(self, nc):
        self.nc = nc
        self.stack = ExitStack()
        self.semnames = ["pe", "act", "dve", "pool"] + [f"d{i}" for i in range(NDMA)]
        self.cnt = {s: 0 for s in self.semnames}
        self.seen = {e: {s: 0 for s in self.semnames} for e in PHYS}
        self.sems = {s: self.stack.enter_context(nc.semaphore("sem_" + s)) for s in self.semnames}
        self.engobjs = {"pe": nc.tensor, "act": nc.scalar, "dve": nc.vector, "pool": nc.gpsimd, "sp": nc.sync}
        self.pending = {e: {} for e in PHYS}
        self.nbuf = 0
        self.dma_rr = 0
        self.scopes = [self.stack]

    def buf(self, name=None, excl=False):
        self.nbuf += 1
        return Buf(name or f"b{self.nbuf}", excl)

    def pbuf(self):
        return self.buf(excl=True)

    def sb(self, name, shape, dtype):
        return self.scopes[-1].enter_context(self.nc.sbuf_tensor(name, list(shape), dtype))

    def ps(self, name, shape, dtype=F32):
        return self.scopes[-1].enter_context(self.nc.psum_tensor(name, list(shape), dtype))

    def push_scope(self):
        st = ExitStack()
        self.scopes.append(st)
        return st

    def pop_scope(self):
        st = self.scopes.pop()
        st.close()
        self.fence()

    def fence(self):
        for e in PHYS:
            for s in self.semnames:
                if self.cnt[s] > self.seen[e][s]:
                    self.pending[e][s] = self.cnt[s]

    def _op(self, phys, sem, inc, fn, reads, writes, extra_waits=()):
        waits = dict(self.pending[phys])
        self.pending[phys] = {}
        xr = [b for b in reads if b.excl]
        if xr:
            reads = [b for b in reads if not b.excl]
            writes = list(writes) + [b for b in xr if b not in writes]
        for (f, c) in extra_waits:
            waits[f] = max(waits.get(f, 0), c)

        def need(f):
            return f != phys or (SAME_ENG_WAIT and phys != "pe")
        for b in reads:
            if b.last_w is not None:
                f, c = b.last_w
                if need(f):
                    waits[f] = max(waits.get(f, 0), c)
        for b in writes:
            if b.last_w is not None:
                f, c = b.last_w
                if need(f):
                    waits[f] = max(waits.get(f, 0), c)
            for (f, c) in b.readers:
                if need(f):
                    waits[f] = max(waits.get(f, 0), c)
        engobj = self.engobjs[phys]
        for f, c in waits.items():
            if c > self.seen[phys][f]:
                self.seen[phys][f] = c
                engobj.wait_ge(self.sems[f], c)
        self.cnt[sem] += inc
        me = (sem, self.cnt[sem])
        ins = fn(engobj)
        ins.then_inc(self.sems[sem], inc)
        for b in reads:
            b.readers.append(me)
        for b in writes:
            b.last_w = me
            b.readers = []
        return me

    def op(self, eng, fn, reads=(), writes=()):
        return self._op(eng, eng, 1, fn, reads, writes)

    def dma(self, fn, reads=(), writes=(), q="sp"):
        k = self.dma_rr % NDMA
        self.dma_rr += 1
        sem = f"d{k}"
        prev = self.cnt[sem]
        extra = [(sem, prev)] if prev > 0 else []
        return self._op(q, sem, 16, fn, reads, writes, extra_waits=extra)

    def finish(self):
        for i in range(NDMA):
            s = f"d{i}"
            if self.cnt[s] > 0:
                self.nc.sync.wait_ge(self.sems[s], self.cnt[s])
        for s in ["pe", "act", "dve", "pool"]:
            if self.cnt[s] > 0:
                self.nc.sync.wait_ge(self.sems[s], self.cnt[s])
        while len(self.scopes) > 1:
            self.scopes.pop().close()
        self.stack.close()


def _rot(lst, i):
    return lst[i % len(lst)]


NT_C = 2048


def build_phase_c(nc, srcs=None):
    def din(name, shape, dt=F32):
        return nc.dram_tensor(name, list(shape), dt, kind="ExternalInput").ap()
    x = din("xc", [NT_C, 1024])
    if srcs is None:
        attn = din("attn", [NT_C, 520])
        yin = din("yin", [NT_C, 512])
    else:
        attn, yin = srcs["attn"], srcs["y"]
    cT2 = din("cT2", [128, 16])
    wada = din("wada_c", [1024, 4096])
    bada = din("bada_c", [1, 4096])
    rows = din("rows_c", [1, 3072])
    bglu = din("bglu", [1, 512])
    wglu = din("wglu", [512, 512])
    wout = din("wout", [1024, 1024])
    wfc1 = din("wfc1", [1024, 4096])
    wfc2 = din("wfc2", [4096, 1024])
    ident = din("ident", [128, 128])
    out = nc.dram_tensor("out", [NT_C, 1024], F32, kind="ExternalOutput").ap()
    x1s = nc.dram_tensor("x1s", [NT_C, 1024], F32).ap()

    P = Prog(nc)
    op, dma = P.op, P.dma

    ident_bf = P.sb("ident_bf", [128, 128], BF16); b_ident = P.buf()
    ones_f = P.sb("ones_f", [1, 128], F32); b_ones_f = P.buf()
    ones_bf = P.sb("ones_bf", [1, 512], BF16); b_ones_bf = P.buf()
    bglu_bf = P.sb("bglu_bf", [1, 512], BF16); b_bglu = P.buf()
    sh2T = P.sb("sh2T", [128, 16], BF16); b_sh2T = P.buf()
    Gcat = P.sb("Gcat", [128, 1024], F32); b_Gcat = P.buf()
    G1 = P.sb("G1", [128, 1024], F32); b_G1 = P.buf()
    S2b = P.sb("S2b", [128, 1024], F32); b_S2b = P.buf()
    G2 = P.sb("G2", [128, 1024], F32); b_G2 = P.buf()
    Gf = P.sb("Gf", [128, 1024], F32); b_Gf = P.buf()

    ident_f = P.sb("ident_f", [128, 128], F32); b_identf = P.buf()
    bglu_f = P.sb("bglu_f", [1, 512], F32); b_bgluf = P.buf()
    dma(lambda e: e.dma_start(out=ident_f[:], in_=ident[:, :]), writes=[b_identf])
    dma(lambda e: e.dma_start(out=bglu_f[:], in_=bglu[:, :]), writes=[b_bgluf])
    op("dve", lambda e: e.tensor_copy(out=ident_bf[:], in_=ident_f[:]), reads=[b_identf], writes=[b_ident])
    op("dve", lambda e: e.tensor_copy(out=bglu_bf[:], in_=bglu_f[:]), reads=[b_bgluf], writes=[b_bglu])
    op("dve", lambda e: e.memset(ones_f[:], 1.0), writes=[b_ones_f])
    op("dve", lambda e: e.memset(ones_bf[:], 1.0), writes=[b_ones_bf])

    P.push_scope()
    ct = P.sb("ct", [128, 16], F32); b_ct = P.buf()
    sct = P.sb("sct", [128, 16], F32); b_sct = P.buf()
    sgc = P.sb("sgc", [128, 16], F32)
    modrow = P.sb("modrow", [1, 4096], F32); b_modrow = P.buf()
    badas = P.sb("badas", [1, 4096], F32); b_badas = P.buf()
    rowss = P.sb("rowss", [1, 3072], F32); b_rowss = P.buf()
    s2row = P.sb("s2row", [1, 1024], F32); b_s2row = P.buf()
    pieces = [P.sb(f"wpiece{i}", [128, 8, 512], F32) for i in range(2)]
    b_pieces = [P.buf() for _ in range(2)]
    ps_row = [P.ps(f"ps_row{i}", [128, 512], F32) for i in range(2)]
    b_ps_row = [P.pbuf() for _ in range(2)]

    dma(lambda e: e.dma_start(out=ct[:], in_=cT2[:, :]), writes=[b_ct])
    dma(lambda e: e.dma_start(out=badas[:], in_=bada[:, :]), writes=[b_badas])
    dma(lambda e: e.dma_start(out=rowss[:], in_=rows[:, :]), writes=[b_rowss])
    op("act", lambda e: e.activation(out=sgc[:], in_=ct[:], func=AF.Sigmoid), reads=[b_ct], writes=[b_sct])
    op("dve", lambda e: e.tensor_tensor(out=sct[:], in0=sgc[:], in1=ct[:], op=ALU.mult), reads=[b_ct, b_sct], writes=[b_sct])
    for j in range(8):
        pc, bpc = _rot(pieces, j), _rot(b_pieces, j)
        pr, bpr = _rot(ps_row, j), _rot(b_ps_row, j)
        dma(lambda e: e.dma_start(out=pc[:], in_=wada[:, j * 512:(j + 1) * 512].rearrange("(k p) n -> p k n", p=128)),
            writes=[bpc])
        for k in range(8):
            op("pe", lambda e: e.matmul(pr[0:1, :], lhsT=sct[:, 2 * k:2 * k + 1], rhs=pc[:, k, :],
                                        start=(k == 0), stop=(k == 7)),
               reads=[b_sct, bpc], writes=[bpr])
        op("dve", lambda e: e.tensor_tensor(out=modrow[0:1, j * 512:(j + 1) * 512], in0=pr[0:1, :],
                                            in1=badas[0:1, j * 512:(j + 1) * 512], op=ALU.add),
           reads=[bpr, b_badas], writes=[b_modrow])
    op("dve", lambda e: e.scalar_tensor_tensor(out=s2row[:], in0=modrow[0:1, 2048:3072], scalar=1.0,
                                                in1=rowss[0:1, 1024:2048], op0=ALU.add, op1=ALU.mult),
       reads=[b_modrow, b_rowss], writes=[b_s2row])
    bc_list = [(Gcat, b_Gcat, rowss, b_rowss, 0), (G1, b_G1, modrow, b_modrow, 0), (S2b, b_S2b, s2row, b_s2row, 0),
               (G2, b_G2, modrow, b_modrow, 3072), (Gf, b_Gf, rowss, b_rowss, 2048)]
    i = 0
    for (dst, bdst, src, bsrc, off) in bc_list:
        for hf in range(2):
            pr, bpr = _rot(ps_row, i), _rot(b_ps_row, i)
            i += 1
            op("pe", lambda e: e.matmul(pr[:, :], lhsT=ones_f[0:1, :], rhs=src[0:1, off + hf * 512: off + (hf + 1) * 512],
                                        start=True, stop=True),
               reads=[b_ones_f, bsrc], writes=[bpr])
            op("act", lambda e: e.activation(out=dst[:, hf * 512:(hf + 1) * 512], in_=pr[:, :], func=AF.Copy),
               reads=[bpr], writes=[bdst])
    pr, bpr = ps_row[0], b_ps_row[0]
    for k in range(8):
        op("pe", lambda e: e.matmul(pr[:, 2 * k:2 * k + 2], lhsT=modrow[0:1, 1024 + k * 128:1024 + (k + 1) * 128],
                                    rhs=ones_f[0:1, 0:2], start=True, stop=True),
           reads=[b_ones_f, b_modrow], writes=[bpr])
    op("dve", lambda e: e.tensor_copy(out=sh2T[:], in_=pr[:, 0:16]), reads=[bpr], writes=[b_sh2T])
    P.pop_scope()

    P.push_scope()
    Wout = P.sb("Wout", [128, 8, 1024], BF16); b_Wout = P.buf()
    Wglu = P.sb("Wglu", [128, 4, 512], BF16); b_Wglu = P.buf()
    stg = [P.sb(f"stgc1_{i}", [128, 2048], F32) for i in range(2)]; b_stg = [P.buf() for _ in range(2)]
    dma(lambda e: e.dma_start(out=stg[0][:].rearrange("p (k n) -> p k n", k=4), in_=wglu.rearrange("(k p) n -> p k n", p=128)),
        writes=[b_stg[0]])
    op("pool", lambda e: e.tensor_copy(out=Wglu[:].rearrange("p k n -> p (k n)"), in_=stg[0][:]), reads=[b_stg[0]], writes=[b_Wglu])
    for kk in range(4):
        sg_, bsg_ = _rot(stg, kk + 1), _rot(b_stg, kk + 1)
        dma(lambda e: e.dma_start(out=sg_[:].rearrange("p (k n) -> p k n", k=2),
                                  in_=wout[kk * 256:(kk + 1) * 256, :].rearrange("(k p) n -> p k n", p=128)), writes=[bsg_])
        op("pool" if kk % 2 else "dve", lambda e: e.tensor_copy(out=Wout[:, 2 * kk:2 * kk + 2, :].rearrange("p k n -> p (k n)"), in_=sg_[:]),
           reads=[bsg_], writes=[b_Wout])
    NB = 3

    def dbl(name, shape, dt, n=2):
        return [P.sb(f"{name}{i}", shape, dt) for i in range(n)], [P.buf() for _ in range(n)]

    def dblp(name, shape, dt, n=2):
        return [P.ps(f"{name}{i}", shape, dt) for i in range(n)], [P.pbuf() for _ in range(n)]
    at, b_at = dbl("at", [128, 8, 65], F32, NB)
    yt, b_yt = dbl("yt", [128, 512], F32, NB)
    xt, b_xt = dbl("xt", [128, 1024], F32, NB)
    rl, b_rl = dbl("rl", [128, 8], F32)
    A, b_A = dbl("A", [128, 8, 64], F32)
    junk = P.sb("junk", [128, 512], BF16); b_junk = P.buf()
    ss, b_ss = dbl("ss", [128, 2], F32)
    sq, b_sq = dbl("sq", [128, 2], F32)
    rstd, b_rstd = dbl("rstd", [128, 2], F32)
    g1, b_g1 = dbl("g1", [128, 512], F32)
    g2, b_g2 = dbl("g2", [128, 512], F32)
    sg, b_sg = dbl("sg", [128, 512], F32)
    yg, b_yg = dbl("yg", [128, 512], F32)
    ygb, b_ygb = dbl("ygb", [128, 512], BF16)
    ygT, b_ygT = dbl("ygT", [128, 4, 128], BF16)
    sig, b_sig = dbl("sig", [128, 512], F32)
    S, b_S = dbl("S", [128, 512], F32)
    mixh, b_mixh = dbl("mixh", [128, 1024], BF16)
    mixT, b_mixT = dbl("mixT", [128, 8, 128], BF16)
    tt, b_tt = dbl("tt", [128, 1024], F32)
    x1t, b_x1t = dbl("x1t", [128, 1024], F32)
    ptr, b_ptr = dblp("ptr", [128, 8, 128], BF16)
    psg, b_psg = dblp("psg", [128, 512], F32)
    pmx = P.ps("pmx", [128, 8, 128], BF16); b_pmx = P.pbuf()
    pso = [P.ps(f"pso{i}", [128, 512], F32) for i in range(2)]; b_pso = [P.pbuf() for _ in range(2)]
    b_x1s = [P.buf() for _ in range(16)]

    def c1_load(t):
        a, ba = _rot(at, t), _rot(b_at, t)
        y_, by = _rot(yt, t), _rot(b_yt, t)
        x_, bx = _rot(xt, t), _rot(b_xt, t)
        dma(lambda e: e.dma_start(out=a[:].rearrange("p h e -> p (h e)"), in_=attn[t * 128:(t + 1) * 128, :]), writes=[ba])
        dma(lambda e: e.dma_start(out=y_[:], in_=yin[t * 128:(t + 1) * 128, :]), writes=[by])
        dma(lambda e: e.dma_start(out=x_[:], in_=x[t * 128:(t + 1) * 128, :]), writes=[bx])

    def c1_a(t):
        a, ba = _rot(at, t), _rot(b_at, t)
        y_, by = _rot(yt, t), _rot(b_yt, t)
        i = t % 2
        op("dve", lambda e: e.reciprocal(out=rl[i][:], in_=a[:, :, 64]), reads=[ba], writes=[b_rl[i]])
        yield
        op("dve", lambda e: e.tensor_tensor(out=A[i][:], in0=a[:, :, 0:64],
                                            in1=rl[i][:, :].unsqueeze(2).to_broadcast([128, 8, 64]), op=ALU.mult),
           reads=[ba, b_rl[i]], writes=[b_A[i]])
        yield
        op("act", lambda e: e.activation(out=junk[:], in_=A[i][:].rearrange("p h d -> p (h d)"), func=AF.Square,
                                         accum_out=ss[i][:, 0:1]),
           reads=[b_A[i]], writes=[b_junk, b_ss[i]])
        yield
        op("pool", lambda e: e.tensor_tensor(out=g1[i][:], in0=y_[:], in1=y_[:], op=ALU.mult), reads=[by], writes=[b_g1[i]])
        yield
        op("pool", lambda e: e.tensor_scalar(out=g1[i][:], in0=g1[i][:], scalar1=0.044715, scalar2=1.0,
                                             op0=ALU.mult, op1=ALU.add), reads=[b_g1[i]], writes=[b_g1[i]])
        yield
        op("pool", lambda e: e.tensor_tensor(out=g2[i][:], in0=g1[i][:], in1=y_[:], op=ALU.mult), reads=[b_g1[i], by], writes=[b_g2[i]])
        yield
        op("act", lambda e: e.activation(out=sg[i][:], in_=g2[i][:], func=AF.Sigmoid, scale=1.5957691216057308),
           reads=[b_g2[i]], writes=[b_sg[i]])
        yield
        op("dve", lambda e: e.tensor_tensor(out=yg[i][:], in0=y_[:], in1=sg[i][:], op=ALU.mult), reads=[by, b_sg[i]], writes=[b_yg[i]])
        yield
        op("pool", lambda e: e.tensor_tensor(out=ygb[i][:], in0=y_[:], in1=sg[i][:], op=ALU.mult), reads=[by, b_sg[i]], writes=[b_ygb[i]])
        yield
        for k in range(4):
            op("pe", lambda e: e.transpose(ptr[i][:, k, :], ygb[i][:, k * 128:(k + 1) * 128], ident_bf[:]),
               reads=[b_ygb[i], b_ident], writes=[b_ptr[i]])
            yield
        op("act", lambda e: e.activation(out=ygT[i][:], in_=ptr[i][:, 0:4, :], func=AF.Copy), reads=[b_ptr[i]], writes=[b_ygT[i]])
        yield
        op("pe", lambda e: e.matmul(psg[i][:], lhsT=ones_bf[0:1, 0:128], rhs=bglu_bf[0:1, :], start=True, stop=False),
           reads=[b_ones_bf, b_bglu], writes=[b_psg[i]])
        yield
        for k in range(4):
            op("pe", lambda e: e.matmul(psg[i][:], lhsT=ygT[i][:, k, :], rhs=Wglu[:, k, :], start=False, stop=(k == 3)),
               reads=[b_ygT[i], b_Wglu], writes=[b_psg[i]])
            yield

    def c1_b(t):
        x_, bx = _rot(xt, t), _rot(b_xt, t)
        i = t % 2
        op("act", lambda e: e.activation(out=sig[i][:], in_=psg[i][:], func=AF.Sigmoid), reads=[b_psg[i]], writes=[b_sig[i]])
        yield
        op("dve", lambda e: e.tensor_tensor(out=S[i][:], in0=yg[i][:], in1=sig[i][:], op=ALU.mult), reads=[b_yg[i], b_sig[i]], writes=[b_S[i]])
        yield
        op("act", lambda e: e.activation(out=junk[:], in_=S[i][:], func=AF.Square, accum_out=ss[i][:, 1:2]),
           reads=[b_S[i]], writes=[b_junk, b_ss[i]])
        yield
        op("act", lambda e: e.activation(out=sq[i][:], in_=ss[i][:], func=AF.Sqrt, scale=1.0 / 512.0, bias=EPS),
           reads=[b_ss[i]], writes=[b_sq[i]])
        yield
        op("dve", lambda e: e.reciprocal(out=rstd[i][:], in_=sq[i][:]), reads=[b_sq[i]], writes=[b_rstd[i]])
        yield
        op("dve", lambda e: e.scalar_tensor_tensor(out=mixh[i][:, 0:512], in0=A[i][:].rearrange("p h d -> p (h d)"),
                                                    scalar=rstd[i][:, 0:1], in1=Gcat[:, 0:512], op0=ALU.mult, op1=ALU.mult),
           reads=[b_A[i], b_rstd[i], b_Gcat], writes=[b_mixh[i]])
        yield
        op("dve", lambda e: e.scalar_tensor_tensor(out=mixh[i][:, 512:1024], in0=S[i][:], scalar=rstd[i][:, 1:2],
                                                    in1=Gcat[:, 512:1024], op0=ALU.mult, op1=ALU.mult),
           reads=[b_S[i], b_rstd[i], b_Gcat], writes=[b_mixh[i]])
        yield
        for k in range(8):
            op("pe", lambda e: e.transpose(pmx[:, k, :], mixh[i][:, k * 128:(k + 1) * 128], ident_bf[:]),
               reads=[b_mixh[i], b_ident], writes=[b_pmx])
            yield
        op("act", lambda e: e.activation(out=mixT[i][:], in_=pmx[:], func=AF.Copy), reads=[b_pmx], writes=[b_mixT[i]])
        yield
        for hf in range(2):
            for k in range(8):
                op("pe", lambda e: e.matmul(pso[hf][:], lhsT=mixT[i][:, k, :], rhs=Wout[:, k, hf * 512:(hf + 1) * 512],
                                            start=(k == 0), stop=(k == 7)),
                   reads=[b_mixT[i], b_Wout], writes=[b_pso[hf]])
                yield
            op("dve", lambda e: e.tensor_tensor(out=tt[i][:, hf * 512:(hf + 1) * 512], in0=pso[hf][:],
                                                in1=G1[:, hf * 512:(hf + 1) * 512], op=ALU.mult),
               reads=[b_pso[hf], b_G1], writes=[b_tt[i]])
            yield
        op("pool", lambda e: e.tensor_tensor(out=x1t[i][:], in0=tt[i][:], in1=x_[:], op=ALU.add), reads=[b_tt[i], bx], writes=[b_x1t[i]])
        yield
        dma(lambda e: e.dma_start(out=x1s[t * 128:(t + 1) * 128, :], in_=x1t[i][:]), reads=[b_x1t[i]], writes=[b_x1s[t]])
        yield

    def lockstep(*gens):
        gens = [g for g in gens if g is not None]
        while gens:
            alive = []
            for g in gens:
                try:
                    next(g)
                    alive.append(g)
                except StopIteration:
                    pass
            gens = alive

    c1_load(0)
    c1_load(1)
    lockstep(c1_a(0))
    for t in range(16):
        if t + 2 < 16:
            c1_load(t + 2)
        lockstep(c1_a(t + 1) if t + 1 < 16 else None, c1_b(t))
    P.pop_scope()

    P.push_scope()
    W2 = P.sb("W2", [128, 32, 1024], BF16); b_W2 = [P.buf() for _ in range(8)]
    stg2 = [P.sb(f"stgc2_{i}", [128, 2048], F32) for i in range(2)]; b_stg2 = [P.buf() for _ in range(2)]
    scount = 0

    def load_w2(i):
        nonlocal scount
        sg_, bsg_ = _rot(stg2, scount), _rot(b_stg2, scount)
        scount += 1
        dma(lambda e: e.dma_start(out=sg_[:].rearrange("p (j n) -> p j n", j=2),
                                  in_=wfc2[256 * i:256 * (i + 1), :].rearrange("(j p) n -> p j n", p=128)), writes=[bsg_])
        op("dve", lambda e: e.tensor_copy(out=W2[:, 2 * i:2 * i + 2, :].rearrange("p j n -> p (j n)"), in_=sg_[:]),
           reads=[bsg_], writes=[b_W2[i // 2]])
    W1p = [P.sb(f"W1p{i}", [128, 8, 256], BF16) for i in range(2)]; b_W1p = [P.buf() for _ in range(2)]
    hT = P.sb("hT", [128, 32, 512], BF16); b_hT = [P.buf() for _ in range(32)]
    h2T = P.sb("h2T", [128, 8, 512], BF16); b_h2T = P.buf()
    x1g = P.sb("x1g", [128, 4, 1024], F32); b_x1g = [P.buf() for _ in range(4)]
    h2 = [P.sb(f"h2_{i}", [128, 1024], BF16) for i in range(2)]; b_h2 = [P.buf() for _ in range(2)]
    junk2 = P.sb("junk2", [128, 1024], BF16); b_junk2 = P.buf()
    ssc = P.sb("ssc", [128, 2], F32); b_ssc = P.buf()
    sqc = P.sb("sqc", [128, 2], F32); b_sqc = P.buf()
    rc = P.sb("rc", [128, 2], F32); b_rc = P.buf()
    ssp = [P.sb(f"ssp{i}", [128, 1], F32) for i in range(2)]; b_ssp = [P.buf() for _ in range(2)]
    sqp = [P.sb(f"sqp{i}", [128, 1], F32) for i in range(2)]; b_sqp = [P.buf() for _ in range(2)]
    rcp = [P.sb(f"rcp{i}", [128, 1], F32) for i in range(2)]; b_rcp = [P.buf() for _ in range(2)]
    b1T = P.sb("b1T", [128, 32], F32); b_b1T = P.buf()
    rl_t = [P.sb(f"rl_t{i}", [128, 512], BF16) for i in range(2)]; b_rl_t = [P.buf() for _ in range(2)]
    t2 = P.sb("t2", [128, 1024], F32); b_t2 = P.buf()
    x2t = t2; b_x2t = b_t2
    ot = [P.sb(f"ot{i}", [128, 1024], F32) for i in range(1)]; b_ot = [P.buf() for _ in range(1)]
    pht = [P.ps(f"pht{i}", [128, 8, 128], BF16) for i in range(2)]; b_pht = [P.pbuf() for _ in range(2)]
    psb = P.ps("psb", [128, 512], F32); b_psb = P.pbuf()
    psf = [P.ps(f"psf{i}", [128, 512], F32) for i in range(2)]; b_psf = [P.pbuf() for _ in range(2)]
    pso2 = [P.ps(f"pso2{i}", [128, 512], F32) for i in range(2)]; b_pso2 = [P.pbuf() for _ in range(2)]
    pcount = 0
    ocount = 0
    for g in range(4):
        for i in range(4):
            t = g * 4 + i
            q_ = i % 2
            dma(lambda e: e.dma_start(out=x1g[:, i, :], in_=x1s[t * 128:(t + 1) * 128, :]), reads=[b_x1s[t]], writes=[b_x1g[i]])
            op("act", lambda e: e.activation(out=junk2[:], in_=x1g[:, i, :], func=AF.Square, accum_out=ssp[q_][:, 0:1]),
               reads=[b_x1g[i]], writes=[b_junk2, b_ssp[q_]])
            op("act", lambda e: e.activation(out=sqp[q_][:], in_=ssp[q_][:], func=AF.Sqrt, scale=1.0 / 1024.0, bias=EPS),
               reads=[b_ssp[q_]], writes=[b_sqp[q_]])
            op("dve", lambda e: e.reciprocal(out=rcp[q_][:], in_=sqp[q_][:]), reads=[b_sqp[q_]], writes=[b_rcp[q_]])
            op("dve", lambda e: e.scalar_tensor_tensor(out=h2[q_][:], in0=x1g[:, i, :], scalar=rcp[q_][:, 0:1], in1=S2b[:],
                                                        op0=ALU.mult, op1=ALU.mult),
               reads=[b_x1g[i], b_rcp[q_], b_S2b], writes=[b_h2[q_]])
            for k in range(8):
                op("pe", lambda e: e.transpose(pht[q_][:, k, :], h2[q_][:, k * 128:(k + 1) * 128], ident_bf[:]),
                   reads=[b_h2[q_], b_ident], writes=[b_pht[q_]])
            op("act", lambda e: e.activation(out=h2T[:, :, i * 128:(i + 1) * 128], in_=pht[q_][:], func=AF.Copy),
               reads=[b_pht[q_]], writes=[b_h2T])
        for pc in range(16):
            w1, bw1 = _rot(W1p, pcount), _rot(b_W1p, pcount)
            pcount += 1
            sg_, bsg_ = _rot(stg2, scount), _rot(b_stg2, scount)
            scount += 1
            dma(lambda e: e.dma_start(out=sg_[:].rearrange("p (k n) -> p k n", k=8),
                                      in_=wfc1[:, pc * 256:(pc + 1) * 256].rearrange("(k p) n -> p k n", p=128)), writes=[bsg_])
            op("dve", lambda e: e.tensor_copy(out=w1[:].rearrange("p k n -> p (k n)"), in_=sg_[:]), reads=[bsg_], writes=[bw1])
            if g == 0:
                load_w2(pc)
            for fc in range(2):
                j = pc * 2 + fc
                if g == 0:
                    for k in range(8):
                        op("pe", lambda e: e.matmul(psb[:, 0:2], lhsT=w1[:, k, fc * 128:(fc + 1) * 128], rhs=sh2T[:, 2 * k:2 * k + 2],
                                                    start=(k == 0), stop=(k == 7)),
                           reads=[b_sh2T, bw1], writes=[b_psb])
                    op("dve", lambda e: e.tensor_copy(out=b1T[:, j:j + 1], in_=psb[:, 0:1]), reads=[b_psb], writes=[b_b1T])
                pf, bpf = _rot(psf, j), _rot(b_psf, j)
                for k in range(8):
                    op("pe", lambda e: e.matmul(pf[:], lhsT=w1[:, k, fc * 128:(fc + 1) * 128], rhs=h2T[:, k, :],
                                                start=(k == 0), stop=(k == 7)),
                       reads=[bw1, b_h2T], writes=[bpf])
                r_, br_ = _rot(rl_t, j), _rot(b_rl_t, j)
                op("act", lambda e: e.activation(out=r_[:], in_=pf[:], func=AF.Relu, bias=b1T[:, j:j + 1]),
                   reads=[bpf, b_b1T], writes=[br_])
                op("pool", lambda e: e.tensor_tensor(out=hT[:, j, :], in0=r_[:], in1=r_[:], op=ALU.mult),
                   reads=[br_], writes=[b_hT[j]])
        for i in range(4):
            t = g * 4 + i
            for hf in range(2):
                po, bpo = _rot(pso2, ocount), _rot(b_pso2, ocount)
                ocount += 1
                for j in range(32):
                    op("pe", lambda e: e.matmul(po[:], lhsT=hT[:, j, i * 128:(i + 1) * 128],
                                                rhs=W2[:, j, hf * 512:(hf + 1) * 512], start=(j == 0), stop=(j == 31)),
                       reads=[b_hT[j], b_W2[j // 4]], writes=[bpo])
                op("dve", lambda e: e.tensor_tensor(out=t2[:, hf * 512:(hf + 1) * 512], in0=po[:],
                                                    in1=G2[:, hf * 512:(hf + 1) * 512], op=ALU.mult),
                   reads=[bpo, b_G2], writes=[b_t2])
            op("pool", lambda e: e.tensor_tensor(out=x2t[:], in0=t2[:], in1=x1g[:, i, :], op=ALU.add),
               reads=[b_t2, b_x1g[i]], writes=[b_x2t])
            op("act", lambda e: e.activation(out=junk2[:], in_=x2t[:], func=AF.Square, accum_out=ssc[:, 1:2]),
               reads=[b_x2t], writes=[b_junk2, b_ssc])
            op("act", lambda e: e.activation(out=sqc[:, 1:2], in_=ssc[:, 1:2], func=AF.Sqrt, scale=1.0 / 1024.0, bias=EPS),
               reads=[b_ssc], writes=[b_sqc])
            op("dve", lambda e: e.reciprocal(out=rc[:, 1:2], in_=sqc[:, 1:2]), reads=[b_sqc], writes=[b_rc])
            o_, bo_ = _rot(ot, t), _rot(b_ot, t)
            op("dve", lambda e: e.scalar_tensor_tensor(out=o_[:], in0=x2t[:], scalar=rc[:, 1:2], in1=Gf[:],
                                                        op0=ALU.mult, op1=ALU.mult),
               reads=[b_x2t, b_rc, b_Gf], writes=[bo_])
            dma(lambda e: e.dma_start(out=out[t * 128:(t + 1) * 128, :], in_=o_[:]), reads=[bo_])
    P.pop_scope()
    P.finish()
    return nc


def _ident():
    return np.eye(128, dtype=np.float32)


def phase_c_inputs(inp, b, qi, attn_full, y_full):
    T0 = qi * NT_C
    c = inp["c"][b]
    cT = np.ascontiguousarray(c.reshape(8, 128).T)
    cT2 = np.repeat(cT[:, :, None], 2, axis=2).reshape(128, 16)
    rows = np.concatenate([inp["g_attn_out"][0], inp["g_ssm_out"][0], inp["g_mlp"][0], inp["g_final"]])[None, :]
    return {
        "xc": np.ascontiguousarray(inp["x"][b, T0:T0 + NT_C]),
        "attn": np.ascontiguousarray(attn_full[b, T0:T0 + NT_C].reshape(NT_C, 520)),
        "yin": np.ascontiguousarray(y_full[b, T0:T0 + NT_C]),
        "cT2": np.ascontiguousarray(cT2),
        "wada_c": np.ascontiguousarray(inp["w_ada"][0][:, 2048:6144]),
        "bada_c": np.ascontiguousarray(inp["b_ada"][0][None, 2048:6144]),
        "rows_c": np.ascontiguousarray(rows.astype(np.float32)),
        "bglu": np.ascontiguousarray(inp["b_glu"][0][None, :]),
        "wglu": np.ascontiguousarray(inp["w_glu"][0]),
        "wout": np.ascontiguousarray(inp["w_out"][0]),
        "wfc1": np.ascontiguousarray(inp["w_fc1"][0]),
        "wfc2": np.ascontiguousarray(inp["w_fc2"][0]),
        "ident": _ident(),
    }


L_SEQ = 8192
NTILE = 64


def build_phase_ab(nc, with_ssm=True, dsts=None, dbg_tiles=NTILE, dbg_attn=True, dbg_stage=9):
    def din(name, shape, dt=F32):
        return nc.dram_tensor(name, list(shape), dt, kind="ExternalInput").ap()
    xb = din("xb", [L_SEQ, 1024])
    cT2 = din("cT2", [128, 16])
    wada = din("wada_a", [1024, 2048])
    bada = din("bada_a", [1, 2048])
    gmix = din("gmix", [1, 1024])
    win = din("win", [1024, 512])
    cosl = din("cosl", [128, 2048])
    sinl = din("sinl", [128, 2048])
    ind = din("ind", [32, L_SEQ])
    cmask = din("cmask", [128, 2048])
    ident = din("ident", [128, 128])
    if dsts is None:
        attn_o = nc.dram_tensor("attn_o", [130, L_SEQ], F32, kind="ExternalOutput").ap()
        y_o = nc.dram_tensor("y_o", [L_SEQ, 128], F32, kind="ExternalOutput").ap()
    else:
        attn_o, y_o = dsts["attn"], dsts["y"]

    P = Prog(nc)
    op, dma = P.op, P.dma

    ident_bf = P.sb("ident_bf", [128, 128], BF16); b_ident = P.buf()
    ident_f = P.sb("ident_f", [128, 128], F32); b_identf = P.buf()
    ones_bf = P.sb("ones_bf", [1, 512], BF16); b_ones_bf = P.buf()
    zeros_bf = P.sb("zeros_bf", [1, 512], BF16); b_zeros_bf = P.buf()
    ones_f = P.sb("ones_f", [1, 128], F32); b_ones_f = P.buf()
    U_tok = P.sb("U_tok", [128, 8, 8, 8, 16], BF16); b_U = [P.buf() for _ in range(8)]
    dma(lambda e: e.dma_start(out=ident_f[:], in_=ident[:, :]), writes=[b_identf])
    op("dve", lambda e: e.tensor_copy(out=ident_bf[:], in_=ident_f[:]), reads=[b_identf], writes=[b_ident])
    op("dve", lambda e: e.memset(ones_f[:], 1.0), writes=[b_ones_f])
    op("dve", lambda e: e.memset(ones_bf[:], 1.0), writes=[b_ones_bf])
    op("dve", lambda e: e.memset(zeros_bf[:], 0.0), writes=[b_zeros_bf])

    P.push_scope()
    QaT = P.sb("QaT", [128, 2, L_SEQ], BF16); b_QaT = [P.buf() for _ in range(NTILE)]
    KaT = P.sb("KaT", [128, 2, L_SEQ], BF16); b_KaT = [P.buf() for _ in range(NTILE)]; b_Kind = P.buf()
    Vaug = P.sb("Vaug", [128, NTILE, 2, 65], BF16); b_V = [P.buf() for _ in range(NTILE)]; b_Vones = P.buf()
    P.push_scope()
    stgI = [P.sb(f"stgI{i}", [128, 2048], F32) for i in range(2)]; b_stgI = [P.buf() for _ in range(2)]
    for cch in range(4):
        sg_, bsg_ = _rot(stgI, cch), _rot(b_stgI, cch)
        dma(lambda e: e.dma_start(out=sg_[64:96, :], in_=ind[:, cch * 2048:(cch + 1) * 2048]), writes=[bsg_])
        for h in range(2):
            op("pool" if h else "dve", lambda e: e.tensor_copy(out=KaT[64:96, h, cch * 2048:(cch + 1) * 2048], in_=sg_[64:96, :]),
               reads=[bsg_], writes=[b_Kind])
    P.pop_scope()
    op("pool", lambda e: e.memset(Vaug[:, :, :, 64:65], 1.0), writes=[b_Vones])

    P.push_scope()
    S1b = P.sb("S1b", [128, 1024], F32); b_S1b = P.buf()
    sh1T = P.sb("sh1T", [128, 16], BF16); b_sh1T = P.buf()
    Win = P.sb("Win", [128, 8, 512], BF16); b_Win = P.buf()
    bin_bf = P.sb("bin_bf", [1, 512], BF16); b_bin = P.buf()
    P.push_scope()
    stgW = [P.sb(f"stgW{i}", [128, 2048], F32) for i in range(2)]; b_stgW = [P.buf() for _ in range(2)]
    for kk in range(2):
        sg_, bsg_ = stgW[kk], b_stgW[kk]
        dma(lambda e: e.dma_start(out=sg_[:].rearrange("p (k n) -> p k n", k=4),
                                  in_=win[kk * 512:(kk + 1) * 512, :].rearrange("(k p) n -> p k n", p=128)), writes=[bsg_])
        op("pool" if kk else "dve", lambda e: e.tensor_copy(out=Win[:, 4 * kk:4 * kk + 4, :].rearrange("p k n -> p (k n)"), in_=sg_[:]),
           reads=[bsg_], writes=[b_Win])
    P.pop_scope()

    P.push_scope()
    ct = P.sb("ct", [128, 16], F32); b_ct = P.buf()
    sct = P.sb("sct", [128, 16], F32); b_sct = P.buf()
    sgc = P.sb("sgc", [128, 16], F32)
    modrow = P.sb("modrow", [1, 2048], F32); b_modrow = P.buf()
    badas = P.sb("badas", [1, 2048], F32); b_badas = P.buf()
    gmixs = P.sb("gmixs", [1, 1024], F32); b_gmixs = P.buf()
    s1row = P.sb("s1row", [1, 1024], F32); b_s1row = P.buf()
    pieces = [P.sb(f"wpiece{i}", [128, 8, 512], F32) for i in range(2)]
    b_pieces = [P.buf() for _ in range(2)]
    ps_row = [P.ps(f"ps_row{i}", [128, 512], F32) for i in range(2)]
    b_ps_row = [P.pbuf() for _ in range(2)]
    dma(lambda e: e.dma_start(out=ct[:], in_=cT2[:, :]), writes=[b_ct])
    dma(lambda e: e.dma_start(out=badas[:], in_=bada[:, :]), writes=[b_badas])
    dma(lambda e: e.dma_start(out=gmixs[:], in_=gmix[:, :]), writes=[b_gmixs])
    op("act", lambda e: e.activation(out=sgc[:], in_=ct[:], func=AF.Sigmoid), reads=[b_ct], writes=[b_sct])
    op("dve", lambda e: e.tensor_tensor(out=sct[:], in0=sgc[:], in1=ct[:], op=ALU.mult), reads=[b_ct, b_sct], writes=[b_sct])
    for j in range(4):
        pc, bpc = _rot(pieces, j), _rot(b_pieces, j)
        pr, bpr = _rot(ps_row, j), _rot(b_ps_row, j)
        dma(lambda e: e.dma_start(out=pc[:], in_=wada[:, j * 512:(j + 1) * 512].rearrange("(k p) n -> p k n", p=128)),
            writes=[bpc])
        for k in range(8):
            op("pe", lambda e: e.matmul(pr[0:1, :], lhsT=sct[:, 2 * k:2 * k + 1], rhs=pc[:, k, :],
                                        start=(k == 0), stop=(k == 7)),
               reads=[b_sct, bpc], writes=[bpr])
        op("dve", lambda e: e.tensor_tensor(out=modrow[0:1, j * 512:(j + 1) * 512], in0=pr[0:1, :],
                                            in1=badas[0:1, j * 512:(j + 1) * 512], op=ALU.add),
           reads=[bpr, b_badas], writes=[b_modrow])
    op("dve", lambda e: e.scalar_tensor_tensor(out=s1row[:], in0=modrow[0:1, 1024:2048], scalar=1.0,
                                                in1=gmixs[0:1, :], op0=ALU.add, op1=ALU.mult),
       reads=[b_modrow, b_gmixs], writes=[b_s1row])
    for hf in range(2):
        pr, bpr = _rot(ps_row, hf), _rot(b_ps_row, hf)
        op("pe", lambda e: e.matmul(pr[:, :], lhsT=ones_f[0:1, :], rhs=s1row[0:1, hf * 512:(hf + 1) * 512],
                                    start=True, stop=True), reads=[b_ones_f, b_s1row], writes=[bpr])
        op("act", lambda e: e.activation(out=S1b[:, hf * 512:(hf + 1) * 512], in_=pr[:, :], func=AF.Copy),
           reads=[bpr], writes=[b_S1b])
    pr, bpr = ps_row[0], b_ps_row[0]
    for k in range(8):
        op("pe", lambda e: e.matmul(pr[:, 2 * k:2 * k + 2], lhsT=modrow[0:1, k * 128:(k + 1) * 128],
                                    rhs=ones_f[0:1, 0:2], start=True, stop=True),
           reads=[b_ones_f, b_modrow], writes=[bpr])
    op("dve", lambda e: e.tensor_copy(out=sh1T[:], in_=pr[:, 0:16]), reads=[bpr], writes=[b_sh1T])
    pr, bpr = ps_row[1], b_ps_row[1]
    for k in range(8):
        op("pe", lambda e: e.matmul(pr[0:1, :], lhsT=sh1T[:, 2 * k:2 * k + 1], rhs=Win[:, k, :],
                                    start=(k == 0), stop=(k == 7)), reads=[b_sh1T, b_Win], writes=[bpr])
    op("act", lambda e: e.activation(out=bin_bf[:], in_=pr[0:1, :], func=AF.Copy), reads=[bpr], writes=[b_bin])
    P.pop_scope()

    def dbl(name, shape, dt, n=2):
        return [P.sb(f"{name}{i}", shape, dt) for i in range(n)], [P.buf() for _ in range(n)]

    def dblp(name, shape, dt, n=2):
        return [P.ps(f"{name}{i}", shape, dt) for i in range(n)], [P.pbuf() for _ in range(n)]
    xt, b_xt = dbl("xt", [128, 1024], F32, 4)
    hb, b_hb = dbl("hb", [128, 1024], BF16)
    junk = P.sb("junk", [128, 1024], BF16); b_junk = P.buf()
    ssx, b_ssx = dbl("ssx", [128, 1], F32)
    sqx, b_sqx = dbl("sqx", [128, 1], F32)
    rx, b_rx = dbl("rx", [128, 1], F32)
    hTs = P.sb("hTs", [128, 8, 1024], BF16); b_hTs = [P.buf() for _ in range(8)]
    cs, b_cs = dbl("cs", [128, 8, 32], F32)
    sn, b_sn = dbl("sn", [128, 8, 32], F32)
    qkr, b_qkr = dbl("qkr", [128, 4, 2, 32], F32)
    r1, b_r1 = dbl("r1", [128, 4, 32], F32)
    r2, b_r2 = dbl("r2", [128, 4, 32], F32)
    r3, b_r3 = dbl("r3", [128, 4, 32], F32)
    r4, b_r4 = dbl("r4", [128, 4, 32], F32)
    ksum = P.sb("ksum", [64, 2, 2], F32); b_ksum = [P.buf(), P.buf()]
    kmT = P.sb("kmT", [64, 2, 32], F32); b_kmT = P.buf()
    qTt, b_qTt = dbl("qTt", [64, 2, 128], F32)
    Gt, b_Gt = dbl("Gt", [128, 2, 32], F32)
    mx8, b_mx8 = dbl("mx8", [128, 2, 8], F32)
    nm, b_nm = dbl("nm", [128, 2, 32], F32)
    Qtok, b_Qtok = dbl("Qtok", [128, 2, 96], BF16)
    pxt = P.ps("pxt", [128, 8, 128], BF16); b_pxt = P.pbuf()
    pqkv, b_pqkv = dblp("pqkv", [128, 512], F32)
    pT, b_pT = dblp("pT", [128, 4, 128], F32)
    pgq, b_pgq = dblp("pgq", [128, 512], F32)
    pu = P.ps("pu", [128, 4, 128], F32); b_pu = P.pbuf()
    pqa = [pgq[i][:, 256:512].bitcast(BF16).rearrange("p (h n) -> p h n", h=4) for i in range(2)]
    for i in range(2):
        op("dve", lambda e: e.memset(Gt[i][:], -1.0e30), writes=[b_Gt[i]])
    op("dve", lambda e: e.memset(kmT[:], 0.0), writes=[b_kmT])

    def load_x(t):
        x_, bx = _rot(xt, t), _rot(b_xt, t)
        dma(lambda e: e.dma_start(out=x_[:], in_=xb[t * 128:(t + 1) * 128, :]), writes=[bx])

    def stage_a(t):
        S_, ti = t // 8, t % 8
        i = t % 2
        x_, bx = _rot(xt, t), _rot(b_xt, t)
        pq_, bpq = pqkv[i], b_pqkv[i]
        if ti == 0:
            c_, bc_ = _rot(cs, S_), _rot(b_cs, S_)
            s_, bs_ = _rot(sn, S_), _rot(b_sn, S_)
            dma(lambda e: e.dma_start(out=c_[:].rearrange("p i d -> p (i d)"), in_=cosl[:, S_ * 256:(S_ + 1) * 256]), writes=[bc_])
            dma(lambda e: e.dma_start(out=s_[:].rearrange("p i d -> p (i d)"), in_=sinl[:, S_ * 256:(S_ + 1) * 256]), writes=[bs_])
        op("act", lambda e: e.activation(out=junk[:], in_=x_[:], func=AF.Square, accum_out=ssx[i][:, 0:1]),
           reads=[bx], writes=[b_junk, b_ssx[i]])
        yield
        op("act", lambda e: e.activation(out=sqx[i][:], in_=ssx[i][:], func=AF.Sqrt, scale=1.0 / 1024.0, bias=EPS),
           reads=[b_ssx[i]], writes=[b_sqx[i]])
        yield
        op("dve", lambda e: e.reciprocal(out=rx[i][:], in_=sqx[i][:]), reads=[b_sqx[i]], writes=[b_rx[i]])
        yield
        op("dve", lambda e: e.scalar_tensor_tensor(out=hb[i][:], in0=x_[:], scalar=rx[i][:, 0:1], in1=S1b[:],
                                                    op0=ALU.mult, op1=ALU.mult),
           reads=[bx, b_rx[i], b_S1b], writes=[b_hb[i]])
        yield
        for k in range(8):
            op("pe", lambda e: e.transpose(pxt[:, k, :], hb[i][:, k * 128:(k + 1) * 128], ident_bf[:]),
               reads=[b_hb[i], b_ident], writes=[b_pxt])
        yield
        op("act", lambda e: e.activation(out=hTs[:, :, ti * 128:(ti + 1) * 128], in_=pxt[:], func=AF.Copy),
           reads=[b_pxt], writes=[b_hTs[ti]])
        yield
        op("pe", lambda e: e.matmul(pq_[:, 0:384], lhsT=ones_bf[0:1, 0:128], rhs=bin_bf[0:1, 0:384], start=True, stop=False),
           reads=[b_ones_bf, b_bin], writes=[bpq])
        for k in range(8):
            op("pe", lambda e: e.matmul(pq_[:, 0:384], lhsT=hTs[:, k, ti * 128:(ti + 1) * 128], rhs=Win[:, k, 0:384],
                                        start=False, stop=(k == 7)),
               reads=[b_hTs[ti], b_Win], writes=[bpq])
        yield

    def u_proj(S_):
        for half in range(2):
            for s4 in range(4):
                s = half * 4 + s4
                op("pe", lambda e: e.matmul(pu[:, s4, :], lhsT=ones_bf[0:1, 0:128], rhs=bin_bf[0:1, 384:512], start=True, stop=False),
                   reads=[b_ones_bf, b_bin], writes=[b_pu])
                for k in range(8):
                    lhs = hTs[:, k, :].rearrange("p (c s) -> p s c", s=8)[:, s, :]
                    op("pe", lambda e: e.matmul(pu[:, s4, :], lhsT=lhs, rhs=Win[:, k, 384:512], start=False, stop=(k == 7)),
                       reads=b_hTs + [b_Win], writes=[b_pu])
            op("act", lambda e: e.activation(out=U_tok[:, S_].rearrange("p g s m -> p s g m")[:, half * 4:half * 4 + 4],
                                             in_=pu[:].rearrange("p s (g m) -> p s g m", g=8), func=AF.Copy),
               reads=[b_pu], writes=[b_U[S_]])

    def stage_b1(t):
        S_, ti = t // 8, t % 8
        i = t % 2
        c_, bc_ = _rot(cs, S_), _rot(b_cs, S_)
        s_, bs_ = _rot(sn, S_), _rot(b_sn, S_)
        pq_, bpq = pqkv[i], b_pqkv[i]
        pq = pq_[:, 0:256].rearrange("p (f two d) -> p f two d", f=4, two=2)
        cb = c_[:, ti, :].unsqueeze(1).to_broadcast([128, 4, 32])
        sb_ = s_[:, ti, :].unsqueeze(1).to_broadcast([128, 4, 32])
        op("dve", lambda e: e.tensor_tensor(out=r1[i][:], in0=pq[:, :, 0, :], in1=cb, op=ALU.mult), reads=[bpq, bc_], writes=[b_r1[i]])
        yield
        op("dve", lambda e: e.tensor_tensor(out=r2[i][:], in0=pq[:, :, 1, :], in1=sb_, op=ALU.mult), reads=[bpq, bs_], writes=[b_r2[i]])
        yield
        op("dve", lambda e: e.tensor_tensor(out=r3[i][:], in0=pq[:, :, 0, :], in1=sb_, op=ALU.mult), reads=[bpq, bs_], writes=[b_r3[i]])
        yield
        op("dve", lambda e: e.tensor_tensor(out=r4[i][:], in0=pq[:, :, 1, :], in1=cb, op=ALU.mult), reads=[bpq, bc_], writes=[b_r4[i]])
        yield
        op("act", lambda e: e.activation(out=Vaug[:, t, :, 0:64], in_=pq_[:, 256:384].rearrange("p (h d) -> p h d", h=2),
                                         func=AF.Copy), reads=[bpq], writes=[b_V[t]])
        yield

    def stage_b2(t):
        i = t % 2
        j = t // 2
        par = t % 2
        tok = slice(t * 128, (t + 1) * 128)
        pT_, bpT = pT[i], b_pT[i]
        pg_, bpg = pgq[i], b_pgq[i]
        pqa_ = pqa[i]
        op("pool", lambda e: e.tensor_tensor(out=qkr[i][:, :, 0, :], in0=r1[i][:], in1=r2[i][:], op=ALU.subtract),
           reads=[b_r1[i], b_r2[i]], writes=[b_qkr[i]])
        yield
        op("pool", lambda e: e.tensor_tensor(out=qkr[i][:, :, 1, :], in0=r3[i][:], in1=r4[i][:], op=ALU.add),
           reads=[b_r3[i], b_r4[i]], writes=[b_qkr[i]])
        yield
        qkf = qkr[i][:].rearrange("p f two d -> p (f two d)")
        for h in range(2):
            op("pe", lambda e: e.transpose(pT_[0:64, h, :], qkf[:, 128 + h * 64:128 + (h + 1) * 64], ident_f[:]),
               reads=[b_qkr[i], b_identf], writes=[bpT])
        if j >= 4:
            for h in range(2):
                op("pe", lambda e: e.transpose(pT_[0:64, 2 + h, :], qkf[:, h * 64:(h + 1) * 64], ident_f[:]),
                   reads=[b_qkr[i], b_identf], writes=[bpT])
        yield
        op("act", lambda e: e.activation(out=KaT[0:64, :, tok], in_=pT_[0:64, 0:2, :], func=AF.Copy),
           reads=[bpT], writes=[b_KaT[t]])
        yield
        op("dve", lambda e: e.tensor_reduce(out=ksum[:, par, :], in_=pT_[0:64, 0:2, :], axis=mybir.AxisListType.X, op=ALU.add),
           reads=[bpT], writes=[b_ksum[par]])
        yield
        if j >= 4:
            op("act", lambda e: e.activation(out=qTt[i][:], in_=pT_[0:64, 2:4, :], func=AF.Copy), reads=[bpT], writes=[b_qTt[i]])
            yield
            for h in range(2):
                op("pe", lambda e: e.matmul(pg_[:, h * 32:(h + 1) * 32], lhsT=qTt[i][:, h, :], rhs=kmT[:, h, :], start=True, stop=True),
                   reads=[b_qTt[i], b_kmT], writes=[bpg])
            yield
            op("dve", lambda e: e.tensor_copy(out=Gt[i][:, :, 0:j], in_=pg_[:, 0:64].rearrange("p (h n) -> p h n", h=2)[:, :, 0:j]),
               reads=[bpg], writes=[b_Gt[i]])
            yield
            for h in range(2):
                op("dve", lambda e: e.max(out=mx8[i][:, h, :], in_=Gt[i][:, h, :]), reads=[b_Gt[i]], writes=[b_mx8[i]])
                yield
            for h in range(2):
                op("dve", lambda e: e.tensor_scalar(out=nm[i][:, h, :], in0=Gt[i][:, h, :], scalar1=mx8[i][:, h, 2:3], scalar2=1.0,
                                                    op0=ALU.is_ge, op1=ALU.subtract), reads=[b_Gt[i], b_mx8[i]], writes=[b_nm[i]])
                yield
            op("dve", lambda e: e.memset(nm[i][:, :, j:j + 1], 0.0), writes=[b_nm[i]])
            yield
            op("dve", lambda e: e.tensor_scalar(out=Qtok[i][:, :, 64:96], in0=nm[i][:], scalar1=-NEGBIG, scalar2=None, op0=ALU.mult),
               reads=[b_nm[i]], writes=[b_Qtok[i]])
            yield
        else:
            op("dve", lambda e: e.memset(Qtok[i][:, :, 64:96], NEGBIG), writes=[b_Qtok[i]])
            yield
            op("dve", lambda e: e.memset(Qtok[i][:, :, 64:64 + j + 1], 0.0), writes=[b_Qtok[i]])
            yield
        op("pool", lambda e: e.tensor_scalar(out=Qtok[i][:, :, 0:64], in0=qkf[:, 0:128].rearrange("p (h d) -> p h d", h=2),
                                             scalar1=0.125, scalar2=None, op0=ALU.mult),
           reads=[b_qkr[i]], writes=[b_Qtok[i]])
        yield
        for h in range(2):
            op("pe", lambda e: e.transpose(pqa_[0:96, h, :], Qtok[i][:, h, :], ident_bf[:]), reads=[b_Qtok[i], b_ident], writes=[bpg])
        yield
        op("act", lambda e: e.activation(out=QaT[0:96, :, tok], in_=pqa_[0:96, 0:2, :], func=AF.Copy),
           reads=[bpg], writes=[b_QaT[t]])
        yield
        if par == 1:
            op("dve", lambda e: e.tensor_tensor(out=kmT[:, :, j], in0=ksum[:, 0, :], in1=ksum[:, 1, :], op=ALU.add),
               reads=[b_ksum[0], b_ksum[1]], writes=[b_kmT])
            yield

    def lockstep(*gens):
        gens = [g for g in gens if g is not None]
        while gens:
            alive = []
            for g in gens:
                try:
                    next(g)
                    alive.append(g)
                except StopIteration:
                    pass
            gens = alive

    import os as _os
    if _os.environ.get("SEQ_LOCK"):
        def lockstep(*gens):
            for g in gens:
                if g is not None:
                    for _ in g:
                        pass
    ntl = dbg_tiles
    for t in range(min(4, ntl)):
        load_x(t)
    if ntl > 0:
        lockstep(stage_a(0))
        if ntl > 1:
            lockstep(stage_a(1))
    for t in range(0, ntl, 2):
        lockstep(stage_b1(t), stage_b1(t + 1) if t + 1 < ntl else None)
        if (t + 2) % 8 == 0 and with_ssm:
            u_proj(t // 8)
        for tt_ in (t + 4, t + 5):
            if tt_ < ntl:
                load_x(tt_)
        def chain(*gs):
            for g_ in gs:
                if g_ is not None:
                    yield from g_
        lockstep(chain(stage_a(t + 2) if t + 2 < ntl else None, stage_a(t + 3) if t + 3 < ntl else None),
                 stage_b2(t), stage_b2(t + 1) if t + 1 < ntl else None)
    P.pop_scope()

    P.push_scope()
    cm = P.sb("cm", [128, 4, 512], BF16); b_cm = P.buf()
    P.push_scope()
    stgM = P.sb("stgM", [128, 2048], F32); b_stgM = P.buf()
    dma(lambda e: e.dma_start(out=stgM[:], in_=cmask[:, :]), writes=[b_stgM])
    op("dve", lambda e: e.tensor_copy(out=cm[:].rearrange("p a c -> p (a c)"), in_=stgM[:]), reads=[b_stgM], writes=[b_cm])
    P.pop_scope()
    Pt = [P.sb(f"Pt{i}", [128, 512], BF16) for i in range(3)]; b_Pt = [P.buf() for _ in range(3)]
    Osb = [P.sb(f"Osb{i}", [65, 512], F32) for i in range(2)]; b_Osb = [P.buf() for _ in range(2)]
    pS = [P.ps(f"pS{i}", [128, 512], F32) for i in range(3)]; b_pS = [P.pbuf() for _ in range(3)]
    pO = [P.ps(f"pO{i}", [128, 512], F32) for i in range(2)]; b_pO = [P.pbuf() for _ in range(2)]
    cnt = 0
    gi = 0
    for h in range(2 if dbg_attn else 0):
        for G in range(16 if dbg_attn is True else dbg_attn):
            po, bpo = _rot(pO, gi), _rot(b_pO, gi)
            ob, bob = _rot(Osb, gi), _rot(b_Osb, gi)
            gi += 1
            nkt = 4 * G + 4
            qs = slice(G * 512, (G + 1) * 512)
            qbufs = b_QaT[4 * G:4 * G + 4]

            def s_mm(kt, c):
                ps_, bps = _rot(pS, c), _rot(b_pS, c)
                op("pe", lambda e: e.matmul(ps_[:], lhsT=KaT[0:96, h, kt * 128:(kt + 1) * 128], rhs=QaT[0:96, h, qs],
                                            start=True, stop=True),
                   reads=[b_KaT[kt], b_Kind] + qbufs, writes=[bps])
            s_mm(0, cnt)
            for kt in range(nkt):
                c = cnt + kt
                if kt + 1 < nkt:
                    s_mm(kt + 1, c + 1)
                ps_, bps = _rot(pS, c), _rot(b_pS, c)
                p_, bp_ = _rot(Pt, c), _rot(b_Pt, c)
                op("act", lambda e: e.activation(out=p_[:], in_=ps_[:], func=AF.Exp), reads=[bps], writes=[bp_])
                a = kt - 4 * G
                c0 = 0
                if a >= 0:
                    op("pool", lambda e: e.tensor_tensor(out=p_[:], in0=p_[:], in1=cm[:, a, :], op=ALU.mult),
                       reads=[bp_, b_cm], writes=[bp_])
                    c0 = a * 128
                op("pe", lambda e: e.matmul(po[0:65, c0:512], lhsT=Vaug[:, kt, h, :], rhs=p_[:, c0:512],
                                            start=(kt == 0), stop=(kt == nkt - 1)),
                   reads=[bp_, b_V[kt], b_Vones], writes=[bpo])
            cnt += nkt
            op("dve", lambda e: e.tensor_copy(out=ob[:], in_=po[0:65, :]), reads=[bpo], writes=[bob])
            dma(lambda e: e.dma_start(out=attn_o[h * 65:(h + 1) * 65, G * 512:(G + 1) * 512], in_=ob[:]), reads=[bob])
    P.pop_scope()
    P.pop_scope()

    if with_ssm:
        build_ssm(nc, P, U_tok, b_U, y_o, ident_bf, b_ident, ident_f, b_identf)
    P.finish()
    return nc


def rope_tables():
    half = 32
    inv = (10000.0 ** (-np.arange(half, dtype=np.float32) / half)).astype(np.float32)
    ang = np.arange(L_SEQ, dtype=np.float32)[:, None] * inv[None, :]
    cos, sin = np.cos(ang).astype(np.float32), np.sin(ang).astype(np.float32)
    cl = np.ascontiguousarray(cos.reshape(NTILE, 128, half).transpose(1, 0, 2).reshape(128, NTILE * half))
    sl = np.ascontiguousarray(sin.reshape(NTILE, 128, half).transpose(1, 0, 2).reshape(128, NTILE * half))
    return cl, sl


def const_tables():
    ind = np.zeros((32, L_SEQ), np.float32)
    for j in range(32):
        ind[j, j * 256:(j + 1) * 256] = 1.0
    cm = np.zeros((128, 4, 512), np.float32)
    kk = np.arange(128)[:, None]
    qq = np.arange(128)[None, :]
    tri = (kk <= qq).astype(np.float32)
    for a in range(4):
        for bq in range(4):
            if bq > a:
                cm[:, a, bq * 128:(bq + 1) * 128] = 1.0
            elif bq == a:
                cm[:, a, bq * 128:(bq + 1) * 128] = tri
    return ind, cm.reshape(128, 2048)


def phase_ab_inputs(inp, b, r):
    c = inp["c"][b]
    cT = np.ascontiguousarray(c.reshape(8, 128).T)
    cT2 = np.repeat(cT[:, :, None], 2, axis=2).reshape(128, 16)
    w = inp["w_in"][0]
    cols = np.concatenate([np.arange(128 * r, 128 * r + 128), 512 + np.arange(128 * r, 128 * r + 128),
                           1024 + np.arange(128 * r, 128 * r + 128), 1536 + np.arange(128 * r, 128 * r + 128)])
    cl, sl = rope_tables()
    ind, cm = const_tables()
    d = {
        "xb": np.ascontiguousarray(inp["x"][b]),
        "cT2": np.ascontiguousarray(cT2),
        "wada_a": np.ascontiguousarray(inp["w_ada"][0][:, 0:2048]),
        "bada_a": np.ascontiguousarray(inp["b_ada"][0][None, 0:2048]),
        "gmix": np.ascontiguousarray(inp["g_mix"][0][None, :]),
        "win": np.ascontiguousarray(w[:, cols]),
        "cosl": cl, "sinl": sl, "ind": ind, "cmask": cm, "ident": _ident(),
    }
    d.update(ssm_inputs(inp, r))
    return d


KV_F, KV_G, KV_O, KV_E, KV_A, KV_H = 0, 8, 16, 24, 32, 47
NKV = 48
NA = 15
TWO_PI = 2.0 * math.pi
CW1 = 6.28125
CW2 = TWO_PI - CW1


def ssm_kvec():
    kv = np.zeros(NKV, np.float32)
    for s in range(8):
        kv[KV_F + s] = -s
        kv[KV_G + s] = s
        kv[KV_O + s] = s + 1
        kv[KV_E + s] = 7 - s
    for i in range(5):
        for m in range(1, 4):
            kv[KV_A + 3 * i + (m - 1)] = 8.0 * m * (4 ** i)
    kv[KV_H] = 0.5
    return kv


def build_ssm(nc, P, U_tok, b_U, y_o, ident_bf, b_ident, ident_f, b_identf):
    def din(name, shape, dt=F32):
        return nc.dram_tensor(name, list(shape), dt, kind="ExternalInput").ap()
    sp_small = din("ssm_small", [128, 8 * 4 + NKV + 2 + 8])
    sp_bc = din("ssm_bc", [128, 4 * 128])
    sp_mat = din("ssm_mat", [128, 256])
    op, dma = P.op, P.dma
    P.push_scope()
    small = P.sb("ssm_small_sb", [128, 8 * 4 + NKV + 2 + 8], F32); b_small = P.buf()
    bc = P.sb("ssm_bc_sb", [128, 4, 8, 16], F32); b_bc = P.buf()
    mats = P.sb("ssm_mat_sb", [128, 2, 128], F32); b_mats = P.buf()
    dma(lambda e: e.dma_start(out=small[:], in_=sp_small[:, :]), writes=[b_small])
    dma(lambda e: e.dma_start(out=bc[:].rearrange("p a g m -> p (a g m)"), in_=sp_bc[:, :]), writes=[b_bc])
    dma(lambda e: e.dma_start(out=mats[:].rearrange("p a n -> p (a n)"), in_=sp_mat[:, :]), writes=[b_mats])
    lamre, lamim, logdt, dsk = small[:, 0:8], small[:, 8:16], small[:, 16:24], small[:, 24:32]
    kvec = small[:, 32:32 + NKV]
    sgn_a, sgn_b = small[:, 32 + NKV:33 + NKV], small[:, 33 + NKV:34 + NKV]
    smask, P2 = mats[:, 0, :], mats[:, 1, :]
    Bs1, Bs2, Cs1, Cs2 = bc[:, 0], bc[:, 1], bc[:, 2], bc[:, 3]

    def T(name, shape, dt=F32):
        return P.sb(name, shape, dt), P.buf()

    def dv(fn, reads, writes):
        return op("dve", fn, reads=reads, writes=writes)

    dt_, b_dt = T("s_dt", [128, 8])
    a_, b_a = T("s_a", [128, 8])
    th, b_th = T("s_th", [128, 8])
    op("act", lambda e: e.activation(out=dt_[:], in_=logdt, func=AF.Exp), reads=[b_small], writes=[b_dt])
    dv(lambda e: e.tensor_tensor(out=a_[:], in0=lamre, in1=dt_[:], op=ALU.mult), [b_small, b_dt], [b_a])
    dv(lambda e: e.tensor_tensor(out=th[:], in0=lamim, in1=dt_[:], op=ALU.mult), [b_small, b_dt], [b_th])
    KA, b_KA = T("s_KA", [128, 8, NKV])
    KT, b_KT = T("s_KT", [128, 8, NKV])
    kb = kvec.unsqueeze(1).to_broadcast([128, 8, NKV])
    dv(lambda e: e.tensor_tensor(out=KA[:], in0=a_[:].unsqueeze(2).to_broadcast([128, 8, NKV]), in1=kb, op=ALU.mult),
       [b_a, b_small], [b_KA])
    dv(lambda e: e.tensor_tensor(out=KT[:], in0=th[:].unsqueeze(2).to_broadcast([128, 8, NKV]), in1=kb, op=ALU.mult),
       [b_th, b_small], [b_KT])
    MAG, b_MAG = T("s_MAG", [128, 8, NKV])
    op("act", lambda e: e.activation(out=MAG[:], in_=KA[:], func=AF.Exp), reads=[b_KA], writes=[b_MAG])
    ni, b_ni = T("s_ni", [128, 8, NKV], I32)
    nf, b_nf = T("s_nf", [128, 8, NKV])
    rr, b_rr = T("s_rr", [128, 8, NKV])
    uu, b_uu = T("s_uu", [128, 8, NKV])
    SIN, b_SIN = T("s_SIN", [128, 8, NKV])
    COS, b_COS = T("s_COS", [128, 8, NKV])

    def sin_of(dst, b_dst, shift):
        dv(lambda e: e.tensor_scalar(out=uu[:], in0=KT[:], scalar1=shift, scalar2=1.0 / TWO_PI, op0=ALU.add, op1=ALU.mult),
           [b_KT], [b_uu])
        dv(lambda e: e.tensor_copy(out=ni[:], in_=uu[:]), [b_uu], [b_ni])
        dv(lambda e: e.tensor_copy(out=nf[:], in_=ni[:]), [b_ni], [b_nf])
        dv(lambda e: e.scalar_tensor_tensor(out=rr[:], in0=nf[:], scalar=-CW1, in1=KT[:], op0=ALU.mult, op1=ALU.add),
           [b_nf, b_KT], [b_rr])
        dv(lambda e: e.scalar_tensor_tensor(out=rr[:], in0=nf[:], scalar=-CW2, in1=rr[:], op0=ALU.mult, op1=ALU.add),
           [b_nf, b_rr], [b_rr])
        dv(lambda e: e.tensor_scalar(out=rr[:], in0=rr[:], scalar1=shift, scalar2=math.pi, op0=ALU.add, op1=ALU.min),
           [b_rr], [b_rr])
        dv(lambda e: e.tensor_scalar(out=rr[:], in0=rr[:], scalar1=-math.pi, scalar2=None, op0=ALU.max), [b_rr], [b_rr])
        op("act", lambda e: e.activation(out=dst[:], in_=rr[:], func=AF.Sin), reads=[b_rr], writes=[b_dst])
    sin_of(SIN, b_SIN, 0.0)
    sin_of(COS, b_COS, math.pi / 2.0)
    PR, b_PR = T("s_PR", [128, 8, NKV])
    PI_, b_PI = T("s_PI", [128, 8, NKV])
    dv(lambda e: e.tensor_tensor(out=PR[:], in0=MAG[:], in1=COS[:], op=ALU.mult), [b_MAG, b_COS], [b_PR])
    dv(lambda e: e.tensor_tensor(out=PI_[:], in0=MAG[:], in1=SIN[:], op=ALU.mult), [b_MAG, b_SIN], [b_PI])
    em1, b_em1 = T("s_em1", [128, 8])
    tq, b_tq = T("s_tq", [128, 8])
    dv(lambda e: e.tensor_scalar(out=tq[:], in0=a_[:], scalar1=0.25, scalar2=1.0, op0=ALU.mult, op1=ALU.add), [b_a], [b_tq])
    dv(lambda e: e.tensor_tensor(out=tq[:], in0=tq[:], in1=a_[:], op=ALU.mult), [b_tq, b_a], [b_tq])
    dv(lambda e: e.tensor_scalar(out=tq[:], in0=tq[:], scalar1=1.0 / 3.0, scalar2=1.0, op0=ALU.mult, op1=ALU.add), [b_tq], [b_tq])
    dv(lambda e: e.tensor_tensor(out=tq[:], in0=tq[:], in1=a_[:], op=ALU.mult), [b_tq, b_a], [b_tq])
    dv(lambda e: e.tensor_scalar(out=tq[:], in0=tq[:], scalar1=0.5, scalar2=1.0, op0=ALU.mult, op1=ALU.add), [b_tq], [b_tq])
    dv(lambda e: e.tensor_tensor(out=em1[:], in0=tq[:], in1=a_[:], op=ALU.mult), [b_tq, b_a], [b_em1])
    cth, sth, shalf = COS[:, :, KV_G + 1], SIN[:, :, KV_G + 1], SIN[:, :, KV_H]
    re1, b_re1 = T("s_re1", [128, 8])
    im1, b_im1 = T("s_im1", [128, 8])
    w1, b_w1 = T("s_w1", [128, 8])
    w2, b_w2 = T("s_w2", [128, 8])
    dv(lambda e: e.tensor_tensor(out=w1[:], in0=shalf, in1=shalf, op=ALU.mult), [b_SIN], [b_w1])
    dv(lambda e: e.tensor_tensor(out=re1[:], in0=em1[:], in1=cth, op=ALU.mult), [b_em1, b_COS], [b_re1])
    dv(lambda e: e.scalar_tensor_tensor(out=re1[:], in0=w1[:], scalar=-2.0, in1=re1[:], op0=ALU.mult, op1=ALU.add),
       [b_w1, b_re1], [b_re1])
    dv(lambda e: e.scalar_tensor_tensor(out=im1[:], in0=em1[:], scalar=1.0, in1=sth, op0=ALU.add, op1=ALU.mult),
       [b_em1, b_SIN], [b_im1])
    den, b_den = T("s_den", [128, 8])
    dv(lambda e: e.tensor_tensor(out=den[:], in0=lamre, in1=lamre, op=ALU.mult), [b_small], [b_den])
    dv(lambda e: e.tensor_tensor(out=w1[:], in0=lamim, in1=lamim, op=ALU.mult), [b_small], [b_w1])
    dv(lambda e: e.tensor_tensor(out=den[:], in0=den[:], in1=w1[:], op=ALU.add), [b_den, b_w1], [b_den])
    dv(lambda e: e.reciprocal(out=den[:], in_=den[:]), [b_den], [b_den])
    cr, b_cr = T("s_cr", [128, 8])
    ci, b_ci = T("s_ci", [128, 8])
    dv(lambda e: e.tensor_tensor(out=w1[:], in0=re1[:], in1=lamre, op=ALU.mult), [b_re1, b_small], [b_w1])
    dv(lambda e: e.tensor_tensor(out=w2[:], in0=im1[:], in1=lamim, op=ALU.mult), [b_im1, b_small], [b_w2])
    dv(lambda e: e.tensor_tensor(out=w1[:], in0=w1[:], in1=w2[:], op=ALU.add), [b_w1, b_w2], [b_w1])
    dv(lambda e: e.tensor_tensor(out=cr[:], in0=w1[:], in1=den[:], op=ALU.mult), [b_w1, b_den], [b_cr])
    dv(lambda e: e.tensor_tensor(out=w1[:], in0=im1[:], in1=lamre, op=ALU.mult), [b_im1, b_small], [b_w1])
    dv(lambda e: e.tensor_tensor(out=w2[:], in0=re1[:], in1=lamim, op=ALU.mult), [b_re1, b_small], [b_w2])
    dv(lambda e: e.tensor_tensor(out=w1[:], in0=w1[:], in1=w2[:], op=ALU.subtract), [b_w1, b_w2], [b_w1])
    dv(lambda e: e.tensor_tensor(out=ci[:], in0=w1[:], in1=den[:], op=ALU.mult), [b_w1, b_den], [b_ci])
    bb1, b_bb1 = T("s_bb1", [128, 8, 16])
    bb2, b_bb2 = T("s_bb2", [128, 8, 16])
    q1, b_q1 = T("s_q1", [128, 8, 16])
    q2, b_q2 = T("s_q2", [128, 8, 16])
    crb = cr[:].unsqueeze(2).to_broadcast([128, 8, 16])
    cib = ci[:].unsqueeze(2).to_broadcast([128, 8, 16])
    dv(lambda e: e.tensor_tensor(out=q1[:], in0=crb, in1=Bs1, op=ALU.mult), [b_cr, b_bc], [b_q1])
    dv(lambda e: e.tensor_tensor(out=q2[:], in0=cib, in1=Bs2, op=ALU.mult), [b_ci, b_bc], [b_q2])
    dv(lambda e: e.scalar_tensor_tensor(out=bb1[:].rearrange("p g m -> p (g m)"), in0=q2[:].rearrange("p g m -> p (g m)"),
                                         scalar=sgn_a, in1=q1[:].rearrange("p g m -> p (g m)"), op0=ALU.mult, op1=ALU.add),
       [b_q1, b_q2, b_small], [b_bb1])
    dv(lambda e: e.tensor_tensor(out=q1[:], in0=crb, in1=Bs2, op=ALU.mult), [b_cr, b_bc], [b_q1])
    dv(lambda e: e.tensor_tensor(out=q2[:], in0=cib, in1=Bs1, op=ALU.mult), [b_ci, b_bc], [b_q2])
    dv(lambda e: e.scalar_tensor_tensor(out=bb2[:].rearrange("p g m -> p (g m)"), in0=q2[:].rearrange("p g m -> p (g m)"),
                                         scalar=sgn_b, in1=q1[:].rearrange("p g m -> p (g m)"), op0=ALU.mult, op1=ALU.add),
       [b_q1, b_q2, b_small], [b_bb2])
    CA, b_CA = T("s_CA", [128, 8, 16])
    CB, b_CB = T("s_CB", [128, 8, 16])
    dv(lambda e: e.tensor_scalar(out=CA[:], in0=Cs1, scalar1=sgn_b, scalar2=None, op0=ALU.mult), [b_bc, b_small], [b_CA])
    dv(lambda e: e.tensor_scalar(out=CB[:], in0=Cs2, scalar1=-1.0, scalar2=None, op0=ALU.mult), [b_bc], [b_CB])
    Fm, b_Fm = T("s_F", [128, 8, 8, 16])
    Fe, b_Fe = T("s_Fe", [128, 8, 8, 16])
    Gm, b_Gm = T("s_G", [128, 8, 8, 16])
    Om, b_Om = T("s_O", [128, 8, 8, 16])
    z1, b_z1 = T("s_z1", [128, 8, 8, 16])
    z2, b_z2 = T("s_z2", [128, 8, 8, 16])

    def outer(dst, bdst, kv0, v1, bv1, v2, bv2, sgn):
        pr = PR[:, :, kv0:kv0 + 8].unsqueeze(3).to_broadcast([128, 8, 8, 16])
        pi = PI_[:, :, kv0:kv0 + 8].unsqueeze(3).to_broadcast([128, 8, 8, 16])
        dv(lambda e: e.tensor_tensor(out=z1[:], in0=pr, in1=v1[:].unsqueeze(2).to_broadcast([128, 8, 8, 16]), op=ALU.mult),
           [b_PR, bv1], [b_z1])
        dv(lambda e: e.tensor_tensor(out=z2[:], in0=pi, in1=v2[:].unsqueeze(2).to_broadcast([128, 8, 8, 16]), op=ALU.mult),
           [b_PI, bv2], [b_z2])
        fl = "p g s m -> p (g s m)"
        if sgn is None:
            dv(lambda e: e.tensor_tensor(out=dst[:].rearrange(fl), in0=z1[:].rearrange(fl), in1=z2[:].rearrange(fl), op=ALU.add),
               [b_z1, b_z2], [bdst])
        else:
            dv(lambda e: e.scalar_tensor_tensor(out=dst[:].rearrange(fl), in0=z2[:].rearrange(fl), scalar=sgn,
                                                 in1=z1[:].rearrange(fl), op0=ALU.mult, op1=ALU.add),
               [b_z1, b_z2, b_small], [bdst])
    outer(Fm, b_Fm, KV_F, bb1, b_bb1, bb2, b_bb2, sgn_a)
    outer(Fe, b_Fe, KV_E, bb1, b_bb1, bb2, b_bb2, sgn_a)
    outer(Gm, b_Gm, KV_G, CA, b_CA, CB, b_CB, None)
    outer(Om, b_Om, KV_O, CA, b_CA, CB, b_CB, None)
    PIA, b_PIA = T("s_PIA", [128, 8, NA])
    dv(lambda e: e.tensor_scalar(out=PIA[:], in0=PI_[:, :, KV_A:KV_A + NA], scalar1=sgn_b, scalar2=None, op0=ALU.mult),
       [b_PI, b_small], [b_PIA])

    Yout = P.sb("s_Yout", [128, 8, 8, 128], F32); b_Yout = P.buf()
    Ug = P.sb("s_Ug", [128, 1024], BF16); b_Ug = P.buf()
    H = P.sb("s_H", [128, 1024], BF16); b_H = P.buf()
    Ysb = P.sb("s_Ysb", [128, 1024], F32); b_Ysb = P.buf()
    Mbf = P.sb("s_Mbf", [128, 128], BF16); b_Mbf = P.buf()
    Ebf = P.sb("s_Ebf", [128, 128], BF16); b_Ebf = P.buf()
    mtmp = P.sb("s_mtmp", [128, 128], F32); b_mtmp = P.buf()
    Amat = P.sb("s_Amat", [128, NA, 128], BF16); b_Amat = P.buf()
    Obf = P.sb("s_Obf", [128, 128], BF16); b_Obf = P.buf()
    put = P.ps("s_put", [128, 8, 128], BF16); b_put = P.pbuf()
    pxy = [P.ps(f"s_pxy{i}", [128, 512], F32) for i in range(2)]; b_pxy = [P.pbuf() for _ in range(2)]
    psc = [P.ps(f"s_psc{i}", [128, 512], F32) for i in range(2)]; b_psc = [P.pbuf() for _ in range(2)]
    pme = P.ps("s_pme", [128, 512], F32); b_pme = P.pbuf()
    pyt = P.ps("s_pyt", [128, 8, 128], F32); b_pyt = P.pbuf()
    for g in range(8):
        Fg = Fm[:, g].rearrange("p s m -> p (s m)")
        Feg = Fe[:, g].rearrange("p s m -> p (s m)")
        Gg = Gm[:, g].rearrange("p s m -> p (s m)")
        Og = Om[:, g].rearrange("p s m -> p (s m)")
        op("pe", lambda e: e.matmul(pme[:, 0:128], lhsT=Fg, rhs=Gg, start=True, stop=True), reads=[b_Fm, b_Gm], writes=[b_pme])
        dv(lambda e: e.tensor_tensor(out=mtmp[:], in0=pme[:, 0:128], in1=smask, op=ALU.mult), [b_pme, b_mats], [b_mtmp])
        dv(lambda e: e.scalar_tensor_tensor(out=Mbf[:], in0=ident_f[:], scalar=dsk[:, g:g + 1], in1=mtmp[:],
                                             op0=ALU.mult, op1=ALU.add), [b_identf, b_small, b_mtmp], [b_Mbf])
        op("pe", lambda e: e.transpose(pme[:, 128:256], Feg, ident_f[:]), reads=[b_Fe, b_identf], writes=[b_pme])
        op("act", lambda e: e.activation(out=Ebf[:], in_=pme[:, 128:256], func=AF.Copy), reads=[b_pme], writes=[b_Ebf])
        op("act", lambda e: e.activation(out=Obf[:], in_=Og, func=AF.Copy), reads=[b_Om], writes=[b_Obf])
        for i in range(NA):
            dv(lambda e: e.tensor_scalar(out=mtmp[:], in0=ident_f[:], scalar1=PR[:, g, KV_A + i:KV_A + i + 1], scalar2=None,
                                         op0=ALU.mult), [b_identf, b_PR], [b_mtmp])
            dv(lambda e: e.scalar_tensor_tensor(out=Amat[:, i, :], in0=P2, scalar=PIA[:, g, i:i + 1], in1=mtmp[:],
                                                 op0=ALU.mult, op1=ALU.add), [b_mats, b_PIA, b_mtmp], [b_Amat])
        for S in range(8):
            op("pe", lambda e: e.transpose(put[:, S, :], U_tok[:, S, g].rearrange("p s m -> p (s m)"), ident_bf[:]),
               reads=[b_U[S], b_ident], writes=[b_put])
        op("act", lambda e: e.activation(out=Ug[:].rearrange("p (S c) -> p S c", S=8), in_=put[:], func=AF.Copy),
           reads=[b_put], writes=[b_Ug])
        for hf in range(2):
            op("pe", lambda e: e.matmul(pxy[hf][:], lhsT=Ebf[:], rhs=Ug[:, hf * 512:(hf + 1) * 512], start=True, stop=True),
               reads=[b_Ebf, b_Ug], writes=[b_pxy[hf]])
            op("act", lambda e: e.activation(out=H[:, hf * 512:(hf + 1) * 512], in_=pxy[hf][:], func=AF.Copy),
               reads=[b_pxy[hf]], writes=[b_H])
        for i in range(5):
            d = 4 ** i
            plan = {0: [], 1: []}
            for m in range(1, 4):
                sh = m * d
                for hb in range(2):
                    lo = max(sh, hb * 512)
                    hi = (hb + 1) * 512
                    if lo < hi:
                        plan[hb].append((m, sh, lo, hi))
            for hb in range(2):
                for idx, (m, sh, lo, hi) in enumerate(plan[hb]):
                    op("pe", lambda e: e.matmul(psc[hb][:, lo - hb * 512:hi - hb * 512], lhsT=Amat[:, 3 * i + m - 1, :],
                                                rhs=H[:, lo - sh:hi - sh], start=(idx == 0), stop=(idx == len(plan[hb]) - 1)),
                       reads=[b_Amat, b_H], writes=[b_psc[hb]])
            for hb in range(2):
                if plan[hb]:
                    lo, hi = plan[hb][0][2], plan[hb][0][3]
                    dv(lambda e: e.tensor_tensor(out=H[:, lo:hi], in0=psc[hb][:, lo - hb * 512:hi - hb * 512], in1=H[:, lo:hi], op=ALU.add),
                       [b_psc[hb], b_H], [b_H])
        for hf in range(2):
            op("pe", lambda e: e.matmul(pxy[hf][:], lhsT=Mbf[:], rhs=Ug[:, hf * 512:(hf + 1) * 512], start=True, stop=False),
               reads=[b_Mbf, b_Ug], writes=[b_pxy[hf]])
            if hf == 0:
                op("pe", lambda e: e.matmul(pxy[0][:, 1:512], lhsT=Obf[:], rhs=H[:, 0:511], start=False, stop=True),
                   reads=[b_Obf, b_H], writes=[b_pxy[0]])
            else:
                op("pe", lambda e: e.matmul(pxy[1][:], lhsT=Obf[:], rhs=H[:, 511:1023], start=False, stop=True),
                   reads=[b_Obf, b_H], writes=[b_pxy[1]])
            op("act", lambda e: e.activation(out=Ysb[:, hf * 512:(hf + 1) * 512], in_=pxy[hf][:], func=AF.Copy),
               reads=[b_pxy[hf]], writes=[b_Ysb])
        for S in range(8):
            op("pe", lambda e: e.transpose(pyt[:, S, :], Ysb[:, S * 128:(S + 1) * 128], ident_f[:]),
               reads=[b_Ysb, b_identf], writes=[b_pyt])
        dv(lambda e: e.tensor_copy(out=Yout[:, :, :, g * 16:(g + 1) * 16],
                                   in_=pyt[:].rearrange("p S (t n) -> p S t n", t=8)), [b_pyt], [b_Yout])
    for S in range(8):
        dma(lambda e: e.dma_start(out=y_o[S * 1024:(S + 1) * 1024, :].rearrange("(c t) ch -> c t ch", t=8), in_=Yout[:, S]),
            reads=[b_Yout])
    P.pop_scope()


def ssm_inputs(inp, r):
    gs = slice(8 * r, 8 * r + 8)
    lam_re = inp["lam_re"][0][gs]
    lam_im = inp["lam_im"][0][gs]
    log_dt = inp["log_dt"][0][gs]
    b_re, b_im = inp["b_re"][0][gs], inp["b_im"][0][gs]
    c_re, c_im = inp["c_re"][0][gs], inp["c_im"][0][gs]
    d = inp["d_skip"][0][gs]
    dup = lambda a: np.concatenate([a, a], axis=0)
    small = np.zeros((128, 8 * 4 + NKV + 2 + 8), np.float32)
    small[:, 0:8] = dup(lam_re.T)
    small[:, 8:16] = dup(lam_im.T)
    small[:, 16:24] = np.broadcast_to(log_dt[None, :], (128, 8))
    small[:, 24:32] = np.tile(d.T, (8, 1))
    small[:, 32:32 + NKV] = ssm_kvec()[None, :]
    small[:64, 32 + NKV] = -1.0; small[64:, 32 + NKV] = 1.0
    small[:64, 33 + NKV] = 1.0; small[64:, 33 + NKV] = -1.0
    bre = b_re.transpose(1, 0, 2); bim = b_im.transpose(1, 0, 2)
    cre = c_re.transpose(2, 0, 1); cim = c_im.transpose(2, 0, 1)
    bcv = np.zeros((128, 4, 8, 16), np.float32)
    bcv[:64, 0], bcv[64:, 0] = bre, bim
    bcv[:64, 1], bcv[64:, 1] = bim, bre
    bcv[:64, 2], bcv[64:, 2] = cre, cim
    bcv[:64, 3], bcv[64:, 3] = cim, cre
    s_idx = np.arange(128) // 16
    smask = (s_idx[None, :] >= s_idx[:, None]).astype(np.float32)
    P2 = np.zeros((128, 128), np.float32)
    P2[np.arange(128), (np.arange(128) + 64) % 128] = 1.0
    return {"ssm_small": small, "ssm_bc": np.ascontiguousarray(bcv.reshape(128, 512)),
            "ssm_mat": np.ascontiguousarray(np.concatenate([smask, P2], axis=1))}


_NC_CACHE = {}


def _get_nc(which):
    if which not in _NC_CACHE:
        nc = bass.Bass("TRN2", target_bir_lowering=False)
        if which == "ab":
            build_phase_ab(nc, with_ssm=True)
        else:
            build_phase_c(nc)
        _NC_CACHE[which] = nc
    return _NC_CACHE[which]


def kernel(**inputs):
    inp = {k: np.asarray(v, dtype=np.float32) for k, v in inputs.items()}
    B = inp["x"].shape[0]
    maps1 = [phase_ab_inputs(inp, ci // 4, ci % 4) for ci in range(8)]
    res1 = run_bass_kernel_spmd(_get_nc("ab"), maps1, core_ids=list(range(8))).results
    attn_full = np.zeros((B, L_SEQ, 8, 65), np.float32)
    y_full = np.zeros((B, L_SEQ, 512), np.float32)
    for ci in range(8):
        b, r = ci // 4, ci % 4
        attn_full[b, :, 2 * r:2 * r + 2, :] = res1[ci]["attn_o"].reshape(2, 65, L_SEQ).transpose(2, 0, 1)
        y_full[b, :, 128 * r:128 * r + 128] = res1[ci]["y_o"]
    maps2 = [phase_c_inputs(inp, ci // 4, ci % 4, attn_full, y_full) for ci in range(8)]
    res2 = run_bass_kernel_spmd(_get_nc("c"), maps2, core_ids=list(range(8))).results
    out = np.zeros((B, L_SEQ, 1024), np.float32)
    for ci in range(8):
        b, qi = ci // 4, ci % 4
        out[b, qi * NT_C:(qi + 1) * NT_C] = res2[ci]["out"]
    return out
```
